# Optimizing a Trainium2 kernel written in Bass

```python
import math
import jax
import jax.numpy as jnp
from jax import lax
import numpy as np

D_MODEL = 1024
BATCH = 16
SEQ = 4096
DEPTH = 4

N_MIXERS = 3
N_GDN = (DEPTH + 2) // 3
N_SB = (DEPTH + 1) // 3
N_DSA = DEPTH // 3

D_FF = 2816
NORM_EPS = 1e-6
ROPE_THETA = 500000.0
Q_BLOCK = 128

GDN_HEADS = 8
GDN_HEAD_DIM = 128
GDN_DIM = GDN_HEADS * GDN_HEAD_DIM
GDN_CONV = 4
GDN_CHUNK = 64
GDN_IN = 4 * GDN_DIM + 2 * GDN_HEADS

SB_HEADS = 8
SB_HEAD_DIM = 128
SB_DIM = SB_HEADS * SB_HEAD_DIM

DSA_HEADS = 8
DSA_Q_RANK = 384
DSA_KV_RANK = 256
DSA_ROPE_DIM = 32
DSA_NOPE_DIM = 96
DSA_QK_DIM = DSA_ROPE_DIM + DSA_NOPE_DIM
DSA_V_DIM = 128
DSA_IDX_HEADS = 8
DSA_IDX_DIM = 64
DSA_TOPK_MAX = 256
DSA_IN = DSA_Q_RANK + DSA_KV_RANK + DSA_ROPE_DIM + DSA_IDX_DIM + DSA_IDX_HEADS

kernel_name = 'hybrid_gdn_stickbreak_dsa_macaron'


def rmsnorm(x, g):
    xf = x.astype(jnp.float32)
    y = xf * lax.rsqrt(jnp.mean(xf * xf, axis=-1, keepdims=True) + NORM_EPS)
    return (y * g.astype(jnp.float32)).astype(x.dtype)


def l2norm(x):
    return x * lax.rsqrt(jnp.sum(x * x, axis=-1, keepdims=True) + NORM_EPS)


def rope(x, pos):
    r = x.shape[-1]
    half = r // 2
    inv_freq = ROPE_THETA ** (-jnp.arange(half, dtype=jnp.float32) * (2.0 / r))
    ang = pos.astype(jnp.float32)[:, None] * inv_freq[None, :]
    shape = (pos.shape[0],) + (1,) * (x.ndim - 3) + (half,)
    cos = jnp.cos(ang).reshape(shape)
    sin = jnp.sin(ang).reshape(shape)
    xf = x.astype(jnp.float32)
    x1, x2 = xf[..., :half], xf[..., half:]
    return jnp.concatenate([x1 * cos - x2 * sin, x2 * cos + x1 * sin], axis=-1).astype(x.dtype)


def partial_rope(x, pos):
    r = x.shape[-1] // 4
    return jnp.concatenate([rope(x[..., :r], pos), x[..., r:]], axis=-1)


def swiglu(h, w_gu, w_down):
    gate, up = jnp.split(h @ w_gu, 2, axis=-1)
    return (jax.nn.silu(gate) * up) @ w_down


def causal_depthwise_conv(x, w):
    kw, ch = w.shape
    return lax.conv_general_dilated(
        x, w[:, None, :].astype(x.dtype), window_strides=(1,), padding=[(kw - 1, 0)],
        dimension_numbers=('NWC', 'WIO', 'NWC'), feature_group_count=ch)


def to_blocks(t, block):
    b, s = t.shape[:2]
    return jnp.swapaxes(t.reshape((b, s // block, block) + t.shape[2:]), 0, 1)


def from_blocks(t):
    nb, b, blk = t.shape[:3]
    return jnp.swapaxes(t, 0, 1).reshape((b, nb * blk) + t.shape[3:])


def chunk_gated_delta_rule(q, k, v, g, beta):
    bsz, seq, nh, dk = q.shape
    dv = v.shape[-1]
    c = GDN_CHUNK
    n = seq // c

    def chunks(t):
        return jnp.moveaxis(t.reshape((bsz, n, c, nh) + t.shape[3:]), 3, 1)

    q, k, v, g, beta = chunks(q), chunks(k), chunks(v), chunks(g), chunks(beta)
    g_cum = jnp.cumsum(g, axis=-1)
    incl = jnp.tril(jnp.ones((c, c), dtype=bool))
    strict = jnp.tril(jnp.ones((c, c), dtype=bool), -1)
    seg = g_cum[..., :, None] - g_cum[..., None, :]
    decay = jnp.where(incl, jnp.exp(jnp.where(incl, seg, 0.0)), 0.0)
    kk = jnp.einsum('bhnid,bhnjd->bhnij', k, k)
    lower = jnp.where(strict, beta[..., :, None] * kk * decay, 0.0)
    eye = jnp.eye(c, dtype=jnp.float32)
    rhs = jnp.concatenate([v * beta[..., None], k * (beta * jnp.exp(g_cum))[..., None]], axis=-1)
    sol = lax.linalg.triangular_solve(lower + eye, rhs, left_side=True, lower=True, unit_diagonal=True)
    u_in, w = sol[..., :dv], sol[..., dv:]
    qk = jnp.einsum('bhnid,bhnjd->bhnij', q, k) * decay
    q_dec = q * jnp.exp(g_cum)[..., None]
    k_dec = k * jnp.exp(g_cum[..., -1:] - g_cum)[..., None]
    g_tot = jnp.exp(g_cum[..., -1])
    xs = tuple(jnp.moveaxis(t, 2, 0) for t in (u_in, w, qk, q_dec, k_dec, g_tot))

    def step(state, inp):
        u_c, w_c, qk_c, qd_c, kd_c, gt_c = inp
        u = u_c - jnp.einsum('bhck,bhkv->bhcv', w_c, state)
        o = jnp.einsum('bhck,bhkv->bhcv', qd_c, state) + jnp.einsum('bhcj,bhjv->bhcv', qk_c, u)
        state = state * gt_c[..., None, None] + jnp.einsum('bhck,bhcv->bhkv', kd_c, u)
        return state, o

    state0 = jnp.zeros((bsz, nh, dk, dv), jnp.float32)
    _, o = lax.scan(step, state0, xs)
    return jnp.transpose(o, (1, 0, 3, 2, 4)).reshape(bsz, seq, nh, dv)


def gated_deltanet(h, w_in, conv_w, a_log, dt_bias, norm_g, w_out):
    bsz, seq, _ = h.shape
    proj = h @ w_in
    qkv = jax.nn.silu(causal_depthwise_conv(proj[..., :3 * GDN_DIM], conv_w))
    z = proj[..., 3 * GDN_DIM:4 * GDN_DIM]
    b_logit = proj[..., 4 * GDN_DIM:4 * GDN_DIM + GDN_HEADS].astype(jnp.float32)
    a = proj[..., 4 * GDN_DIM + GDN_HEADS:].astype(jnp.float32)

    def heads(t):
        return t.reshape(bsz, seq, GDN_HEADS, GDN_HEAD_DIM).astype(jnp.float32)

    q, k, v = (heads(t) for t in jnp.split(qkv, 3, axis=-1))
    q = l2norm(q) * GDN_HEAD_DIM ** -0.5
    k = l2norm(k)
    beta = jax.nn.sigmoid(b_logit)
    g = -jnp.exp(a_log.astype(jnp.float32)) * jax.nn.softplus(a + dt_bias.astype(jnp.float32))
    o = chunk_gated_delta_rule(q, k, v, g, beta)
    o = rmsnorm(o, norm_g) * jax.nn.silu(heads(z))
    return o.reshape(bsz, seq, GDN_DIM).astype(h.dtype) @ w_out


def stick_breaking_attention(h, w_in, w_out):
    bsz, seq, _ = h.shape
    q, k, v = (t.reshape(bsz, seq, SB_HEADS, SB_HEAD_DIM).astype(jnp.float32)
               for t in jnp.split(h @ w_in, 3, axis=-1))
    scale = SB_HEAD_DIM ** -0.5
    key_pos = jnp.arange(seq)

    def block(args):
        q_blk, t0 = args
        z = jnp.einsum('bqhd,bkhd->bhqk', q_blk, k) * scale
        q_pos = t0 + jnp.arange(Q_BLOCK)
        earlier = key_pos[None, :] < q_pos[:, None]
        log_beta = jax.nn.log_sigmoid(z)
        log_fail = jnp.where(earlier, log_beta - z, 0.0)
        log_a = log_beta + lax.cumsum(log_fail, axis=3, reverse=True) - log_fail
        att = jnp.where(earlier, jnp.exp(log_a), 0.0)
        return jnp.einsum('bhqk,bkhd->bqhd', att, v)

    starts = jnp.arange(seq // Q_BLOCK, dtype=jnp.int32) * Q_BLOCK
    o = from_blocks(lax.map(block, (to_blocks(q, Q_BLOCK), starts)))
    return o.reshape(bsz, seq, SB_DIM).astype(h.dtype) @ w_out


def dsa_sparse_attention(h, w_in, cq_g, ckv_g, kidx_g, w_uq, w_qidx, w_uk, w_uv, w_out):
    bsz, seq, _ = h.shape
    topk = min(DSA_TOPK_MAX, seq // 4)
    pos = jnp.arange(seq)
    proj = h @ w_in
    o1 = DSA_Q_RANK
    o2 = o1 + DSA_KV_RANK
    o3 = o2 + DSA_ROPE_DIM
    o4 = o3 + DSA_IDX_DIM
    c_q = rmsnorm(proj[..., :o1], cq_g)
    c_kv = rmsnorm(proj[..., o1:o2], ckv_g)
    k_rope = rope(proj[..., o2:o3], pos)
    k_idx = partial_rope(rmsnorm(proj[..., o3:o4], kidx_g), pos).astype(jnp.float32)
    w_idx = proj[..., o4:].astype(jnp.float32) * (DSA_IDX_HEADS ** -0.5 * DSA_IDX_DIM ** -0.5)
    q = (c_q @ w_uq).reshape(bsz, seq, DSA_HEADS, DSA_QK_DIM)
    q_rope = rope(q[..., :DSA_ROPE_DIM], pos)
    q_nope = q[..., DSA_ROPE_DIM:]
    q_idx = partial_rope((c_q @ w_qidx).reshape(bsz, seq, DSA_IDX_HEADS, DSA_IDX_DIM), pos)
    q_abs = jnp.einsum('bshn,chn->bshc', q_nope, w_uk.reshape(DSA_KV_RANK, DSA_HEADS, DSA_NOPE_DIM))
    q_lat = jnp.concatenate([q_abs, q_rope], axis=-1).astype(jnp.float32)
    k_lat = jnp.concatenate([c_kv, k_rope], axis=-1).astype(jnp.float32)
    scale = DSA_QK_DIM ** -0.5
    batch_ix = jnp.arange(bsz)[:, None, None]

    def block(args):
        qi, wi, ql, t0 = args
        q_pos = t0 + jnp.arange(Q_BLOCK)
        logits = jnp.einsum('bqhd,bkd->bqhk', qi, k_idx)
        score = jnp.einsum('bqh,bqhk->bqk', wi, jax.nn.relu(logits))
        score = jnp.where(pos[None, None, :] <= q_pos[None, :, None], score, -jnp.inf)
        _, idx = lax.top_k(score, topk)
        sel = k_lat[batch_ix, idx]
        att = jnp.einsum('bqhr,bqkr->bqhk', ql, sel) * scale
        att = jnp.where((idx <= q_pos[None, :, None])[:, :, None, :], att, -jnp.inf)
        p = jax.nn.softmax(att, axis=-1)
        return jnp.einsum('bqhk,bqkc->bqhc', p, sel[..., :DSA_KV_RANK])

    starts = jnp.arange(seq // Q_BLOCK, dtype=jnp.int32) * Q_BLOCK
    o_lat = from_blocks(lax.map(block, (to_blocks(q_idx.astype(jnp.float32), Q_BLOCK),
                                        to_blocks(w_idx, Q_BLOCK),
                                        to_blocks(q_lat, Q_BLOCK), starts)))
    o = jnp.einsum('bshc,chv->bshv', o_lat,
                   w_uv.reshape(DSA_KV_RANK, DSA_HEADS, DSA_V_DIM).astype(jnp.float32))
    return o.reshape(bsz, seq, DSA_HEADS * DSA_V_DIM).astype(h.dtype) @ w_out


def setup_inputs(seed: int = 0) -> dict:
    key = jax.random.key(seed)
    ks = jax.random.split(key, 26)
    f32 = jnp.float32

    def dense(k, shape):
        return jax.random.normal(k, shape, f32) * shape[-2] ** -0.5

    def gain(k, shape):
        return 1.0 + 0.02 * jax.random.normal(k, shape, f32)

    x = jax.random.normal(ks[0], (BATCH, SEQ, D_MODEL), f32)
    dt = jnp.exp(jax.random.uniform(ks[12], (N_GDN, GDN_HEADS), f32, math.log(1e-3), math.log(1e-1)))
    return {
        'x': x,
        'ffn1_norm': gain(ks[1], (DEPTH, D_MODEL)),
        'ffn1_w_gu': dense(ks[2], (DEPTH, D_MODEL, 2 * D_FF)),
        'ffn1_w_down': dense(ks[3], (DEPTH, D_FF, D_MODEL)),
        'mix_norm': gain(ks[4], (DEPTH, D_MODEL)),
        'ffn2_norm': gain(ks[5], (DEPTH, D_MODEL)),
        'ffn2_w_gu': dense(ks[6], (DEPTH, D_MODEL, 2 * D_FF)),
        'ffn2_w_down': dense(ks[7], (DEPTH, D_FF, D_MODEL)),
        'gdn_w_in': dense(ks[8], (N_GDN, D_MODEL, GDN_IN)),
        'gdn_conv': jax.random.normal(ks[10], (N_GDN, GDN_CONV, 3 * GDN_DIM), f32) * GDN_CONV ** -0.5,
        'gdn_a_log': jnp.log(jax.random.uniform(ks[11], (N_GDN, GDN_HEADS), f32, 1.0, 16.0)),
        'gdn_dt_bias': dt + jnp.log(-jnp.expm1(-dt)),
        'gdn_norm': gain(ks[13], (N_GDN, GDN_HEAD_DIM)),
        'gdn_w_out': dense(ks[9], (N_GDN, GDN_DIM, D_MODEL)),
        'sb_w_in': dense(ks[14], (N_SB, D_MODEL, 3 * SB_DIM)),
        'sb_w_out': dense(ks[15], (N_SB, SB_DIM, D_MODEL)),
        'dsa_w_in': dense(ks[16], (N_DSA, D_MODEL, DSA_IN)),
        'dsa_cq_norm': gain(ks[17], (N_DSA, DSA_Q_RANK)),
        'dsa_ckv_norm': gain(ks[18], (N_DSA, DSA_KV_RANK)),
        'dsa_kidx_norm': gain(ks[19], (N_DSA, DSA_IDX_DIM)),
        'dsa_w_uq': dense(ks[20], (N_DSA, DSA_Q_RANK, DSA_HEADS * DSA_QK_DIM)),
        'dsa_w_qidx': dense(ks[21], (N_DSA, DSA_Q_RANK, DSA_IDX_HEADS * DSA_IDX_DIM)),
        'dsa_w_uk': dense(ks[22], (N_DSA, DSA_KV_RANK, DSA_HEADS * DSA_NOPE_DIM)),
        'dsa_w_uv': dense(ks[23], (N_DSA, DSA_KV_RANK, DSA_HEADS * DSA_V_DIM)),
        'dsa_w_out': dense(ks[24], (N_DSA, DSA_HEADS * DSA_V_DIM, D_MODEL)),
        'final_norm': gain(ks[25], (D_MODEL,)),
    }


def reference(x, ffn1_norm, ffn1_w_gu, ffn1_w_down, mix_norm, ffn2_norm, ffn2_w_gu, ffn2_w_down,
              gdn_w_in, gdn_conv, gdn_a_log, gdn_dt_bias, gdn_norm, gdn_w_out,
              sb_w_in, sb_w_out,
              dsa_w_in, dsa_cq_norm, dsa_ckv_norm, dsa_kidx_norm, dsa_w_uq, dsa_w_qidx,
              dsa_w_uk, dsa_w_uv, dsa_w_out, final_norm):
    for i in range(DEPTH):
        kind, j = i % N_MIXERS, i // N_MIXERS
        x = x + 0.5 * swiglu(rmsnorm(x, ffn1_norm[i]), ffn1_w_gu[i], ffn1_w_down[i])
        h = rmsnorm(x, mix_norm[i])
        if kind == 0:
            m = gated_deltanet(h, gdn_w_in[j], gdn_conv[j], gdn_a_log[j], gdn_dt_bias[j],
                               gdn_norm[j], gdn_w_out[j])
        elif kind == 1:
            m = stick_breaking_attention(h, sb_w_in[j], sb_w_out[j])
        else:
            m = dsa_sparse_attention(h, dsa_w_in[j], dsa_cq_norm[j], dsa_ckv_norm[j],
                                     dsa_kidx_norm[j], dsa_w_uq[j], dsa_w_qidx[j],
                                     dsa_w_uk[j], dsa_w_uv[j], dsa_w_out[j])
        x = x + m.astype(x.dtype)
        x = x + 0.5 * swiglu(rmsnorm(x, ffn2_norm[i]), ffn2_w_gu[i], ffn2_w_down[i])
    return rmsnorm(x, final_norm)
```

```python
import contextlib
from contextlib import ExitStack

import numpy as np
import concourse.bass as bass
import concourse.mybir as mybir
from concourse.bass_utils import run_bass_kernel_spmd

F32 = mybir.dt.float32
BF16 = mybir.dt.bfloat16
AF = mybir.ActivationFunctionType
ALU = mybir.AluOpType
AX = mybir.AxisListType

D_MODEL = 1024
D_FF = 2816
EPS = 1e-6
N_CORES = 8

PE, ACT, DVE, POOL, SP = "pe", "act", "dve", "pool", "sp"
SEM_EPOCH = 20000
NDMA_SLOTS = 24


class V:
    __slots__ = ("ap", "key")

    def __init__(self, ap, key):
        self.ap = ap
        self.key = key


class T:
    _n = 0

    def __init__(self, handle, name, ap=None, is_psum=False):
        self.h = handle
        self.name = name
        T._n += 1
        self.id = T._n
        self._ap = ap
        self.is_psum = is_psum

    def base(self):
        return self._ap if self._ap is not None else self.h

    def __getitem__(self, idx):
        return V(self.base()[idx], (self.id, None, self.is_psum))

    def k(self, sub, idx=slice(None)):
        return V(self.base()[idx], (self.id, None if self.is_psum else sub, self.is_psum))

    def v(self, ap, sub=None):
        return V(ap, (self.id, None if self.is_psum else sub, self.is_psum))


class Op:
    __slots__ = ("eng", "fn", "reads", "writes", "is_dma", "idx", "deps", "signal", "sig_no", "slot", "slot_val")

    def __init__(self, eng, fn, reads, writes, is_dma=False):
        self.eng = eng
        self.fn = fn
        self.reads = reads
        self.writes = writes
        self.is_dma = is_dma
        self.deps = []
        self.signal = False
        self.sig_no = None


class Prog:
    def __init__(self, nc, es):
        self.nc = nc
        self.es_outer = es
        self.es = es
        self.ops = []
        self.engs = {PE: nc.tensor, ACT: nc.scalar, DVE: nc.vector, POOL: nc.gpsimd, SP: nc.sync}
        self.sig_count = {e: 0 for e in self.engs}
        self.dma_count = {e: 0 for e in self.engs}
        self.sems = {e: [] for e in self.engs}
        self.dsems = {e: [] for e in self.engs}
        self.n_emitted = 0

    def sbuf(self, name, shape, dt):
        T._n += 1
        name = f"{name}_{T._n}"
        h = self.es.enter_context(self.nc.sbuf_tensor(name, list(shape), dt))
        return T(h, name)

    def psum(self, name, shape, dt=F32):
        T._n += 1
        name = f"{name}_{T._n}"
        h = self.es.enter_context(self.nc.psum_tensor(name, list(shape), dt))
        return T(h, name, is_psum=True)

    def dram(self, name, shape, dt, kind="Internal"):
        h = self.nc.dram_tensor(name, list(shape), dt, kind=kind)
        return T(h, name, ap=h.ap())

    @contextlib.contextmanager
    def phase(self):
        old = self.es
        with ExitStack() as es:
            self.es = es
            yield
            self.flush()
        self.es = old

    def op(self, eng, fn, reads, writes, is_dma=False):
        rk = [v.key for v in reads]
        wk = [v.key for v in writes]
        wk += [k for k in rk if len(k) == 3 and k[2] is True]
        o = Op(eng, fn, rk, wk, is_dma)
        o.idx = len(self.ops)
        self.ops.append(o)
        return o

    def dma(self, out, in_, q=SP, **kw):
        return self.op(q, lambda e: e.dma_start(out=out.ap, in_=in_.ap, **kw), [in_], [out], is_dma=True)

    def mm(self, out, lhsT, rhs, start=True, stop=True):
        return self.op(PE, lambda e: e.matmul(out.ap, lhsT=lhsT.ap, rhs=rhs.ap, start=start, stop=stop),
                       [lhsT, rhs] + ([] if start else [out]), [out])

    def transpose(self, out, in_, ident):
        return self.op(PE, lambda e: e.transpose(out.ap, in_.ap, ident.ap), [in_, ident], [out])

    def act(self, out, in_, func, bias=None, scale=None, accum_out=None):
        kw = {}
        reads = [in_]
        writes = [out]
        if bias is not None:
            if isinstance(bias, V):
                kw["bias"] = bias.ap
                reads.append(bias)
            else:
                kw["bias"] = bias
        if scale is not None:
            if isinstance(scale, V):
                kw["scale"] = scale.ap
                reads.append(scale)
            else:
                kw["scale"] = scale
        if accum_out is not None:
            kw["accum_out"] = accum_out.ap
            writes.append(accum_out)
        return self.op(ACT, lambda e: e.activation(out.ap, in_.ap, func, **kw), reads, writes)

    def copy(self, out, in_, eng=DVE):
        if eng == ACT:
            return self.op(ACT, lambda e: e.copy(out.ap, in_.ap), [in_], [out])
        return self.op(eng, lambda e: e.tensor_copy(out=out.ap, in_=in_.ap), [in_], [out])

    def tt(self, out, in0, in1, op, eng=DVE):
        return self.op(eng, lambda e: e.tensor_tensor(out.ap, in0.ap, in1.ap, op), [in0, in1], [out])

    def ts(self, out, in0, s1, op0, s2=None, op1=None, accum_out=None, eng=DVE):
        reads = [in0]
        writes = [out]
        a1 = s1.ap if isinstance(s1, V) else s1
        a2 = s2.ap if isinstance(s2, V) else s2
        if isinstance(s1, V):
            reads.append(s1)
        if isinstance(s2, V):
            reads.append(s2)
        kw = {}
        if op1 is not None:
            kw["op1"] = op1
        if accum_out is not None:
            kw["accum_out"] = accum_out.ap
            writes.append(accum_out)
        return self.op(eng, lambda e: e.tensor_scalar(out.ap, in0.ap, a1, a2, op0, **kw), reads, writes)

    def stt(self, out, in0, s, in1, op0, op1, eng=DVE):
        reads = [in0, in1]
        a = s.ap if isinstance(s, V) else s
        if isinstance(s, V):
            reads.append(s)
        return self.op(eng, lambda e: e.scalar_tensor_tensor(out.ap, in0.ap, a, in1.ap, op0, op1), reads, [out])

    def reduce(self, out, in_, op, axis=AX.X, eng=DVE):
        return self.op(eng, lambda e: e.tensor_reduce(out.ap, in_.ap, axis, op), [in_], [out])

    def recip(self, out, in_):
        return self.op(DVE, lambda e: e.reciprocal(out.ap, in_.ap), [in_], [out])

    def rsqrt(self, out, in_, mult, add):
        self.act(out, in_, AF.Ln, bias=add, scale=mult)
        return self.act(out, out, AF.Exp, scale=-0.5)

    def memset(self, out, val, eng=DVE):
        return self.op(eng, lambda e: e.memset(out.ap, val), [], [out])

    def _sem(self, eng, n):
        i = n // SEM_EPOCH
        while len(self.sems[eng]) <= i:
            self.sems[eng].append(self.es_outer.enter_context(self.nc.semaphore(f"s_{eng}_{len(self.sems[eng])}")))
        return self.sems[eng][i], n % SEM_EPOCH + 1

    def _dsem(self, eng, slot):
        while len(self.dsems[eng]) <= slot:
            self.dsems[eng].append(self.es_outer.enter_context(self.nc.semaphore(f"d_{eng}_{len(self.dsems[eng])}")))
        return self.dsems[eng][slot]

    def flush(self):
        ops = self.ops
        if not ops:
            return
        last_w = {}
        readers = {}
        for o in ops:
            deps = set()
            for k in o.reads:
                w = last_w.get(k)
                if w is not None:
                    deps.add(w)
            for k in o.writes:
                w = last_w.get(k)
                if w is not None:
                    deps.add(w)
                for r in readers.get(k, ()):
                    deps.add(r)
            deps.discard(o.idx)
            o.deps = deps
            for k in o.reads:
                readers.setdefault(k, []).append(o.idx)
            for k in o.writes:
                last_w[k] = o.idx
                readers[k] = []
        waited = {e: {} for e in self.engs}
        waited_dma = {e: set() for e in self.engs}
        for o in ops:
            best = {}
            keep = []
            for d in o.deps:
                p = ops[d]
                if p.is_dma:
                    if d not in waited_dma[o.eng]:
                        waited_dma[o.eng].add(d)
                        keep.append(d)
                    continue
                if p.eng == PE and o.eng == PE and not o.is_dma:
                    continue
                if waited[o.eng].get(p.eng, -1) >= d:
                    continue
                if best.get(p.eng, -1) < d:
                    best[p.eng] = d
            for e, d in best.items():
                keep.append(d)
                waited[o.eng][e] = d
            o.deps = sorted(keep)
            for d in o.deps:
                ops[d].signal = True
        last_op = {}
        for o in ops:
            if not o.is_dma:
                last_op[o.eng] = o
        for o in last_op.values():
            o.signal = True
        last_dma = {}
        for o in ops:
            if o.is_dma:
                o.slot = self.dma_count[o.eng] % NDMA_SLOTS
                o.slot_val = 16 * (self.dma_count[o.eng] // NDMA_SLOTS + 1)
                self.dma_count[o.eng] += 1
                last_dma[(o.eng, o.slot)] = o
            elif o.signal:
                o.sig_no = self.sig_count[o.eng]
                self.sig_count[o.eng] += 1

        def sem_of(p):
            if p.is_dma:
                return self._dsem(p.eng, p.slot), p.slot_val
            return self._sem(p.eng, p.sig_no)

        for o in ops:
            e = self.engs[o.eng]
            for d in o.deps:
                s, v = sem_of(ops[d])
                e.wait_ge(s, v)
            if o.is_dma and o.slot_val > 16:
                e.wait_ge(self._dsem(o.eng, o.slot), o.slot_val - 16)
            ins = o.fn(e)
            if o.is_dma:
                ins.then_inc(self._dsem(o.eng, o.slot), 16)
            elif o.signal:
                s, _ = self._sem(o.eng, o.sig_no)
                ins.then_inc(s, 1)
        for en, e in self.engs.items():
            for pe_, o in last_op.items():
                if pe_ == en:
                    continue
                s, v = sem_of(o)
                e.wait_ge(s, v)
            for o in last_dma.values():
                s, v = sem_of(o)
                e.wait_ge(s, v)
        self.n_emitted += len(ops)
        self.ops = []


class Cfg:
    def __init__(self, S=4096, NSEQ=2, layers=(0, 1, 2, 3), ffn=True, mixers=True, final=True):
        self.S = S
        self.NSEQ = NSEQ
        self.NTOK = S * NSEQ
        self.layers = tuple(layers)
        self.ffn = ffn
        self.mixers = mixers
        self.final = final
        self.topk = min(256, S // 4)


INPUT_SHAPES = {
    'ffn1_norm': (4, 1024), 'ffn1_w_gu': (4, 1024, 5632), 'ffn1_w_down': (4, 2816, 1024),
    'mix_norm': (4, 1024), 'ffn2_norm': (4, 1024), 'ffn2_w_gu': (4, 1024, 5632), 'ffn2_w_down': (4, 2816, 1024),
    'gdn_w_in': (2, 1024, 4112), 'gdn_conv': (2, 4, 3072), 'gdn_a_log': (2, 8), 'gdn_dt_bias': (2, 8),
    'gdn_norm': (2, 128), 'gdn_w_out': (2, 1024, 1024),
    'sb_w_in': (1, 1024, 3072), 'sb_w_out': (1, 1024, 1024),
    'dsa_w_in': (1, 1024, 744), 'dsa_cq_norm': (1, 384), 'dsa_ckv_norm': (1, 256), 'dsa_kidx_norm': (1, 64),
    'dsa_w_uq': (1, 384, 1024), 'dsa_w_qidx': (1, 384, 512), 'dsa_w_uk': (1, 256, 768), 'dsa_w_uv': (1, 256, 1024),
    'dsa_w_out': (1, 1024, 1024), 'final_norm': (1024,),
}


class Ctx:
    pass


def make_consts(P, K):
    K.ident_f = P.sbuf("ident_f", [128, 128], F32)
    K.ident_b = P.sbuf("ident_b", [128, 128], BF16)
    K.ones_f = P.sbuf("ones_f", [128, 128], F32)
    K.ones_b = P.sbuf("ones_b", [128, 128], BF16)
    P.memset(K.ones_f[:], 1.0)
    P.memset(K.ones_b[:], 1.0)
    P.memset(K.ident_f[:], 1.0)
    P.op(POOL, lambda e: e.affine_select(K.ident_f[:].ap, K.ident_f[:].ap, [[-1, 128]], ALU.is_equal, 0.0,
                                         base=0, channel_multiplier=1), [K.ident_f[:]], [K.ident_f[:]])
    P.copy(K.ident_b[:], K.ident_f[:])
    K.m_ge = P.sbuf("m_ge", [128, 128], F32)
    K.m_gt = P.sbuf("m_gt", [128, 128], F32)
    K.m_lt = P.sbuf("m_lt", [128, 128], F32)
    for t, pat, cm, cmp_ in ((K.m_ge, 1, -1, ALU.is_ge), (K.m_gt, 1, -1, ALU.is_gt), (K.m_lt, -1, 1, ALU.is_gt)):
        P.memset(t[:], 1.0)
        P.op(POOL, (lambda t, pat, cm, cmp_: lambda e: e.affine_select(t[:].ap, t[:].ap, [[pat, 128]], cmp_, 0.0,
                                                                       base=0, channel_multiplier=cm))(t, pat, cm, cmp_),
             [t[:]], [t[:]])


def norm_T(P, K, xs, gb, hT_view, scr, psT, n_feat=1024, eps=EPS):
    junk, ss, rstd, xn = scr
    P.act(junk[:, 0:n_feat], xs, AF.Square, accum_out=ss[:])
    P.rsqrt(rstd[:], ss[:], 1.0 / n_feat, eps)
    P.stt(xn[:, 0:n_feat], xs, rstd[:], gb, ALU.mult, ALU.mult)
    nch = n_feat // 128
    for kc in range(nch):
        P.transpose(psT[:, kc * 128:(kc + 1) * 128], xn[:, kc * 128:(kc + 1) * 128], K.ident_b[:])
    P.copy(hT_view, psT.v(psT.base()[:, 0:n_feat].rearrange("p (c t) -> p c t", t=128)), eng=ACT)


def ffn_phase(P, K, C, src, dst, g_row, w_gu, w_down, tag):
    TT = 256
    NS = TT // 128
    NFC = D_FF // 128
    with P.phase():
        wgu = P.sbuf("wgu", [128, 8, 2 * D_FF], BF16)
        wd = P.sbuf("wd", [128, NFC, D_MODEL], BF16)
        gb = P.sbuf("gb", [128, D_MODEL], F32)
        P.dma(gb[:], V(g_row.partition_broadcast(128), ("w", tag, "g")))
        wgu_src = w_gu.rearrange("(kc p) f -> p kc f", p=128)
        for fi in range(11):
            P.dma(wgu.k(fi, (slice(None), slice(None), slice(fi * 512, (fi + 1) * 512))),
                  V(wgu_src[:, :, fi * 512:(fi + 1) * 512], ("w", tag, "gu")), q=POOL)
        wd_src = w_down.rearrange("(fc p) d -> p fc d", p=128)
        for pi in range(2):
            P.dma(wd.k(pi, (slice(None), slice(pi * 11, (pi + 1) * 11), slice(None))),
                  V(wd_src[:, pi * 11:(pi + 1) * 11, :], ("w", tag, "d")), q=POOL)
        xt = [P.sbuf(f"xt{i}", [128, D_MODEL], F32) for i in range(2 * NS)]
        hT = [P.sbuf(f"hT{i}", [128, 8, TT], BF16) for i in range(2)]
        aT = P.sbuf("aT", [128, NFC, TT], BF16)
        sg = [P.sbuf(f"sg{i}", [128, TT], F32) for i in range(2)]
        scr = (P.sbuf("junk", [128, D_MODEL], BF16), P.sbuf("ss", [128, 1], F32),
               P.sbuf("rstd", [128, 1], F32), P.sbuf("xn", [128, D_MODEL], BF16))
        psT = [P.psum(f"psT{i}", [128, D_MODEL], BF16) for i in range(2)]
        psg = [P.psum(f"psg{i}", [128, TT], F32) for i in range(2)]
        psu = [P.psum(f"psu{i}", [128, TT], F32) for i in range(2)]
        psd = [P.psum(f"psd{i}", [128, 512], F32) for i in range(2)]
        ntile = C.NTOK // TT
        nd = 0
        for ti in range(ntile):
            r0 = ti * TT
            xs = [xt[(ti % 2) * NS + s] for s in range(NS)]
            h = hT[ti % 2]
            for s in range(NS):
                P.dma(xs[s][:], src.k(("r", r0 // 128 + s), (slice(r0 + s * 128, r0 + (s + 1) * 128), slice(None))))
                norm_T(P, K, xs[s][:], gb[:], h[:, :, s * 128:(s + 1) * 128], scr, psT[s % 2])
            for fc in range(NFC):
                pg = psg[fc % 2]
                pu = psu[fc % 2]
                for kc in range(8):
                    P.mm(pg[:], wgu.k(fc // 4, (slice(None), kc, slice(fc * 128, (fc + 1) * 128))), h[:, kc, :],
                         start=(kc == 0), stop=(kc == 7))
                cu = NFC + fc
                for kc in range(8):
                    P.mm(pu[:], wgu.k(cu // 4, (slice(None), kc, slice(cu * 128, (cu + 1) * 128))), h[:, kc, :],
                         start=(kc == 0), stop=(kc == 7))
                P.act(sg[fc % 2][:], pg[:], AF.Silu)
                P.tt(aT[:, fc, :], sg[fc % 2][:], pu[:], ALU.mult)
            for s in range(NS):
                for half in range(2):
                    pd = psd[nd % 2]
                    nd += 1
                    for fc in range(NFC):
                        P.mm(pd[:], aT[:, fc, s * 128:(s + 1) * 128],
                             wd.k(fc // 11, (slice(None), fc, slice(half * 512, (half + 1) * 512))),
                             start=(fc == 0), stop=(fc == NFC - 1))
                    P.stt(xs[s][:, half * 512:(half + 1) * 512], pd[:], 0.5, xs[s][:, half * 512:(half + 1) * 512],
                          ALU.mult, ALU.add)
                P.dma(dst.k(("r", r0 // 128 + s), (slice(r0 + s * 128, r0 + (s + 1) * 128), slice(None))), xs[s][:],
                      q=POOL)


def final_phase(P, K, C, src, dst, g_row):
    with P.phase():
        gb = P.sbuf("gb", [128, D_MODEL], F32)
        P.dma(gb[:], V(g_row.partition_broadcast(128), ("w", "fin", "g")))
        xt = [P.sbuf(f"xt{i}", [128, D_MODEL], F32) for i in range(4)]
        junk = P.sbuf("junk", [128, D_MODEL], BF16)
        ss = [P.sbuf(f"ss{i}", [128, 1], F32) for i in range(4)]
        for ti in range(C.NTOK // 128):
            x = xt[ti % 4]
            s = ss[ti % 4]
            rows = (slice(ti * 128, (ti + 1) * 128), slice(None))
            P.dma(x[:], src.k(("r", ti), rows))
            P.act(junk[:], x[:], AF.Square, accum_out=s[:])
            P.rsqrt(s[:], s[:], 1.0 / D_MODEL, EPS)
            P.stt(x[:], x[:], s[:], gb[:], ALU.mult, ALU.mult)
            P.dma(dst.k(("r", ti), rows), x[:], q=POOL)


def copy_phase(P, C, src, dst):
    with P.phase():
        xt = [P.sbuf(f"xt{i}", [128, D_MODEL], F32) for i in range(4)]
        for ti in range(C.NTOK // 128):
            rows = (slice(ti * 128, (ti + 1) * 128), slice(None))
            P.dma(xt[ti % 4][:], src.k(("r", ti), rows))
            P.dma(dst.k(("r", ti), rows), xt[ti % 4][:], q=POOL)


def build(C):
    nc = bass.Bass("TRN2", target_bir_lowering=False)
    with ExitStack() as es:
        P = Prog(nc, es)
        K = Ctx()
        x_in = P.dram("x", [C.NTOK, D_MODEL], F32, kind="ExternalInput")
        y_out = P.dram("y", [C.NTOK, D_MODEL], F32, kind="ExternalOutput")
        xr = P.dram("xr", [C.NTOK, D_MODEL], F32)
        W = {}
        for name, shp in INPUT_SHAPES.items():
            W[name] = nc.dram_tensor(name, list(shp), F32, kind="ExternalInput").ap()
        SC = {}
        if C.mixers and any(l % 3 in (1, 2) for l in C.layers):
            SC["qs"] = P.dram("sc_qs", [8, 128, C.NTOK], BF16)
            SC["ks"] = P.dram("sc_ks", [8, 128, C.NTOK], BF16)
            SC["vs"] = P.dram("sc_vs", [C.NTOK, 1024], BF16)
            SC["os"] = P.dram("sc_os", [8, 128, C.NTOK], BF16)
        if C.mixers and any(l % 3 == 2 for l in C.layers):
            NT_ = C.S // 128
            SC["kl"] = P.dram("sc_kl", [3, 128, C.NTOK], BF16)
            SC["kvtm"] = P.dram("sc_kvtm", [C.NTOK, 256], BF16)
            SC["kiT"] = P.dram("sc_kiT", [64, C.NTOK], BF16)
            SC["wi"] = P.dram("sc_wi", [C.NTOK, 8], F32)
            SC["qaT"] = P.dram("sc_qaT", [8, 2, 128, C.NTOK], BF16)
            SC["qrT"] = P.dram("sc_qrT", [8, 33, C.NTOK], BF16)
            SC["qiT"] = P.dram("sc_qiT", [8, 64, C.NTOK], BF16)
            SC["negT"] = P.dram("sc_negT", [C.NSEQ, NT_, 128, C.S], BF16)
        SC["ropeA"] = nc.dram_tensor("ropeA", [C.S, 256], F32, kind="ExternalInput").ap()
        SC["ropeB"] = nc.dram_tensor("ropeB", [C.S, 128], F32, kind="ExternalInput").ap()
        make_consts(P, K)
        P.flush()
        cur = x_in
        for li in C.layers:
            kind, j = li % 3, li // 3
            if C.ffn:
                ffn_phase(P, K, C, cur, xr, W['ffn1_norm'][li], W['ffn1_w_gu'][li], W['ffn1_w_down'][li], f"f1_{li}")
                cur = xr
            if C.mixers:
                if cur is x_in:
                    copy_phase(P, C, x_in, xr)
                    cur = xr
                if kind == 0:
                    gdn_phase(P, K, C, xr, W, li, j)
                elif kind == 1:
                    sb_phase(P, K, C, xr, W, li, j, SC)
                else:
                    dsa_phase(P, K, C, xr, W, li, j, SC)
            if C.ffn:
                ffn_phase(P, K, C, cur, xr, W['ffn2_norm'][li], W['ffn2_w_gu'][li], W['ffn2_w_down'][li], f"f2_{li}")
                cur = xr
        if C.final:
            final_phase(P, K, C, cur, y_out, W['final_norm'])
        else:
            copy_phase(P, C, cur, y_out)
        P.flush()
        C.n_ops = P.n_emitted
    return nc


def sl(a, b):
    return slice(a, b)


ALL = slice(None)


def run_interleaved(gens):
    alive = list(gens)
    while alive:
        for g in list(alive):
            try:
                next(g)
            except StopIteration:
                alive.remove(g)


def gdn_phase(P, K, C, xr, W, li, j):
    S = C.S
    NT = S // 128
    with P.phase():
        win = P.sbuf("win", [128, 8, 4112], BF16)
        wsrc = W['gdn_w_in'][j].rearrange("(kc p) f -> p kc f", p=128)
        pieces = [(i * 512, (i + 1) * 512) for i in range(8)] + [(4096, 4112)]
        for pi, (a, b) in enumerate(pieces):
            P.dma(win.k(pi, (ALL, ALL, sl(a, b))), V(wsrc[:, :, a:b], ("w", "gdn_in")), q=POOL)
        wout = P.sbuf("wout", [128, 8, 1024], BF16)
        P.dma(wout[:], V(W['gdn_w_out'][j].rearrange("(kc p) d -> p kc d", p=128), ("w", "gdn_out")), q=POOL)
        gb = P.sbuf("gb", [128, D_MODEL], F32)
        P.dma(gb[:], V(W['mix_norm'][li].partition_broadcast(128), ("w", "mixg")))
        gnb8 = P.sbuf("gnb8", [128, 1024], F32)
        for h in range(8):
            P.dma(gnb8[:, h * 128:(h + 1) * 128], V(W['gdn_norm'][j].partition_broadcast(128), ("w", "gn")))
        alb = P.sbuf("alb", [128, 8], F32)
        dtb = P.sbuf("dtb", [128, 8], F32)
        nea = P.sbuf("nea", [128, 8], F32)
        P.dma(alb[:], V(W['gdn_a_log'][j].partition_broadcast(128), ("w", "alog")))
        P.dma(dtb[:], V(W['gdn_dt_bias'][j].partition_broadcast(128), ("w", "dtb")))
        P.act(nea[:], alb[:], AF.Exp)
        P.ts(nea[:], nea[:], -1.0, ALU.mult)
        cw4 = P.sbuf("cw4", [96, 128], F32)
        P.dma(cw4[:], V(W['gdn_conv'][j].rearrange("i (c p) -> (i c) p", p=128), ("w", "conv")))
        cw = P.sbuf("cw", [128, 96], F32)
        diagw = P.sbuf("diagw", [128, 96, 128], BF16)
        B0 = P.psum("B0", [128, 1024], BF16)
        PB = [None] + [P.psum(f"PB{i}", [128, 512], F32) for i in range(1, 8)]
        P.transpose(PB[1][:, 0:96], cw4[:], K.ident_f[0:96, 0:96])
        P.copy(cw[:], PB[1][:, 0:96])
        for ci in range(96):
            P.ts(diagw.k(ci, (ALL, ci, ALL)), K.ident_f[:], cw[:, ci:ci + 1], ALU.mult)
        xt = [P.sbuf(f"xt{i}", [128, D_MODEL], F32) for i in range(2)]
        hT = [P.sbuf(f"hT{i}", [128, 8, 128], BF16) for i in range(2)]
        scr = (P.sbuf("junk", [128, D_MODEL], BF16), P.sbuf("ss", [128, 1], F32),
               P.sbuf("rstd", [128, 1], F32), P.sbuf("xn", [128, D_MODEL], BF16))
        cb = P.sbuf("cb", [128, 24, 131], BF16)
        sil = [P.sbuf(f"sil{i}", [128, 512], F32) for i in range(2)]
        sqb = [P.sbuf(f"sqb{i}", [128, 512], BF16) for i in range(2)]
        rs = [P.sbuf(f"rs{i}", [128, 512], F32) for i in range(2)]
        qkT = P.sbuf("qkT", [128, 16, 128], BF16)
        vT = P.sbuf("vT", [128, 8, 128], BF16)
        gz = P.sbuf("gz", [128, 1024], F32)
        sm = {n: P.sbuf(n, [128, 8], F32) for n in ("eb", "beta", "nbeta", "ta", "g", "egc", "gtot", "dk", "kdsc")}
        gc = P.sbuf("gc", [128, 16], F32)
        S_f = P.sbuf("S_f", [128, 8, 128], F32)
        S_b = P.sbuf("S_b", [128, 8, 128], BF16)
        gh = [P.sbuf(f"gh{i}", [128, 128], F32) for i in range(4)]
        E2 = [P.sbuf(f"E2{i}", [128, 256], F32) for i in range(4)]
        EMi = [P.sbuf(f"EMi{i}", [128, 128], F32) for i in range(4)]
        EMs = [P.sbuf(f"EMs{i}", [128, 128], F32) for i in range(4)]
        WW = [[P.sbuf(f"WW{p}{i}", [128, 256], F32) for i in range(2)] for p in range(4)]
        Rm = [P.sbuf(f"R{p}", [128, 128], F32) for p in range(4)]
        Rb = [P.sbuf(f"Rb{p}", [128, 128], BF16) for p in range(4)]
        vtm = [P.sbuf(f"vtm{p}", [128, 128], BF16) for p in range(4)]
        Xk = [P.sbuf(f"Xk{h}", [128, 128], BF16) for h in range(8)]
        kdec = [P.sbuf(f"kdec{h}", [128, 128], BF16) for h in range(8)]
        qkd = [P.sbuf(f"qkd{h}", [128, 128], BF16) for h in range(8)]
        qdT = [P.sbuf(f"qdT{h}", [128, 128], BF16) for h in range(8)]
        uinb = [P.sbuf(f"uinb{h}", [128, 128], F32) for h in range(8)]
        wT = [P.sbuf(f"wT{h}", [128, 128], BF16) for h in range(8)]
        uu = [P.sbuf(f"u{h}", [128, 128], BF16) for h in range(8)]
        ssq = [P.sbuf(f"ssq{h}", [128, 1], F32) for h in range(8)]
        rsq = [P.sbuf(f"rsq{h}", [128, 1], F32) for h in range(8)]
        junk2 = [P.sbuf(f"junk2{i}", [128, 128], BF16) for i in range(2)]
        og = P.sbuf("og", [128, 1024], BF16)
        ogT = P.sbuf("ogT", [128, 8, 128], BF16)

        def cs(i, n=1):
            return sl(i * 128, (i + n) * 128)

        for sq_i in range(C.NSEQ):
            for h in range(8):
                P.memset(S_f.k(h, (ALL, h, ALL)), 0.0)
                P.memset(S_b.k(h, (ALL, h, ALL)), 0.0)
            for gq in range(6):
                P.memset(cb.k(gq, (ALL, sl(gq * 4, gq * 4 + 4), sl(0, 3))), 0.0)
            for t in range(NT):
                r0 = sq_i * S + t * 128
                ti = r0 // 128
                xs = xt[t % 2]
                h_ = hT[t % 2]
                rows = (sl(r0, r0 + 128), ALL)
                P.dma(xs[:], xr.k(("r", ti), rows))
                norm_T(P, K, xs[:], gb[:], h_[:, :, :], scr, B0)

                def stage_a(gq):
                    pp = PB[1 + gq % 2]
                    pc = PB[3 + gq % 2]
                    for cc in range(4):
                        c = gq * 4 + cc
                        for kc in range(8):
                            P.mm(pp[:, cs(cc)], win.k(c // 4, (ALL, kc, cs(c))), h_[:, kc, :],
                                 start=(kc == 0), stop=(kc == 7))
                    cbg = cb.k(gq, (ALL, sl(gq * 4, gq * 4 + 4), sl(3, 131)))
                    P.copy(cbg, pp.v(pp.base()[:, :].rearrange("p (c t) -> p c t", t=128)), eng=ACT)
                    yield
                    for cc in range(4):
                        c = gq * 4 + cc
                        for i in range(4):
                            P.mm(pc[:, cs(cc)], diagw.k(i * 24 + c, (ALL, i * 24 + c, ALL)),
                                 cb.k(gq, (ALL, c, sl(i, i + 128))), start=(i == 0), stop=(i == 3))
                    if gq < 4:
                        s_ = sil[gq % 2]
                        P.act(s_[:], pc[:], AF.Silu)
                        P.tt(sqb[gq % 2][:], s_[:], s_[:], ALU.mult, eng=POOL)
                    else:
                        P.act(vT.v(vT.base()[:, (gq - 4) * 4:(gq - 4) * 4 + 4, :]),
                              pc.v(pc.base()[:, :].rearrange("p (c t) -> p c t", t=128)), AF.Silu)
                    P.copy(cb.k(gq, (ALL, sl(gq * 4, gq * 4 + 4), sl(0, 3))),
                           cb.k(gq, (ALL, sl(gq * 4, gq * 4 + 4), sl(128, 131))), eng=POOL)
                    yield
                    if gq < 4:
                        for cc in range(4):
                            P.mm(PB[5][:, cs(cc)], K.ones_b[:], sqb[gq % 2][:, cs(cc)])
                        r_ = rs[gq % 2]
                        if gq < 2:
                            P.rsqrt(r_[:], PB[5][:], 128.0, 128.0 * EPS)
                        else:
                            P.rsqrt(r_[:], PB[5][:], 1.0, EPS)
                        P.tt(qkT.v(qkT.base()[:, gq * 4:gq * 4 + 4, :]),
                             s_.v(s_.base()[:, :].rearrange("p (c t) -> p c t", t=128)),
                             r_.v(r_.base()[:, :].rearrange("p (c t) -> p c t", t=128)), ALU.mult)
                    yield

                gens = [stage_a(gq) for gq in range(6)]
                for step in range(6 + 2):
                    for gq in range(6):
                        if 0 <= step - gq < 3:
                            next(gens[gq])
                for hf in range(2):
                    for kc in range(8):
                        P.mm(PB[1 + hf][:], h_[:, kc, :], win.k(6 + hf, (ALL, kc, sl(3072 + hf * 512, 3072 + (hf + 1) * 512))),
                             start=(kc == 0), stop=(kc == 7))
                    P.act(gz[:, hf * 512:(hf + 1) * 512], PB[1 + hf][:], AF.Silu)
                P.tt(gz[:], gz[:], gnb8[:], ALU.mult, eng=POOL)
                for kc in range(8):
                    P.mm(PB[6][:, 0:16], h_[:, kc, :], win.k(8, (ALL, kc, sl(4096, 4112))), start=(kc == 0), stop=(kc == 7))
                P.act(sm["eb"][:], PB[6][:, 0:8], AF.Exp, scale=-1.0)
                P.tt(sm["ta"][:], PB[6][:, 8:16], dtb[:], ALU.add)
                P.ts(sm["eb"][:], sm["eb"][:], 1.0, ALU.add)
                P.recip(sm["beta"][:], sm["eb"][:])
                P.ts(sm["nbeta"][:], sm["beta"][:], -1.0, ALU.mult)
                P.act(sm["ta"][:], sm["ta"][:], AF.Exp)
                P.act(sm["ta"][:], sm["ta"][:], AF.Ln, bias=1.0)
                P.tt(sm["g"][:], sm["ta"][:], nea[:], ALU.mult)
                P.mm(PB[7][:, 0:8], K.m_ge[:], sm["g"][:])
                P.mm(PB[7][:, 8:16], K.ones_f[:], sm["g"][:])
                P.copy(gc[:], PB[7][:, 0:16])
                P.act(sm["egc"][:], gc[:, 0:8], AF.Exp)
                P.act(sm["gtot"][:], gc[:, 8:16], AF.Exp)
                P.tt(sm["dk"][:], gc[:, 8:16], gc[:, 0:8], ALU.subtract)
                P.act(sm["kdsc"][:], sm["dk"][:], AF.Exp)
                beta, nbeta, g = sm["beta"], sm["nbeta"], sm["g"]

                def head_prep(h):
                    p = h % 4
                    bD, bW, bR, bU = PB[1 + h % 2], PB[3 + h % 2], PB[5 + h % 2], PB[7]
                    kTh = qkT[:, 8 + h, :]
                    qTh = qkT[:, h, :]
                    P.ts(gh[p][:], K.m_ge[:], g[:, h:h + 1], ALU.mult)
                    P.mm(bD[:, 0:128], K.ones_f[:], gh[p][:])
                    P.mm(bD[:, 128:256], K.m_lt[:], gh[p][:])
                    P.mm(bD[:, 256:384], kTh, kTh)
                    P.mm(bD[:, 384:512], kTh, qTh)
                    P.act(E2[p][:], bD[:, 0:256], AF.Exp)
                    P.tt(EMi[p][:], E2[p][:, 128:256], K.m_ge[:], ALU.mult, eng=POOL)
                    P.tt(EMs[p][:], E2[p][:, 128:256], K.m_gt[:], ALU.mult, eng=POOL)
                    W_ = WW[p]
                    R_ = Rm[p]
                    P.stt(W_[0][:, 0:128], bD[:, 256:384], nbeta[:, h:h + 1], EMs[p][:], ALU.mult, ALU.mult)
                    P.tt(qkd[h][:], bD[:, 384:512], EMi[p][:], ALU.mult)
                    P.tt(qdT[h][:], qTh, E2[p][:, 0:128], ALU.mult, eng=POOL)
                    yield
                    P.transpose(bW[:, 128:256], W_[0][:, 0:128], K.ident_f[:])
                    P.copy(W_[0][:, 128:256], bW[:, 128:256], eng=ACT)
                    P.tt(R_[:], W_[0][:, 0:128], K.ident_f[:], ALU.add)
                    yield
                    for m in range(1, 7):
                        a, b = (m - 1) % 2, m % 2
                        Wa, WTa = W_[a][:, 0:128], W_[a][:, 128:256]
                        if m < 6:
                            P.mm(bW[:, 0:128], WTa, Wa)
                        P.mm(bW[:, 128:256], Wa, WTa)
                        if m < 6:
                            P.copy(W_[b][:], bW[:, 0:256], eng=(ACT if m % 2 else DVE))
                        else:
                            P.copy(W_[b][:, 128:256], bW[:, 128:256], eng=ACT)
                        yield
                        P.mm(bR[:, 0:128], W_[b][:, 128:256], R_[:])
                        P.tt(R_[:], bR[:, 0:128], R_[:], ALU.add)
                        yield
                    P.copy(Rb[p][:], R_[:], eng=ACT)
                    P.transpose(B0[:, 0:128], kTh, K.ident_b[:])
                    P.transpose(B0[:, 128:256], vT[:, h, :], K.ident_b[:])
                    P.ts(Xk[h][:], B0[:, 0:128], sm["egc"][:, h:h + 1], ALU.mult)
                    P.ts(kdec[h][:], B0[:, 0:128], sm["kdsc"][:, h:h + 1], ALU.mult)
                    P.copy(vtm[p][:], B0[:, 128:256], eng=ACT)
                    yield
                    P.mm(bU[:, 0:128], Rb[p][:], vtm[p][:])
                    P.mm(bU[:, 128:256], Xk[h][:], Rb[p][:])
                    P.ts(uinb[h][:], bU[:, 0:128], beta[:, h:h + 1], ALU.mult)
                    P.copy(wT[h][:], bU[:, 128:256], eng=ACT)
                    yield

                for h0 in range(0, 8, 4):
                    run_interleaved([head_prep(h0 + q) for q in range(4)])

                def recur(hg):
                    hs = [hg * 4 + q for q in range(4)]
                    bT, bO = PB[1 + 2 * hg], PB[2 + 2 * hg]
                    for q, h in enumerate(hs):
                        P.mm(bT[:, cs(q)], wT[h][:], S_b.k(h, (ALL, h, ALL)))
                    yield
                    for q, h in enumerate(hs):
                        P.stt(uu[h][:], bT[:, cs(q)], nbeta[:, h:h + 1], uinb[h][:], ALU.mult, ALU.add)
                    yield
                    for q, h in enumerate(hs):
                        P.mm(bO[:, cs(q)], qdT[h][:], S_b.k(h, (ALL, h, ALL)), start=True, stop=False)
                        P.mm(bO[:, cs(q)], qkd[h][:], uu[h][:], start=False, stop=True)
                    for q, h in enumerate(hs):
                        P.mm(bT[:, cs(q)], kdec[h][:], uu[h][:])
                    yield
                    for q, h in enumerate(hs):
                        Sfh = S_f.k(h, (ALL, h, ALL))
                        P.stt(Sfh, Sfh, sm["gtot"][:, h:h + 1], bT[:, cs(q)], ALU.mult, ALU.add)
                        P.copy(S_b.k(h, (ALL, h, ALL)), Sfh, eng=POOL)
                    yield
                    for q, h in enumerate(hs):
                        P.act(junk2[hg][:], bO[:, cs(q)], AF.Square, accum_out=ssq[h][:])
                    yield
                    for q, h in enumerate(hs):
                        P.rsqrt(rsq[h][:], ssq[h][:], 1.0 / 128, EPS)
                    yield
                    for q, h in enumerate(hs):
                        P.stt(og.k(h, (ALL, cs(h))), bO[:, cs(q)], rsq[h][:], gz[:, cs(h)], ALU.mult, ALU.mult)
                    yield

                run_interleaved([recur(0), recur(1)])
                for hc in range(8):
                    P.transpose(B0[:, cs(hc)], og.k(hc, (ALL, cs(hc))), K.ident_b[:])
                P.copy(ogT[:, :, :], B0.v(B0.base()[:, :].rearrange("p (c t) -> p c t", t=128)), eng=ACT)
                for half in range(2):
                    bo = PB[5 + half]
                    for hc in range(8):
                        P.mm(bo[:], ogT[:, hc, :], wout[:, hc, half * 512:(half + 1) * 512], start=(hc == 0), stop=(hc == 7))
                    P.tt(xs[:, half * 512:(half + 1) * 512], bo[:], xs[:, half * 512:(half + 1) * 512], ALU.add)
                P.dma(xr.k(("r", ti), rows), xs[:], q=POOL)


_uid = [0]


def dv(t, ap):
    _uid[0] += 1
    return V(ap, (t.id, ("u", _uid[0]), False))


def outproj_phase(P, K, C, xr, w_out, os_):
    with P.phase():
        wout = P.sbuf("wout", [128, 8, 1024], BF16)
        P.dma(wout[:], V(w_out.rearrange("(kc p) d -> p kc d", p=128), ("w", "wout")), q=POOL)
        PB = [P.psum(f"PO{i}", [128, 512], F32) for i in range(4)]
        oT = [P.sbuf(f"oT{i}", [128, 8, 512], BF16) for i in range(2)]
        xt = [P.sbuf(f"xt{i}", [128, D_MODEL], F32) for i in range(4)]
        nb = 0
        for ti in range(C.NTOK // 512):
            r0 = ti * 512
            o_ = oT[ti % 2]
            P.dma(o_[:], dv(os_, os_.base()[:, :, r0:r0 + 512].rearrange("h p t -> p h t")))
            for s_ in range(4):
                x = xt[s_]
                rows = (sl(r0 + s_ * 128, r0 + (s_ + 1) * 128), ALL)
                P.dma(x[:], xr.k(("r", ti * 4 + s_), rows))
                for half in range(2):
                    pb = PB[nb % 4]
                    nb += 1
                    for h in range(8):
                        P.mm(pb[:], o_[:, h, s_ * 128:(s_ + 1) * 128], wout[:, h, half * 512:(half + 1) * 512],
                             start=(h == 0), stop=(h == 7))
                    P.tt(x[:, half * 512:(half + 1) * 512], pb[:], x[:, half * 512:(half + 1) * 512], ALU.add)
                P.dma(xr.k(("r", ti * 4 + s_), rows), x[:], q=POOL)


def sb_phase(P, K, C, xr, W, li, j, SC):
    S = C.S
    NT = S // 128
    NG = S // 512
    qs, ks, vs, os_ = SC["qs"], SC["ks"], SC["vs"], SC["os"]
    scale = 128.0 ** -0.5

    def cs(i, n=1):
        return sl(i * 128, (i + n) * 128)

    with P.phase():
        win = P.sbuf("win", [128, 8, 3072], BF16)
        wsrc = W['sb_w_in'][j].rearrange("(kc p) f -> p kc f", p=128)
        for pi in range(6):
            P.dma(win.k(pi, (ALL, ALL, sl(pi * 512, (pi + 1) * 512))), V(wsrc[:, :, pi * 512:(pi + 1) * 512], ("w", "sb_in")), q=POOL)
        gb = P.sbuf("gb", [128, D_MODEL], F32)
        P.dma(gb[:], V(W['mix_norm'][li].partition_broadcast(128), ("w", "mixg")))
        B0 = P.psum("B0", [128, 1024], BF16)
        PB = [None] + [P.psum(f"PB{i}", [128, 512], F32) for i in range(1, 8)]
        xt = [P.sbuf(f"xt{i}", [128, D_MODEL], F32) for i in range(4)]
        hT = [P.sbuf(f"hT{i}", [128, 8, 512], BF16) for i in range(2)]
        scr = (P.sbuf("junk", [128, D_MODEL], BF16), P.sbuf("ss", [128, 1], F32),
               P.sbuf("rstd", [128, 1], F32), P.sbuf("xn", [128, D_MODEL], BF16))
        qst = [P.sbuf(f"qst{i}", [128, 512], BF16) for i in range(4)]
        vst = [P.sbuf(f"vst{i}", [128, 1024], BF16) for i in range(2)]
        for ti in range(C.NTOK // 512):
            r0 = ti * 512
            h_ = hT[ti % 2]
            for s_ in range(4):
                rows = (sl(r0 + s_ * 128, r0 + (s_ + 1) * 128), ALL)
                P.dma(xt[s_][:], xr.k(("r", ti * 4 + s_), rows))
                norm_T(P, K, xt[s_][:], gb[:], h_[:, :, s_ * 128:(s_ + 1) * 128], scr, B0)
            for c in range(16):
                pb = PB[1 + c % 4]
                for kc in range(8):
                    P.mm(pb[:], win.k(c // 4, (ALL, kc, cs(c))), h_[:, kc, :], start=(kc == 0), stop=(kc == 7))
                st = qst[c % 4]
                if c < 8:
                    P.act(st[:], pb[:], AF.Copy, scale=scale)
                    P.dma(qs.k(("q", c, ti), (c, ALL, sl(r0, r0 + 512))), st[:], q=SP)
                else:
                    P.copy(st[:], pb[:], eng=DVE)
                    P.dma(ks.k(("k", c - 8, ti), (c - 8, ALL, sl(r0, r0 + 512))), st[:], q=SP)
            for s_ in range(4):
                vv = vst[s_ % 2]
                for half in range(2):
                    pb = PB[5 + half]
                    for kc in range(8):
                        P.mm(pb[:], h_[:, kc, s_ * 128:(s_ + 1) * 128],
                             win.k(4 + half, (ALL, kc, sl(2048 + half * 512, 2048 + (half + 1) * 512))),
                             start=(kc == 0), stop=(kc == 7))
                    P.copy(vv[:, half * 512:(half + 1) * 512], pb[:], eng=(ACT if half else DVE))
                P.dma(vs.k(("v", ti * 4 + s_), (sl(r0 + s_ * 128, r0 + (s_ + 1) * 128), ALL)), vv[:], q=SP)

    with P.phase():
        PA = [P.psum(f"PA{i}", [128, 512], F32) for i in range(6)]
        negtri_f = P.sbuf("negtri_f", [128, 128], F32)
        negtri = P.sbuf("negtri", [128, 128], BF16)
        negones = P.sbuf("negones", [128, 128], BF16)
        P.tt(negtri_f[:], K.m_lt[:], K.ident_f[:], ALU.add)
        P.ts(negtri[:], negtri_f[:], -1.0, ALU.mult)
        P.memset(negones[:], -1.0)
        maskf = [P.sbuf(f"maskf{r}", [128, 512], F32) for r in range(4)]
        maskb = [P.sbuf(f"maskb{r}", [128, 512], BF16) for r in range(4)]
        maskf_ = maskf
        maskf = maskb
        for r in range(4):
            P.memset(maskf_[r][:], 1.0)
            P.op(POOL, (lambda t, r: lambda e: e.affine_select(t[:].ap, t[:].ap, [[1, 512]], ALU.is_gt, 0.0,
                                                               base=-r * 128, channel_multiplier=-1))(maskf_[r], r),
                 [maskf_[r][:]], [maskf_[r][:]])
            P.copy(maskb[r][:], maskf_[r][:])
        hd = [[{"k": P.sbuf(f"kT{a}{b}", [128, S], BF16), "q": P.sbuf(f"qT{a}{b}", [128, S], BF16),
                "v": P.sbuf(f"v{a}{b}", [128, NT, 128], BF16)} for b in range(2)] for a in range(2)]
        wk = [{"e": [P.sbuf(f"e{b}{i}", [128, 512], F32) for i in range(2)],
               "sp": [P.sbuf(f"sp{b}{i}", [128, 512], BF16) for i in range(2)],
               "accb": P.sbuf(f"accb{b}", [128, 512], BF16),
               "att": [P.sbuf(f"att{b}{i}", [128, 512], BF16) for i in range(2)],
               "acc": P.sbuf(f"acc{b}", [128, 512], F32),
               "ost": P.sbuf(f"ost{b}", [128, 512], BF16),
               "banks": (PA[3 * b], PA[3 * b + 1], PA[3 * b + 2])} for b in range(2)]

        def load_pair(sq_i, hp, a):
            for b in range(2):
                h = 2 * hp + b
                d = hd[a][b]
                cols = sl(sq_i * S, (sq_i + 1) * S)
                P.dma(d["k"][:], ks.k(("kall",), (h, ALL, cols)))
                P.dma(d["q"][:], qs.k(("qall",), (h, ALL, cols)))
                P.dma(d["v"][:], vs.v(vs.base()[sq_i * S:(sq_i + 1) * S, h * 128:(h + 1) * 128].rearrange("(t p) d -> p t d", p=128), ("vall",)))

        def stream(sq_i, h, g, d, w):
            bA, bB, bO = w["banks"]
            kT, qT, v = d["k"], d["q"], d["v"]
            qcols = sl(g * 512, (g + 1) * 512)
            nkb = 4 * g + 4
            for idx, kb in enumerate(range(nkb - 1, -1, -1)):
                first = idx == 0
                last = kb == 0
                r = kb - 4 * g
                e, sp, att = w["e"][idx % 2], w["sp"][idx % 2], w["att"][idx % 2]
                P.mm(bA[:], kT[:, cs(kb)], qT[:, qcols])
                yield
                P.act(e[:], bA[:], AF.Exp)
                P.act(sp[:], e[:], AF.Ln, bias=1.0)
                if r >= 0:
                    P.tt(sp[:], sp[:], maskf[r][:], ALU.mult, eng=POOL)
                yield
                P.mm(bB[:], kT[:, cs(kb)], qT[:, qcols], start=True, stop=False)
                P.mm(bB[:], negtri[:], sp[:], start=False, stop=first)
                if not first:
                    P.mm(bB[:], negones[:], w["accb"][:], start=False, stop=True)
                yield
                P.act(att[:], bB[:], AF.Exp)
                if r >= 0:
                    P.tt(att[:], att[:], maskb[r][:], ALU.mult)
                if not last:
                    if first:
                        P.copy(w["acc"][:], sp[:])
                    else:
                        P.tt(w["acc"][:], w["acc"][:], sp[:], ALU.add)
                    P.copy(w["accb"][:], w["acc"][:])
                yield
                P.mm(bO[:], v[:, kb, :], att[:], start=first, stop=last)
                yield
            P.copy(w["ost"][:], bO[:])
            P.dma(os_.k(("o", h, sq_i, g), (h, ALL, sl(sq_i * S + g * 512, sq_i * S + (g + 1) * 512))), w["ost"][:], q=POOL)

        pairs = [(sq_i, hp) for sq_i in range(C.NSEQ) for hp in range(4)]
        load_pair(pairs[0][0], pairs[0][1], 0)
        for pi, (sq_i, hp) in enumerate(pairs):
            a = pi % 2
            if pi + 1 < len(pairs):
                load_pair(pairs[pi + 1][0], pairs[pi + 1][1], 1 - a)
            for g in range(NG):
                run_interleaved([stream(sq_i, 2 * hp + b, g, hd[a][b], wk[b]) for b in range(2)])

    outproj_phase(P, K, C, xr, W['sb_w_out'][j], os_)


def rope_tm(P, x1, x2, cos, sin, o1, o2, t1, t2):
    P.tt(t1, x1, cos, ALU.mult)
    P.tt(t2, x2, sin, ALU.mult)
    P.tt(o1, t1, t2, ALU.subtract)
    P.tt(t1, x2, cos, ALU.mult)
    P.tt(t2, x1, sin, ALU.mult)
    P.tt(o2, t1, t2, ALU.add)


def dsa_phase(P, K, C, xr, W, li, j, SC):
    S = C.S
    NT = S // 128
    NG = S // 512
    topk = C.topk
    NIT = 16
    att_scale = 128.0 ** -0.5
    widx_scale = (8.0 ** -0.5) * (64.0 ** -0.5)
    NEG = -30000.0
    kl, kvtm, kiT, wiS = SC["kl"], SC["kvtm"], SC["kiT"], SC["wi"]
    qaT, qrT, qiT, negT, os_ = SC["qaT"], SC["qrT"], SC["qiT"], SC["negT"], SC["os"]
    ropeA, ropeB = SC["ropeA"], SC["ropeB"]

    def cs(i, n=1):
        return sl(i * 128, (i + n) * 128)

    with P.phase():
        B0 = P.psum("B0", [128, 1024], BF16)
        PB = [None] + [P.psum(f"PB{i}", [128, 512], F32) for i in range(1, 8)]
        win = P.sbuf("win", [128, 8, 744], BF16)
        P.dma(win[:], V(W['dsa_w_in'][j].rearrange("(kc p) f -> p kc f", p=128), ("w", "dsa_in")), q=POOL)
        wuq4 = W['dsa_w_uq'][j].rearrange("(kc p) (h d) -> p kc h d", p=128, d=128)
        wuq_r = P.sbuf("wuq_r", [128, 3, 8, 32], BF16)
        for kc in range(3):
            P.dma(wuq_r[:, kc, :, :], V(wuq4[:, kc, :, 0:32], ("w", "uq_r")), q=POOL)
        wuq_n = P.sbuf("wuq_n", [128, 3, 8, 96], BF16)
        for kc in range(3):
            P.dma(wuq_n[:, kc, :, :], V(wuq4[:, kc, :, 32:128], ("w", "uq_n")), q=POOL)
        wuk = P.sbuf("wuk", [128, 2, 768], BF16)
        P.dma(wuk[:], V(W['dsa_w_uk'][j].rearrange("(cc p) f -> p cc f", p=128), ("w", "uk")), q=POOL)
        wqidx = P.sbuf("wqidx", [128, 3, 512], BF16)
        P.dma(wqidx[:], V(W['dsa_w_qidx'][j].rearrange("(kc p) f -> p kc f", p=128), ("w", "qidx")), q=POOL)
        gb = P.sbuf("gb", [128, D_MODEL], F32)
        P.dma(gb[:], V(W['mix_norm'][li].partition_broadcast(128), ("w", "mixg")))
        cqg = P.sbuf("cqg", [128, 384], F32)
        ckvg = P.sbuf("ckvg", [128, 256], F32)
        kidxg = P.sbuf("kidxg", [128, 64], F32)
        P.dma(cqg[:], V(W['dsa_cq_norm'][j].partition_broadcast(128), ("w", "cqg")))
        P.dma(ckvg[:], V(W['dsa_ckv_norm'][j].partition_broadcast(128), ("w", "ckvg")))
        P.dma(kidxg[:], V(W['dsa_kidx_norm'][j].partition_broadcast(128), ("w", "kidxg")))
        AT = P.sbuf("AT", [96, 8, 384], BF16)
        BT = P.sbuf("BT", [96, 8, 256], BF16)
        Wabs = P.sbuf("Wabs", [128, 3, 8, 256], BF16)
        for h in range(8):
            for kc in range(3):
                P.transpose(B0[0:96, cs(kc)], wuq_n[:, kc, h, :], K.ident_b[:])
            for cc in range(2):
                P.transpose(B0[0:96, cs(3 + cc)], wuk[:, cc, h * 96:(h + 1) * 96], K.ident_b[:])
            P.copy(AT[:, h, :], B0[0:96, 0:384], eng=ACT)
            P.copy(BT[:, h, :], B0[0:96, 384:640])
        for h in range(8):
            for kc in range(3):
                n = h * 3 + kc
                pb = PB[1 + n % 4]
                P.mm(pb[:, 0:256], AT[:, h, cs(kc)], BT[:, h, :])
                P.copy(Wabs[:, kc, h, :], pb[:, 0:256], eng=(ACT if n % 2 else DVE))
        xt = [P.sbuf(f"xt{i}", [128, D_MODEL], F32) for i in range(2)]
        hT = [P.sbuf(f"hT{i}", [128, 8, 128], BF16) for i in range(2)]
        scr = (P.sbuf("junk", [128, D_MODEL], BF16), P.sbuf("ss", [128, 1], F32),
               P.sbuf("rstd", [128, 1], F32), P.sbuf("xn", [128, D_MODEL], BF16))
        junk = scr[0]
        ra = [P.sbuf(f"ra{i}", [128, 256], F32) for i in range(2)]
        rb = [P.sbuf(f"rb{i}", [128, 128], F32) for i in range(2)]
        pj = P.sbuf("pj", [128, 744], F32)
        sA = {n: P.sbuf(n, [128, 1], F32) for n in ("ssA", "rsA", "ssB", "rsB", "ssC", "rsC", "kn2a", "kn2b", "kn2", "rm", "nkmax")}
        km1 = P.sbuf("km1", [1, 1], F32)
        cqn = P.sbuf("cqn", [128, 384], BF16)
        cqT = P.sbuf("cqT", [128, 3, 128], BF16)
        ckv = P.sbuf("ckv", [128, 256], BF16)
        ckT = P.sbuf("ckT", [128, 2, 128], BF16)
        kra = P.sbuf("kra", [128, 33], F32)
        krb = P.sbuf("krb", [128, 33], BF16)
        krT = P.sbuf("krT", [33, 128], BF16)
        t1 = P.sbuf("t1", [128, 128], F32)
        t2 = P.sbuf("t2", [128, 128], F32)
        kin = P.sbuf("kin", [128, 64], F32)
        kib = P.sbuf("kib", [128, 64], BF16)
        kiTt = P.sbuf("kiTt", [64, 128], BF16)
        wit = P.sbuf("wit", [128, 8], F32)
        qab = P.sbuf("qab", [128, 16, 128], BF16)
        sqa = P.sbuf("sqa", [128, 16, 128], BF16)
        qr = P.sbuf("qr", [128, 8, 32], F32)
        qra = P.sbuf("qra", [128, 8, 33], F32)
        qrb = P.sbuf("qrb", [128, 8, 33], BF16)
        qrTt = P.sbuf("qrTt", [33, 8, 128], BF16)
        t3 = P.sbuf("t3", [128, 8, 32], F32)
        qr2 = P.sbuf("qr2", [128, 8], F32)
        qn = P.sbuf("qn", [128, 8], F32)
        qi = P.sbuf("qi", [128, 8, 64], F32)
        qib = P.sbuf("qib", [128, 8, 64], BF16)
        qiTt = P.sbuf("qiTt", [64, 8, 128], BF16)
        P.memset(kra[:, 32:33], 1.0)

        def v3(t, a, b, d):
            return t.v(t.base()[:, a:b].rearrange("p (h d) -> p h d", d=d))

        for sq_i in range(C.NSEQ):
            P.memset(sA["rm"][:], 0.0)
            for t in range(NT):
                r0 = sq_i * S + t * 128
                ti = r0 // 128
                toks = sl(r0, r0 + 128)
                xs, h_ = xt[t % 2], hT[t % 2]
                ra_, rb_ = ra[t % 2], rb[t % 2]
                P.dma(xs[:], xr.k(("r", ti), (toks, ALL)))
                P.dma(ra_[:], V(ropeA[t * 128:(t + 1) * 128, :], ("w", "ropeA")))
                P.dma(rb_[:], V(ropeB[t * 128:(t + 1) * 128, :], ("w", "ropeB")))
                norm_T(P, K, xs[:], gb[:], h_[:, :, :], scr, B0)
                for kc in range(8):
                    P.mm(PB[1][:], h_[:, kc, :], win[:, kc, 0:512], start=(kc == 0), stop=(kc == 7))
                for kc in range(8):
                    P.mm(PB[2][:, 0:232], h_[:, kc, :], win[:, kc, 512:744], start=(kc == 0), stop=(kc == 7))
                P.copy(pj[:, 0:512], PB[1][:], eng=ACT)
                P.copy(pj[:, 512:744], PB[2][:, 0:232])
                P.act(junk[:, 0:384], pj[:, 0:384], AF.Square, accum_out=sA["ssA"][:])
                P.rsqrt(sA["rsA"][:], sA["ssA"][:], 1.0 / 384, EPS)
                P.stt(cqn[:], pj[:, 0:384], sA["rsA"][:], cqg[:], ALU.mult, ALU.mult)
                for cc in range(3):
                    P.transpose(B0[:, cs(cc)], cqn[:, cs(cc)], K.ident_b[:])
                P.copy(cqT[:, :, :], B0.v(B0.base()[:, 0:384].rearrange("p (c t) -> p c t", t=128)), eng=ACT)
                P.act(junk[:, 0:256], pj[:, 384:640], AF.Square, accum_out=sA["ssB"][:])
                P.rsqrt(sA["rsB"][:], sA["ssB"][:], 1.0 / 256, EPS)
                P.stt(ckv[:], pj[:, 384:640], sA["rsB"][:], ckvg[:], ALU.mult, ALU.mult)
                P.act(junk[:, 0:256], ckv[:], AF.Square, accum_out=sA["kn2a"][:])
                P.dma(dv(kvtm, kvtm.base()[toks, :]), ckv[:])
                for cc in range(2):
                    P.transpose(B0[:, cs(cc)], ckv[:, cs(cc)], K.ident_b[:])
                P.copy(ckT[:, :, :], B0.v(B0.base()[:, 0:256].rearrange("p (c t) -> p c t", t=128)))
                P.dma(dv(kl, kl.base()[0:2, :, toks].rearrange("c p t -> p c t")), ckT[:])
                rope_tm(P, pj[:, 640:656], pj[:, 656:672], ra_[:, 0:16], ra_[:, 128:144],
                        kra[:, 0:16], kra[:, 16:32], t1[:, 0:16], t2[:, 0:16])
                P.act(junk[:, 0:32], kra[:, 0:32], AF.Square, accum_out=sA["kn2b"][:])
                P.tt(sA["kn2"][:], sA["kn2a"][:], sA["kn2b"][:], ALU.add)
                P.tt(sA["rm"][:], sA["rm"][:], sA["kn2"][:], ALU.max)
                P.copy(krb[:], kra[:], eng=POOL)
                P.transpose(B0[0:33, 0:128], krb[:], K.ident_b[:])
                P.copy(krT[:], B0[0:33, 0:128])
                P.dma(dv(kl, kl.base()[2, 0:33, toks]), krT[:])
                P.act(junk[:, 0:64], pj[:, 672:736], AF.Square, accum_out=sA["ssC"][:])
                P.rsqrt(sA["rsC"][:], sA["ssC"][:], 1.0 / 64, EPS)
                P.stt(kin[:], pj[:, 672:736], sA["rsC"][:], kidxg[:], ALU.mult, ALU.mult)
                P.copy(kib[:], kin[:], eng=POOL)
                rope_tm(P, kin[:, 0:8], kin[:, 8:16], rb_[:, 0:8], rb_[:, 64:72],
                        kib[:, 0:8], kib[:, 8:16], t1[:, 0:8], t2[:, 0:8])
                P.transpose(B0[0:64, 0:128], kib[:], K.ident_b[:])
                P.copy(kiTt[:], B0[0:64, 0:128], eng=ACT)
                P.dma(dv(kiT, kiT.base()[:, toks]), kiTt[:])
                P.ts(wit[:], pj[:, 736:744], widx_scale, ALU.mult)
                P.dma(dv(wiS, wiS.base()[toks, :]), wit[:])
                P.transpose(PB[3][0:1, 0:128], sA["rm"][:], K.ident_f[:])
                P.reduce(km1[:], PB[3][0:1, 0:128], ALU.max)
                P.act(km1[:], km1[:], AF.Ln, bias=1e-30)
                P.act(km1[:], km1[:], AF.Exp, scale=0.5)
                P.ts(km1[:], km1[:], -1.0, ALU.mult)
                P.mm(PB[3][:, 128:129], K.ones_f[0:1, 0:128], km1[0:1, 0:1])
                P.copy(sA["nkmax"][:], PB[3][:, 128:129])
                for b4 in range(4):
                    pb = PB[4 + b4 % 2]
                    for i in range(4):
                        idx = b4 * 4 + i
                        h, cc = idx // 2, idx % 2
                        for kc in range(3):
                            P.mm(pb[:, cs(i)], Wabs[:, kc, h, cs(cc)], cqT[:, kc, :], start=(kc == 0), stop=(kc == 2))
                    P.copy(qab[:, b4 * 4:(b4 + 1) * 4, :], pb.v(pb.base()[:, :].rearrange("p (c t) -> p c t", t=128)),
                           eng=(ACT if b4 % 2 else DVE))
                P.dma(dv(qaT, qaT.base()[:, :, :, toks].rearrange("h c p t -> p (h c) t")), qab[:])
                P.tt(sqa[:], qab[:], qab[:], ALU.mult, eng=POOL)
                for idx in range(16):
                    h, cc = idx // 2, idx % 2
                    P.mm(PB[6][:, h:h + 1], sqa[:, idx, :], K.ones_b[:, 0:1], start=(cc == 0), stop=(cc == 1))
                for kc in range(3):
                    P.mm(PB[7][:, 0:256], cqT[:, kc, :], wuq_r.v(wuq_r.base()[:, kc, :, :].rearrange("p h d -> p (h d)")),
                         start=(kc == 0), stop=(kc == 2))
                P.copy(qr[:, :, :], PB[7].v(PB[7].base()[:, 0:256].rearrange("p (h d) -> p h d", d=32)), eng=ACT)
                rope_tm(P, qr[:, :, 0:16], qr[:, :, 16:32], v3(ra_, 0, 128, 16), v3(ra_, 128, 256, 16),
                        qra[:, :, 0:16], qra[:, :, 16:32], v3(t1, 0, 128, 16), v3(t2, 0, 128, 16))
                P.tt(t3[:, :, :], qra[:, :, 0:32], qra[:, :, 0:32], ALU.mult)
                P.reduce(qr2[:], t3[:, :, :], ALU.add)
                P.tt(qn[:], qr2[:], PB[6][:, 0:8], ALU.add)
                P.act(qn[:], qn[:], AF.Ln, bias=1e-30)
                P.act(qn[:], qn[:], AF.Exp, scale=0.5)
                P.ts(qra[:, :, 32:33], qn.v(qn.base()[:, :].rearrange("p (h o) -> p h o", o=1)), sA["nkmax"][:], ALU.mult)
                P.copy(qrb[:, :, :], qra[:, :, :], eng=POOL)
                for h in range(8):
                    P.transpose(B0[0:33, cs(h)], qrb[:, h, :], K.ident_b[:])
                P.copy(qrTt[:, :, :], B0.v(B0.base()[0:33, :].rearrange("p (h t) -> p h t", t=128)), eng=ACT)
                P.dma(dv(qrT, qrT.base()[:, :, toks].rearrange("h p t -> p h t")), qrTt[:])
                for kc in range(3):
                    P.mm(PB[1][:], cqT[:, kc, :], wqidx[:, kc, :], start=(kc == 0), stop=(kc == 2))
                P.copy(qi[:, :, :], PB[1].v(PB[1].base()[:, :].rearrange("p (h d) -> p h d", d=64)))
                P.copy(qib[:, :, :], qi[:, :, :], eng=POOL)
                rope_tm(P, qi[:, :, 0:8], qi[:, :, 8:16], v3(rb_, 0, 64, 8), v3(rb_, 64, 128, 8),
                        qib[:, :, 0:8], qib[:, :, 8:16], v3(t1, 0, 64, 8), v3(t2, 0, 64, 8))
                for h in range(8):
                    P.transpose(B0[0:64, cs(h)], qib[:, h, :], K.ident_b[:])
                P.copy(qiTt[:, :, :], B0.v(B0.base()[0:64, :].rearrange("p (h t) -> p h t", t=128)))
                P.dma(dv(qiT, qiT.base()[:, :, toks].rearrange("h p t -> p h t")), qiTt[:])

    with P.phase():
        B0 = P.psum("B0", [128, 1024], BF16)
        PI = [P.psum(f"PI{i}", [128, 512], F32) for i in range(2)]
        PS = [P.psum(f"PS{i}", [128, 512], F32) for i in range(2)]
        kis = P.sbuf("kis", [64, S], BF16)
        sc = [P.sbuf(f"sc{i}", [128, S], F32) for i in range(2)]
        jk = P.sbuf("jk", [128, S], BF16)
        neg = [P.sbuf(f"neg{i}", [128, S], BF16) for i in range(2)]
        ngt = [P.sbuf(f"ngt{i}", [128, NT, 128], BF16) for i in range(2)]
        rr = [[P.sbuf(f"rr{i}{k}", [128, 512], F32) for k in range(2)] for i in range(2)]
        dg = [P.sbuf(f"dg{i}", [128, 8, 128], F32) for i in range(2)]
        qiq = [P.sbuf(f"qiq{i}", [64, 8, 128], BF16) for i in range(2)]
        wiq = [P.sbuf(f"wiq{i}", [128, 8], F32) for i in range(2)]
        m_le = P.sbuf("m_le", [128, 128], F32)
        nbig = P.sbuf("nbig", [128, 128], F32)
        P.ts(m_le[:], K.m_gt[:], -1.0, ALU.mult, 1.0, ALU.add)
        P.ts(nbig[:], K.m_gt[:], -1e30, ALU.mult)
        pw2 = P.sbuf("pw2", [128, NIT], F32)
        for it in range(NIT):
            P.memset(pw2[:, it:it + 1], 2.0 ** -(it + 1))
        sB = [{n: P.sbuf(f"{n}{i}", [128, 1], F32) for n in ("lo", "hi", "mid", "cnt", "t")} for i in range(2)]
        stp = [P.sbuf(f"stp{i}", [128, NIT], F32) for i in range(2)]
        jks = [jk, P.sbuf("jk2", [128, S], BF16)]

        def qblock(sq_i, qb, par):
            L = (qb + 1) * 128
            Lg = (4 * (qb // 4) + 4) * 128
            r0 = sq_i * S + qb * 128
            toks = sl(r0, r0 + 128)
            s_, q_, w_, n_, g_ = sc[par], qiq[par], wiq[par], neg[par], ngt[par]
            lo, hi, mid, cnt, tt_ = (sB[par][n] for n in ("lo", "hi", "mid", "cnt", "t"))
            st_, jk_ = stp[par], jks[par]
            pb, sb_, d_ = PI[par], PS[par], dg[par]
            P.dma(q_[:], dv(qiT, qiT.base()[:, :, toks].rearrange("h p t -> p h t")))
            P.dma(w_[:], dv(wiS, wiS.base()[toks, :]))
            for h in range(8):
                P.ts(d_[:, h, :], K.ident_f[:], w_[:, h:h + 1], ALU.mult)
            yield
            for kg in range((L + 511) // 512):
                w = min(512, L - kg * 512)
                cols = sl(kg * 512, kg * 512 + w)
                for h in range(8):
                    r_ = rr[par][h % 2]
                    P.mm(pb[:, 0:w], q_[:, h, :], kis[:, cols])
                    P.act(r_[:, 0:w], pb[:, 0:w], AF.Relu)
                    yield
                    P.mm(sb_[:, 0:w], d_[:, h, :], r_[:, 0:w], start=(h == 0), stop=(h == 7))
                P.copy(s_[:, cols], sb_[:, 0:w], eng=ACT)
                yield
            P.reduce(hi[:], s_[:, 0:L], ALU.max)
            P.reduce(lo[:], s_[:, 0:L], ALU.min)
            yield
            P.tt(hi[:], hi[:], lo[:], ALU.subtract)
            P.ts(lo[:], lo[:], -1.0, ALU.add)
            P.ts(hi[:], hi[:], 2.0, ALU.add)
            P.ts(st_[:], pw2[:], hi[:], ALU.mult)
            blk = s_[:, qb * 128:L]
            P.tt(blk, blk, m_le[:], ALU.mult)
            P.tt(blk, blk, nbig[:], ALU.add)
            yield
            for it in range(NIT):
                P.tt(mid[:], lo[:], st_[:, it:it + 1], ALU.add)
                P.ts(jk_[:, 0:L], s_[:, 0:L], mid[:], ALU.is_gt, 0.0, ALU.add, accum_out=cnt[:])
                yield
                P.ts(tt_[:], cnt[:], float(topk) - 0.5, ALU.is_ge, st_[:, it:it + 1], ALU.mult)
                P.tt(lo[:], lo[:], tt_[:], ALU.add)
                yield
            P.ts(n_[:, 0:L], s_[:, 0:L], lo[:], ALU.is_gt)
            if Lg > L:
                P.memset(n_[:, L:Lg], 0.0, eng=POOL)
            yield
            nkb = Lg // 128
            for kb0 in range(0, nkb, 8):
                n8 = min(8, nkb - kb0)
                for i in range(n8):
                    P.transpose(B0[:, cs(i)], n_[:, cs(kb0 + i)], K.ident_b[:])
                P.copy(g_[:, kb0:kb0 + n8, :], B0.v(B0.base()[:, 0:n8 * 128].rearrange("p (c t) -> p c t", t=128)),
                       eng=ACT)
                yield
            P.dma(dv(negT, negT.base()[sq_i, 0:nkb, :, qb * 128:(qb + 1) * 128].rearrange("kb k q -> k kb q")),
                  g_[:, 0:nkb, :], q=POOL)

        for sq_i in range(C.NSEQ):
            P.dma(kis[:], dv(kiT, kiT.base()[:, sq_i * S:(sq_i + 1) * S]))
            order = []
            a, b = 0, NT - 1
            while a < b:
                order.append((a, b))
                a += 1
                b -= 1
            for (qa_i, qb_i) in order:
                run_interleaved([qblock(sq_i, qa_i, 0), qblock(sq_i, qb_i, 1)])
            if a == b:
                run_interleaved([qblock(sq_i, a, 0)])

    with P.phase():
        PA = [P.psum(f"PA{i}", [128, 512], F32) for i in range(2)]
        O0 = P.psum("O0", [128, 512], F32)
        O1 = P.psum("O1", [128, 512], F32)
        Dn = P.psum("Dn", [128, 512], F32)
        Ov = P.psum("Ov", [128, 512], F32)
        wuv = P.sbuf("wuv", [128, 2, 1024], BF16)
        P.dma(wuv[:], V(W['dsa_w_uv'][j].rearrange("(cc p) f -> p cc f", p=128), ("w", "uv")), q=POOL)
        kl01 = P.sbuf("kl01", [128, 2, S], BF16)
        kl2 = P.sbuf("kl2", [33, S], BF16)
        ckv = P.sbuf("ckvs", [128, NT, 256], BF16)
        ngc = [P.sbuf(f"ngc{i}", [128, NT, 512], BF16) for i in range(2)]
        qa = [P.sbuf(f"qa{i}", [128, 2, 512], BF16) for i in range(2)]
        qrr = [P.sbuf(f"qrr{i}", [33, 512], BF16) for i in range(2)]
        pT = [P.sbuf(f"pT{i}", [128, 512], BF16) for i in range(2)]
        ol = P.sbuf("ol", [128, 2, 512], BF16)
        rden = P.sbuf("rden", [128, 512], F32)
        ost = [P.sbuf(f"ost{i}", [128, 512], BF16) for i in range(2)]
        cnt_ = 0
        for sq_i in range(C.NSEQ):
            seq = sl(sq_i * S, (sq_i + 1) * S)
            P.dma(kl01[:], dv(kl, kl.base()[0:2, :, seq].rearrange("c p t -> p c t")))
            P.dma(kl2[:], dv(kl, kl.base()[2, 0:33, seq]))
            P.dma(ckv[:], dv(kvtm, kvtm.base()[seq, :].rearrange("(t p) c -> p t c", p=128)))
            for g in range(NG):
                nkb = 4 * g + 4
                ng_ = ngc[g % 2]
                gt = sl(sq_i * S + g * 512, sq_i * S + (g + 1) * 512)
                P.dma(ng_[:, 0:nkb, :], dv(negT, negT.base()[sq_i, 0:nkb, :, g * 512:(g + 1) * 512].rearrange("kb k q -> k kb q")))
                for h in range(8):
                    qa_, qr_ = qa[cnt_ % 2], qrr[cnt_ % 2]
                    o_ = ost[cnt_ % 2]
                    cnt_ += 1
                    P.dma(qa_[:], dv(qaT, qaT.base()[h, :, :, gt].rearrange("c p t -> p c t")))
                    P.dma(qr_[:], dv(qrT, qrT.base()[h, 0:33, gt]))

                    def pv(i):
                        p_ = pT[i % 2]
                        P.mm(O0[:], ckv[:, i, 0:128], p_[:], start=(i == 0), stop=(i == nkb - 1))
                        P.mm(O1[:], ckv[:, i, 128:256], p_[:], start=(i == 0), stop=(i == nkb - 1))
                        P.mm(Dn[:], K.ones_b[:], p_[:], start=(i == 0), stop=(i == nkb - 1))

                    for kb in range(nkb):
                        A = PA[kb % 2]
                        P.mm(A[:], kl01[:, 0, cs(kb)], qa_[:, 0, :], start=True, stop=False)
                        P.mm(A[:], kl01[:, 1, cs(kb)], qa_[:, 1, :], start=False, stop=False)
                        P.mm(A[:], kl2[0:33, cs(kb)], qr_[0:33, :], start=False, stop=True)
                        if kb > 0:
                            pv(kb - 1)
                        P.act(pT[kb % 2][:], A[:], AF.Exp, scale=att_scale)
                        P.tt(pT[kb % 2][:], pT[kb % 2][:], ng_[:, kb, :], ALU.mult)
                    pv(nkb - 1)
                    P.copy(ol[:, 0, :], O0[:], eng=ACT)
                    P.copy(ol[:, 1, :], O1[:])
                    P.recip(rden[:], Dn[:])
                    P.mm(Ov[:], wuv[:, 0, cs(h)], ol[:, 0, :], start=True, stop=False)
                    P.mm(Ov[:], wuv[:, 1, cs(h)], ol[:, 1, :], start=False, stop=True)
                    P.tt(o_[:], Ov[:], rden[:], ALU.mult)
                    P.dma(dv(os_, os_.base()[h, :, gt]), o_[:], q=POOL)

    outproj_phase(P, K, C, xr, W['dsa_w_out'][j], os_)


def rope_tables(S):
    pos = np.arange(S, dtype=np.float32)[:, None]

    def tab(r):
        half = r // 2
        inv = (np.float32(500000.0) ** (-np.arange(half, dtype=np.float32) * np.float32(2.0 / r))).astype(np.float32)
        ang = pos * inv[None, :]
        c = np.tile(np.cos(ang).astype(np.float32), (1, 8))
        s_ = np.tile(np.sin(ang).astype(np.float32), (1, 8))
        return np.ascontiguousarray(np.concatenate([c, s_], axis=1), dtype=np.float32)

    return tab(32), tab(16)


def run(C, inputs, n_cores):
    nc = build(C)
    x = np.ascontiguousarray(inputs['x'], dtype=np.float32).reshape(n_cores, C.NTOK, D_MODEL)
    in_maps = []
    for c in range(n_cores):
        m = {"x": x[c]}
        m["ropeA"], m["ropeB"] = rope_tables(C.S)
        for name in INPUT_SHAPES:
            m[name] = np.ascontiguousarray(inputs[name], dtype=np.float32)
        in_maps.append(m)
    res = run_bass_kernel_spmd(nc, in_maps, core_ids=list(range(n_cores)))
    return np.stack([np.asarray(r["y"]) for r in res.results], axis=0)


def kernel(**inputs):
    C = Cfg()
    x = np.asarray(inputs['x'])
    B, S, D = x.shape
    y = run(C, inputs, N_CORES)
    return y.reshape(B, S, D).astype(np.float32)
```

```python
import contextlib
from contextlib import ExitStack

import numpy as np
import concourse.bass as bass
import concourse.mybir as mybir
from concourse.bass_utils import run_bass_kernel_spmd

F32 = mybir.dt.float32
BF16 = mybir.dt.bfloat16
AF = mybir.ActivationFunctionType
ALU = mybir.AluOpType
AX = mybir.AxisListType

D_MODEL = 1024
D_FF = 2816
EPS = 1e-6
N_CORES = 8

PE, ACT, DVE, POOL, SP = "pe", "act", "dve", "pool", "sp"
SEM_EPOCH = 20000
NDMA_SLOTS = 24


class V:
    __slots__ = ("ap", "key")

    def __init__(self, ap, key):
        self.ap = ap
        self.key = key


class T:
    _n = 0

    def __init__(self, handle, name, ap=None, is_psum=False):
        self.h = handle
        self.name = name
        T._n += 1
        self.id = T._n
        self._ap = ap
        self.is_psum = is_psum

    def base(self):
        return self._ap if self._ap is not None else self.h

    def __getitem__(self, idx):
        return V(self.base()[idx], (self.id, None, self.is_psum))

    def k(self, sub, idx=slice(None)):
        return V(self.base()[idx], (self.id, None if self.is_psum else sub, self.is_psum))

    def v(self, ap, sub=None):
        return V(ap, (self.id, None if self.is_psum else sub, self.is_psum))


class Op:
    __slots__ = ("eng", "fn", "reads", "writes", "is_dma", "idx", "deps", "signal", "sig_no", "slot", "slot_val")

    def __init__(self, eng, fn, reads, writes, is_dma=False):
        self.eng = eng
        self.fn = fn
        self.reads = reads
        self.writes = writes
        self.is_dma = is_dma
        self.deps = []
        self.signal = False
        self.sig_no = None


class Prog:
    def __init__(self, nc, es):
        self.nc = nc
        self.es_outer = es
        self.es = es
        self.ops = []
        self.engs = {PE: nc.tensor, ACT: nc.scalar, DVE: nc.vector, POOL: nc.gpsimd, SP: nc.sync}
        self.sig_count = {e: 0 for e in self.engs}
        self.dma_count = {e: 0 for e in self.engs}
        self.sems = {e: [] for e in self.engs}
        self.dsems = {e: [] for e in self.engs}
        self.n_emitted = 0

    def sbuf(self, name, shape, dt):
        T._n += 1
        name = f"{name}_{T._n}"
        h = self.es.enter_context(self.nc.sbuf_tensor(name, list(shape), dt))
        return T(h, name)

    def psum(self, name, shape, dt=F32):
        T._n += 1
        name = f"{name}_{T._n}"
        h = self.es.enter_context(self.nc.psum_tensor(name, list(shape), dt))
        return T(h, name, is_psum=True)

    def dram(self, name, shape, dt, kind="Internal"):
        h = self.nc.dram_tensor(name, list(shape), dt, kind=kind)
        return T(h, name, ap=h.ap())

    @contextlib.contextmanager
    def phase(self):
        old = self.es
        with ExitStack() as es:
            self.es = es
            yield
            self.flush()
        self.es = old

    def op(self, eng, fn, reads, writes, is_dma=False):
        rk = [v.key for v in reads]
        wk = [v.key for v in writes]
        wk += [k for k in rk if len(k) == 3 and k[2] is True]
        o = Op(eng, fn, rk, wk, is_dma)
        o.idx = len(self.ops)
        self.ops.append(o)
        return o

    def dma(self, out, in_, q=SP, **kw):
        return self.op(q, lambda e: e.dma_start(out=out.ap, in_=in_.ap, **kw), [in_], [out], is_dma=True)

    def mm(self, out, lhsT, rhs, start=True, stop=True):
        return self.op(PE, lambda e: e.matmul(out.ap, lhsT=lhsT.ap, rhs=rhs.ap, start=start, stop=stop),
                       [lhsT, rhs] + ([] if start else [out]), [out])

    def transpose(self, out, in_, ident):
        return self.op(PE, lambda e: e.transpose(out.ap, in_.ap, ident.ap), [in_, ident], [out])

    def act(self, out, in_, func, bias=None, scale=None, accum_out=None):
        kw = {}
        reads = [in_]
        writes = [out]
        if bias is not None:
            if isinstance(bias, V):
                kw["bias"] = bias.ap
                reads.append(bias)
            else:
                kw["bias"] = bias
        if scale is not None:
            if isinstance(scale, V):
                kw["scale"] = scale.ap
                reads.append(scale)
            else:
                kw["scale"] = scale
        if accum_out is not None:
            kw["accum_out"] = accum_out.ap
            writes.append(accum_out)
        return self.op(ACT, lambda e: e.activation(out.ap, in_.ap, func, **kw), reads, writes)

    def copy(self, out, in_, eng=DVE):
        if eng == ACT:
            return self.op(ACT, lambda e: e.copy(out.ap, in_.ap), [in_], [out])
        return self.op(eng, lambda e: e.tensor_copy(out=out.ap, in_=in_.ap), [in_], [out])

    def tt(self, out, in0, in1, op, eng=DVE):
        return self.op(eng, lambda e: e.tensor_tensor(out.ap, in0.ap, in1.ap, op), [in0, in1], [out])

    def ts(self, out, in0, s1, op0, s2=None, op1=None, accum_out=None, eng=DVE):
        reads = [in0]
        writes = [out]
        a1 = s1.ap if isinstance(s1, V) else s1
        a2 = s2.ap if isinstance(s2, V) else s2
        if isinstance(s1, V):
            reads.append(s1)
        if isinstance(s2, V):
            reads.append(s2)
        kw = {}
        if op1 is not None:
            kw["op1"] = op1
        if accum_out is not None:
            kw["accum_out"] = accum_out.ap
            writes.append(accum_out)
        return self.op(eng, lambda e: e.tensor_scalar(out.ap, in0.ap, a1, a2, op0, **kw), reads, writes)

    def stt(self, out, in0, s, in1, op0, op1, eng=DVE):
        reads = [in0, in1]
        a = s.ap if isinstance(s, V) else s
        if isinstance(s, V):
            reads.append(s)
        return self.op(eng, lambda e: e.scalar_tensor_tensor(out.ap, in0.ap, a, in1.ap, op0, op1), reads, [out])

    def reduce(self, out, in_, op, axis=AX.X, eng=DVE):
        return self.op(eng, lambda e: e.tensor_reduce(out.ap, in_.ap, axis, op), [in_], [out])

    def recip(self, out, in_):
        return self.op(DVE, lambda e: e.reciprocal(out.ap, in_.ap), [in_], [out])

    def rsqrt(self, out, in_, mult, add):
        self.act(out, in_, AF.Ln, bias=add, scale=mult)
        return self.act(out, out, AF.Exp, scale=-0.5)

    def memset(self, out, val, eng=DVE):
        return self.op(eng, lambda e: e.memset(out.ap, val), [], [out])

    def _sem(self, eng, n):
        i = n // SEM_EPOCH
        while len(self.sems[eng]) <= i:
            self.sems[eng].append(self.es_outer.enter_context(self.nc.semaphore(f"s_{eng}_{len(self.sems[eng])}")))
        return self.sems[eng][i], n % SEM_EPOCH + 1

    def _dsem(self, eng, slot):
        while len(self.dsems[eng]) <= slot:
            self.dsems[eng].append(self.es_outer.enter_context(self.nc.semaphore(f"d_{eng}_{len(self.dsems[eng])}")))
        return self.dsems[eng][slot]

    def flush(self):
        ops = self.ops
        if not ops:
            return
        last_w = {}
        readers = {}
        for o in ops:
            deps = set()
            for k in o.reads:
                w = last_w.get(k)
                if w is not None:
                    deps.add(w)
            for k in o.writes:
                w = last_w.get(k)
                if w is not None:
                    deps.add(w)
                for r in readers.get(k, ()):
                    deps.add(r)
            deps.discard(o.idx)
            o.deps = deps
            for k in o.reads:
                readers.setdefault(k, []).append(o.idx)
            for k in o.writes:
                last_w[k] = o.idx
                readers[k] = []
        waited = {e: {} for e in self.engs}
        waited_dma = {e: set() for e in self.engs}
        for o in ops:
            best = {}
            keep = []
            for d in o.deps:
                p = ops[d]
                if p.is_dma:
                    if d not in waited_dma[o.eng]:
                        waited_dma[o.eng].add(d)
                        keep.append(d)
                    continue
                if p.eng == PE and o.eng == PE and not o.is_dma:
                    continue
                if waited[o.eng].get(p.eng, -1) >= d:
                    continue
                if best.get(p.eng, -1) < d:
                    best[p.eng] = d
            for e, d in best.items():
                keep.append(d)
                waited[o.eng][e] = d
            o.deps = sorted(keep)
            for d in o.deps:
                ops[d].signal = True
        last_op = {}
        for o in ops:
            if not o.is_dma:
                last_op[o.eng] = o
        for o in last_op.values():
            o.signal = True
        last_dma = {}
        for o in ops:
            if o.is_dma:
                o.slot = self.dma_count[o.eng] % NDMA_SLOTS
                o.slot_val = 16 * (self.dma_count[o.eng] // NDMA_SLOTS + 1)
                self.dma_count[o.eng] += 1
                last_dma[(o.eng, o.slot)] = o
            elif o.signal:
                o.sig_no = self.sig_count[o.eng]
                self.sig_count[o.eng] += 1

        def sem_of(p):
            if p.is_dma:
                return self._dsem(p.eng, p.slot), p.slot_val
            return self._sem(p.eng, p.sig_no)

        for o in ops:
            e = self.engs[o.eng]
            for d in o.deps:
                s, v = sem_of(ops[d])
                e.wait_ge(s, v)
            if o.is_dma and o.slot_val > 16:
                e.wait_ge(self._dsem(o.eng, o.slot), o.slot_val - 16)
            ins = o.fn(e)
            if o.is_dma:
                ins.then_inc(self._dsem(o.eng, o.slot), 16)
            elif o.signal:
                s, _ = self._sem(o.eng, o.sig_no)
                ins.then_inc(s, 1)
        for en, e in self.engs.items():
            for pe_, o in last_op.items():
                if pe_ == en:
                    continue
                s, v = sem_of(o)
                e.wait_ge(s, v)
            for o in last_dma.values():
                s, v = sem_of(o)
                e.wait_ge(s, v)
        self.n_emitted += len(ops)
        self.ops = []


class Cfg:
    def __init__(self, S=4096, NSEQ=2, layers=(0, 1, 2, 3), ffn=True, mixers=True, final=True):
        self.S = S
        self.NSEQ = NSEQ
        self.NTOK = S * NSEQ
        self.layers = tuple(layers)
        self.ffn = ffn
        self.mixers = mixers
        self.final = final
        self.topk = min(256, S // 4)


INPUT_SHAPES = {
    'ffn1_norm': (4, 1024), 'ffn1_w_gu': (4, 1024, 5632), 'ffn1_w_down': (4, 2816, 1024),
    'mix_norm': (4, 1024), 'ffn2_norm': (4, 1024), 'ffn2_w_gu': (4, 1024, 5632), 'ffn2_w_down': (4, 2816, 1024),
    'gdn_w_in': (2, 1024, 4112), 'gdn_conv': (2, 4, 3072), 'gdn_a_log': (2, 8), 'gdn_dt_bias': (2, 8),
    'gdn_norm': (2, 128), 'gdn_w_out': (2, 1024, 1024),
    'sb_w_in': (1, 1024, 3072), 'sb_w_out': (1, 1024, 1024),
    'dsa_w_in': (1, 1024, 744), 'dsa_cq_norm': (1, 384), 'dsa_ckv_norm': (1, 256), 'dsa_kidx_norm': (1, 64),
    'dsa_w_uq': (1, 384, 1024), 'dsa_w_qidx': (1, 384, 512), 'dsa_w_uk': (1, 256, 768), 'dsa_w_uv': (1, 256, 1024),
    'dsa_w_out': (1, 1024, 1024), 'final_norm': (1024,),
}


class Ctx:
    pass


def make_consts(P, K):
    K.ident_f = P.sbuf("ident_f", [128, 128], F32)
    K.ident_b = P.sbuf("ident_b", [128, 128], BF16)
    K.ones_f = P.sbuf("ones_f", [128, 128], F32)
    K.ones_b = P.sbuf("ones_b", [128, 128], BF16)
    P.memset(K.ones_f[:], 1.0)
    P.memset(K.ones_b[:], 1.0)
    P.memset(K.ident_f[:], 1.0)
    P.op(POOL, lambda e: e.affine_select(K.ident_f[:].ap, K.ident_f[:].ap, [[-1, 128]], ALU.is_equal, 0.0,
                                         base=0, channel_multiplier=1), [K.ident_f[:]], [K.ident_f[:]])
    P.copy(K.ident_b[:], K.ident_f[:])
    K.m_ge = P.sbuf("m_ge", [128, 128], F32)
    K.m_gt = P.sbuf("m_gt", [128, 128], F32)
    K.m_lt = P.sbuf("m_lt", [128, 128], F32)
    for t, pat, cm, cmp_ in ((K.m_ge, 1, -1, ALU.is_ge), (K.m_gt, 1, -1, ALU.is_gt), (K.m_lt, -1, 1, ALU.is_gt)):
        P.memset(t[:], 1.0)
        P.op(POOL, (lambda t, pat, cm, cmp_: lambda e: e.affine_select(t[:].ap, t[:].ap, [[pat, 128]], cmp_, 0.0,
                                                                       base=0, channel_multiplier=cm))(t, pat, cm, cmp_),
             [t[:]], [t[:]])


def norm_T(P, K, xs, gb, hT_view, scr, psT, n_feat=1024, eps=EPS):
    junk, ss, rstd, xn = scr
    P.act(junk[:, 0:n_feat], xs, AF.Square, accum_out=ss[:])
    P.rsqrt(rstd[:], ss[:], 1.0 / n_feat, eps)
    P.stt(xn[:, 0:n_feat], xs, rstd[:], gb, ALU.mult, ALU.mult)
    nch = n_feat // 128
    for kc in range(nch):
        P.transpose(psT[:, kc * 128:(kc + 1) * 128], xn[:, kc * 128:(kc + 1) * 128], K.ident_b[:])
    P.copy(hT_view, psT.v(psT.base()[:, 0:n_feat].rearrange("p (c t) -> p c t", t=128)), eng=ACT)


def ffn_phase(P, K, C, src, dst, g_row, w_gu, w_down, tag):
    TT = 256
    NS = TT // 128
    NFC = D_FF // 128
    with P.phase():
        wgu = P.sbuf("wgu", [128, 8, 2 * D_FF], BF16)
        wd = P.sbuf("wd", [128, NFC, D_MODEL], BF16)
        gb = P.sbuf("gb", [128, D_MODEL], F32)
        P.dma(gb[:], V(g_row.partition_broadcast(128), ("w", tag, "g")))
        wgu_src = w_gu.rearrange("(kc p) f -> p kc f", p=128)
        for fi in range(11):
            P.dma(wgu.k(fi, (slice(None), slice(None), slice(fi * 512, (fi + 1) * 512))),
                  V(wgu_src[:, :, fi * 512:(fi + 1) * 512], ("w", tag, "gu")), q=POOL)
        wd_src = w_down.rearrange("(fc p) d -> p fc d", p=128)
        for pi in range(2):
            P.dma(wd.k(pi, (slice(None), slice(pi * 11, (pi + 1) * 11), slice(None))),
                  V(wd_src[:, pi * 11:(pi + 1) * 11, :], ("w", tag, "d")), q=POOL)
        xt = [P.sbuf(f"xt{i}", [128, D_MODEL], F32) for i in range(2 * NS)]
        hT = [P.sbuf(f"hT{i}", [128, 8, TT], BF16) for i in range(2)]
        aT = P.sbuf("aT", [128, NFC, TT], BF16)
        sg = [P.sbuf(f"sg{i}", [128, TT], F32) for i in range(2)]
        scr = (P.sbuf("junk", [128, D_MODEL], BF16), P.sbuf("ss", [128, 1], F32),
               P.sbuf("rstd", [128, 1], F32), P.sbuf("xn", [128, D_MODEL], BF16))
        psT = [P.psum(f"psT{i}", [128, D_MODEL], BF16) for i in range(2)]
        psg = [P.psum(f"psg{i}", [128, TT], F32) for i in range(2)]
        psu = [P.psum(f"psu{i}", [128, TT], F32) for i in range(2)]
        psd = [P.psum(f"psd{i}", [128, 512], F32) for i in range(2)]
        ntile = C.NTOK // TT
        nd = 0
        for ti in range(ntile):
            r0 = ti * TT
            xs = [xt[(ti % 2) * NS + s] for s in range(NS)]
            h = hT[ti % 2]
            for s in range(NS):
                P.dma(xs[s][:], src.k(("r", r0 // 128 + s), (slice(r0 + s * 128, r0 + (s + 1) * 128), slice(None))))
                norm_T(P, K, xs[s][:], gb[:], h[:, :, s * 128:(s + 1) * 128], scr, psT[s % 2])
            for fc in range(NFC):
                pg = psg[fc % 2]
                pu = psu[fc % 2]
                for kc in range(8):
                    P.mm(pg[:], wgu.k(fc // 4, (slice(None), kc, slice(fc * 128, (fc + 1) * 128))), h[:, kc, :],
                         start=(kc == 0), stop=(kc == 7))
                cu = NFC + fc
                for kc in range(8):
                    P.mm(pu[:], wgu.k(cu // 4, (slice(None), kc, slice(cu * 128, (cu + 1) * 128))), h[:, kc, :],
                         start=(kc == 0), stop=(kc == 7))
                P.act(sg[fc % 2][:], pg[:], AF.Silu)
                P.tt(aT[:, fc, :], sg[fc % 2][:], pu[:], ALU.mult)
            for s in range(NS):
                for half in range(2):
                    pd = psd[nd % 2]
                    nd += 1
                    for fc in range(NFC):
                        P.mm(pd[:], aT[:, fc, s * 128:(s + 1) * 128],
                             wd.k(fc // 11, (slice(None), fc, slice(half * 512, (half + 1) * 512))),
                             start=(fc == 0), stop=(fc == NFC - 1))
                    P.stt(xs[s][:, half * 512:(half + 1) * 512], pd[:], 0.5, xs[s][:, half * 512:(half + 1) * 512],
                          ALU.mult, ALU.add)
                P.dma(dst.k(("r", r0 // 128 + s), (slice(r0 + s * 128, r0 + (s + 1) * 128), slice(None))), xs[s][:],
                      q=POOL)


def final_phase(P, K, C, src, dst, g_row):
    with P.phase():
        gb = P.sbuf("gb", [128, D_MODEL], F32)
        P.dma(gb[:], V(g_row.partition_broadcast(128), ("w", "fin", "g")))
        xt = [P.sbuf(f"xt{i}", [128, D_MODEL], F32) for i in range(4)]
        junk = P.sbuf("junk", [128, D_MODEL], BF16)
        ss = [P.sbuf(f"ss{i}", [128, 1], F32) for i in range(4)]
        for ti in range(C.NTOK // 128):
            x = xt[ti % 4]
            s = ss[ti % 4]
            rows = (slice(ti * 128, (ti + 1) * 128), slice(None))
            P.dma(x[:], src.k(("r", ti), rows))
            P.act(junk[:], x[:], AF.Square, accum_out=s[:])
            P.rsqrt(s[:], s[:], 1.0 / D_MODEL, EPS)
            P.stt(x[:], x[:], s[:], gb[:], ALU.mult, ALU.mult)
            P.dma(dst.k(("r", ti), rows), x[:], q=POOL)


def copy_phase(P, C, src, dst):
    with P.phase():
        xt = [P.sbuf(f"xt{i}", [128, D_MODEL], F32) for i in range(4)]
        for ti in range(C.NTOK // 128):
            rows = (slice(ti * 128, (ti + 1) * 128), slice(None))
            P.dma(xt[ti % 4][:], src.k(("r", ti), rows))
            P.dma(dst.k(("r", ti), rows), xt[ti % 4][:], q=POOL)


def build(C):
    nc = bass.Bass("TRN2", target_bir_lowering=False)
    with ExitStack() as es:
        P = Prog(nc, es)
        K = Ctx()
        x_in = P.dram("x", [C.NTOK, D_MODEL], F32, kind="ExternalInput")
        y_out = P.dram("y", [C.NTOK, D_MODEL], F32, kind="ExternalOutput")
        xr = P.dram("xr", [C.NTOK, D_MODEL], F32)
        W = {}
        for name, shp in INPUT_SHAPES.items():
            W[name] = nc.dram_tensor(name, list(shp), F32, kind="ExternalInput").ap()
        SC = {}
        if C.mixers and any(l % 3 in (1, 2) for l in C.layers):
            SC["qs"] = P.dram("sc_qs", [8, 128, C.NTOK], BF16)
            SC["ks"] = P.dram("sc_ks", [8, 128, C.NTOK], BF16)
            SC["vs"] = P.dram("sc_vs", [C.NTOK, 1024], BF16)
            SC["os"] = P.dram("sc_os", [8, 128, C.NTOK], BF16)
        if C.mixers and any(l % 3 == 2 for l in C.layers):
            NT_ = C.S // 128
            SC["kl"] = P.dram("sc_kl", [3, 128, C.NTOK], BF16)
            SC["kvtm"] = P.dram("sc_kvtm", [C.NTOK, 256], BF16)
            SC["kiT"] = P.dram("sc_kiT", [64, C.NTOK], BF16)
            SC["wi"] = P.dram("sc_wi", [C.NTOK, 8], F32)
            SC["qaT"] = P.dram("sc_qaT", [8, 2, 128, C.NTOK], BF16)
            SC["qrT"] = P.dram("sc_qrT", [8, 33, C.NTOK], BF16)
            SC["qiT"] = P.dram("sc_qiT", [8, 64, C.NTOK], BF16)
            SC["negT"] = P.dram("sc_negT", [C.NSEQ, NT_, 128, C.S], BF16)
        SC["ropeA"] = nc.dram_tensor("ropeA", [C.S, 256], F32, kind="ExternalInput").ap()
        SC["ropeB"] = nc.dram_tensor("ropeB", [C.S, 128], F32, kind="ExternalInput").ap()
        make_consts(P, K)
        P.flush()
        cur = x_in
        for li in C.layers:
            kind, j = li % 3, li // 3
            if C.ffn:
                ffn_phase(P, K, C, cur, xr, W['ffn1_norm'][li], W['ffn1_w_gu'][li], W['ffn1_w_down'][li], f"f1_{li}")
                cur = xr
            if C.mixers:
                if cur is x_in:
                    copy_phase(P, C, x_in, xr)
                    cur = xr
                if kind == 0:
                    gdn_phase(P, K, C, xr, W, li, j)
                elif kind == 1:
                    sb_phase(P, K, C, xr, W, li, j, SC)
                else:
                    dsa_phase(P, K, C, xr, W, li, j, SC)
            if C.ffn:
                ffn_phase(P, K, C, cur, xr, W['ffn2_norm'][li], W['ffn2_w_gu'][li], W['ffn2_w_down'][li], f"f2_{li}")
                cur = xr
        if C.final:
            final_phase(P, K, C, cur, y_out, W['final_norm'])
        else:
            copy_phase(P, C, cur, y_out)
        P.flush()
        C.n_ops = P.n_emitted
    return nc


def sl(a, b):
    return slice(a, b)


ALL = slice(None)


def run_interleaved(gens):
    alive = list(gens)
    while alive:
        for g in list(alive):
            try:
                next(g)
            except StopIteration:
                alive.remove(g)


def gdn_phase(P, K, C, xr, W, li, j):
    S = C.S
    NT = S // 128
    with P.phase():
        win = P.sbuf("win", [128, 8, 4112], BF16)
        wsrc = W['gdn_w_in'][j].rearrange("(kc p) f -> p kc f", p=128)
        pieces = [(i * 512, (i + 1) * 512) for i in range(8)] + [(4096, 4112)]
        for pi, (a, b) in enumerate(pieces):
            P.dma(win.k(pi, (ALL, ALL, sl(a, b))), V(wsrc[:, :, a:b], ("w", "gdn_in")), q=POOL)
        wout = P.sbuf("wout", [128, 8, 1024], BF16)
        P.dma(wout[:], V(W['gdn_w_out'][j].rearrange("(kc p) d -> p kc d", p=128), ("w", "gdn_out")), q=POOL)
        gb = P.sbuf("gb", [128, D_MODEL], F32)
        P.dma(gb[:], V(W['mix_norm'][li].partition_broadcast(128), ("w", "mixg")))
        gnb8 = P.sbuf("gnb8", [128, 1024], F32)
        for h in range(8):
            P.dma(gnb8[:, h * 128:(h + 1) * 128], V(W['gdn_norm'][j].partition_broadcast(128), ("w", "gn")))
        alb = P.sbuf("alb", [128, 8], F32)
        dtb = P.sbuf("dtb", [128, 8], F32)
        nea = P.sbuf("nea", [128, 8], F32)
        P.dma(alb[:], V(W['gdn_a_log'][j].partition_broadcast(128), ("w", "alog")))
        P.dma(dtb[:], V(W['gdn_dt_bias'][j].partition_broadcast(128), ("w", "dtb")))
        P.act(nea[:], alb[:], AF.Exp)
        P.ts(nea[:], nea[:], -1.0, ALU.mult)
        cw4 = P.sbuf("cw4", [96, 128], F32)
        P.dma(cw4[:], V(W['gdn_conv'][j].rearrange("i (c p) -> (i c) p", p=128), ("w", "conv")))
        cw = P.sbuf("cw", [128, 96], F32)
        diagw = P.sbuf("diagw", [128, 96, 128], BF16)
        B0 = P.psum("B0", [128, 1024], BF16)
        PB = [None] + [P.psum(f"PB{i}", [128, 512], F32) for i in range(1, 8)]
        P.transpose(PB[1][:, 0:96], cw4[:], K.ident_f[0:96, 0:96])
        P.copy(cw[:], PB[1][:, 0:96])
        for ci in range(96):
            P.ts(diagw.k(ci, (ALL, ci, ALL)), K.ident_f[:], cw[:, ci:ci + 1], ALU.mult)
        xt = [P.sbuf(f"xt{i}", [128, D_MODEL], F32) for i in range(2)]
        hT = [P.sbuf(f"hT{i}", [128, 8, 128], BF16) for i in range(2)]
        scr = (P.sbuf("junk", [128, D_MODEL], BF16), P.sbuf("ss", [128, 1], F32),
               P.sbuf("rstd", [128, 1], F32), P.sbuf("xn", [128, D_MODEL], BF16))
        cb = P.sbuf("cb", [128, 24, 131], BF16)
        sil = [P.sbuf(f"sil{i}", [128, 512], F32) for i in range(2)]
        sqb = [P.sbuf(f"sqb{i}", [128, 512], BF16) for i in range(2)]
        rs = [P.sbuf(f"rs{i}", [128, 512], F32) for i in range(2)]
        qkT = P.sbuf("qkT", [128, 16, 128], BF16)
        vT = P.sbuf("vT", [128, 8, 128], BF16)
        gz = P.sbuf("gz", [128, 1024], F32)
        sm = {n: P.sbuf(n, [128, 8], F32) for n in ("eb", "beta", "nbeta", "ta", "g", "egc", "gtot", "dk", "kdsc")}
        gc = P.sbuf("gc", [128, 16], F32)
        S_f = P.sbuf("S_f", [128, 8, 128], F32)
        S_b = P.sbuf("S_b", [128, 8, 128], BF16)
        gh = [P.sbuf(f"gh{i}", [128, 128], F32) for i in range(4)]
        E2 = [P.sbuf(f"E2{i}", [128, 256], F32) for i in range(4)]
        EMi = [P.sbuf(f"EMi{i}", [128, 128], F32) for i in range(4)]
        EMs = [P.sbuf(f"EMs{i}", [128, 128], F32) for i in range(4)]
        WW = [[P.sbuf(f"WW{p}{i}", [128, 256], F32) for i in range(2)] for p in range(4)]
        Rm = [P.sbuf(f"R{p}", [128, 128], F32) for p in range(4)]
        Rb = [P.sbuf(f"Rb{p}", [128, 128], BF16) for p in range(4)]
        vtm = [P.sbuf(f"vtm{p}", [128, 128], BF16) for p in range(4)]
        Xk = [P.sbuf(f"Xk{h}", [128, 128], BF16) for h in range(8)]
        kdec = [P.sbuf(f"kdec{h}", [128, 128], BF16) for h in range(8)]
        qkd = [P.sbuf(f"qkd{h}", [128, 128], BF16) for h in range(8)]
        qdT = [P.sbuf(f"qdT{h}", [128, 128], BF16) for h in range(8)]
        uinb = [P.sbuf(f"uinb{h}", [128, 128], F32) for h in range(8)]
        wT = [P.sbuf(f"wT{h}", [128, 128], BF16) for h in range(8)]
        uu = [P.sbuf(f"u{h}", [128, 128], BF16) for h in range(8)]
        ssq = [P.sbuf(f"ssq{h}", [128, 1], F32) for h in range(8)]
        rsq = [P.sbuf(f"rsq{h}", [128, 1], F32) for h in range(8)]
        junk2 = [P.sbuf(f"junk2{i}", [128, 128], BF16) for i in range(2)]
        og = P.sbuf("og", [128, 1024], BF16)
        ogT = P.sbuf("ogT", [128, 8, 128], BF16)

        def cs(i, n=1):
            return sl(i * 128, (i + n) * 128)

        for sq_i in range(C.NSEQ):
            for h in range(8):
                P.memset(S_f.k(h, (ALL, h, ALL)), 0.0)
                P.memset(S_b.k(h, (ALL, h, ALL)), 0.0)
            for gq in range(6):
                P.memset(cb.k(gq, (ALL, sl(gq * 4, gq * 4 + 4), sl(0, 3))), 0.0)
            for t in range(NT):
                r0 = sq_i * S + t * 128
                ti = r0 // 128
                xs = xt[t % 2]
                h_ = hT[t % 2]
                rows = (sl(r0, r0 + 128), ALL)
                P.dma(xs[:], xr.k(("r", ti), rows))
                norm_T(P, K, xs[:], gb[:], h_[:, :, :], scr, B0)

                def stage_a(gq):
                    pp = PB[1 + gq % 2]
                    pc = PB[3 + gq % 2]
                    for cc in range(4):
                        c = gq * 4 + cc
                        for kc in range(8):
                            P.mm(pp[:, cs(cc)], win.k(c // 4, (ALL, kc, cs(c))), h_[:, kc, :],
                                 start=(kc == 0), stop=(kc == 7))
                    cbg = cb.k(gq, (ALL, sl(gq * 4, gq * 4 + 4), sl(3, 131)))
                    P.copy(cbg, pp.v(pp.base()[:, :].rearrange("p (c t) -> p c t", t=128)), eng=ACT)
                    yield
                    for cc in range(4):
                        c = gq * 4 + cc
                        for i in range(4):
                            P.mm(pc[:, cs(cc)], diagw.k(i * 24 + c, (ALL, i * 24 + c, ALL)),
                                 cb.k(gq, (ALL, c, sl(i, i + 128))), start=(i == 0), stop=(i == 3))
                    if gq < 4:
                        s_ = sil[gq % 2]
                        P.act(s_[:], pc[:], AF.Silu)
                        P.tt(sqb[gq % 2][:], s_[:], s_[:], ALU.mult, eng=POOL)
                    else:
                        P.act(vT.v(vT.base()[:, (gq - 4) * 4:(gq - 4) * 4 + 4, :]),
                              pc.v(pc.base()[:, :].rearrange("p (c t) -> p c t", t=128)), AF.Silu)
                    P.copy(cb.k(gq, (ALL, sl(gq * 4, gq * 4 + 4), sl(0, 3))),
                           cb.k(gq, (ALL, sl(gq * 4, gq * 4 + 4), sl(128, 131))), eng=POOL)
                    yield
                    if gq < 4:
                        for cc in range(4):
                            P.mm(PB[5][:, cs(cc)], K.ones_b[:], sqb[gq % 2][:, cs(cc)])
                        r_ = rs[gq % 2]
                        if gq < 2:
                            P.rsqrt(r_[:], PB[5][:], 128.0, 128.0 * EPS)
                        else:
                            P.rsqrt(r_[:], PB[5][:], 1.0, EPS)
                        P.tt(qkT.v(qkT.base()[:, gq * 4:gq * 4 + 4, :]),
                             s_.v(s_.base()[:, :].rearrange("p (c t) -> p c t", t=128)),
                             r_.v(r_.base()[:, :].rearrange("p (c t) -> p c t", t=128)), ALU.mult)
                    yield

                gens = [stage_a(gq) for gq in range(6)]
                for step in range(6 + 2):
                    for gq in range(6):
                        if 0 <= step - gq < 3:
                            next(gens[gq])
                for hf in range(2):
                    for kc in range(8):
                        P.mm(PB[1 + hf][:], h_[:, kc, :], win.k(6 + hf, (ALL, kc, sl(3072 + hf * 512, 3072 + (hf + 1) * 512))),
                             start=(kc == 0), stop=(kc == 7))
                    P.act(gz[:, hf * 512:(hf + 1) * 512], PB[1 + hf][:], AF.Silu)
                P.tt(gz[:], gz[:], gnb8[:], ALU.mult, eng=POOL)
                for kc in range(8):
                    P.mm(PB[6][:, 0:16], h_[:, kc, :], win.k(8, (ALL, kc, sl(4096, 4112))), start=(kc == 0), stop=(kc == 7))
                P.act(sm["eb"][:], PB[6][:, 0:8], AF.Exp, scale=-1.0)
                P.tt(sm["ta"][:], PB[6][:, 8:16], dtb[:], ALU.add)
                P.ts(sm["eb"][:], sm["eb"][:], 1.0, ALU.add)
                P.recip(sm["beta"][:], sm["eb"][:])
                P.ts(sm["nbeta"][:], sm["beta"][:], -1.0, ALU.mult)
                P.act(sm["ta"][:], sm["ta"][:], AF.Exp)
                P.act(sm["ta"][:], sm["ta"][:], AF.Ln, bias=1.0)
                P.tt(sm["g"][:], sm["ta"][:], nea[:], ALU.mult)
                P.mm(PB[7][:, 0:8], K.m_ge[:], sm["g"][:])
                P.mm(PB[7][:, 8:16], K.ones_f[:], sm["g"][:])
                P.copy(gc[:], PB[7][:, 0:16])
                P.act(sm["egc"][:], gc[:, 0:8], AF.Exp)
                P.act(sm["gtot"][:], gc[:, 8:16], AF.Exp)
                P.tt(sm["dk"][:], gc[:, 8:16], gc[:, 0:8], ALU.subtract)
                P.act(sm["kdsc"][:], sm["dk"][:], AF.Exp)
                beta, nbeta, g = sm["beta"], sm["nbeta"], sm["g"]

                def head_prep(h):
                    p = h % 4
                    bD, bW, bR, bU = PB[1 + h % 2], PB[3 + h % 2], PB[5 + h % 2], PB[7]
                    kTh = qkT[:, 8 + h, :]
                    qTh = qkT[:, h, :]
                    P.ts(gh[p][:], K.m_ge[:], g[:, h:h + 1], ALU.mult)
                    P.mm(bD[:, 0:128], K.ones_f[:], gh[p][:])
                    P.mm(bD[:, 128:256], K.m_lt[:], gh[p][:])
                    P.mm(bD[:, 256:384], kTh, kTh)
                    P.mm(bD[:, 384:512], kTh, qTh)
                    P.act(E2[p][:], bD[:, 0:256], AF.Exp)
                    P.tt(EMi[p][:], E2[p][:, 128:256], K.m_ge[:], ALU.mult, eng=POOL)
                    P.tt(EMs[p][:], E2[p][:, 128:256], K.m_gt[:], ALU.mult, eng=POOL)
                    W_ = WW[p]
                    R_ = Rm[p]
                    P.stt(W_[0][:, 0:128], bD[:, 256:384], nbeta[:, h:h + 1], EMs[p][:], ALU.mult, ALU.mult)
                    P.tt(qkd[h][:], bD[:, 384:512], EMi[p][:], ALU.mult)
                    P.tt(qdT[h][:], qTh, E2[p][:, 0:128], ALU.mult, eng=POOL)
                    yield
                    P.transpose(bW[:, 128:256], W_[0][:, 0:128], K.ident_f[:])
                    P.copy(W_[0][:, 128:256], bW[:, 128:256], eng=ACT)
                    P.tt(R_[:], W_[0][:, 0:128], K.ident_f[:], ALU.add)
                    yield
                    for m in range(1, 7):
                        a, b = (m - 1) % 2, m % 2
                        Wa, WTa = W_[a][:, 0:128], W_[a][:, 128:256]
                        if m < 6:
                            P.mm(bW[:, 0:128], WTa, Wa)
                        P.mm(bW[:, 128:256], Wa, WTa)
                        if m < 6:
                            P.copy(W_[b][:], bW[:, 0:256], eng=(ACT if m % 2 else DVE))
                        else:
                            P.copy(W_[b][:, 128:256], bW[:, 128:256], eng=ACT)
                        yield
                        P.mm(bR[:, 0:128], W_[b][:, 128:256], R_[:])
                        P.tt(R_[:], bR[:, 0:128], R_[:], ALU.add)
                        yield
                    P.copy(Rb[p][:], R_[:], eng=ACT)
                    P.transpose(B0[:, 0:128], kTh, K.ident_b[:])
                    P.transpose(B0[:, 128:256], vT[:, h, :], K.ident_b[:])
                    P.ts(Xk[h][:], B0[:, 0:128], sm["egc"][:, h:h + 1], ALU.mult)
                    P.ts(kdec[h][:], B0[:, 0:128], sm["kdsc"][:, h:h + 1], ALU.mult)
                    P.copy(vtm[p][:], B0[:, 128:256], eng=ACT)
                    yield
                    P.mm(bU[:, 0:128], Rb[p][:], vtm[p][:])
                    P.mm(bU[:, 128:256], Xk[h][:], Rb[p][:])
                    P.ts(uinb[h][:], bU[:, 0:128], beta[:, h:h + 1], ALU.mult)
                    P.copy(wT[h][:], bU[:, 128:256], eng=ACT)
                    yield

                for h0 in range(0, 8, 4):
                    run_interleaved([head_prep(h0 + q) for q in range(4)])

                def recur(hg):
                    hs = [hg * 4 + q for q in range(4)]
                    bT, bO = PB[1 + 2 * hg], PB[2 + 2 * hg]
                    for q, h in enumerate(hs):
                        P.mm(bT[:, cs(q)], wT[h][:], S_b.k(h, (ALL, h, ALL)))
                    yield
                    for q, h in enumerate(hs):
                        P.stt(uu[h][:], bT[:, cs(q)], nbeta[:, h:h + 1], uinb[h][:], ALU.mult, ALU.add)
                    yield
                    for q, h in enumerate(hs):
                        P.mm(bO[:, cs(q)], qdT[h][:], S_b.k(h, (ALL, h, ALL)), start=True, stop=False)
                        P.mm(bO[:, cs(q)], qkd[h][:], uu[h][:], start=False, stop=True)
                    for q, h in enumerate(hs):
                        P.mm(bT[:, cs(q)], kdec[h][:], uu[h][:])
                    yield
                    for q, h in enumerate(hs):
                        Sfh = S_f.k(h, (ALL, h, ALL))
                        P.stt(Sfh, Sfh, sm["gtot"][:, h:h + 1], bT[:, cs(q)], ALU.mult, ALU.add)
                        P.copy(S_b.k(h, (ALL, h, ALL)), Sfh, eng=POOL)
                    yield
                    for q, h in enumerate(hs):
                        P.act(junk2[hg][:], bO[:, cs(q)], AF.Square, accum_out=ssq[h][:])
                    yield
                    for q, h in enumerate(hs):
                        P.rsqrt(rsq[h][:], ssq[h][:], 1.0 / 128, EPS)
                    yield
                    for q, h in enumerate(hs):
                        P.stt(og.k(h, (ALL, cs(h))), bO[:, cs(q)], rsq[h][:], gz[:, cs(h)], ALU.mult, ALU.mult)
                    yield

                run_interleaved([recur(0), recur(1)])
                for hc in range(8):
                    P.transpose(B0[:, cs(hc)], og.k(hc, (ALL, cs(hc))), K.ident_b[:])
                P.copy(ogT[:, :, :], B0.v(B0.base()[:, :].rearrange("p (c t) -> p c t", t=128)), eng=ACT)
                for half in range(2):
                    bo = PB[5 + half]
                    for hc in range(8):
                        P.mm(bo[:], ogT[:, hc, :], wout[:, hc, half * 512:(half + 1) * 512], start=(hc == 0), stop=(hc == 7))
                    P.tt(xs[:, half * 512:(half + 1) * 512], bo[:], xs[:, half * 512:(half + 1) * 512], ALU.add)
                P.dma(xr.k(("r", ti), rows), xs[:], q=POOL)


_uid = [0]


def dv(t, ap):
    _uid[0] += 1
    return V(ap, (t.id, ("u", _uid[0]), False))


def outproj_phase(P, K, C, xr, w_out, os_):
    with P.phase():
        wout = P.sbuf("wout", [128, 8, 1024], BF16)
        P.dma(wout[:], V(w_out.rearrange("(kc p) d -> p kc d", p=128), ("w", "wout")), q=POOL)
        PB = [P.psum(f"PO{i}", [128, 512], F32) for i in range(4)]
        oT = [P.sbuf(f"oT{i}", [128, 8, 512], BF16) for i in range(2)]
        xt = [P.sbuf(f"xt{i}", [128, D_MODEL], F32) for i in range(4)]
        nb = 0
        for ti in range(C.NTOK // 512):
            r0 = ti * 512
            o_ = oT[ti % 2]
            P.dma(o_[:], dv(os_, os_.base()[:, :, r0:r0 + 512].rearrange("h p t -> p h t")))
            for s_ in range(4):
                x = xt[s_]
                rows = (sl(r0 + s_ * 128, r0 + (s_ + 1) * 128), ALL)
                P.dma(x[:], xr.k(("r", ti * 4 + s_), rows))
                for half in range(2):
                    pb = PB[nb % 4]
                    nb += 1
                    for h in range(8):
                        P.mm(pb[:], o_[:, h, s_ * 128:(s_ + 1) * 128], wout[:, h, half * 512:(half + 1) * 512],
                             start=(h == 0), stop=(h == 7))
                    P.tt(x[:, half * 512:(half + 1) * 512], pb[:], x[:, half * 512:(half + 1) * 512], ALU.add)
                P.dma(xr.k(("r", ti * 4 + s_), rows), x[:], q=POOL)


def sb_phase(P, K, C, xr, W, li, j, SC):
    S = C.S
    NT = S // 128
    NG = S // 512
    qs, ks, vs, os_ = SC["qs"], SC["ks"], SC["vs"], SC["os"]
    scale = 128.0 ** -0.5

    def cs(i, n=1):
        return sl(i * 128, (i + n) * 128)

    with P.phase():
        win = P.sbuf("win", [128, 8, 3072], BF16)
        wsrc = W['sb_w_in'][j].rearrange("(kc p) f -> p kc f", p=128)
        for pi in range(6):
            P.dma(win.k(pi, (ALL, ALL, sl(pi * 512, (pi + 1) * 512))), V(wsrc[:, :, pi * 512:(pi + 1) * 512], ("w", "sb_in")), q=POOL)
        gb = P.sbuf("gb", [128, D_MODEL], F32)
        P.dma(gb[:], V(W['mix_norm'][li].partition_broadcast(128), ("w", "mixg")))
        B0 = P.psum("B0", [128, 1024], BF16)
        PB = [None] + [P.psum(f"PB{i}", [128, 512], F32) for i in range(1, 8)]
        xt = [P.sbuf(f"xt{i}", [128, D_MODEL], F32) for i in range(4)]
        hT = [P.sbuf(f"hT{i}", [128, 8, 512], BF16) for i in range(2)]
        scr = (P.sbuf("junk", [128, D_MODEL], BF16), P.sbuf("ss", [128, 1], F32),
               P.sbuf("rstd", [128, 1], F32), P.sbuf("xn", [128, D_MODEL], BF16))
        qst = [P.sbuf(f"qst{i}", [128, 512], BF16) for i in range(4)]
        vst = [P.sbuf(f"vst{i}", [128, 1024], BF16) for i in range(2)]
        for ti in range(C.NTOK // 512):
            r0 = ti * 512
            h_ = hT[ti % 2]
            for s_ in range(4):
                rows = (sl(r0 + s_ * 128, r0 + (s_ + 1) * 128), ALL)
                P.dma(xt[s_][:], xr.k(("r", ti * 4 + s_), rows))
                norm_T(P, K, xt[s_][:], gb[:], h_[:, :, s_ * 128:(s_ + 1) * 128], scr, B0)
            for c in range(16):
                pb = PB[1 + c % 4]
                for kc in range(8):
                    P.mm(pb[:], win.k(c // 4, (ALL, kc, cs(c))), h_[:, kc, :], start=(kc == 0), stop=(kc == 7))
                st = qst[c % 4]
                if c < 8:
                    P.act(st[:], pb[:], AF.Copy, scale=scale)
                    P.dma(qs.k(("q", c, ti), (c, ALL, sl(r0, r0 + 512))), st[:], q=SP)
                else:
                    P.copy(st[:], pb[:], eng=DVE)
                    P.dma(ks.k(("k", c - 8, ti), (c - 8, ALL, sl(r0, r0 + 512))), st[:], q=SP)
            for s_ in range(4):
                vv = vst[s_ % 2]
                for half in range(2):
                    pb = PB[5 + half]
                    for kc in range(8):
                        P.mm(pb[:], h_[:, kc, s_ * 128:(s_ + 1) * 128],
                             win.k(4 + half, (ALL, kc, sl(2048 + half * 512, 2048 + (half + 1) * 512))),
                             start=(kc == 0), stop=(kc == 7))
                    P.copy(vv[:, half * 512:(half + 1) * 512], pb[:], eng=(ACT if half else DVE))
                P.dma(vs.k(("v", ti * 4 + s_), (sl(r0 + s_ * 128, r0 + (s_ + 1) * 128), ALL)), vv[:], q=SP)

    with P.phase():
        PA = [P.psum(f"PA{i}", [128, 512], F32) for i in range(6)]
        negtri_f = P.sbuf("negtri_f", [128, 128], F32)
        negtri = P.sbuf("negtri", [128, 128], BF16)
        negones = P.sbuf("negones", [128, 128], BF16)
        P.tt(negtri_f[:], K.m_lt[:], K.ident_f[:], ALU.add)
        P.ts(negtri[:], negtri_f[:], -1.0, ALU.mult)
        P.memset(negones[:], -1.0)
        maskf = [P.sbuf(f"maskf{r}", [128, 512], F32) for r in range(4)]
        maskb = [P.sbuf(f"maskb{r}", [128, 512], BF16) for r in range(4)]
        maskf_ = maskf
        maskf = maskb
        for r in range(4):
            P.memset(maskf_[r][:], 1.0)
            P.op(POOL, (lambda t, r: lambda e: e.affine_select(t[:].ap, t[:].ap, [[1, 512]], ALU.is_gt, 0.0,
                                                               base=-r * 128, channel_multiplier=-1))(maskf_[r], r),
                 [maskf_[r][:]], [maskf_[r][:]])
            P.copy(maskb[r][:], maskf_[r][:])
        hd = [[{"k": P.sbuf(f"kT{a}{b}", [128, S], BF16), "q": P.sbuf(f"qT{a}{b}", [128, S], BF16),
                "v": P.sbuf(f"v{a}{b}", [128, NT, 128], BF16)} for b in range(2)] for a in range(2)]
        wk = [{"e": [P.sbuf(f"e{b}{i}", [128, 512], F32) for i in range(2)],
               "sp": [P.sbuf(f"sp{b}{i}", [128, 512], BF16) for i in range(2)],
               "accb": P.sbuf(f"accb{b}", [128, 512], BF16),
               "att": [P.sbuf(f"att{b}{i}", [128, 512], BF16) for i in range(2)],
               "acc": P.sbuf(f"acc{b}", [128, 512], F32),
               "ost": P.sbuf(f"ost{b}", [128, 512], BF16),
               "banks": (PA[3 * b], PA[3 * b + 1], PA[3 * b + 2])} for b in range(2)]

        def load_pair(sq_i, hp, a):
            for b in range(2):
                h = 2 * hp + b
                d = hd[a][b]
                cols = sl(sq_i * S, (sq_i + 1) * S)
                P.dma(d["k"][:], ks.k(("kall",), (h, ALL, cols)))
                P.dma(d["q"][:], qs.k(("qall",), (h, ALL, cols)))
                P.dma(d["v"][:], vs.v(vs.base()[sq_i * S:(sq_i + 1) * S, h * 128:(h + 1) * 128].rearrange("(t p) d -> p t d", p=128), ("vall",)))

        def stream(sq_i, h, g, d, w):
            bA, bB, bO = w["banks"]
            kT, qT, v = d["k"], d["q"], d["v"]
            qcols = sl(g * 512, (g + 1) * 512)
            nkb = 4 * g + 4
            for idx, kb in enumerate(range(nkb - 1, -1, -1)):
                first = idx == 0
                last = kb == 0
                r = kb - 4 * g
                e, sp, att = w["e"][idx % 2], w["sp"][idx % 2], w["att"][idx % 2]
                P.mm(bA[:], kT[:, cs(kb)], qT[:, qcols])
                yield
                P.act(e[:], bA[:], AF.Exp)
                P.act(sp[:], e[:], AF.Ln, bias=1.0)
                if r >= 0:
                    P.tt(sp[:], sp[:], maskf[r][:], ALU.mult, eng=POOL)
                yield
                P.mm(bB[:], kT[:, cs(kb)], qT[:, qcols], start=True, stop=False)
                P.mm(bB[:], negtri[:], sp[:], start=False, stop=first)
                if not first:
                    P.mm(bB[:], negones[:], w["accb"][:], start=False, stop=True)
                yield
                P.act(att[:], bB[:], AF.Exp)
                if r >= 0:
                    P.tt(att[:], att[:], maskb[r][:], ALU.mult)
                if not last:
                    if first:
                        P.copy(w["acc"][:], sp[:])
                    else:
                        P.tt(w["acc"][:], w["acc"][:], sp[:], ALU.add)
                    P.copy(w["accb"][:], w["acc"][:])
                yield
                P.mm(bO[:], v[:, kb, :], att[:], start=first, stop=last)
                yield
            P.copy(w["ost"][:], bO[:])
            P.dma(os_.k(("o", h, sq_i, g), (h, ALL, sl(sq_i * S + g * 512, sq_i * S + (g + 1) * 512))), w["ost"][:], q=POOL)

        pairs = [(sq_i, hp) for sq_i in range(C.NSEQ) for hp in range(4)]
        load_pair(pairs[0][0], pairs[0][1], 0)
        for pi, (sq_i, hp) in enumerate(pairs):
            a = pi % 2
            if pi + 1 < len(pairs):
                load_pair(pairs[pi + 1][0], pairs[pi + 1][1], 1 - a)
            for g in range(NG):
                run_interleaved([stream(sq_i, 2 * hp + b, g, hd[a][b], wk[b]) for b in range(2)])

    outproj_phase(P, K, C, xr, W['sb_w_out'][j], os_)


def rope_tm(P, x1, x2, cos, sin, o1, o2, t1, t2):
    P.tt(t1, x1, cos, ALU.mult)
    P.tt(t2, x2, sin, ALU.mult)
    P.tt(o1, t1, t2, ALU.subtract)
    P.tt(t1, x2, cos, ALU.mult)
    P.tt(t2, x1, sin, ALU.mult)
    P.tt(o2, t1, t2, ALU.add)


def dsa_phase(P, K, C, xr, W, li, j, SC):
    S = C.S
    NT = S // 128
    NG = S // 512
    topk = C.topk
    NIT = 16
    att_scale = 128.0 ** -0.5
    widx_scale = (8.0 ** -0.5) * (64.0 ** -0.5)
    NEG = -30000.0
    kl, kvtm, kiT, wiS = SC["kl"], SC["kvtm"], SC["kiT"], SC["wi"]
    qaT, qrT, qiT, negT, os_ = SC["qaT"], SC["qrT"], SC["qiT"], SC["negT"], SC["os"]
    ropeA, ropeB = SC["ropeA"], SC["ropeB"]

    def cs(i, n=1):
        return sl(i * 128, (i + n) * 128)

    with P.phase():
        B0 = P.psum("B0", [128, 1024], BF16)
        PB = [None] + [P.psum(f"PB{i}", [128, 512], F32) for i in range(1, 8)]
        win = P.sbuf("win", [128, 8, 744], BF16)
        P.dma(win[:], V(W['dsa_w_in'][j].rearrange("(kc p) f -> p kc f", p=128), ("w", "dsa_in")), q=POOL)
        wuq4 = W['dsa_w_uq'][j].rearrange("(kc p) (h d) -> p kc h d", p=128, d=128)
        wuq_r = P.sbuf("wuq_r", [128, 3, 8, 32], BF16)
        for kc in range(3):
            P.dma(wuq_r[:, kc, :, :], V(wuq4[:, kc, :, 0:32], ("w", "uq_r")), q=POOL)
        wuq_n = P.sbuf("wuq_n", [128, 3, 8, 96], BF16)
        for kc in range(3):
            P.dma(wuq_n[:, kc, :, :], V(wuq4[:, kc, :, 32:128], ("w", "uq_n")), q=POOL)
        wuk = P.sbuf("wuk", [128, 2, 768], BF16)
        P.dma(wuk[:], V(W['dsa_w_uk'][j].rearrange("(cc p) f -> p cc f", p=128), ("w", "uk")), q=POOL)
        wqidx = P.sbuf("wqidx", [128, 3, 512], BF16)
        P.dma(wqidx[:], V(W['dsa_w_qidx'][j].rearrange("(kc p) f -> p kc f", p=128), ("w", "qidx")), q=POOL)
        gb = P.sbuf("gb", [128, D_MODEL], F32)
        P.dma(gb[:], V(W['mix_norm'][li].partition_broadcast(128), ("w", "mixg")))
        cqg = P.sbuf("cqg", [128, 384], F32)
        ckvg = P.sbuf("ckvg", [128, 256], F32)
        kidxg = P.sbuf("kidxg", [128, 64], F32)
        P.dma(cqg[:], V(W['dsa_cq_norm'][j].partition_broadcast(128), ("w", "cqg")))
        P.dma(ckvg[:], V(W['dsa_ckv_norm'][j].partition_broadcast(128), ("w", "ckvg")))
        P.dma(kidxg[:], V(W['dsa_kidx_norm'][j].partition_broadcast(128), ("w", "kidxg")))
        AT = P.sbuf("AT", [96, 8, 384], BF16)
        BT = P.sbuf("BT", [96, 8, 256], BF16)
        Wabs = P.sbuf("Wabs", [128, 3, 8, 256], BF16)
        for h in range(8):
            for kc in range(3):
                P.transpose(B0[0:96, cs(kc)], wuq_n[:, kc, h, :], K.ident_b[:])
            for cc in range(2):
                P.transpose(B0[0:96, cs(3 + cc)], wuk[:, cc, h * 96:(h + 1) * 96], K.ident_b[:])
            P.copy(AT[:, h, :], B0[0:96, 0:384], eng=ACT)
            P.copy(BT[:, h, :], B0[0:96, 384:640])
        for h in range(8):
            for kc in range(3):
                n = h * 3 + kc
                pb = PB[1 + n % 4]
                P.mm(pb[:, 0:256], AT[:, h, cs(kc)], BT[:, h, :])
                P.copy(Wabs[:, kc, h, :], pb[:, 0:256], eng=(ACT if n % 2 else DVE))
        xt = [P.sbuf(f"xt{i}", [128, D_MODEL], F32) for i in range(2)]
        hT = [P.sbuf(f"hT{i}", [128, 8, 128], BF16) for i in range(2)]
        scr = (P.sbuf("junk", [128, D_MODEL], BF16), P.sbuf("ss", [128, 1], F32),
               P.sbuf("rstd", [128, 1], F32), P.sbuf("xn", [128, D_MODEL], BF16))
        junk = scr[0]
        ra = [P.sbuf(f"ra{i}", [128, 256], F32) for i in range(2)]
        rb = [P.sbuf(f"rb{i}", [128, 128], F32) for i in range(2)]
        pj = P.sbuf("pj", [128, 744], F32)
        sA = {n: P.sbuf(n, [128, 1], F32) for n in ("ssA", "rsA", "ssB", "rsB", "ssC", "rsC", "kn2a", "kn2b", "kn2", "rm", "nkmax")}
        km1 = P.sbuf("km1", [1, 1], F32)
        cqn = P.sbuf("cqn", [128, 384], BF16)
        cqT = P.sbuf("cqT", [128, 3, 128], BF16)
        ckv = P.sbuf("ckv", [128, 256], BF16)
        ckT = P.sbuf("ckT", [128, 2, 128], BF16)
        kra = P.sbuf("kra", [128, 33], F32)
        krb = P.sbuf("krb", [128, 33], BF16)
        krT = P.sbuf("krT", [33, 128], BF16)
        t1 = P.sbuf("t1", [128, 128], F32)
        t2 = P.sbuf("t2", [128, 128], F32)
        kin = P.sbuf("kin", [128, 64], F32)
        kib = P.sbuf("kib", [128, 64], BF16)
        kiTt = P.sbuf("kiTt", [64, 128], BF16)
        wit = P.sbuf("wit", [128, 8], F32)
        qab = P.sbuf("qab", [128, 16, 128], BF16)
        sqa = P.sbuf("sqa", [128, 16, 128], BF16)
        qr = P.sbuf("qr", [128, 8, 32], F32)
        qra = P.sbuf("qra", [128, 8, 33], F32)
        qrb = P.sbuf("qrb", [128, 8, 33], BF16)
        qrTt = P.sbuf("qrTt", [33, 8, 128], BF16)
        t3 = P.sbuf("t3", [128, 8, 32], F32)
        qr2 = P.sbuf("qr2", [128, 8], F32)
        qn = P.sbuf("qn", [128, 8], F32)
        qi = P.sbuf("qi", [128, 8, 64], F32)
        qib = P.sbuf("qib", [128, 8, 64], BF16)
        qiTt = P.sbuf("qiTt", [64, 8, 128], BF16)
        P.memset(kra[:, 32:33], 1.0)

        def v3(t, a, b, d):
            return t.v(t.base()[:, a:b].rearrange("p (h d) -> p h d", d=d))

        for sq_i in range(C.NSEQ):
            P.memset(sA["rm"][:], 0.0)
            for t in range(NT):
                r0 = sq_i * S + t * 128
                ti = r0 // 128
                toks = sl(r0, r0 + 128)
                xs, h_ = xt[t % 2], hT[t % 2]
                ra_, rb_ = ra[t % 2], rb[t % 2]
                P.dma(xs[:], xr.k(("r", ti), (toks, ALL)))
                P.dma(ra_[:], V(ropeA[t * 128:(t + 1) * 128, :], ("w", "ropeA")))
                P.dma(rb_[:], V(ropeB[t * 128:(t + 1) * 128, :], ("w", "ropeB")))
                norm_T(P, K, xs[:], gb[:], h_[:, :, :], scr, B0)
                for kc in range(8):
                    P.mm(PB[1][:], h_[:, kc, :], win[:, kc, 0:512], start=(kc == 0), stop=(kc == 7))
                for kc in range(8):
                    P.mm(PB[2][:, 0:232], h_[:, kc, :], win[:, kc, 512:744], start=(kc == 0), stop=(kc == 7))
                P.copy(pj[:, 0:512], PB[1][:], eng=ACT)
                P.copy(pj[:, 512:744], PB[2][:, 0:232])
                P.act(junk[:, 0:384], pj[:, 0:384], AF.Square, accum_out=sA["ssA"][:])
                P.rsqrt(sA["rsA"][:], sA["ssA"][:], 1.0 / 384, EPS)
                P.stt(cqn[:], pj[:, 0:384], sA["rsA"][:], cqg[:], ALU.mult, ALU.mult)
                for cc in range(3):
                    P.transpose(B0[:, cs(cc)], cqn[:, cs(cc)], K.ident_b[:])
                P.copy(cqT[:, :, :], B0.v(B0.base()[:, 0:384].rearrange("p (c t) -> p c t", t=128)), eng=ACT)
                P.act(junk[:, 0:256], pj[:, 384:640], AF.Square, accum_out=sA["ssB"][:])
                P.rsqrt(sA["rsB"][:], sA["ssB"][:], 1.0 / 256, EPS)
                P.stt(ckv[:], pj[:, 384:640], sA["rsB"][:], ckvg[:], ALU.mult, ALU.mult)
                P.act(junk[:, 0:256], ckv[:], AF.Square, accum_out=sA["kn2a"][:])
                P.dma(dv(kvtm, kvtm.base()[toks, :]), ckv[:])
                for cc in range(2):
                    P.transpose(B0[:, cs(cc)], ckv[:, cs(cc)], K.ident_b[:])
                P.copy(ckT[:, :, :], B0.v(B0.base()[:, 0:256].rearrange("p (c t) -> p c t", t=128)))
                P.dma(dv(kl, kl.base()[0:2, :, toks].rearrange("c p t -> p c t")), ckT[:])
                rope_tm(P, pj[:, 640:656], pj[:, 656:672], ra_[:, 0:16], ra_[:, 128:144],
                        kra[:, 0:16], kra[:, 16:32], t1[:, 0:16], t2[:, 0:16])
                P.act(junk[:, 0:32], kra[:, 0:32], AF.Square, accum_out=sA["kn2b"][:])
                P.tt(sA["kn2"][:], sA["kn2a"][:], sA["kn2b"][:], ALU.add)
                P.tt(sA["rm"][:], sA["rm"][:], sA["kn2"][:], ALU.max)
                P.copy(krb[:], kra[:], eng=POOL)
                P.transpose(B0[0:33, 0:128], krb[:], K.ident_b[:])
                P.copy(krT[:], B0[0:33, 0:128])
                P.dma(dv(kl, kl.base()[2, 0:33, toks]), krT[:])
                P.act(junk[:, 0:64], pj[:, 672:736], AF.Square, accum_out=sA["ssC"][:])
                P.rsqrt(sA["rsC"][:], sA["ssC"][:], 1.0 / 64, EPS)
                P.stt(kin[:], pj[:, 672:736], sA["rsC"][:], kidxg[:], ALU.mult, ALU.mult)
                P.copy(kib[:], kin[:], eng=POOL)
                rope_tm(P, kin[:, 0:8], kin[:, 8:16], rb_[:, 0:8], rb_[:, 64:72],
                        kib[:, 0:8], kib[:, 8:16], t1[:, 0:8], t2[:, 0:8])
                P.transpose(B0[0:64, 0:128], kib[:], K.ident_b[:])
                P.copy(kiTt[:], B0[0:64, 0:128], eng=ACT)
                P.dma(dv(kiT, kiT.base()[:, toks]), kiTt[:])
                P.ts(wit[:], pj[:, 736:744], widx_scale, ALU.mult)
                P.dma(dv(wiS, wiS.base()[toks, :]), wit[:])
                P.transpose(PB[3][0:1, 0:128], sA["rm"][:], K.ident_f[:])
                P.reduce(km1[:], PB[3][0:1, 0:128], ALU.max)
                P.act(km1[:], km1[:], AF.Ln, bias=1e-30)
                P.act(km1[:], km1[:], AF.Exp, scale=0.5)
                P.ts(km1[:], km1[:], -1.0, ALU.mult)
                P.mm(PB[3][:, 128:129], K.ones_f[0:1, 0:128], km1[0:1, 0:1])
                P.copy(sA["nkmax"][:], PB[3][:, 128:129])
                for b4 in range(4):
                    pb = PB[4 + b4 % 2]
                    for i in range(4):
                        idx = b4 * 4 + i
                        h, cc = idx // 2, idx % 2
                        for kc in range(3):
                            P.mm(pb[:, cs(i)], Wabs[:, kc, h, cs(cc)], cqT[:, kc, :], start=(kc == 0), stop=(kc == 2))
                    P.copy(qab[:, b4 * 4:(b4 + 1) * 4, :], pb.v(pb.base()[:, :].rearrange("p (c t) -> p c t", t=128)),
                           eng=(ACT if b4 % 2 else DVE))
                P.dma(dv(qaT, qaT.base()[:, :, :, toks].rearrange("h c p t -> p (h c) t")), qab[:])
                P.tt(sqa[:], qab[:], qab[:], ALU.mult, eng=POOL)
                for idx in range(16):
                    h, cc = idx // 2, idx % 2
                    P.mm(PB[6][:, h:h + 1], sqa[:, idx, :], K.ones_b[:, 0:1], start=(cc == 0), stop=(cc == 1))
                for kc in range(3):
                    P.mm(PB[7][:, 0:256], cqT[:, kc, :], wuq_r.v(wuq_r.base()[:, kc, :, :].rearrange("p h d -> p (h d)")),
                         start=(kc == 0), stop=(kc == 2))
                P.copy(qr[:, :, :], PB[7].v(PB[7].base()[:, 0:256].rearrange("p (h d) -> p h d", d=32)), eng=ACT)
                rope_tm(P, qr[:, :, 0:16], qr[:, :, 16:32], v3(ra_, 0, 128, 16), v3(ra_, 128, 256, 16),
                        qra[:, :, 0:16], qra[:, :, 16:32], v3(t1, 0, 128, 16), v3(t2, 0, 128, 16))
                P.tt(t3[:, :, :], qra[:, :, 0:32], qra[:, :, 0:32], ALU.mult)
                P.reduce(qr2[:], t3[:, :, :], ALU.add)
                P.tt(qn[:], qr2[:], PB[6][:, 0:8], ALU.add)
                P.act(qn[:], qn[:], AF.Ln, bias=1e-30)
                P.act(qn[:], qn[:], AF.Exp, scale=0.5)
                P.ts(qra[:, :, 32:33], qn.v(qn.base()[:, :].rearrange("p (h o) -> p h o", o=1)), sA["nkmax"][:], ALU.mult)
                P.copy(qrb[:, :, :], qra[:, :, :], eng=POOL)
                for h in range(8):
                    P.transpose(B0[0:33, cs(h)], qrb[:, h, :], K.ident_b[:])
                P.copy(qrTt[:, :, :], B0.v(B0.base()[0:33, :].rearrange("p (h t) -> p h t", t=128)), eng=ACT)
                P.dma(dv(qrT, qrT.base()[:, :, toks].rearrange("h p t -> p h t")), qrTt[:])
                for kc in range(3):
                    P.mm(PB[1][:], cqT[:, kc, :], wqidx[:, kc, :], start=(kc == 0), stop=(kc == 2))
                P.copy(qi[:, :, :], PB[1].v(PB[1].base()[:, :].rearrange("p (h d) -> p h d", d=64)))
                P.copy(qib[:, :, :], qi[:, :, :], eng=POOL)
                rope_tm(P, qi[:, :, 0:8], qi[:, :, 8:16], v3(rb_, 0, 64, 8), v3(rb_, 64, 128, 8),
                        qib[:, :, 0:8], qib[:, :, 8:16], v3(t1, 0, 64, 8), v3(t2, 0, 64, 8))
                for h in range(8):
                    P.transpose(B0[0:64, cs(h)], qib[:, h, :], K.ident_b[:])
                P.copy(qiTt[:, :, :], B0.v(B0.base()[0:64, :].rearrange("p (h t) -> p h t", t=128)))
                P.dma(dv(qiT, qiT.base()[:, :, toks].rearrange("h p t -> p h t")), qiTt[:])

    with P.phase():
        B0 = P.psum("B0", [128, 1024], BF16)
        PI = [P.psum(f"PI{i}", [128, 512], F32) for i in range(2)]
        PS = [P.psum(f"PS{i}", [128, 512], F32) for i in range(2)]
        kis = P.sbuf("kis", [64, S], BF16)
        sc = [P.sbuf(f"sc{i}", [128, S], F32) for i in range(2)]
        jk = P.sbuf("jk", [128, S], BF16)
        neg = [P.sbuf(f"neg{i}", [128, S], BF16) for i in range(2)]
        ngt = [P.sbuf(f"ngt{i}", [128, NT, 128], BF16) for i in range(2)]
        rr = [[P.sbuf(f"rr{i}{k}", [128, 512], F32) for k in range(2)] for i in range(2)]
        dg = [P.sbuf(f"dg{i}", [128, 8, 128], F32) for i in range(2)]
        qiq = [P.sbuf(f"qiq{i}", [64, 8, 128], BF16) for i in range(2)]
        wiq = [P.sbuf(f"wiq{i}", [128, 8], F32) for i in range(2)]
        m_le = P.sbuf("m_le", [128, 128], F32)
        nbig = P.sbuf("nbig", [128, 128], F32)
        P.ts(m_le[:], K.m_gt[:], -1.0, ALU.mult, 1.0, ALU.add)
        P.ts(nbig[:], K.m_gt[:], -1e30, ALU.mult)
        pw2 = P.sbuf("pw2", [128, NIT], F32)
        for it in range(NIT):
            P.memset(pw2[:, it:it + 1], 2.0 ** -(it + 1))
        sB = [{n: P.sbuf(f"{n}{i}", [128, 1], F32) for n in ("lo", "hi", "mid", "cnt", "t")} for i in range(2)]
        stp = [P.sbuf(f"stp{i}", [128, NIT], F32) for i in range(2)]
        jks = [jk, P.sbuf("jk2", [128, S], BF16)]

        def idx_gen(sq_i, qb):
            par = qb % 2
            L = (qb + 1) * 128
            r0 = sq_i * S + qb * 128
            toks = sl(r0, r0 + 128)
            s_, q_, w_, d_ = sc[par], qiq[par], wiq[par], dg[par]
            P.dma(q_[:], dv(qiT, qiT.base()[:, :, toks].rearrange("h p t -> p h t")))
            P.dma(w_[:], dv(wiS, wiS.base()[toks, :]))
            for h in range(8):
                P.ts(d_[:, h, :], K.ident_f[:], w_[:, h:h + 1], ALU.mult)
            yield
            n = 0
            for kg in range((L + 511) // 512):
                w = min(512, L - kg * 512)
                cols = sl(kg * 512, kg * 512 + w)
                sb_ = PS[kg % 2]
                pend = None
                for h in range(8):
                    pb, r_ = PI[n % 2], rr[n % 2][0]
                    n += 1
                    P.mm(pb[:, 0:w], q_[:, h, :], kis[:, cols])
                    if pend is not None:
                        P.mm(sb_[:, 0:w], d_[:, pend[0], :], pend[1][:, 0:w], start=(pend[0] == 0), stop=False)
                    P.act(r_[:, 0:w], pb[:, 0:w], AF.Relu)
                    pend = (h, r_)
                    yield
                P.mm(sb_[:, 0:w], d_[:, 7, :], pend[1][:, 0:w], start=False, stop=True)
                P.copy(s_[:, cols], sb_[:, 0:w], eng=ACT)
                yield

        def bis_gen(sq_i, qb):
            par = qb % 2
            L = (qb + 1) * 128
            Lg = (4 * (qb // 4) + 4) * 128
            s_, n_, g_ = sc[par], neg[par], ngt[par]
            lo, hi, mid, cnt, tt_ = (sB[par][n] for n in ("lo", "hi", "mid", "cnt", "t"))
            st_, jk_ = stp[par], jks[par]
            P.reduce(hi[:], s_[:, 0:L], ALU.max)
            P.reduce(lo[:], s_[:, 0:L], ALU.min)
            yield
            P.tt(hi[:], hi[:], lo[:], ALU.subtract)
            P.ts(lo[:], lo[:], -1.0, ALU.add)
            P.ts(hi[:], hi[:], 2.0, ALU.add)
            P.ts(st_[:], pw2[:], hi[:], ALU.mult)
            blk = s_[:, qb * 128:L]
            P.tt(blk, blk, m_le[:], ALU.mult)
            P.tt(blk, blk, nbig[:], ALU.add)
            yield
            for it in range(NIT):
                P.tt(mid[:], lo[:], st_[:, it:it + 1], ALU.add)
                P.ts(jk_[:, 0:L], s_[:, 0:L], mid[:], ALU.is_gt, 0.0, ALU.add, accum_out=cnt[:])
                yield
                P.ts(tt_[:], cnt[:], float(topk) - 0.5, ALU.is_ge, st_[:, it:it + 1], ALU.mult)
                P.tt(lo[:], lo[:], tt_[:], ALU.add)
                yield
            P.ts(n_[:, 0:L], s_[:, 0:L], lo[:], ALU.is_gt)
            if Lg > L:
                P.memset(n_[:, L:Lg], 0.0, eng=POOL)
            yield
            nkb = Lg // 128
            for kb0 in range(0, nkb, 8):
                n8 = min(8, nkb - kb0)
                for i in range(n8):
                    P.transpose(B0[:, cs(i)], n_[:, cs(kb0 + i)], K.ident_b[:])
                P.copy(g_[:, kb0:kb0 + n8, :], B0.v(B0.base()[:, 0:n8 * 128].rearrange("p (c t) -> p c t", t=128)),
                       eng=ACT)
                yield
            P.dma(dv(negT, negT.base()[sq_i, 0:nkb, :, qb * 128:(qb + 1) * 128].rearrange("kb k q -> k kb q")),
                  g_[:, 0:nkb, :], q=POOL)

        prev = None
        for sq_i in range(C.NSEQ):
            P.dma(kis[:], dv(kiT, kiT.base()[:, sq_i * S:(sq_i + 1) * S]))
            for qb in range(NT):
                gens = [idx_gen(sq_i, qb)]
                if prev is not None:
                    gens.append(bis_gen(*prev))
                run_interleaved(gens)
                prev = (sq_i, qb)
        run_interleaved([bis_gen(*prev)])

    with P.phase():
        PA = [P.psum(f"PA{i}", [128, 512], F32) for i in range(2)]
        O0 = P.psum("O0", [128, 512], F32)
        O1 = P.psum("O1", [128, 512], F32)
        Dn = P.psum("Dn", [128, 512], F32)
        Ov = P.psum("Ov", [128, 512], F32)
        wuv = P.sbuf("wuv", [128, 2, 1024], BF16)
        P.dma(wuv[:], V(W['dsa_w_uv'][j].rearrange("(cc p) f -> p cc f", p=128), ("w", "uv")), q=POOL)
        kl01 = P.sbuf("kl01", [128, 2, S], BF16)
        kl2 = P.sbuf("kl2", [33, S], BF16)
        ckv = P.sbuf("ckvs", [128, NT, 256], BF16)
        ngc = [P.sbuf(f"ngc{i}", [128, NT, 512], BF16) for i in range(2)]
        qa = [P.sbuf(f"qa{i}", [128, 2, 512], BF16) for i in range(2)]
        qrr = [P.sbuf(f"qrr{i}", [33, 512], BF16) for i in range(2)]
        pT = [P.sbuf(f"pT{i}", [128, 512], BF16) for i in range(2)]
        ol = P.sbuf("ol", [128, 2, 512], BF16)
        rden = P.sbuf("rden", [128, 512], F32)
        ost = [P.sbuf(f"ost{i}", [128, 512], BF16) for i in range(2)]
        cnt_ = 0
        for sq_i in range(C.NSEQ):
            seq = sl(sq_i * S, (sq_i + 1) * S)
            P.dma(kl01[:], dv(kl, kl.base()[0:2, :, seq].rearrange("c p t -> p c t")))
            P.dma(kl2[:], dv(kl, kl.base()[2, 0:33, seq]))
            P.dma(ckv[:], dv(kvtm, kvtm.base()[seq, :].rearrange("(t p) c -> p t c", p=128)))
            for g in range(NG):
                nkb = 4 * g + 4
                ng_ = ngc[g % 2]
                gt = sl(sq_i * S + g * 512, sq_i * S + (g + 1) * 512)
                P.dma(ng_[:, 0:nkb, :], dv(negT, negT.base()[sq_i, 0:nkb, :, g * 512:(g + 1) * 512].rearrange("kb k q -> k kb q")))
                for h in range(8):
                    qa_, qr_ = qa[cnt_ % 2], qrr[cnt_ % 2]
                    o_ = ost[cnt_ % 2]
                    cnt_ += 1
                    P.dma(qa_[:], dv(qaT, qaT.base()[h, :, :, gt].rearrange("c p t -> p c t")))
                    P.dma(qr_[:], dv(qrT, qrT.base()[h, 0:33, gt]))

                    def pv(i):
                        p_ = pT[i % 2]
                        P.mm(O0[:], ckv[:, i, 0:128], p_[:], start=(i == 0), stop=(i == nkb - 1))
                        P.mm(O1[:], ckv[:, i, 128:256], p_[:], start=(i == 0), stop=(i == nkb - 1))
                        P.mm(Dn[:], K.ones_b[:], p_[:], start=(i == 0), stop=(i == nkb - 1))

                    for kb in range(nkb):
                        A = PA[kb % 2]
                        P.mm(A[:], kl01[:, 0, cs(kb)], qa_[:, 0, :], start=True, stop=False)
                        P.mm(A[:], kl01[:, 1, cs(kb)], qa_[:, 1, :], start=False, stop=False)
                        P.mm(A[:], kl2[0:33, cs(kb)], qr_[0:33, :], start=False, stop=True)
                        if kb > 0:
                            pv(kb - 1)
                        P.act(pT[kb % 2][:], A[:], AF.Exp, scale=att_scale)
                        P.tt(pT[kb % 2][:], pT[kb % 2][:], ng_[:, kb, :], ALU.mult)
                    pv(nkb - 1)
                    P.copy(ol[:, 0, :], O0[:], eng=ACT)
                    P.copy(ol[:, 1, :], O1[:])
                    P.recip(rden[:], Dn[:])
                    P.mm(Ov[:], wuv[:, 0, cs(h)], ol[:, 0, :], start=True, stop=False)
                    P.mm(Ov[:], wuv[:, 1, cs(h)], ol[:, 1, :], start=False, stop=True)
                    P.tt(o_[:], Ov[:], rden[:], ALU.mult)
                    P.dma(dv(os_, os_.base()[h, :, gt]), o_[:], q=POOL)

    outproj_phase(P, K, C, xr, W['dsa_w_out'][j], os_)


def rope_tables(S):
    pos = np.arange(S, dtype=np.float32)[:, None]

    def tab(r):
        half = r // 2
        inv = (np.float32(500000.0) ** (-np.arange(half, dtype=np.float32) * np.float32(2.0 / r))).astype(np.float32)
        ang = pos * inv[None, :]
        c = np.tile(np.cos(ang).astype(np.float32), (1, 8))
        s_ = np.tile(np.sin(ang).astype(np.float32), (1, 8))
        return np.ascontiguousarray(np.concatenate([c, s_], axis=1), dtype=np.float32)

    return tab(32), tab(16)


def run(C, inputs, n_cores):
    nc = build(C)
    x = np.ascontiguousarray(inputs['x'], dtype=np.float32).reshape(n_cores, C.NTOK, D_MODEL)
    in_maps = []
    for c in range(n_cores):
        m = {"x": x[c]}
        m["ropeA"], m["ropeB"] = rope_tables(C.S)
        for name in INPUT_SHAPES:
            m[name] = np.ascontiguousarray(inputs[name], dtype=np.float32)
        in_maps.append(m)
    res = run_bass_kernel_spmd(nc, in_maps, core_ids=list(range(n_cores)))
    return np.stack([np.asarray(r["y"]) for r in res.results], axis=0)


def kernel(**inputs):
    C = Cfg()
    x = np.asarray(inputs['x'])
    B, S, D = x.shape
    y = run(C, inputs, N_CORES)
    return y.reshape(B, S, D).astype(np.float32)
```

```python
import contextlib
from contextlib import ExitStack

import numpy as np
import concourse.bass as bass
import concourse.mybir as mybir
from concourse.bass_utils import run_bass_kernel_spmd

F32 = mybir.dt.float32
BF16 = mybir.dt.bfloat16
AF = mybir.ActivationFunctionType
ALU = mybir.AluOpType
AX = mybir.AxisListType

D_MODEL = 1024
D_FF = 2816
EPS = 1e-6
N_CORES = 8

PE, ACT, DVE, POOL, SP = "pe", "act", "dve", "pool", "sp"
SEM_EPOCH = 20000
NDMA_SLOTS = 24


class V:
    __slots__ = ("ap", "key")

    def __init__(self, ap, key):
        self.ap = ap
        self.key = key


class T:
    _n = 0

    def __init__(self, handle, name, ap=None, is_psum=False):
        self.h = handle
        self.name = name
        T._n += 1
        self.id = T._n
        self._ap = ap
        self.is_psum = is_psum

    def base(self):
        return self._ap if self._ap is not None else self.h

    def __getitem__(self, idx):
        return V(self.base()[idx], (self.id, None, self.is_psum))

    def k(self, sub, idx=slice(None)):
        return V(self.base()[idx], (self.id, None if self.is_psum else sub, self.is_psum))

    def v(self, ap, sub=None):
        return V(ap, (self.id, None if self.is_psum else sub, self.is_psum))


class Op:
    __slots__ = ("eng", "fn", "reads", "writes", "is_dma", "idx", "deps", "signal", "sig_no", "slot", "slot_val")

    def __init__(self, eng, fn, reads, writes, is_dma=False):
        self.eng = eng
        self.fn = fn
        self.reads = reads
        self.writes = writes
        self.is_dma = is_dma
        self.deps = []
        self.signal = False
        self.sig_no = None


class Prog:
    def __init__(self, nc, es):
        self.nc = nc
        self.es_outer = es
        self.es = es
        self.ops = []
        self.engs = {PE: nc.tensor, ACT: nc.scalar, DVE: nc.vector, POOL: nc.gpsimd, SP: nc.sync}
        self.sig_count = {e: 0 for e in self.engs}
        self.dma_count = {e: 0 for e in self.engs}
        self.sems = {e: [] for e in self.engs}
        self.dsems = {e: [] for e in self.engs}
        self.n_emitted = 0

    def sbuf(self, name, shape, dt):
        T._n += 1
        name = f"{name}_{T._n}"
        h = self.es.enter_context(self.nc.sbuf_tensor(name, list(shape), dt))
        return T(h, name)

    def psum(self, name, shape, dt=F32):
        T._n += 1
        name = f"{name}_{T._n}"
        h = self.es.enter_context(self.nc.psum_tensor(name, list(shape), dt))
        return T(h, name, is_psum=True)

    def dram(self, name, shape, dt, kind="Internal"):
        h = self.nc.dram_tensor(name, list(shape), dt, kind=kind)
        return T(h, name, ap=h.ap())

    @contextlib.contextmanager
    def phase(self):
        old = self.es
        with ExitStack() as es:
            self.es = es
            yield
            self.flush()
        self.es = old

    def op(self, eng, fn, reads, writes, is_dma=False):
        rk = [v.key for v in reads]
        wk = [v.key for v in writes]
        wk += [k for k in rk if len(k) == 3 and k[2] is True]
        o = Op(eng, fn, rk, wk, is_dma)
        o.idx = len(self.ops)
        self.ops.append(o)
        return o

    def dma(self, out, in_, q=SP, **kw):
        return self.op(q, lambda e: e.dma_start(out=out.ap, in_=in_.ap, **kw), [in_], [out], is_dma=True)

    def mm(self, out, lhsT, rhs, start=True, stop=True):
        return self.op(PE, lambda e: e.matmul(out.ap, lhsT=lhsT.ap, rhs=rhs.ap, start=start, stop=stop),
                       [lhsT, rhs] + ([] if start else [out]), [out])

    def transpose(self, out, in_, ident):
        return self.op(PE, lambda e: e.transpose(out.ap, in_.ap, ident.ap), [in_, ident], [out])

    def act(self, out, in_, func, bias=None, scale=None, accum_out=None):
        kw = {}
        reads = [in_]
        writes = [out]
        if bias is not None:
            if isinstance(bias, V):
                kw["bias"] = bias.ap
                reads.append(bias)
            else:
                kw["bias"] = bias
        if scale is not None:
            if isinstance(scale, V):
                kw["scale"] = scale.ap
                reads.append(scale)
            else:
                kw["scale"] = scale
        if accum_out is not None:
            kw["accum_out"] = accum_out.ap
            writes.append(accum_out)
        return self.op(ACT, lambda e: e.activation(out.ap, in_.ap, func, **kw), reads, writes)

    def copy(self, out, in_, eng=DVE):
        if eng == ACT:
            return self.op(ACT, lambda e: e.copy(out.ap, in_.ap), [in_], [out])
        return self.op(eng, lambda e: e.tensor_copy(out=out.ap, in_=in_.ap), [in_], [out])

    def tt(self, out, in0, in1, op, eng=DVE):
        return self.op(eng, lambda e: e.tensor_tensor(out.ap, in0.ap, in1.ap, op), [in0, in1], [out])

    def ts(self, out, in0, s1, op0, s2=None, op1=None, accum_out=None, eng=DVE):
        reads = [in0]
        writes = [out]
        a1 = s1.ap if isinstance(s1, V) else s1
        a2 = s2.ap if isinstance(s2, V) else s2
        if isinstance(s1, V):
            reads.append(s1)
        if isinstance(s2, V):
            reads.append(s2)
        kw = {}
        if op1 is not None:
            kw["op1"] = op1
        if accum_out is not None:
            kw["accum_out"] = accum_out.ap
            writes.append(accum_out)
        return self.op(eng, lambda e: e.tensor_scalar(out.ap, in0.ap, a1, a2, op0, **kw), reads, writes)

    def stt(self, out, in0, s, in1, op0, op1, eng=DVE):
        reads = [in0, in1]
        a = s.ap if isinstance(s, V) else s
        if isinstance(s, V):
            reads.append(s)
        return self.op(eng, lambda e: e.scalar_tensor_tensor(out.ap, in0.ap, a, in1.ap, op0, op1), reads, [out])

    def reduce(self, out, in_, op, axis=AX.X, eng=DVE):
        return self.op(eng, lambda e: e.tensor_reduce(out.ap, in_.ap, axis, op), [in_], [out])

    def recip(self, out, in_):
        return self.op(DVE, lambda e: e.reciprocal(out.ap, in_.ap), [in_], [out])

    def rsqrt(self, out, in_, mult, add):
        self.act(out, in_, AF.Ln, bias=add, scale=mult)
        return self.act(out, out, AF.Exp, scale=-0.5)

    def memset(self, out, val, eng=DVE):
        return self.op(eng, lambda e: e.memset(out.ap, val), [], [out])

    def _sem(self, eng, n):
        i = n // SEM_EPOCH
        while len(self.sems[eng]) <= i:
            self.sems[eng].append(self.es_outer.enter_context(self.nc.semaphore(f"s_{eng}_{len(self.sems[eng])}")))
        return self.sems[eng][i], n % SEM_EPOCH + 1

    def _dsem(self, eng, slot):
        while len(self.dsems[eng]) <= slot:
            self.dsems[eng].append(self.es_outer.enter_context(self.nc.semaphore(f"d_{eng}_{len(self.dsems[eng])}")))
        return self.dsems[eng][slot]

    def flush(self):
        ops = self.ops
        if not ops:
            return
        last_w = {}
        readers = {}
        for o in ops:
            deps = set()
            for k in o.reads:
                w = last_w.get(k)
                if w is not None:
                    deps.add(w)
            for k in o.writes:
                w = last_w.get(k)
                if w is not None:
                    deps.add(w)
                for r in readers.get(k, ()):
                    deps.add(r)
            deps.discard(o.idx)
            o.deps = deps
            for k in o.reads:
                readers.setdefault(k, []).append(o.idx)
            for k in o.writes:
                last_w[k] = o.idx
                readers[k] = []
        waited = {e: {} for e in self.engs}
        waited_dma = {e: set() for e in self.engs}
        for o in ops:
            best = {}
            keep = []
            for d in o.deps:
                p = ops[d]
                if p.is_dma:
                    if d not in waited_dma[o.eng]:
                        waited_dma[o.eng].add(d)
                        keep.append(d)
                    continue
                if p.eng == PE and o.eng == PE and not o.is_dma:
                    continue
                if waited[o.eng].get(p.eng, -1) >= d:
                    continue
                if best.get(p.eng, -1) < d:
                    best[p.eng] = d
            for e, d in best.items():
                keep.append(d)
                waited[o.eng][e] = d
            o.deps = sorted(keep)
            for d in o.deps:
                ops[d].signal = True
        last_op = {}
        for o in ops:
            if not o.is_dma:
                last_op[o.eng] = o
        for o in last_op.values():
            o.signal = True
        last_dma = {}
        for o in ops:
            if o.is_dma:
                o.slot = self.dma_count[o.eng] % NDMA_SLOTS
                o.slot_val = 16 * (self.dma_count[o.eng] // NDMA_SLOTS + 1)
                self.dma_count[o.eng] += 1
                last_dma[(o.eng, o.slot)] = o
            elif o.signal:
                o.sig_no = self.sig_count[o.eng]
                self.sig_count[o.eng] += 1

        def sem_of(p):
            if p.is_dma:
                return self._dsem(p.eng, p.slot), p.slot_val
            return self._sem(p.eng, p.sig_no)

        for o in ops:
            e = self.engs[o.eng]
            for d in o.deps:
                s, v = sem_of(ops[d])
                e.wait_ge(s, v)
            if o.is_dma and o.slot_val > 16:
                e.wait_ge(self._dsem(o.eng, o.slot), o.slot_val - 16)
            ins = o.fn(e)
            if o.is_dma:
                ins.then_inc(self._dsem(o.eng, o.slot), 16)
            elif o.signal:
                s, _ = self._sem(o.eng, o.sig_no)
                ins.then_inc(s, 1)
        for en, e in self.engs.items():
            for pe_, o in last_op.items():
                if pe_ == en:
                    continue
                s, v = sem_of(o)
                e.wait_ge(s, v)
            for o in last_dma.values():
                s, v = sem_of(o)
                e.wait_ge(s, v)
        self.n_emitted += len(ops)
        self.ops = []


class Cfg:
    def __init__(self, S=4096, NSEQ=2, layers=(0, 1, 2, 3), ffn=True, mixers=True, final=True):
        self.S = S
        self.NSEQ = NSEQ
        self.NTOK = S * NSEQ
        self.layers = tuple(layers)
        self.ffn = ffn
        self.mixers = mixers
        self.final = final
        self.topk = min(256, S // 4)


INPUT_SHAPES = {
    'ffn1_norm': (4, 1024), 'ffn1_w_gu': (4, 1024, 5632), 'ffn1_w_down': (4, 2816, 1024),
    'mix_norm': (4, 1024), 'ffn2_norm': (4, 1024), 'ffn2_w_gu': (4, 1024, 5632), 'ffn2_w_down': (4, 2816, 1024),
    'gdn_w_in': (2, 1024, 4112), 'gdn_conv': (2, 4, 3072), 'gdn_a_log': (2, 8), 'gdn_dt_bias': (2, 8),
    'gdn_norm': (2, 128), 'gdn_w_out': (2, 1024, 1024),
    'sb_w_in': (1, 1024, 3072), 'sb_w_out': (1, 1024, 1024),
    'dsa_w_in': (1, 1024, 744), 'dsa_cq_norm': (1, 384), 'dsa_ckv_norm': (1, 256), 'dsa_kidx_norm': (1, 64),
    'dsa_w_uq': (1, 384, 1024), 'dsa_w_qidx': (1, 384, 512), 'dsa_w_uk': (1, 256, 768), 'dsa_w_uv': (1, 256, 1024),
    'dsa_w_out': (1, 1024, 1024), 'final_norm': (1024,),
}


class Ctx:
    pass


def make_consts(P, K):
    K.ident_f = P.sbuf("ident_f", [128, 128], F32)
    K.ident_b = P.sbuf("ident_b", [128, 128], BF16)
    K.ones_f = P.sbuf("ones_f", [128, 128], F32)
    K.ones_b = P.sbuf("ones_b", [128, 128], BF16)
    P.memset(K.ones_f[:], 1.0)
    P.memset(K.ones_b[:], 1.0)
    P.memset(K.ident_f[:], 1.0)
    P.op(POOL, lambda e: e.affine_select(K.ident_f[:].ap, K.ident_f[:].ap, [[-1, 128]], ALU.is_equal, 0.0,
                                         base=0, channel_multiplier=1), [K.ident_f[:]], [K.ident_f[:]])
    P.copy(K.ident_b[:], K.ident_f[:])
    K.m_ge = P.sbuf("m_ge", [128, 128], F32)
    K.m_gt = P.sbuf("m_gt", [128, 128], F32)
    K.m_lt = P.sbuf("m_lt", [128, 128], F32)
    for t, pat, cm, cmp_ in ((K.m_ge, 1, -1, ALU.is_ge), (K.m_gt, 1, -1, ALU.is_gt), (K.m_lt, -1, 1, ALU.is_gt)):
        P.memset(t[:], 1.0)
        P.op(POOL, (lambda t, pat, cm, cmp_: lambda e: e.affine_select(t[:].ap, t[:].ap, [[pat, 128]], cmp_, 0.0,
                                                                       base=0, channel_multiplier=cm))(t, pat, cm, cmp_),
             [t[:]], [t[:]])


def norm_T(P, K, xs, gb, hT_view, scr, psT, n_feat=1024, eps=EPS):
    junk, ss, rstd, xn = scr
    P.act(junk[:, 0:n_feat], xs, AF.Square, accum_out=ss[:])
    P.rsqrt(rstd[:], ss[:], 1.0 / n_feat, eps)
    P.stt(xn[:, 0:n_feat], xs, rstd[:], gb, ALU.mult, ALU.mult)
    nch = n_feat // 128
    for kc in range(nch):
        P.transpose(psT[:, kc * 128:(kc + 1) * 128], xn[:, kc * 128:(kc + 1) * 128], K.ident_b[:])
    P.copy(hT_view, psT.v(psT.base()[:, 0:n_feat].rearrange("p (c t) -> p c t", t=128)), eng=ACT)


def ffn_phase(P, K, C, src, dst, g_row, w_gu, w_down, tag):
    TT = 256
    NS = TT // 128
    NFC = D_FF // 128
    with P.phase():
        wgu = P.sbuf("wgu", [128, 8, 2 * D_FF], BF16)
        wd = P.sbuf("wd", [128, NFC, D_MODEL], BF16)
        gb = P.sbuf("gb", [128, D_MODEL], F32)
        P.dma(gb[:], V(g_row.partition_broadcast(128), ("w", tag, "g")))
        wgu_src = w_gu.rearrange("(kc p) f -> p kc f", p=128)
        for fi in range(11):
            P.dma(wgu.k(fi, (slice(None), slice(None), slice(fi * 512, (fi + 1) * 512))),
                  V(wgu_src[:, :, fi * 512:(fi + 1) * 512], ("w", tag, "gu")), q=POOL)
        wd_src = w_down.rearrange("(fc p) d -> p fc d", p=128)
        for pi in range(2):
            P.dma(wd.k(pi, (slice(None), slice(pi * 11, (pi + 1) * 11), slice(None))),
                  V(wd_src[:, pi * 11:(pi + 1) * 11, :], ("w", tag, "d")), q=POOL)
        xt = [P.sbuf(f"xt{i}", [128, D_MODEL], F32) for i in range(2 * NS)]
        hT = [P.sbuf(f"hT{i}", [128, 8, TT], BF16) for i in range(2)]
        aT = P.sbuf("aT", [128, NFC, TT], BF16)
        sg = [P.sbuf(f"sg{i}", [128, TT], F32) for i in range(2)]
        scr = (P.sbuf("junk", [128, D_MODEL], BF16), P.sbuf("ss", [128, 1], F32),
               P.sbuf("rstd", [128, 1], F32), P.sbuf("xn", [128, D_MODEL], BF16))
        psT = [P.psum(f"psT{i}", [128, D_MODEL], BF16) for i in range(2)]
        psg = [P.psum(f"psg{i}", [128, TT], F32) for i in range(2)]
        psu = [P.psum(f"psu{i}", [128, TT], F32) for i in range(2)]
        psd = [P.psum(f"psd{i}", [128, 512], F32) for i in range(2)]
        ntile = C.NTOK // TT
        nd = 0
        for ti in range(ntile):
            r0 = ti * TT
            xs = [xt[(ti % 2) * NS + s] for s in range(NS)]
            h = hT[ti % 2]
            for s in range(NS):
                P.dma(xs[s][:], src.k(("r", r0 // 128 + s), (slice(r0 + s * 128, r0 + (s + 1) * 128), slice(None))))
                norm_T(P, K, xs[s][:], gb[:], h[:, :, s * 128:(s + 1) * 128], scr, psT[s % 2])
            for fc in range(NFC):
                pg = psg[fc % 2]
                pu = psu[fc % 2]
                for kc in range(8):
                    P.mm(pg[:], wgu.k(fc // 4, (slice(None), kc, slice(fc * 128, (fc + 1) * 128))), h[:, kc, :],
                         start=(kc == 0), stop=(kc == 7))
                cu = NFC + fc
                for kc in range(8):
                    P.mm(pu[:], wgu.k(cu // 4, (slice(None), kc, slice(cu * 128, (cu + 1) * 128))), h[:, kc, :],
                         start=(kc == 0), stop=(kc == 7))
                P.act(sg[fc % 2][:], pg[:], AF.Silu)
                P.tt(aT[:, fc, :], sg[fc % 2][:], pu[:], ALU.mult)
            for s in range(NS):
                for half in range(2):
                    pd = psd[nd % 2]
                    nd += 1
                    for fc in range(NFC):
                        P.mm(pd[:], aT[:, fc, s * 128:(s + 1) * 128],
                             wd.k(fc // 11, (slice(None), fc, slice(half * 512, (half + 1) * 512))),
                             start=(fc == 0), stop=(fc == NFC - 1))
                    P.stt(xs[s][:, half * 512:(half + 1) * 512], pd[:], 0.5, xs[s][:, half * 512:(half + 1) * 512],
                          ALU.mult, ALU.add)
                P.dma(dst.k(("r", r0 // 128 + s), (slice(r0 + s * 128, r0 + (s + 1) * 128), slice(None))), xs[s][:],
                      q=POOL)


def final_phase(P, K, C, src, dst, g_row):
    with P.phase():
        gb = P.sbuf("gb", [128, D_MODEL], F32)
        P.dma(gb[:], V(g_row.partition_broadcast(128), ("w", "fin", "g")))
        xt = [P.sbuf(f"xt{i}", [128, D_MODEL], F32) for i in range(4)]
        junk = P.sbuf("junk", [128, D_MODEL], BF16)
        ss = [P.sbuf(f"ss{i}", [128, 1], F32) for i in range(4)]
        for ti in range(C.NTOK // 128):
            x = xt[ti % 4]
            s = ss[ti % 4]
            rows = (slice(ti * 128, (ti + 1) * 128), slice(None))
            P.dma(x[:], src.k(("r", ti), rows))
            P.act(junk[:], x[:], AF.Square, accum_out=s[:])
            P.rsqrt(s[:], s[:], 1.0 / D_MODEL, EPS)
            P.stt(x[:], x[:], s[:], gb[:], ALU.mult, ALU.mult)
            P.dma(dst.k(("r", ti), rows), x[:], q=POOL)


def copy_phase(P, C, src, dst):
    with P.phase():
        xt = [P.sbuf(f"xt{i}", [128, D_MODEL], F32) for i in range(4)]
        for ti in range(C.NTOK // 128):
            rows = (slice(ti * 128, (ti + 1) * 128), slice(None))
            P.dma(xt[ti % 4][:], src.k(("r", ti), rows))
            P.dma(dst.k(("r", ti), rows), xt[ti % 4][:], q=POOL)


def build(C):
    nc = bass.Bass("TRN2", target_bir_lowering=False)
    with ExitStack() as es:
        P = Prog(nc, es)
        K = Ctx()
        x_in = P.dram("x", [C.NTOK, D_MODEL], F32, kind="ExternalInput")
        y_out = P.dram("y", [C.NTOK, D_MODEL], F32, kind="ExternalOutput")
        xr = P.dram("xr", [C.NTOK, D_MODEL], F32)
        W = {}
        for name, shp in INPUT_SHAPES.items():
            W[name] = nc.dram_tensor(name, list(shp), F32, kind="ExternalInput").ap()
        SC = {}
        if C.mixers and any(l % 3 in (1, 2) for l in C.layers):
            SC["qs"] = P.dram("sc_qs", [8, 128, C.NTOK], BF16)
            SC["ks"] = P.dram("sc_ks", [8, 128, C.NTOK], BF16)
            SC["vs"] = P.dram("sc_vs", [C.NTOK, 1024], BF16)
            SC["os"] = P.dram("sc_os", [8, 128, C.NTOK], BF16)
        if C.mixers and any(l % 3 == 2 for l in C.layers):
            NT_ = C.S // 128
            SC["kl"] = P.dram("sc_kl", [3, 128, C.NTOK], BF16)
            SC["kvtm"] = P.dram("sc_kvtm", [C.NTOK, 256], BF16)
            SC["kiT"] = P.dram("sc_kiT", [64, C.NTOK], BF16)
            SC["wi"] = P.dram("sc_wi", [C.NTOK, 8], F32)
            SC["qaT"] = P.dram("sc_qaT", [8, 2, 128, C.NTOK], BF16)
            SC["qrT"] = P.dram("sc_qrT", [8, 33, C.NTOK], BF16)
            SC["qiT"] = P.dram("sc_qiT", [8, 64, C.NTOK], BF16)
            SC["negT"] = P.dram("sc_negT", [C.NSEQ, NT_, 128, C.S], BF16)
        SC["ropeA"] = nc.dram_tensor("ropeA", [C.S, 256], F32, kind="ExternalInput").ap()
        SC["ropeB"] = nc.dram_tensor("ropeB", [C.S, 128], F32, kind="ExternalInput").ap()
        make_consts(P, K)
        P.flush()
        cur = x_in
        for li in C.layers:
            kind, j = li % 3, li // 3
            if C.ffn:
                ffn_phase(P, K, C, cur, xr, W['ffn1_norm'][li], W['ffn1_w_gu'][li], W['ffn1_w_down'][li], f"f1_{li}")
                cur = xr
            if C.mixers:
                if cur is x_in:
                    copy_phase(P, C, x_in, xr)
                    cur = xr
                if kind == 0:
                    gdn_phase(P, K, C, xr, W, li, j)
                elif kind == 1:
                    sb_phase(P, K, C, xr, W, li, j, SC)
                else:
                    dsa_phase(P, K, C, xr, W, li, j, SC)
            if C.ffn:
                ffn_phase(P, K, C, cur, xr, W['ffn2_norm'][li], W['ffn2_w_gu'][li], W['ffn2_w_down'][li], f"f2_{li}")
                cur = xr
        if C.final:
            final_phase(P, K, C, cur, y_out, W['final_norm'])
        else:
            copy_phase(P, C, cur, y_out)
        P.flush()
        C.n_ops = P.n_emitted
    return nc


def sl(a, b):
    return slice(a, b)


ALL = slice(None)


def run_interleaved(gens):
    alive = list(gens)
    while alive:
        for g in list(alive):
            try:
                next(g)
            except StopIteration:
                alive.remove(g)


def gdn_phase(P, K, C, xr, W, li, j):
    S = C.S
    NT = S // 128
    with P.phase():
        win = P.sbuf("win", [128, 8, 4112], BF16)
        wsrc = W['gdn_w_in'][j].rearrange("(kc p) f -> p kc f", p=128)
        pieces = [(i * 512, (i + 1) * 512) for i in range(8)] + [(4096, 4112)]
        for pi, (a, b) in enumerate(pieces):
            P.dma(win.k(pi, (ALL, ALL, sl(a, b))), V(wsrc[:, :, a:b], ("w", "gdn_in")), q=POOL)
        wout = P.sbuf("wout", [128, 8, 1024], BF16)
        P.dma(wout[:], V(W['gdn_w_out'][j].rearrange("(kc p) d -> p kc d", p=128), ("w", "gdn_out")), q=POOL)
        gb = P.sbuf("gb", [128, D_MODEL], F32)
        P.dma(gb[:], V(W['mix_norm'][li].partition_broadcast(128), ("w", "mixg")))
        gnb8 = P.sbuf("gnb8", [128, 1024], F32)
        for h in range(8):
            P.dma(gnb8[:, h * 128:(h + 1) * 128], V(W['gdn_norm'][j].partition_broadcast(128), ("w", "gn")))
        alb = P.sbuf("alb", [128, 8], F32)
        dtb = P.sbuf("dtb", [128, 8], F32)
        nea = P.sbuf("nea", [128, 8], F32)
        P.dma(alb[:], V(W['gdn_a_log'][j].partition_broadcast(128), ("w", "alog")))
        P.dma(dtb[:], V(W['gdn_dt_bias'][j].partition_broadcast(128), ("w", "dtb")))
        P.act(nea[:], alb[:], AF.Exp)
        P.ts(nea[:], nea[:], -1.0, ALU.mult)
        cw4 = P.sbuf("cw4", [96, 128], F32)
        P.dma(cw4[:], V(W['gdn_conv'][j].rearrange("i (c p) -> (i c) p", p=128), ("w", "conv")))
        cw = P.sbuf("cw", [128, 96], F32)
        diagw = P.sbuf("diagw", [128, 96, 128], BF16)
        B0 = P.psum("B0", [128, 1024], BF16)
        PB = [None] + [P.psum(f"PB{i}", [128, 512], F32) for i in range(1, 8)]
        P.transpose(PB[1][:, 0:96], cw4[:], K.ident_f[0:96, 0:96])
        P.copy(cw[:], PB[1][:, 0:96])
        for ci in range(96):
            P.ts(diagw.k(ci, (ALL, ci, ALL)), K.ident_f[:], cw[:, ci:ci + 1], ALU.mult)
        xt = [P.sbuf(f"xt{i}", [128, D_MODEL], F32) for i in range(2)]
        hT = [P.sbuf(f"hT{i}", [128, 8, 128], BF16) for i in range(2)]
        scr = (P.sbuf("junk", [128, D_MODEL], BF16), P.sbuf("ss", [128, 1], F32),
               P.sbuf("rstd", [128, 1], F32), P.sbuf("xn", [128, D_MODEL], BF16))
        cb = P.sbuf("cb", [128, 24, 131], BF16)
        sil = [P.sbuf(f"sil{i}", [128, 512], F32) for i in range(2)]
        sqb = [P.sbuf(f"sqb{i}", [128, 512], BF16) for i in range(2)]
        rs = [P.sbuf(f"rs{i}", [128, 512], F32) for i in range(2)]
        qkT = P.sbuf("qkT", [128, 16, 128], BF16)
        vT = P.sbuf("vT", [128, 8, 128], BF16)
        gz = P.sbuf("gz", [128, 1024], F32)
        sm = {n: P.sbuf(n, [128, 8], F32) for n in ("eb", "beta", "nbeta", "ta", "g", "egc", "gtot", "dk", "kdsc")}
        gc = P.sbuf("gc", [128, 16], F32)
        S_f = P.sbuf("S_f", [128, 8, 128], F32)
        S_b = P.sbuf("S_b", [128, 8, 128], BF16)
        gh = [P.sbuf(f"gh{i}", [128, 128], F32) for i in range(4)]
        E2 = [P.sbuf(f"E2{i}", [128, 256], F32) for i in range(4)]
        EMi = [P.sbuf(f"EMi{i}", [128, 128], F32) for i in range(4)]
        EMs = [P.sbuf(f"EMs{i}", [128, 128], F32) for i in range(4)]
        WW = [[P.sbuf(f"WW{p}{i}", [128, 256], F32) for i in range(2)] for p in range(4)]
        Rm = [P.sbuf(f"R{p}", [128, 128], F32) for p in range(4)]
        Rb = [P.sbuf(f"Rb{p}", [128, 128], BF16) for p in range(4)]
        vtm = [P.sbuf(f"vtm{p}", [128, 128], BF16) for p in range(4)]
        Xk = [P.sbuf(f"Xk{h}", [128, 128], BF16) for h in range(8)]
        kdec = [P.sbuf(f"kdec{h}", [128, 128], BF16) for h in range(8)]
        qkd = [P.sbuf(f"qkd{h}", [128, 128], BF16) for h in range(8)]
        qdT = [P.sbuf(f"qdT{h}", [128, 128], BF16) for h in range(8)]
        uinb = [P.sbuf(f"uinb{h}", [128, 128], F32) for h in range(8)]
        wT = [P.sbuf(f"wT{h}", [128, 128], BF16) for h in range(8)]
        uu = [P.sbuf(f"u{h}", [128, 128], BF16) for h in range(8)]
        ssq = [P.sbuf(f"ssq{h}", [128, 1], F32) for h in range(8)]
        rsq = [P.sbuf(f"rsq{h}", [128, 1], F32) for h in range(8)]
        junk2 = [P.sbuf(f"junk2{i}", [128, 128], BF16) for i in range(2)]
        og = P.sbuf("og", [128, 1024], BF16)
        ogT = P.sbuf("ogT", [128, 8, 128], BF16)

        def cs(i, n=1):
            return sl(i * 128, (i + n) * 128)

        for sq_i in range(C.NSEQ):
            for h in range(8):
                P.memset(S_f.k(h, (ALL, h, ALL)), 0.0)
                P.memset(S_b.k(h, (ALL, h, ALL)), 0.0)
            for gq in range(6):
                P.memset(cb.k(gq, (ALL, sl(gq * 4, gq * 4 + 4), sl(0, 3))), 0.0)
            for t in range(NT):
                r0 = sq_i * S + t * 128
                ti = r0 // 128
                xs = xt[t % 2]
                h_ = hT[t % 2]
                rows = (sl(r0, r0 + 128), ALL)
                P.dma(xs[:], xr.k(("r", ti), rows))
                norm_T(P, K, xs[:], gb[:], h_[:, :, :], scr, B0)

                def stage_a(gq):
                    pp = PB[1 + gq % 2]
                    pc = PB[3 + gq % 2]
                    for cc in range(4):
                        c = gq * 4 + cc
                        for kc in range(8):
                            P.mm(pp[:, cs(cc)], win.k(c // 4, (ALL, kc, cs(c))), h_[:, kc, :],
                                 start=(kc == 0), stop=(kc == 7))
                    cbg = cb.k(gq, (ALL, sl(gq * 4, gq * 4 + 4), sl(3, 131)))
                    P.copy(cbg, pp.v(pp.base()[:, :].rearrange("p (c t) -> p c t", t=128)), eng=ACT)
                    yield
                    for cc in range(4):
                        c = gq * 4 + cc
                        for i in range(4):
                            P.mm(pc[:, cs(cc)], diagw.k(i * 24 + c, (ALL, i * 24 + c, ALL)),
                                 cb.k(gq, (ALL, c, sl(i, i + 128))), start=(i == 0), stop=(i == 3))
                    if gq < 4:
                        s_ = sil[gq % 2]
                        P.act(s_[:], pc[:], AF.Silu)
                        P.tt(sqb[gq % 2][:], s_[:], s_[:], ALU.mult, eng=POOL)
                    else:
                        P.act(vT.v(vT.base()[:, (gq - 4) * 4:(gq - 4) * 4 + 4, :]),
                              pc.v(pc.base()[:, :].rearrange("p (c t) -> p c t", t=128)), AF.Silu)
                    P.copy(cb.k(gq, (ALL, sl(gq * 4, gq * 4 + 4), sl(0, 3))),
                           cb.k(gq, (ALL, sl(gq * 4, gq * 4 + 4), sl(128, 131))), eng=POOL)
                    yield
                    if gq < 4:
                        for cc in range(4):
                            P.mm(PB[5][:, cs(cc)], K.ones_b[:], sqb[gq % 2][:, cs(cc)])
                        r_ = rs[gq % 2]
                        if gq < 2:
                            P.rsqrt(r_[:], PB[5][:], 128.0, 128.0 * EPS)
                        else:
                            P.rsqrt(r_[:], PB[5][:], 1.0, EPS)
                        P.tt(qkT.v(qkT.base()[:, gq * 4:gq * 4 + 4, :]),
                             s_.v(s_.base()[:, :].rearrange("p (c t) -> p c t", t=128)),
                             r_.v(r_.base()[:, :].rearrange("p (c t) -> p c t", t=128)), ALU.mult)
                    yield

                gens = [stage_a(gq) for gq in range(6)]
                for step in range(6 + 2):
                    for gq in range(6):
                        if 0 <= step - gq < 3:
                            next(gens[gq])
                for hf in range(2):
                    for kc in range(8):
                        P.mm(PB[1 + hf][:], h_[:, kc, :], win.k(6 + hf, (ALL, kc, sl(3072 + hf * 512, 3072 + (hf + 1) * 512))),
                             start=(kc == 0), stop=(kc == 7))
                    P.act(gz[:, hf * 512:(hf + 1) * 512], PB[1 + hf][:], AF.Silu)
                P.tt(gz[:], gz[:], gnb8[:], ALU.mult, eng=POOL)
                for kc in range(8):
                    P.mm(PB[6][:, 0:16], h_[:, kc, :], win.k(8, (ALL, kc, sl(4096, 4112))), start=(kc == 0), stop=(kc == 7))
                P.act(sm["eb"][:], PB[6][:, 0:8], AF.Exp, scale=-1.0)
                P.tt(sm["ta"][:], PB[6][:, 8:16], dtb[:], ALU.add)
                P.ts(sm["eb"][:], sm["eb"][:], 1.0, ALU.add)
                P.recip(sm["beta"][:], sm["eb"][:])
                P.ts(sm["nbeta"][:], sm["beta"][:], -1.0, ALU.mult)
                P.act(sm["ta"][:], sm["ta"][:], AF.Exp)
                P.act(sm["ta"][:], sm["ta"][:], AF.Ln, bias=1.0)
                P.tt(sm["g"][:], sm["ta"][:], nea[:], ALU.mult)
                P.mm(PB[7][:, 0:8], K.m_ge[:], sm["g"][:])
                P.mm(PB[7][:, 8:16], K.ones_f[:], sm["g"][:])
                P.copy(gc[:], PB[7][:, 0:16])
                P.act(sm["egc"][:], gc[:, 0:8], AF.Exp)
                P.act(sm["gtot"][:], gc[:, 8:16], AF.Exp)
                P.tt(sm["dk"][:], gc[:, 8:16], gc[:, 0:8], ALU.subtract)
                P.act(sm["kdsc"][:], sm["dk"][:], AF.Exp)
                beta, nbeta, g = sm["beta"], sm["nbeta"], sm["g"]

                def head_prep(h):
                    p = h % 4
                    bD, bW, bR, bU = PB[1 + h % 2], PB[3 + h % 2], PB[5 + h % 2], PB[7]
                    kTh = qkT[:, 8 + h, :]
                    qTh = qkT[:, h, :]
                    P.ts(gh[p][:], K.m_ge[:], g[:, h:h + 1], ALU.mult)
                    P.mm(bD[:, 0:128], K.ones_f[:], gh[p][:])
                    P.mm(bD[:, 128:256], K.m_lt[:], gh[p][:])
                    P.mm(bD[:, 256:384], kTh, kTh)
                    P.mm(bD[:, 384:512], kTh, qTh)
                    P.act(E2[p][:], bD[:, 0:256], AF.Exp)
                    P.tt(EMi[p][:], E2[p][:, 128:256], K.m_ge[:], ALU.mult, eng=POOL)
                    P.tt(EMs[p][:], E2[p][:, 128:256], K.m_gt[:], ALU.mult, eng=POOL)
                    W_ = WW[p]
                    R_ = Rm[p]
                    P.stt(W_[0][:, 0:128], bD[:, 256:384], nbeta[:, h:h + 1], EMs[p][:], ALU.mult, ALU.mult)
                    P.tt(qkd[h][:], bD[:, 384:512], EMi[p][:], ALU.mult)
                    P.tt(qdT[h][:], qTh, E2[p][:, 0:128], ALU.mult, eng=POOL)
                    yield
                    P.transpose(bW[:, 128:256], W_[0][:, 0:128], K.ident_f[:])
                    P.copy(W_[0][:, 128:256], bW[:, 128:256], eng=ACT)
                    P.tt(R_[:], W_[0][:, 0:128], K.ident_f[:], ALU.add)
                    yield
                    for m in range(1, 7):
                        a, b = (m - 1) % 2, m % 2
                        Wa, WTa = W_[a][:, 0:128], W_[a][:, 128:256]
                        if m < 6:
                            P.mm(bW[:, 0:128], WTa, Wa)
                        P.mm(bW[:, 128:256], Wa, WTa)
                        if m < 6:
                            P.copy(W_[b][:], bW[:, 0:256], eng=(ACT if m % 2 else DVE))
                        else:
                            P.copy(W_[b][:, 128:256], bW[:, 128:256], eng=ACT)
                        yield
                        P.mm(bR[:, 0:128], W_[b][:, 128:256], R_[:])
                        P.tt(R_[:], bR[:, 0:128], R_[:], ALU.add)
                        yield
                    P.copy(Rb[p][:], R_[:], eng=ACT)
                    P.transpose(B0[:, 0:128], kTh, K.ident_b[:])
                    P.transpose(B0[:, 128:256], vT[:, h, :], K.ident_b[:])
                    P.ts(Xk[h][:], B0[:, 0:128], sm["egc"][:, h:h + 1], ALU.mult)
                    P.ts(kdec[h][:], B0[:, 0:128], sm["kdsc"][:, h:h + 1], ALU.mult)
                    P.copy(vtm[p][:], B0[:, 128:256], eng=ACT)
                    yield
                    P.mm(bU[:, 0:128], Rb[p][:], vtm[p][:])
                    P.mm(bU[:, 128:256], Xk[h][:], Rb[p][:])
                    P.ts(uinb[h][:], bU[:, 0:128], beta[:, h:h + 1], ALU.mult)
                    P.copy(wT[h][:], bU[:, 128:256], eng=ACT)
                    yield

                for h0 in range(0, 8, 4):
                    run_interleaved([head_prep(h0 + q) for q in range(4)])

                def recur(hg):
                    hs = [hg * 4 + q for q in range(4)]
                    bT, bO = PB[1 + 2 * hg], PB[2 + 2 * hg]
                    for q, h in enumerate(hs):
                        P.mm(bT[:, cs(q)], wT[h][:], S_b.k(h, (ALL, h, ALL)))
                    yield
                    for q, h in enumerate(hs):
                        P.stt(uu[h][:], bT[:, cs(q)], nbeta[:, h:h + 1], uinb[h][:], ALU.mult, ALU.add)
                    yield
                    for q, h in enumerate(hs):
                        P.mm(bO[:, cs(q)], qdT[h][:], S_b.k(h, (ALL, h, ALL)), start=True, stop=False)
                        P.mm(bO[:, cs(q)], qkd[h][:], uu[h][:], start=False, stop=True)
                    for q, h in enumerate(hs):
                        P.mm(bT[:, cs(q)], kdec[h][:], uu[h][:])
                    yield
                    for q, h in enumerate(hs):
                        Sfh = S_f.k(h, (ALL, h, ALL))
                        P.stt(Sfh, Sfh, sm["gtot"][:, h:h + 1], bT[:, cs(q)], ALU.mult, ALU.add)
                        P.copy(S_b.k(h, (ALL, h, ALL)), Sfh, eng=POOL)
                    yield
                    for q, h in enumerate(hs):
                        P.act(junk2[hg][:], bO[:, cs(q)], AF.Square, accum_out=ssq[h][:])
                    yield
                    for q, h in enumerate(hs):
                        P.rsqrt(rsq[h][:], ssq[h][:], 1.0 / 128, EPS)
                    yield
                    for q, h in enumerate(hs):
                        P.stt(og.k(h, (ALL, cs(h))), bO[:, cs(q)], rsq[h][:], gz[:, cs(h)], ALU.mult, ALU.mult)
                    yield

                run_interleaved([recur(0), recur(1)])
                for hc in range(8):
                    P.transpose(B0[:, cs(hc)], og.k(hc, (ALL, cs(hc))), K.ident_b[:])
                P.copy(ogT[:, :, :], B0.v(B0.base()[:, :].rearrange("p (c t) -> p c t", t=128)), eng=ACT)
                for half in range(2):
                    bo = PB[5 + half]
                    for hc in range(8):
                        P.mm(bo[:], ogT[:, hc, :], wout[:, hc, half * 512:(half + 1) * 512], start=(hc == 0), stop=(hc == 7))
                    P.tt(xs[:, half * 512:(half + 1) * 512], bo[:], xs[:, half * 512:(half + 1) * 512], ALU.add)
                P.dma(xr.k(("r", ti), rows), xs[:], q=POOL)


_uid = [0]


def dv(t, ap):
    _uid[0] += 1
    return V(ap, (t.id, ("u", _uid[0]), False))


def outproj_phase(P, K, C, xr, w_out, os_):
    with P.phase():
        wout = P.sbuf("wout", [128, 8, 1024], BF16)
        P.dma(wout[:], V(w_out.rearrange("(kc p) d -> p kc d", p=128), ("w", "wout")), q=POOL)
        PB = [P.psum(f"PO{i}", [128, 512], F32) for i in range(4)]
        oT = [P.sbuf(f"oT{i}", [128, 8, 512], BF16) for i in range(2)]
        xt = [P.sbuf(f"xt{i}", [128, D_MODEL], F32) for i in range(4)]
        nb = 0
        for ti in range(C.NTOK // 512):
            r0 = ti * 512
            o_ = oT[ti % 2]
            P.dma(o_[:], dv(os_, os_.base()[:, :, r0:r0 + 512].rearrange("h p t -> p h t")))
            for s_ in range(4):
                x = xt[s_]
                rows = (sl(r0 + s_ * 128, r0 + (s_ + 1) * 128), ALL)
                P.dma(x[:], xr.k(("r", ti * 4 + s_), rows))
                for half in range(2):
                    pb = PB[nb % 4]
                    nb += 1
                    for h in range(8):
                        P.mm(pb[:], o_[:, h, s_ * 128:(s_ + 1) * 128], wout[:, h, half * 512:(half + 1) * 512],
                             start=(h == 0), stop=(h == 7))
                    P.tt(x[:, half * 512:(half + 1) * 512], pb[:], x[:, half * 512:(half + 1) * 512], ALU.add)
                P.dma(xr.k(("r", ti * 4 + s_), rows), x[:], q=POOL)


def sb_phase(P, K, C, xr, W, li, j, SC):
    S = C.S
    NT = S // 128
    NG = S // 512
    qs, ks, vs, os_ = SC["qs"], SC["ks"], SC["vs"], SC["os"]
    scale = 128.0 ** -0.5

    def cs(i, n=1):
        return sl(i * 128, (i + n) * 128)

    with P.phase():
        win = P.sbuf("win", [128, 8, 3072], BF16)
        wsrc = W['sb_w_in'][j].rearrange("(kc p) f -> p kc f", p=128)
        for pi in range(6):
            P.dma(win.k(pi, (ALL, ALL, sl(pi * 512, (pi + 1) * 512))), V(wsrc[:, :, pi * 512:(pi + 1) * 512], ("w", "sb_in")), q=POOL)
        gb = P.sbuf("gb", [128, D_MODEL], F32)
        P.dma(gb[:], V(W['mix_norm'][li].partition_broadcast(128), ("w", "mixg")))
        B0 = P.psum("B0", [128, 1024], BF16)
        PB = [None] + [P.psum(f"PB{i}", [128, 512], F32) for i in range(1, 8)]
        xt = [P.sbuf(f"xt{i}", [128, D_MODEL], F32) for i in range(4)]
        hT = [P.sbuf(f"hT{i}", [128, 8, 512], BF16) for i in range(2)]
        scr = (P.sbuf("junk", [128, D_MODEL], BF16), P.sbuf("ss", [128, 1], F32),
               P.sbuf("rstd", [128, 1], F32), P.sbuf("xn", [128, D_MODEL], BF16))
        qst = [P.sbuf(f"qst{i}", [128, 512], BF16) for i in range(4)]
        vst = [P.sbuf(f"vst{i}", [128, 1024], BF16) for i in range(2)]
        for ti in range(C.NTOK // 512):
            r0 = ti * 512
            h_ = hT[ti % 2]
            for s_ in range(4):
                rows = (sl(r0 + s_ * 128, r0 + (s_ + 1) * 128), ALL)
                P.dma(xt[s_][:], xr.k(("r", ti * 4 + s_), rows))
                norm_T(P, K, xt[s_][:], gb[:], h_[:, :, s_ * 128:(s_ + 1) * 128], scr, B0)
            for c in range(16):
                pb = PB[1 + c % 4]
                for kc in range(8):
                    P.mm(pb[:], win.k(c // 4, (ALL, kc, cs(c))), h_[:, kc, :], start=(kc == 0), stop=(kc == 7))
                st = qst[c % 4]
                if c < 8:
                    P.act(st[:], pb[:], AF.Copy, scale=scale)
                    P.dma(qs.k(("q", c, ti), (c, ALL, sl(r0, r0 + 512))), st[:], q=SP)
                else:
                    P.copy(st[:], pb[:], eng=DVE)
                    P.dma(ks.k(("k", c - 8, ti), (c - 8, ALL, sl(r0, r0 + 512))), st[:], q=SP)
            for s_ in range(4):
                vv = vst[s_ % 2]
                for half in range(2):
                    pb = PB[5 + half]
                    for kc in range(8):
                        P.mm(pb[:], h_[:, kc, s_ * 128:(s_ + 1) * 128],
                             win.k(4 + half, (ALL, kc, sl(2048 + half * 512, 2048 + (half + 1) * 512))),
                             start=(kc == 0), stop=(kc == 7))
                    P.copy(vv[:, half * 512:(half + 1) * 512], pb[:], eng=(ACT if half else DVE))
                P.dma(vs.k(("v", ti * 4 + s_), (sl(r0 + s_ * 128, r0 + (s_ + 1) * 128), ALL)), vv[:], q=SP)

    with P.phase():
        PA = [P.psum(f"PA{i}", [128, 512], F32) for i in range(6)]
        negtri_f = P.sbuf("negtri_f", [128, 128], F32)
        negtri = P.sbuf("negtri", [128, 128], BF16)
        negones = P.sbuf("negones", [128, 128], BF16)
        P.tt(negtri_f[:], K.m_lt[:], K.ident_f[:], ALU.add)
        P.ts(negtri[:], negtri_f[:], -1.0, ALU.mult)
        P.memset(negones[:], -1.0)
        maskf = [P.sbuf(f"maskf{r}", [128, 512], F32) for r in range(4)]
        maskb = [P.sbuf(f"maskb{r}", [128, 512], BF16) for r in range(4)]
        maskf_ = maskf
        maskf = maskb
        for r in range(4):
            P.memset(maskf_[r][:], 1.0)
            P.op(POOL, (lambda t, r: lambda e: e.affine_select(t[:].ap, t[:].ap, [[1, 512]], ALU.is_gt, 0.0,
                                                               base=-r * 128, channel_multiplier=-1))(maskf_[r], r),
                 [maskf_[r][:]], [maskf_[r][:]])
            P.copy(maskb[r][:], maskf_[r][:])
        hd = [[{"k": P.sbuf(f"kT{a}{b}", [128, S], BF16), "q": P.sbuf(f"qT{a}{b}", [128, S], BF16),
                "v": P.sbuf(f"v{a}{b}", [128, NT, 128], BF16)} for b in range(2)] for a in range(2)]
        wk = [{"e": [P.sbuf(f"e{b}{i}", [128, 512], F32) for i in range(2)],
               "sp": [P.sbuf(f"sp{b}{i}", [128, 512], BF16) for i in range(2)],
               "accb": P.sbuf(f"accb{b}", [128, 512], BF16),
               "att": [P.sbuf(f"att{b}{i}", [128, 512], BF16) for i in range(2)],
               "acc": P.sbuf(f"acc{b}", [128, 512], F32),
               "ost": P.sbuf(f"ost{b}", [128, 512], BF16),
               "banks": (PA[3 * b], PA[3 * b + 1], PA[3 * b + 2])} for b in range(2)]

        def load_pair(sq_i, hp, a):
            for b in range(2):
                h = 2 * hp + b
                d = hd[a][b]
                cols = sl(sq_i * S, (sq_i + 1) * S)
                P.dma(d["k"][:], ks.k(("kall",), (h, ALL, cols)))
                P.dma(d["q"][:], qs.k(("qall",), (h, ALL, cols)))
                P.dma(d["v"][:], vs.v(vs.base()[sq_i * S:(sq_i + 1) * S, h * 128:(h + 1) * 128].rearrange("(t p) d -> p t d", p=128), ("vall",)))

        def stream(sq_i, h, g, d, w):
            bA, bB, bO = w["banks"]
            kT, qT, v = d["k"], d["q"], d["v"]
            qcols = sl(g * 512, (g + 1) * 512)
            nkb = 4 * g + 4
            kbs = list(range(nkb - 1, -1, -1))
            P.mm(bA[:], kT[:, cs(kbs[0])], qT[:, qcols])
            yield
            for idx, kb in enumerate(kbs):
                first = idx == 0
                last = kb == 0
                r = kb - 4 * g
                e, sp, att = w["e"][idx % 2], w["sp"][idx % 2], w["att"][idx % 2]
                P.act(e[:], bA[:], AF.Exp)
                P.act(sp[:], e[:], AF.Ln, bias=1.0)
                if r >= 0:
                    P.tt(sp[:], sp[:], maskf[r][:], ALU.mult, eng=POOL)
                yield
                P.mm(bB[:], kT[:, cs(kb)], qT[:, qcols], start=True, stop=False)
                P.mm(bB[:], negtri[:], sp[:], start=False, stop=first)
                if not first:
                    P.mm(bB[:], negones[:], w["accb"][:], start=False, stop=True)
                if not last:
                    P.mm(bA[:], kT[:, cs(kbs[idx + 1])], qT[:, qcols])
                yield
                P.act(att[:], bB[:], AF.Exp)
                if r >= 0:
                    P.tt(att[:], att[:], maskb[r][:], ALU.mult)
                if not last:
                    if first:
                        P.copy(w["acc"][:], sp[:])
                    else:
                        P.tt(w["acc"][:], w["acc"][:], sp[:], ALU.add)
                    P.copy(w["accb"][:], w["acc"][:])
                yield
                P.mm(bO[:], v[:, kb, :], att[:], start=first, stop=last)
                yield
            P.copy(w["ost"][:], bO[:])
            P.dma(os_.k(("o", h, sq_i, g), (h, ALL, sl(sq_i * S + g * 512, sq_i * S + (g + 1) * 512))), w["ost"][:], q=POOL)

        pairs = [(sq_i, hp) for sq_i in range(C.NSEQ) for hp in range(4)]
        load_pair(pairs[0][0], pairs[0][1], 0)
        for pi, (sq_i, hp) in enumerate(pairs):
            a = pi % 2
            if pi + 1 < len(pairs):
                load_pair(pairs[pi + 1][0], pairs[pi + 1][1], 1 - a)
            for g in range(NG):
                run_interleaved([stream(sq_i, 2 * hp + b, g, hd[a][b], wk[b]) for b in range(2)])

    outproj_phase(P, K, C, xr, W['sb_w_out'][j], os_)


def rope_tm(P, x1, x2, cos, sin, o1, o2, t1, t2):
    P.tt(t1, x1, cos, ALU.mult)
    P.tt(t2, x2, sin, ALU.mult)
    P.tt(o1, t1, t2, ALU.subtract)
    P.tt(t1, x2, cos, ALU.mult)
    P.tt(t2, x1, sin, ALU.mult)
    P.tt(o2, t1, t2, ALU.add)


def dsa_phase(P, K, C, xr, W, li, j, SC):
    S = C.S
    NT = S // 128
    NG = S // 512
    topk = C.topk
    NIT = 16
    att_scale = 128.0 ** -0.5
    widx_scale = (8.0 ** -0.5) * (64.0 ** -0.5)
    NEG = -30000.0
    kl, kvtm, kiT, wiS = SC["kl"], SC["kvtm"], SC["kiT"], SC["wi"]
    qaT, qrT, qiT, negT, os_ = SC["qaT"], SC["qrT"], SC["qiT"], SC["negT"], SC["os"]
    ropeA, ropeB = SC["ropeA"], SC["ropeB"]

    def cs(i, n=1):
        return sl(i * 128, (i + n) * 128)

    with P.phase():
        B0 = P.psum("B0", [128, 1024], BF16)
        PB = [None] + [P.psum(f"PB{i}", [128, 512], F32) for i in range(1, 8)]
        win = P.sbuf("win", [128, 8, 744], BF16)
        P.dma(win[:], V(W['dsa_w_in'][j].rearrange("(kc p) f -> p kc f", p=128), ("w", "dsa_in")), q=POOL)
        wuq4 = W['dsa_w_uq'][j].rearrange("(kc p) (h d) -> p kc h d", p=128, d=128)
        wuq_r = P.sbuf("wuq_r", [128, 3, 8, 32], BF16)
        for kc in range(3):
            P.dma(wuq_r[:, kc, :, :], V(wuq4[:, kc, :, 0:32], ("w", "uq_r")), q=POOL)
        wuq_n = P.sbuf("wuq_n", [128, 3, 8, 96], BF16)
        for kc in range(3):
            P.dma(wuq_n[:, kc, :, :], V(wuq4[:, kc, :, 32:128], ("w", "uq_n")), q=POOL)
        wuk = P.sbuf("wuk", [128, 2, 768], BF16)
        P.dma(wuk[:], V(W['dsa_w_uk'][j].rearrange("(cc p) f -> p cc f", p=128), ("w", "uk")), q=POOL)
        wqidx = P.sbuf("wqidx", [128, 3, 512], BF16)
        P.dma(wqidx[:], V(W['dsa_w_qidx'][j].rearrange("(kc p) f -> p kc f", p=128), ("w", "qidx")), q=POOL)
        gb = P.sbuf("gb", [128, D_MODEL], F32)
        P.dma(gb[:], V(W['mix_norm'][li].partition_broadcast(128), ("w", "mixg")))
        cqg = P.sbuf("cqg", [128, 384], F32)
        ckvg = P.sbuf("ckvg", [128, 256], F32)
        kidxg = P.sbuf("kidxg", [128, 64], F32)
        P.dma(cqg[:], V(W['dsa_cq_norm'][j].partition_broadcast(128), ("w", "cqg")))
        P.dma(ckvg[:], V(W['dsa_ckv_norm'][j].partition_broadcast(128), ("w", "ckvg")))
        P.dma(kidxg[:], V(W['dsa_kidx_norm'][j].partition_broadcast(128), ("w", "kidxg")))
        AT = P.sbuf("AT", [96, 8, 384], BF16)
        BT = P.sbuf("BT", [96, 8, 256], BF16)
        Wabs = P.sbuf("Wabs", [128, 3, 8, 256], BF16)
        for h in range(8):
            for kc in range(3):
                P.transpose(B0[0:96, cs(kc)], wuq_n[:, kc, h, :], K.ident_b[:])
            for cc in range(2):
                P.transpose(B0[0:96, cs(3 + cc)], wuk[:, cc, h * 96:(h + 1) * 96], K.ident_b[:])
            P.copy(AT[:, h, :], B0[0:96, 0:384], eng=ACT)
            P.copy(BT[:, h, :], B0[0:96, 384:640])
        for h in range(8):
            for kc in range(3):
                n = h * 3 + kc
                pb = PB[1 + n % 4]
                P.mm(pb[:, 0:256], AT[:, h, cs(kc)], BT[:, h, :])
                P.copy(Wabs[:, kc, h, :], pb[:, 0:256], eng=(ACT if n % 2 else DVE))
        xt = [P.sbuf(f"xt{i}", [128, D_MODEL], F32) for i in range(2)]
        hT = [P.sbuf(f"hT{i}", [128, 8, 128], BF16) for i in range(2)]
        scr = (P.sbuf("junk", [128, D_MODEL], BF16), P.sbuf("ss", [128, 1], F32),
               P.sbuf("rstd", [128, 1], F32), P.sbuf("xn", [128, D_MODEL], BF16))
        junk = scr[0]
        ra = [P.sbuf(f"ra{i}", [128, 256], F32) for i in range(2)]
        rb = [P.sbuf(f"rb{i}", [128, 128], F32) for i in range(2)]
        pj = P.sbuf("pj", [128, 744], F32)
        sA = {n: P.sbuf(n, [128, 1], F32) for n in ("ssA", "rsA", "ssB", "rsB", "ssC", "rsC", "kn2a", "kn2b", "kn2", "rm", "nkmax")}
        km1 = P.sbuf("km1", [1, 1], F32)
        cqn = P.sbuf("cqn", [128, 384], BF16)
        cqT = P.sbuf("cqT", [128, 3, 128], BF16)
        ckv = P.sbuf("ckv", [128, 256], BF16)
        ckT = P.sbuf("ckT", [128, 2, 128], BF16)
        kra = P.sbuf("kra", [128, 33], F32)
        krb = P.sbuf("krb", [128, 33], BF16)
        krT = P.sbuf("krT", [33, 128], BF16)
        t1 = P.sbuf("t1", [128, 128], F32)
        t2 = P.sbuf("t2", [128, 128], F32)
        kin = P.sbuf("kin", [128, 64], F32)
        kib = P.sbuf("kib", [128, 64], BF16)
        kiTt = P.sbuf("kiTt", [64, 128], BF16)
        wit = P.sbuf("wit", [128, 8], F32)
        qab = P.sbuf("qab", [128, 16, 128], BF16)
        sqa = P.sbuf("sqa", [128, 16, 128], BF16)
        qr = P.sbuf("qr", [128, 8, 32], F32)
        qra = P.sbuf("qra", [128, 8, 33], F32)
        qrb = P.sbuf("qrb", [128, 8, 33], BF16)
        qrTt = P.sbuf("qrTt", [33, 8, 128], BF16)
        t3 = P.sbuf("t3", [128, 8, 32], F32)
        qr2 = P.sbuf("qr2", [128, 8], F32)
        qn = P.sbuf("qn", [128, 8], F32)
        qi = P.sbuf("qi", [128, 8, 64], F32)
        qib = P.sbuf("qib", [128, 8, 64], BF16)
        qiTt = P.sbuf("qiTt", [64, 8, 128], BF16)
        P.memset(kra[:, 32:33], 1.0)

        def v3(t, a, b, d):
            return t.v(t.base()[:, a:b].rearrange("p (h d) -> p h d", d=d))

        for sq_i in range(C.NSEQ):
            P.memset(sA["rm"][:], 0.0)
            for t in range(NT):
                r0 = sq_i * S + t * 128
                ti = r0 // 128
                toks = sl(r0, r0 + 128)
                xs, h_ = xt[t % 2], hT[t % 2]
                ra_, rb_ = ra[t % 2], rb[t % 2]
                P.dma(xs[:], xr.k(("r", ti), (toks, ALL)))
                P.dma(ra_[:], V(ropeA[t * 128:(t + 1) * 128, :], ("w", "ropeA")))
                P.dma(rb_[:], V(ropeB[t * 128:(t + 1) * 128, :], ("w", "ropeB")))
                norm_T(P, K, xs[:], gb[:], h_[:, :, :], scr, B0)
                for kc in range(8):
                    P.mm(PB[1][:], h_[:, kc, :], win[:, kc, 0:512], start=(kc == 0), stop=(kc == 7))
                for kc in range(8):
                    P.mm(PB[2][:, 0:232], h_[:, kc, :], win[:, kc, 512:744], start=(kc == 0), stop=(kc == 7))
                P.copy(pj[:, 0:512], PB[1][:], eng=ACT)
                P.copy(pj[:, 512:744], PB[2][:, 0:232])
                P.act(junk[:, 0:384], pj[:, 0:384], AF.Square, accum_out=sA["ssA"][:])
                P.rsqrt(sA["rsA"][:], sA["ssA"][:], 1.0 / 384, EPS)
                P.stt(cqn[:], pj[:, 0:384], sA["rsA"][:], cqg[:], ALU.mult, ALU.mult)
                for cc in range(3):
                    P.transpose(B0[:, cs(cc)], cqn[:, cs(cc)], K.ident_b[:])
                P.copy(cqT[:, :, :], B0.v(B0.base()[:, 0:384].rearrange("p (c t) -> p c t", t=128)), eng=ACT)
                P.act(junk[:, 0:256], pj[:, 384:640], AF.Square, accum_out=sA["ssB"][:])
                P.rsqrt(sA["rsB"][:], sA["ssB"][:], 1.0 / 256, EPS)
                P.stt(ckv[:], pj[:, 384:640], sA["rsB"][:], ckvg[:], ALU.mult, ALU.mult)
                P.act(junk[:, 0:256], ckv[:], AF.Square, accum_out=sA["kn2a"][:])
                P.dma(dv(kvtm, kvtm.base()[toks, :]), ckv[:])
                for cc in range(2):
                    P.transpose(B0[:, cs(cc)], ckv[:, cs(cc)], K.ident_b[:])
                P.copy(ckT[:, :, :], B0.v(B0.base()[:, 0:256].rearrange("p (c t) -> p c t", t=128)))
                P.dma(dv(kl, kl.base()[0:2, :, toks].rearrange("c p t -> p c t")), ckT[:])
                rope_tm(P, pj[:, 640:656], pj[:, 656:672], ra_[:, 0:16], ra_[:, 128:144],
                        kra[:, 0:16], kra[:, 16:32], t1[:, 0:16], t2[:, 0:16])
                P.act(junk[:, 0:32], kra[:, 0:32], AF.Square, accum_out=sA["kn2b"][:])
                P.tt(sA["kn2"][:], sA["kn2a"][:], sA["kn2b"][:], ALU.add)
                P.tt(sA["rm"][:], sA["rm"][:], sA["kn2"][:], ALU.max)
                P.copy(krb[:], kra[:], eng=POOL)
                P.transpose(B0[0:33, 0:128], krb[:], K.ident_b[:])
                P.copy(krT[:], B0[0:33, 0:128])
                P.dma(dv(kl, kl.base()[2, 0:33, toks]), krT[:])
                P.act(junk[:, 0:64], pj[:, 672:736], AF.Square, accum_out=sA["ssC"][:])
                P.rsqrt(sA["rsC"][:], sA["ssC"][:], 1.0 / 64, EPS)
                P.stt(kin[:], pj[:, 672:736], sA["rsC"][:], kidxg[:], ALU.mult, ALU.mult)
                P.copy(kib[:], kin[:], eng=POOL)
                rope_tm(P, kin[:, 0:8], kin[:, 8:16], rb_[:, 0:8], rb_[:, 64:72],
                        kib[:, 0:8], kib[:, 8:16], t1[:, 0:8], t2[:, 0:8])
                P.transpose(B0[0:64, 0:128], kib[:], K.ident_b[:])
                P.copy(kiTt[:], B0[0:64, 0:128], eng=ACT)
                P.dma(dv(kiT, kiT.base()[:, toks]), kiTt[:])
                P.ts(wit[:], pj[:, 736:744], widx_scale, ALU.mult)
                P.dma(dv(wiS, wiS.base()[toks, :]), wit[:])
                P.transpose(PB[3][0:1, 0:128], sA["rm"][:], K.ident_f[:])
                P.reduce(km1[:], PB[3][0:1, 0:128], ALU.max)
                P.act(km1[:], km1[:], AF.Ln, bias=1e-30)
                P.act(km1[:], km1[:], AF.Exp, scale=0.5)
                P.ts(km1[:], km1[:], -1.0, ALU.mult)
                P.mm(PB[3][:, 128:129], K.ones_f[0:1, 0:128], km1[0:1, 0:1])
                P.copy(sA["nkmax"][:], PB[3][:, 128:129])
                for b4 in range(4):
                    pb = PB[4 + b4 % 2]
                    for i in range(4):
                        idx = b4 * 4 + i
                        h, cc = idx // 2, idx % 2
                        for kc in range(3):
                            P.mm(pb[:, cs(i)], Wabs[:, kc, h, cs(cc)], cqT[:, kc, :], start=(kc == 0), stop=(kc == 2))
                    P.copy(qab[:, b4 * 4:(b4 + 1) * 4, :], pb.v(pb.base()[:, :].rearrange("p (c t) -> p c t", t=128)),
                           eng=(ACT if b4 % 2 else DVE))
                P.dma(dv(qaT, qaT.base()[:, :, :, toks].rearrange("h c p t -> p (h c) t")), qab[:])
                P.tt(sqa[:], qab[:], qab[:], ALU.mult, eng=POOL)
                for idx in range(16):
                    h, cc = idx // 2, idx % 2
                    P.mm(PB[6][:, h:h + 1], sqa[:, idx, :], K.ones_b[:, 0:1], start=(cc == 0), stop=(cc == 1))
                for kc in range(3):
                    P.mm(PB[7][:, 0:256], cqT[:, kc, :], wuq_r.v(wuq_r.base()[:, kc, :, :].rearrange("p h d -> p (h d)")),
                         start=(kc == 0), stop=(kc == 2))
                P.copy(qr[:, :, :], PB[7].v(PB[7].base()[:, 0:256].rearrange("p (h d) -> p h d", d=32)), eng=ACT)
                rope_tm(P, qr[:, :, 0:16], qr[:, :, 16:32], v3(ra_, 0, 128, 16), v3(ra_, 128, 256, 16),
                        qra[:, :, 0:16], qra[:, :, 16:32], v3(t1, 0, 128, 16), v3(t2, 0, 128, 16))
                P.tt(t3[:, :, :], qra[:, :, 0:32], qra[:, :, 0:32], ALU.mult)
                P.reduce(qr2[:], t3[:, :, :], ALU.add)
                P.tt(qn[:], qr2[:], PB[6][:, 0:8], ALU.add)
                P.act(qn[:], qn[:], AF.Ln, bias=1e-30)
                P.act(qn[:], qn[:], AF.Exp, scale=0.5)
                P.ts(qra[:, :, 32:33], qn.v(qn.base()[:, :].rearrange("p (h o) -> p h o", o=1)), sA["nkmax"][:], ALU.mult)
                P.copy(qrb[:, :, :], qra[:, :, :], eng=POOL)
                for h in range(8):
                    P.transpose(B0[0:33, cs(h)], qrb[:, h, :], K.ident_b[:])
                P.copy(qrTt[:, :, :], B0.v(B0.base()[0:33, :].rearrange("p (h t) -> p h t", t=128)), eng=ACT)
                P.dma(dv(qrT, qrT.base()[:, :, toks].rearrange("h p t -> p h t")), qrTt[:])
                for kc in range(3):
                    P.mm(PB[1][:], cqT[:, kc, :], wqidx[:, kc, :], start=(kc == 0), stop=(kc == 2))
                P.copy(qi[:, :, :], PB[1].v(PB[1].base()[:, :].rearrange("p (h d) -> p h d", d=64)))
                P.copy(qib[:, :, :], qi[:, :, :], eng=POOL)
                rope_tm(P, qi[:, :, 0:8], qi[:, :, 8:16], v3(rb_, 0, 64, 8), v3(rb_, 64, 128, 8),
                        qib[:, :, 0:8], qib[:, :, 8:16], v3(t1, 0, 64, 8), v3(t2, 0, 64, 8))
                for h in range(8):
                    P.transpose(B0[0:64, cs(h)], qib[:, h, :], K.ident_b[:])
                P.copy(qiTt[:, :, :], B0.v(B0.base()[0:64, :].rearrange("p (h t) -> p h t", t=128)))
                P.dma(dv(qiT, qiT.base()[:, :, toks].rearrange("h p t -> p h t")), qiTt[:])

    with P.phase():
        B0 = P.psum("B0", [128, 1024], BF16)
        PI = [P.psum(f"PI{i}", [128, 512], F32) for i in range(2)]
        PS = [P.psum(f"PS{i}", [128, 512], F32) for i in range(2)]
        kis = P.sbuf("kis", [64, S], BF16)
        sc = [P.sbuf(f"sc{i}", [128, S], F32) for i in range(2)]
        jk = P.sbuf("jk", [128, S], BF16)
        neg = [P.sbuf(f"neg{i}", [128, S], BF16) for i in range(2)]
        ngt = [P.sbuf(f"ngt{i}", [128, NT, 128], BF16) for i in range(2)]
        rr = [[P.sbuf(f"rr{i}{k}", [128, 512], F32) for k in range(2)] for i in range(2)]
        dg = [P.sbuf(f"dg{i}", [128, 8, 128], F32) for i in range(2)]
        qiq = [P.sbuf(f"qiq{i}", [64, 8, 128], BF16) for i in range(2)]
        wiq = [P.sbuf(f"wiq{i}", [128, 8], F32) for i in range(2)]
        m_le = P.sbuf("m_le", [128, 128], F32)
        nbig = P.sbuf("nbig", [128, 128], F32)
        P.ts(m_le[:], K.m_gt[:], -1.0, ALU.mult, 1.0, ALU.add)
        P.ts(nbig[:], K.m_gt[:], -1e30, ALU.mult)
        pw2 = P.sbuf("pw2", [128, NIT], F32)
        for it in range(NIT):
            P.memset(pw2[:, it:it + 1], 2.0 ** -(it + 1))
        sB = [{n: P.sbuf(f"{n}{i}", [128, 1], F32) for n in ("lo", "hi", "mid", "cnt", "t")} for i in range(2)]
        stp = [P.sbuf(f"stp{i}", [128, NIT], F32) for i in range(2)]
        jks = [jk, P.sbuf("jk2", [128, S], BF16)]

        def idx_gen(sq_i, qb):
            par = qb % 2
            L = (qb + 1) * 128
            r0 = sq_i * S + qb * 128
            toks = sl(r0, r0 + 128)
            s_, q_, w_, d_ = sc[par], qiq[par], wiq[par], dg[par]
            P.dma(q_[:], dv(qiT, qiT.base()[:, :, toks].rearrange("h p t -> p h t")))
            P.dma(w_[:], dv(wiS, wiS.base()[toks, :]))
            for h in range(8):
                P.ts(d_[:, h, :], K.ident_f[:], w_[:, h:h + 1], ALU.mult)
            yield
            n = 0
            for kg in range((L + 511) // 512):
                w = min(512, L - kg * 512)
                cols = sl(kg * 512, kg * 512 + w)
                sb_ = PS[kg % 2]
                pend = None
                for h in range(8):
                    pb, r_ = PI[n % 2], rr[n % 2][0]
                    n += 1
                    P.mm(pb[:, 0:w], q_[:, h, :], kis[:, cols])
                    if pend is not None:
                        P.mm(sb_[:, 0:w], d_[:, pend[0], :], pend[1][:, 0:w], start=(pend[0] == 0), stop=False)
                    P.act(r_[:, 0:w], pb[:, 0:w], AF.Relu)
                    pend = (h, r_)
                    yield
                P.mm(sb_[:, 0:w], d_[:, 7, :], pend[1][:, 0:w], start=False, stop=True)
                P.copy(s_[:, cols], sb_[:, 0:w], eng=ACT)
                yield

        def bis_gen(sq_i, qb):
            par = qb % 2
            L = (qb + 1) * 128
            Lg = (4 * (qb // 4) + 4) * 128
            s_, n_, g_ = sc[par], neg[par], ngt[par]
            lo, hi, mid, cnt, tt_ = (sB[par][n] for n in ("lo", "hi", "mid", "cnt", "t"))
            st_, jk_ = stp[par], jks[par]
            P.reduce(hi[:], s_[:, 0:L], ALU.max)
            P.reduce(lo[:], s_[:, 0:L], ALU.min)
            yield
            P.tt(hi[:], hi[:], lo[:], ALU.subtract)
            P.ts(lo[:], lo[:], -1.0, ALU.add)
            P.ts(hi[:], hi[:], 2.0, ALU.add)
            P.ts(st_[:], pw2[:], hi[:], ALU.mult)
            blk = s_[:, qb * 128:L]
            P.tt(blk, blk, m_le[:], ALU.mult)
            P.tt(blk, blk, nbig[:], ALU.add)
            yield
            for it in range(NIT):
                P.tt(mid[:], lo[:], st_[:, it:it + 1], ALU.add)
                P.ts(jk_[:, 0:L], s_[:, 0:L], mid[:], ALU.is_gt, 0.0, ALU.add, accum_out=cnt[:])
                yield
                P.ts(tt_[:], cnt[:], float(topk) - 0.5, ALU.is_ge, st_[:, it:it + 1], ALU.mult)
                P.tt(lo[:], lo[:], tt_[:], ALU.add)
                yield
            P.ts(n_[:, 0:L], s_[:, 0:L], lo[:], ALU.is_gt)
            if Lg > L:
                P.memset(n_[:, L:Lg], 0.0, eng=POOL)
            yield
            nkb = Lg // 128
            for kb0 in range(0, nkb, 8):
                n8 = min(8, nkb - kb0)
                for i in range(n8):
                    P.transpose(B0[:, cs(i)], n_[:, cs(kb0 + i)], K.ident_b[:])
                P.copy(g_[:, kb0:kb0 + n8, :], B0.v(B0.base()[:, 0:n8 * 128].rearrange("p (c t) -> p c t", t=128)),
                       eng=ACT)
                yield
            P.dma(dv(negT, negT.base()[sq_i, 0:nkb, :, qb * 128:(qb + 1) * 128].rearrange("kb k q -> k kb q")),
                  g_[:, 0:nkb, :], q=POOL)

        prev = None
        for sq_i in range(C.NSEQ):
            P.dma(kis[:], dv(kiT, kiT.base()[:, sq_i * S:(sq_i + 1) * S]))
            for qb in range(NT):
                gens = [idx_gen(sq_i, qb)]
                if prev is not None:
                    gens.append(bis_gen(*prev))
                run_interleaved(gens)
                prev = (sq_i, qb)
        run_interleaved([bis_gen(*prev)])

    with P.phase():
        PA = [P.psum(f"PA{i}", [128, 512], F32) for i in range(2)]
        O0 = P.psum("O0", [128, 512], F32)
        O1 = P.psum("O1", [128, 512], F32)
        Dn = P.psum("Dn", [128, 512], F32)
        Ov = P.psum("Ov", [128, 512], F32)
        wuv = P.sbuf("wuv", [128, 2, 1024], BF16)
        P.dma(wuv[:], V(W['dsa_w_uv'][j].rearrange("(cc p) f -> p cc f", p=128), ("w", "uv")), q=POOL)
        kl01 = P.sbuf("kl01", [128, 2, S], BF16)
        kl2 = P.sbuf("kl2", [33, S], BF16)
        ckv = P.sbuf("ckvs", [128, NT, 256], BF16)
        ngc = [P.sbuf(f"ngc{i}", [128, NT, 512], BF16) for i in range(2)]
        qa = [P.sbuf(f"qa{i}", [128, 2, 512], BF16) for i in range(2)]
        qrr = [P.sbuf(f"qrr{i}", [33, 512], BF16) for i in range(2)]
        pT = [P.sbuf(f"pT{i}", [128, 512], BF16) for i in range(2)]
        ol = P.sbuf("ol", [128, 2, 512], BF16)
        rden = P.sbuf("rden", [128, 512], F32)
        ost = [P.sbuf(f"ost{i}", [128, 512], BF16) for i in range(2)]
        cnt_ = 0
        for sq_i in range(C.NSEQ):
            seq = sl(sq_i * S, (sq_i + 1) * S)
            P.dma(kl01[:], dv(kl, kl.base()[0:2, :, seq].rearrange("c p t -> p c t")))
            P.dma(kl2[:], dv(kl, kl.base()[2, 0:33, seq]))
            P.dma(ckv[:], dv(kvtm, kvtm.base()[seq, :].rearrange("(t p) c -> p t c", p=128)))
            for g in range(NG):
                nkb = 4 * g + 4
                ng_ = ngc[g % 2]
                gt = sl(sq_i * S + g * 512, sq_i * S + (g + 1) * 512)
                P.dma(ng_[:, 0:nkb, :], dv(negT, negT.base()[sq_i, 0:nkb, :, g * 512:(g + 1) * 512].rearrange("kb k q -> k kb q")))
                for h in range(8):
                    qa_, qr_ = qa[cnt_ % 2], qrr[cnt_ % 2]
                    o_ = ost[cnt_ % 2]
                    cnt_ += 1
                    P.dma(qa_[:], dv(qaT, qaT.base()[h, :, :, gt].rearrange("c p t -> p c t")))
                    P.dma(qr_[:], dv(qrT, qrT.base()[h, 0:33, gt]))

                    def pv(i):
                        p_ = pT[i % 2]
                        P.mm(O0[:], ckv[:, i, 0:128], p_[:], start=(i == 0), stop=(i == nkb - 1))
                        P.mm(O1[:], ckv[:, i, 128:256], p_[:], start=(i == 0), stop=(i == nkb - 1))
                        P.mm(Dn[:], K.ones_b[:], p_[:], start=(i == 0), stop=(i == nkb - 1))

                    for kb in range(nkb):
                        A = PA[kb % 2]
                        P.mm(A[:], kl01[:, 0, cs(kb)], qa_[:, 0, :], start=True, stop=False)
                        P.mm(A[:], kl01[:, 1, cs(kb)], qa_[:, 1, :], start=False, stop=False)
                        P.mm(A[:], kl2[0:33, cs(kb)], qr_[0:33, :], start=False, stop=True)
                        if kb > 0:
                            pv(kb - 1)
                        P.act(pT[kb % 2][:], A[:], AF.Exp, scale=att_scale)
                        P.tt(pT[kb % 2][:], pT[kb % 2][:], ng_[:, kb, :], ALU.mult)
                    pv(nkb - 1)
                    P.copy(ol[:, 0, :], O0[:], eng=ACT)
                    P.copy(ol[:, 1, :], O1[:])
                    P.recip(rden[:], Dn[:])
                    P.mm(Ov[:], wuv[:, 0, cs(h)], ol[:, 0, :], start=True, stop=False)
                    P.mm(Ov[:], wuv[:, 1, cs(h)], ol[:, 1, :], start=False, stop=True)
                    P.tt(o_[:], Ov[:], rden[:], ALU.mult)
                    P.dma(dv(os_, os_.base()[h, :, gt]), o_[:], q=POOL)

    outproj_phase(P, K, C, xr, W['dsa_w_out'][j], os_)


def rope_tables(S):
    pos = np.arange(S, dtype=np.float32)[:, None]

    def tab(r):
        half = r // 2
        inv = (np.float32(500000.0) ** (-np.arange(half, dtype=np.float32) * np.float32(2.0 / r))).astype(np.float32)
        ang = pos * inv[None, :]
        c = np.tile(np.cos(ang).astype(np.float32), (1, 8))
        s_ = np.tile(np.sin(ang).astype(np.float32), (1, 8))
        return np.ascontiguousarray(np.concatenate([c, s_], axis=1), dtype=np.float32)

    return tab(32), tab(16)


def run(C, inputs, n_cores):
    nc = build(C)
    x = np.ascontiguousarray(inputs['x'], dtype=np.float32).reshape(n_cores, C.NTOK, D_MODEL)
    in_maps = []
    for c in range(n_cores):
        m = {"x": x[c]}
        m["ropeA"], m["ropeB"] = rope_tables(C.S)
        for name in INPUT_SHAPES:
            m[name] = np.ascontiguousarray(inputs[name], dtype=np.float32)
        in_maps.append(m)
    res = run_bass_kernel_spmd(nc, in_maps, core_ids=list(range(n_cores)))
    return np.stack([np.asarray(r["y"]) for r in res.results], axis=0)


def kernel(**inputs):
    C = Cfg()
    x = np.asarray(inputs['x'])
    B, S, D = x.shape
    y = run(C, inputs, N_CORES)
    return y.reshape(B, S, D).astype(np.float32)
```

```python
import contextlib
from contextlib import ExitStack

import numpy as np
import concourse.bass as bass
import concourse.mybir as mybir
from concourse.bass_utils import run_bass_kernel_spmd

F32 = mybir.dt.float32
BF16 = mybir.dt.bfloat16
AF = mybir.ActivationFunctionType
ALU = mybir.AluOpType
AX = mybir.AxisListType

D_MODEL = 1024
D_FF = 2816
EPS = 1e-6
N_CORES = 8

PE, ACT, DVE, POOL, SP = "pe", "act", "dve", "pool", "sp"
SEM_EPOCH = 20000
NDMA_SLOTS = 24


class V:
    __slots__ = ("ap", "key")

    def __init__(self, ap, key):
        self.ap = ap
        self.key = key


class T:
    _n = 0

    def __init__(self, handle, name, ap=None, is_psum=False):
        self.h = handle
        self.name = name
        T._n += 1
        self.id = T._n
        self._ap = ap
        self.is_psum = is_psum

    def base(self):
        return self._ap if self._ap is not None else self.h

    def __getitem__(self, idx):
        return V(self.base()[idx], (self.id, None, self.is_psum))

    def k(self, sub, idx=slice(None)):
        return V(self.base()[idx], (self.id, None if self.is_psum else sub, self.is_psum))

    def v(self, ap, sub=None):
        return V(ap, (self.id, None if self.is_psum else sub, self.is_psum))


class Op:
    __slots__ = ("eng", "fn", "reads", "writes", "is_dma", "idx", "deps", "signal", "sig_no", "slot", "slot_val")

    def __init__(self, eng, fn, reads, writes, is_dma=False):
        self.eng = eng
        self.fn = fn
        self.reads = reads
        self.writes = writes
        self.is_dma = is_dma
        self.deps = []
        self.signal = False
        self.sig_no = None


class Prog:
    def __init__(self, nc, es):
        self.nc = nc
        self.es_outer = es
        self.es = es
        self.ops = []
        self.engs = {PE: nc.tensor, ACT: nc.scalar, DVE: nc.vector, POOL: nc.gpsimd, SP: nc.sync}
        self.sig_count = {e: 0 for e in self.engs}
        self.dma_count = {e: 0 for e in self.engs}
        self.sems = {e: [] for e in self.engs}
        self.dsems = {e: [] for e in self.engs}
        self.n_emitted = 0

    def sbuf(self, name, shape, dt):
        T._n += 1
        name = f"{name}_{T._n}"
        h = self.es.enter_context(self.nc.sbuf_tensor(name, list(shape), dt))
        return T(h, name)

    def psum(self, name, shape, dt=F32):
        T._n += 1
        name = f"{name}_{T._n}"
        h = self.es.enter_context(self.nc.psum_tensor(name, list(shape), dt))
        return T(h, name, is_psum=True)

    def dram(self, name, shape, dt, kind="Internal"):
        h = self.nc.dram_tensor(name, list(shape), dt, kind=kind)
        return T(h, name, ap=h.ap())

    @contextlib.contextmanager
    def phase(self):
        old = self.es
        with ExitStack() as es:
            self.es = es
            yield
            self.flush()
        self.es = old

    def op(self, eng, fn, reads, writes, is_dma=False):
        rk = [v.key for v in reads]
        wk = [v.key for v in writes]
        wk += [k for k in rk if len(k) == 3 and k[2] is True]
        o = Op(eng, fn, rk, wk, is_dma)
        o.idx = len(self.ops)
        self.ops.append(o)
        return o

    def dma(self, out, in_, q=SP, **kw):
        return self.op(q, lambda e: e.dma_start(out=out.ap, in_=in_.ap, **kw), [in_], [out], is_dma=True)

    def mm(self, out, lhsT, rhs, start=True, stop=True):
        return self.op(PE, lambda e: e.matmul(out.ap, lhsT=lhsT.ap, rhs=rhs.ap, start=start, stop=stop),
                       [lhsT, rhs] + ([] if start else [out]), [out])

    def transpose(self, out, in_, ident):
        return self.op(PE, lambda e: e.transpose(out.ap, in_.ap, ident.ap), [in_, ident], [out])

    def act(self, out, in_, func, bias=None, scale=None, accum_out=None):
        kw = {}
        reads = [in_]
        writes = [out]
        if bias is not None:
            if isinstance(bias, V):
                kw["bias"] = bias.ap
                reads.append(bias)
            else:
                kw["bias"] = bias
        if scale is not None:
            if isinstance(scale, V):
                kw["scale"] = scale.ap
                reads.append(scale)
            else:
                kw["scale"] = scale
        if accum_out is not None:
            kw["accum_out"] = accum_out.ap
            writes.append(accum_out)
        return self.op(ACT, lambda e: e.activation(out.ap, in_.ap, func, **kw), reads, writes)

    def copy(self, out, in_, eng=DVE):
        if eng == ACT:
            return self.op(ACT, lambda e: e.copy(out.ap, in_.ap), [in_], [out])
        return self.op(eng, lambda e: e.tensor_copy(out=out.ap, in_=in_.ap), [in_], [out])

    def tt(self, out, in0, in1, op, eng=DVE):
        return self.op(eng, lambda e: e.tensor_tensor(out.ap, in0.ap, in1.ap, op), [in0, in1], [out])

    def ts(self, out, in0, s1, op0, s2=None, op1=None, accum_out=None, eng=DVE):
        reads = [in0]
        writes = [out]
        a1 = s1.ap if isinstance(s1, V) else s1
        a2 = s2.ap if isinstance(s2, V) else s2
        if isinstance(s1, V):
            reads.append(s1)
        if isinstance(s2, V):
            reads.append(s2)
        kw = {}
        if op1 is not None:
            kw["op1"] = op1
        if accum_out is not None:
            kw["accum_out"] = accum_out.ap
            writes.append(accum_out)
        return self.op(eng, lambda e: e.tensor_scalar(out.ap, in0.ap, a1, a2, op0, **kw), reads, writes)

    def stt(self, out, in0, s, in1, op0, op1, eng=DVE):
        reads = [in0, in1]
        a = s.ap if isinstance(s, V) else s
        if isinstance(s, V):
            reads.append(s)
        return self.op(eng, lambda e: e.scalar_tensor_tensor(out.ap, in0.ap, a, in1.ap, op0, op1), reads, [out])

    def reduce(self, out, in_, op, axis=AX.X, eng=DVE):
        return self.op(eng, lambda e: e.tensor_reduce(out.ap, in_.ap, axis, op), [in_], [out])

    def recip(self, out, in_):
        return self.op(DVE, lambda e: e.reciprocal(out.ap, in_.ap), [in_], [out])

    def rsqrt(self, out, in_, mult, add):
        self.act(out, in_, AF.Ln, bias=add, scale=mult)
        return self.act(out, out, AF.Exp, scale=-0.5)

    def memset(self, out, val, eng=DVE):
        return self.op(eng, lambda e: e.memset(out.ap, val), [], [out])

    def _sem(self, eng, n):
        i = n // SEM_EPOCH
        while len(self.sems[eng]) <= i:
            self.sems[eng].append(self.es_outer.enter_context(self.nc.semaphore(f"s_{eng}_{len(self.sems[eng])}")))
        return self.sems[eng][i], n % SEM_EPOCH + 1

    def _dsem(self, eng, slot):
        while len(self.dsems[eng]) <= slot:
            self.dsems[eng].append(self.es_outer.enter_context(self.nc.semaphore(f"d_{eng}_{len(self.dsems[eng])}")))
        return self.dsems[eng][slot]

    def flush(self):
        ops = self.ops
        if not ops:
            return
        last_w = {}
        readers = {}
        for o in ops:
            deps = set()
            for k in o.reads:
                w = last_w.get(k)
                if w is not None:
                    deps.add(w)
            for k in o.writes:
                w = last_w.get(k)
                if w is not None:
                    deps.add(w)
                for r in readers.get(k, ()):
                    deps.add(r)
            deps.discard(o.idx)
            o.deps = deps
            for k in o.reads:
                readers.setdefault(k, []).append(o.idx)
            for k in o.writes:
                last_w[k] = o.idx
                readers[k] = []
        waited = {e: {} for e in self.engs}
        waited_dma = {e: set() for e in self.engs}
        for o in ops:
            best = {}
            keep = []
            for d in o.deps:
                p = ops[d]
                if p.is_dma:
                    if d not in waited_dma[o.eng]:
                        waited_dma[o.eng].add(d)
                        keep.append(d)
                    continue
                if p.eng == PE and o.eng == PE and not o.is_dma:
                    continue
                if waited[o.eng].get(p.eng, -1) >= d:
                    continue
                if best.get(p.eng, -1) < d:
                    best[p.eng] = d
            for e, d in best.items():
                keep.append(d)
                waited[o.eng][e] = d
            o.deps = sorted(keep)
            for d in o.deps:
                ops[d].signal = True
        last_op = {}
        for o in ops:
            if not o.is_dma:
                last_op[o.eng] = o
        for o in last_op.values():
            o.signal = True
        last_dma = {}
        for o in ops:
            if o.is_dma:
                o.slot = self.dma_count[o.eng] % NDMA_SLOTS
                o.slot_val = 16 * (self.dma_count[o.eng] // NDMA_SLOTS + 1)
                self.dma_count[o.eng] += 1
                last_dma[(o.eng, o.slot)] = o
            elif o.signal:
                o.sig_no = self.sig_count[o.eng]
                self.sig_count[o.eng] += 1

        def sem_of(p):
            if p.is_dma:
                return self._dsem(p.eng, p.slot), p.slot_val
            return self._sem(p.eng, p.sig_no)

        for o in ops:
            e = self.engs[o.eng]
            for d in o.deps:
                s, v = sem_of(ops[d])
                e.wait_ge(s, v)
            if o.is_dma and o.slot_val > 16:
                e.wait_ge(self._dsem(o.eng, o.slot), o.slot_val - 16)
            ins = o.fn(e)
            if o.is_dma:
                ins.then_inc(self._dsem(o.eng, o.slot), 16)
            elif o.signal:
                s, _ = self._sem(o.eng, o.sig_no)
                ins.then_inc(s, 1)
        for en, e in self.engs.items():
            for pe_, o in last_op.items():
                if pe_ == en:
                    continue
                s, v = sem_of(o)
                e.wait_ge(s, v)
            for o in last_dma.values():
                s, v = sem_of(o)
                e.wait_ge(s, v)
        self.n_emitted += len(ops)
        self.ops = []


class Cfg:
    def __init__(self, S=4096, NSEQ=2, layers=(0, 1, 2, 3), ffn=True, mixers=True, final=True):
        self.S = S
        self.NSEQ = NSEQ
        self.NTOK = S * NSEQ
        self.layers = tuple(layers)
        self.ffn = ffn
        self.mixers = mixers
        self.final = final
        self.topk = min(256, S // 4)


INPUT_SHAPES = {
    'ffn1_norm': (4, 1024), 'ffn1_w_gu': (4, 1024, 5632), 'ffn1_w_down': (4, 2816, 1024),
    'mix_norm': (4, 1024), 'ffn2_norm': (4, 1024), 'ffn2_w_gu': (4, 1024, 5632), 'ffn2_w_down': (4, 2816, 1024),
    'gdn_w_in': (2, 1024, 4112), 'gdn_conv': (2, 4, 3072), 'gdn_a_log': (2, 8), 'gdn_dt_bias': (2, 8),
    'gdn_norm': (2, 128), 'gdn_w_out': (2, 1024, 1024),
    'sb_w_in': (1, 1024, 3072), 'sb_w_out': (1, 1024, 1024),
    'dsa_w_in': (1, 1024, 744), 'dsa_cq_norm': (1, 384), 'dsa_ckv_norm': (1, 256), 'dsa_kidx_norm': (1, 64),
    'dsa_w_uq': (1, 384, 1024), 'dsa_w_qidx': (1, 384, 512), 'dsa_w_uk': (1, 256, 768), 'dsa_w_uv': (1, 256, 1024),
    'dsa_w_out': (1, 1024, 1024), 'final_norm': (1024,),
}


class Ctx:
    pass


def make_consts(P, K):
    K.ident_f = P.sbuf("ident_f", [128, 128], F32)
    K.ident_b = P.sbuf("ident_b", [128, 128], BF16)
    K.ones_f = P.sbuf("ones_f", [128, 128], F32)
    K.ones_b = P.sbuf("ones_b", [128, 128], BF16)
    P.memset(K.ones_f[:], 1.0)
    P.memset(K.ones_b[:], 1.0)
    P.memset(K.ident_f[:], 1.0)
    P.op(POOL, lambda e: e.affine_select(K.ident_f[:].ap, K.ident_f[:].ap, [[-1, 128]], ALU.is_equal, 0.0,
                                         base=0, channel_multiplier=1), [K.ident_f[:]], [K.ident_f[:]])
    P.copy(K.ident_b[:], K.ident_f[:])
    K.m_ge = P.sbuf("m_ge", [128, 128], F32)
    K.m_gt = P.sbuf("m_gt", [128, 128], F32)
    K.m_lt = P.sbuf("m_lt", [128, 128], F32)
    for t, pat, cm, cmp_ in ((K.m_ge, 1, -1, ALU.is_ge), (K.m_gt, 1, -1, ALU.is_gt), (K.m_lt, -1, 1, ALU.is_gt)):
        P.memset(t[:], 1.0)
        P.op(POOL, (lambda t, pat, cm, cmp_: lambda e: e.affine_select(t[:].ap, t[:].ap, [[pat, 128]], cmp_, 0.0,
                                                                       base=0, channel_multiplier=cm))(t, pat, cm, cmp_),
             [t[:]], [t[:]])


def norm_T(P, K, xs, gb, hT_view, scr, psT, n_feat=1024, eps=EPS):
    junk, ss, rstd, xn = scr
    P.act(junk[:, 0:n_feat], xs, AF.Square, accum_out=ss[:])
    P.rsqrt(rstd[:], ss[:], 1.0 / n_feat, eps)
    P.stt(xn[:, 0:n_feat], xs, rstd[:], gb, ALU.mult, ALU.mult)
    nch = n_feat // 128
    for kc in range(nch):
        P.transpose(psT[:, kc * 128:(kc + 1) * 128], xn[:, kc * 128:(kc + 1) * 128], K.ident_b[:])
    P.copy(hT_view, psT.v(psT.base()[:, 0:n_feat].rearrange("p (c t) -> p c t", t=128)), eng=ACT)


def ffn_phase(P, K, C, src, dst, g_row, w_gu, w_down, tag):
    TT = 256
    NS = TT // 128
    NFC = D_FF // 128
    with P.phase():
        wgu = P.sbuf("wgu", [128, 8, 2 * D_FF], BF16)
        wd = P.sbuf("wd", [128, NFC, D_MODEL], BF16)
        gb = P.sbuf("gb", [128, D_MODEL], F32)
        P.dma(gb[:], V(g_row.partition_broadcast(128), ("w", tag, "g")))
        wgu_src = w_gu.rearrange("(kc p) f -> p kc f", p=128)
        for fi in range(11):
            P.dma(wgu.k(fi, (slice(None), slice(None), slice(fi * 512, (fi + 1) * 512))),
                  V(wgu_src[:, :, fi * 512:(fi + 1) * 512], ("w", tag, "gu")), q=POOL)
        wd_src = w_down.rearrange("(fc p) d -> p fc d", p=128)
        for pi in range(2):
            P.dma(wd.k(pi, (slice(None), slice(pi * 11, (pi + 1) * 11), slice(None))),
                  V(wd_src[:, pi * 11:(pi + 1) * 11, :], ("w", tag, "d")), q=POOL)
        xt = [P.sbuf(f"xt{i}", [128, D_MODEL], F32) for i in range(2 * NS)]
        hT = [P.sbuf(f"hT{i}", [128, 8, TT], BF16) for i in range(2)]
        aT = P.sbuf("aT", [128, NFC, TT], BF16)
        sg = [P.sbuf(f"sg{i}", [128, TT], F32) for i in range(2)]
        scr = (P.sbuf("junk", [128, D_MODEL], BF16), P.sbuf("ss", [128, 1], F32),
               P.sbuf("rstd", [128, 1], F32), P.sbuf("xn", [128, D_MODEL], BF16))
        psT = [P.psum(f"psT{i}", [128, D_MODEL], BF16) for i in range(2)]
        psg = [P.psum(f"psg{i}", [128, TT], F32) for i in range(2)]
        psu = [P.psum(f"psu{i}", [128, TT], F32) for i in range(2)]
        psd = [P.psum(f"psd{i}", [128, 512], F32) for i in range(2)]
        ntile = C.NTOK // TT
        nd = 0
        for ti in range(ntile):
            r0 = ti * TT
            xs = [xt[(ti % 2) * NS + s] for s in range(NS)]
            h = hT[ti % 2]
            for s in range(NS):
                P.dma(xs[s][:], src.k(("r", r0 // 128 + s), (slice(r0 + s * 128, r0 + (s + 1) * 128), slice(None))))
                norm_T(P, K, xs[s][:], gb[:], h[:, :, s * 128:(s + 1) * 128], scr, psT[s % 2])
            for fc in range(NFC):
                pg = psg[fc % 2]
                pu = psu[fc % 2]
                for kc in range(8):
                    P.mm(pg[:], wgu.k(fc // 4, (slice(None), kc, slice(fc * 128, (fc + 1) * 128))), h[:, kc, :],
                         start=(kc == 0), stop=(kc == 7))
                cu = NFC + fc
                for kc in range(8):
                    P.mm(pu[:], wgu.k(cu // 4, (slice(None), kc, slice(cu * 128, (cu + 1) * 128))), h[:, kc, :],
                         start=(kc == 0), stop=(kc == 7))
                P.act(sg[fc % 2][:], pg[:], AF.Silu)
                P.tt(aT[:, fc, :], sg[fc % 2][:], pu[:], ALU.mult)
            for s in range(NS):
                for half in range(2):
                    pd = psd[nd % 2]
                    nd += 1
                    for fc in range(NFC):
                        P.mm(pd[:], aT[:, fc, s * 128:(s + 1) * 128],
                             wd.k(fc // 11, (slice(None), fc, slice(half * 512, (half + 1) * 512))),
                             start=(fc == 0), stop=(fc == NFC - 1))
                    P.stt(xs[s][:, half * 512:(half + 1) * 512], pd[:], 0.5, xs[s][:, half * 512:(half + 1) * 512],
                          ALU.mult, ALU.add)
                P.dma(dst.k(("r", r0 // 128 + s), (slice(r0 + s * 128, r0 + (s + 1) * 128), slice(None))), xs[s][:],
                      q=POOL)


def final_phase(P, K, C, src, dst, g_row):
    with P.phase():
        gb = P.sbuf("gb", [128, D_MODEL], F32)
        P.dma(gb[:], V(g_row.partition_broadcast(128), ("w", "fin", "g")))
        xt = [P.sbuf(f"xt{i}", [128, D_MODEL], F32) for i in range(4)]
        junk = P.sbuf("junk", [128, D_MODEL], BF16)
        ss = [P.sbuf(f"ss{i}", [128, 1], F32) for i in range(4)]
        for ti in range(C.NTOK // 128):
            x = xt[ti % 4]
            s = ss[ti % 4]
            rows = (slice(ti * 128, (ti + 1) * 128), slice(None))
            P.dma(x[:], src.k(("r", ti), rows))
            P.act(junk[:], x[:], AF.Square, accum_out=s[:])
            P.rsqrt(s[:], s[:], 1.0 / D_MODEL, EPS)
            P.stt(x[:], x[:], s[:], gb[:], ALU.mult, ALU.mult)
            P.dma(dst.k(("r", ti), rows), x[:], q=POOL)


def copy_phase(P, C, src, dst):
    with P.phase():
        xt = [P.sbuf(f"xt{i}", [128, D_MODEL], F32) for i in range(4)]
        for ti in range(C.NTOK // 128):
            rows = (slice(ti * 128, (ti + 1) * 128), slice(None))
            P.dma(xt[ti % 4][:], src.k(("r", ti), rows))
            P.dma(dst.k(("r", ti), rows), xt[ti % 4][:], q=POOL)


def build(C):
    nc = bass.Bass("TRN2", target_bir_lowering=False)
    with ExitStack() as es:
        P = Prog(nc, es)
        K = Ctx()
        x_in = P.dram("x", [C.NTOK, D_MODEL], F32, kind="ExternalInput")
        y_out = P.dram("y", [C.NTOK, D_MODEL], F32, kind="ExternalOutput")
        xr = P.dram("xr", [C.NTOK, D_MODEL], F32)
        W = {}
        for name, shp in INPUT_SHAPES.items():
            W[name] = nc.dram_tensor(name, list(shp), F32, kind="ExternalInput").ap()
        SC = {}
        if C.mixers and any(l % 3 in (1, 2) for l in C.layers):
            SC["qs"] = P.dram("sc_qs", [8, 128, C.NTOK], BF16)
            SC["ks"] = P.dram("sc_ks", [8, 128, C.NTOK], BF16)
            SC["vs"] = P.dram("sc_vs", [C.NTOK, 1024], BF16)
            SC["os"] = P.dram("sc_os", [8, 128, C.NTOK], BF16)
        if C.mixers and any(l % 3 == 2 for l in C.layers):
            NT_ = C.S // 128
            SC["kl"] = P.dram("sc_kl", [3, 128, C.NTOK], BF16)
            SC["kvtm"] = P.dram("sc_kvtm", [C.NTOK, 256], BF16)
            SC["kiT"] = P.dram("sc_kiT", [64, C.NTOK], BF16)
            SC["wi"] = P.dram("sc_wi", [C.NTOK, 8], F32)
            SC["qaT"] = P.dram("sc_qaT", [8, 2, 128, C.NTOK], BF16)
            SC["qrT"] = P.dram("sc_qrT", [8, 33, C.NTOK], BF16)
            SC["qiT"] = P.dram("sc_qiT", [8, 64, C.NTOK], BF16)
            SC["negT"] = P.dram("sc_negT", [C.NSEQ, NT_, 128, C.S], BF16)
        SC["ropeA"] = nc.dram_tensor("ropeA", [C.S, 256], F32, kind="ExternalInput").ap()
        SC["ropeB"] = nc.dram_tensor("ropeB", [C.S, 128], F32, kind="ExternalInput").ap()
        make_consts(P, K)
        P.flush()
        cur = x_in
        for li in C.layers:
            kind, j = li % 3, li // 3
            if C.ffn:
                ffn_phase(P, K, C, cur, xr, W['ffn1_norm'][li], W['ffn1_w_gu'][li], W['ffn1_w_down'][li], f"f1_{li}")
                cur = xr
            if C.mixers:
                if cur is x_in:
                    copy_phase(P, C, x_in, xr)
                    cur = xr
                if kind == 0:
                    gdn_phase(P, K, C, xr, W, li, j)
                elif kind == 1:
                    sb_phase(P, K, C, xr, W, li, j, SC)
                else:
                    dsa_phase(P, K, C, xr, W, li, j, SC)
            if C.ffn:
                ffn_phase(P, K, C, cur, xr, W['ffn2_norm'][li], W['ffn2_w_gu'][li], W['ffn2_w_down'][li], f"f2_{li}")
                cur = xr
        if C.final:
            final_phase(P, K, C, cur, y_out, W['final_norm'])
        else:
            copy_phase(P, C, cur, y_out)
        P.flush()
        C.n_ops = P.n_emitted
    return nc


def sl(a, b):
    return slice(a, b)


ALL = slice(None)


def run_interleaved(gens):
    alive = list(gens)
    while alive:
        for g in list(alive):
            try:
                next(g)
            except StopIteration:
                alive.remove(g)


def gdn_phase(P, K, C, xr, W, li, j):
    S = C.S
    NT = S // 128
    with P.phase():
        win = P.sbuf("win", [128, 8, 4112], BF16)
        wsrc = W['gdn_w_in'][j].rearrange("(kc p) f -> p kc f", p=128)
        pieces = [(i * 512, (i + 1) * 512) for i in range(8)] + [(4096, 4112)]
        for pi, (a, b) in enumerate(pieces):
            P.dma(win.k(pi, (ALL, ALL, sl(a, b))), V(wsrc[:, :, a:b], ("w", "gdn_in")), q=POOL)
        wout = P.sbuf("wout", [128, 8, 1024], BF16)
        P.dma(wout[:], V(W['gdn_w_out'][j].rearrange("(kc p) d -> p kc d", p=128), ("w", "gdn_out")), q=POOL)
        gb = P.sbuf("gb", [128, D_MODEL], F32)
        P.dma(gb[:], V(W['mix_norm'][li].partition_broadcast(128), ("w", "mixg")))
        gnb8 = P.sbuf("gnb8", [128, 1024], F32)
        for h in range(8):
            P.dma(gnb8[:, h * 128:(h + 1) * 128], V(W['gdn_norm'][j].partition_broadcast(128), ("w", "gn")))
        alb = P.sbuf("alb", [128, 8], F32)
        dtb = P.sbuf("dtb", [128, 8], F32)
        nea = P.sbuf("nea", [128, 8], F32)
        P.dma(alb[:], V(W['gdn_a_log'][j].partition_broadcast(128), ("w", "alog")))
        P.dma(dtb[:], V(W['gdn_dt_bias'][j].partition_broadcast(128), ("w", "dtb")))
        P.act(nea[:], alb[:], AF.Exp)
        P.ts(nea[:], nea[:], -1.0, ALU.mult)
        cw4 = P.sbuf("cw4", [96, 128], F32)
        P.dma(cw4[:], V(W['gdn_conv'][j].rearrange("i (c p) -> (i c) p", p=128), ("w", "conv")))
        cw = P.sbuf("cw", [128, 96], F32)
        diagw = P.sbuf("diagw", [128, 96, 128], BF16)
        B0 = P.psum("B0", [128, 1024], BF16)
        PB = [None] + [P.psum(f"PB{i}", [128, 512], F32) for i in range(1, 8)]
        P.transpose(PB[1][:, 0:96], cw4[:], K.ident_f[0:96, 0:96])
        P.copy(cw[:], PB[1][:, 0:96])
        for ci in range(96):
            P.ts(diagw.k(ci, (ALL, ci, ALL)), K.ident_f[:], cw[:, ci:ci + 1], ALU.mult)
        xt = [P.sbuf(f"xt{i}", [128, D_MODEL], F32) for i in range(2)]
        hT = [P.sbuf(f"hT{i}", [128, 8, 128], BF16) for i in range(2)]
        scr = (P.sbuf("junk", [128, D_MODEL], BF16), P.sbuf("ss", [128, 1], F32),
               P.sbuf("rstd", [128, 1], F32), P.sbuf("xn", [128, D_MODEL], BF16))
        cb = P.sbuf("cb", [128, 24, 131], BF16)
        sil = [P.sbuf(f"sil{i}", [128, 512], F32) for i in range(2)]
        sqb = [P.sbuf(f"sqb{i}", [128, 512], BF16) for i in range(2)]
        rs = [P.sbuf(f"rs{i}", [128, 512], F32) for i in range(2)]
        qkT = P.sbuf("qkT", [128, 16, 128], BF16)
        vT = P.sbuf("vT", [128, 8, 128], BF16)
        gz = P.sbuf("gz", [128, 1024], F32)
        sm = {n: P.sbuf(n, [128, 8], F32) for n in ("eb", "beta", "nbeta", "ta", "g", "egc", "gtot", "dk", "kdsc")}
        gc = P.sbuf("gc", [128, 16], F32)
        S_f = P.sbuf("S_f", [128, 8, 128], F32)
        S_b = P.sbuf("S_b", [128, 8, 128], BF16)
        gh = [P.sbuf(f"gh{i}", [128, 128], F32) for i in range(4)]
        E2 = [P.sbuf(f"E2{i}", [128, 256], F32) for i in range(4)]
        EMi = [P.sbuf(f"EMi{i}", [128, 128], F32) for i in range(4)]
        EMs = [P.sbuf(f"EMs{i}", [128, 128], F32) for i in range(4)]
        WW = [[P.sbuf(f"WW{p}{i}", [128, 256], F32) for i in range(2)] for p in range(4)]
        Rm = [P.sbuf(f"R{p}", [128, 128], F32) for p in range(4)]
        Rb = [P.sbuf(f"Rb{p}", [128, 128], BF16) for p in range(4)]
        vtm = [P.sbuf(f"vtm{p}", [128, 128], BF16) for p in range(4)]
        Xk = [P.sbuf(f"Xk{h}", [128, 128], BF16) for h in range(8)]
        kdec = [P.sbuf(f"kdec{h}", [128, 128], BF16) for h in range(8)]
        qkd = [P.sbuf(f"qkd{h}", [128, 128], BF16) for h in range(8)]
        qdT = [P.sbuf(f"qdT{h}", [128, 128], BF16) for h in range(8)]
        uinb = [P.sbuf(f"uinb{h}", [128, 128], F32) for h in range(8)]
        wT = [P.sbuf(f"wT{h}", [128, 128], BF16) for h in range(8)]
        uu = [P.sbuf(f"u{h}", [128, 128], BF16) for h in range(8)]
        ssq = [P.sbuf(f"ssq{h}", [128, 1], F32) for h in range(8)]
        rsq = [P.sbuf(f"rsq{h}", [128, 1], F32) for h in range(8)]
        junk2 = [P.sbuf(f"junk2{i}", [128, 128], BF16) for i in range(2)]
        og = P.sbuf("og", [128, 1024], BF16)
        ogT = P.sbuf("ogT", [128, 8, 128], BF16)

        def cs(i, n=1):
            return sl(i * 128, (i + n) * 128)

        for sq_i in range(C.NSEQ):
            for h in range(8):
                P.memset(S_f.k(h, (ALL, h, ALL)), 0.0)
                P.memset(S_b.k(h, (ALL, h, ALL)), 0.0)
            for gq in range(6):
                P.memset(cb.k(gq, (ALL, sl(gq * 4, gq * 4 + 4), sl(0, 3))), 0.0)
            for t in range(NT):
                r0 = sq_i * S + t * 128
                ti = r0 // 128
                xs = xt[t % 2]
                h_ = hT[t % 2]
                rows = (sl(r0, r0 + 128), ALL)
                P.dma(xs[:], xr.k(("r", ti), rows))
                norm_T(P, K, xs[:], gb[:], h_[:, :, :], scr, B0)

                def stage_a(gq):
                    pp = PB[1 + gq % 2]
                    pc = PB[3 + gq % 2]
                    for cc in range(4):
                        c = gq * 4 + cc
                        for kc in range(8):
                            P.mm(pp[:, cs(cc)], win.k(c // 4, (ALL, kc, cs(c))), h_[:, kc, :],
                                 start=(kc == 0), stop=(kc == 7))
                    cbg = cb.k(gq, (ALL, sl(gq * 4, gq * 4 + 4), sl(3, 131)))
                    P.copy(cbg, pp.v(pp.base()[:, :].rearrange("p (c t) -> p c t", t=128)), eng=ACT)
                    yield
                    for cc in range(4):
                        c = gq * 4 + cc
                        for i in range(4):
                            P.mm(pc[:, cs(cc)], diagw.k(i * 24 + c, (ALL, i * 24 + c, ALL)),
                                 cb.k(gq, (ALL, c, sl(i, i + 128))), start=(i == 0), stop=(i == 3))
                    if gq < 4:
                        s_ = sil[gq % 2]
                        P.act(s_[:], pc[:], AF.Silu)
                        P.tt(sqb[gq % 2][:], s_[:], s_[:], ALU.mult)
                    else:
                        P.act(vT.v(vT.base()[:, (gq - 4) * 4:(gq - 4) * 4 + 4, :]),
                              pc.v(pc.base()[:, :].rearrange("p (c t) -> p c t", t=128)), AF.Silu)
                    P.copy(cb.k(gq, (ALL, sl(gq * 4, gq * 4 + 4), sl(0, 3))),
                           cb.k(gq, (ALL, sl(gq * 4, gq * 4 + 4), sl(128, 131))), eng=POOL)
                    yield
                    if gq < 4:
                        for cc in range(4):
                            P.mm(PB[5][:, cs(cc)], K.ones_b[:], sqb[gq % 2][:, cs(cc)])
                        r_ = rs[gq % 2]
                        if gq < 2:
                            P.rsqrt(r_[:], PB[5][:], 128.0, 128.0 * EPS)
                        else:
                            P.rsqrt(r_[:], PB[5][:], 1.0, EPS)
                        P.tt(qkT.v(qkT.base()[:, gq * 4:gq * 4 + 4, :]),
                             s_.v(s_.base()[:, :].rearrange("p (c t) -> p c t", t=128)),
                             r_.v(r_.base()[:, :].rearrange("p (c t) -> p c t", t=128)), ALU.mult)
                    yield

                gens = [stage_a(gq) for gq in range(6)]
                for step in range(6 + 2):
                    for gq in range(6):
                        if 0 <= step - gq < 3:
                            next(gens[gq])
                for hf in range(2):
                    for kc in range(8):
                        P.mm(PB[1 + hf][:], h_[:, kc, :], win.k(6 + hf, (ALL, kc, sl(3072 + hf * 512, 3072 + (hf + 1) * 512))),
                             start=(kc == 0), stop=(kc == 7))
                    P.act(gz[:, hf * 512:(hf + 1) * 512], PB[1 + hf][:], AF.Silu)
                P.tt(gz[:], gz[:], gnb8[:], ALU.mult)
                for kc in range(8):
                    P.mm(PB[6][:, 0:16], h_[:, kc, :], win.k(8, (ALL, kc, sl(4096, 4112))), start=(kc == 0), stop=(kc == 7))
                P.act(sm["eb"][:], PB[6][:, 0:8], AF.Exp, scale=-1.0)
                P.tt(sm["ta"][:], PB[6][:, 8:16], dtb[:], ALU.add)
                P.ts(sm["eb"][:], sm["eb"][:], 1.0, ALU.add)
                P.recip(sm["beta"][:], sm["eb"][:])
                P.ts(sm["nbeta"][:], sm["beta"][:], -1.0, ALU.mult)
                P.act(sm["ta"][:], sm["ta"][:], AF.Exp)
                P.act(sm["ta"][:], sm["ta"][:], AF.Ln, bias=1.0)
                P.tt(sm["g"][:], sm["ta"][:], nea[:], ALU.mult)
                P.mm(PB[7][:, 0:8], K.m_ge[:], sm["g"][:])
                P.mm(PB[7][:, 8:16], K.ones_f[:], sm["g"][:])
                P.copy(gc[:], PB[7][:, 0:16])
                P.act(sm["egc"][:], gc[:, 0:8], AF.Exp)
                P.act(sm["gtot"][:], gc[:, 8:16], AF.Exp)
                P.tt(sm["dk"][:], gc[:, 8:16], gc[:, 0:8], ALU.subtract)
                P.act(sm["kdsc"][:], sm["dk"][:], AF.Exp)
                beta, nbeta, g = sm["beta"], sm["nbeta"], sm["g"]

                def head_prep(h):
                    p = h % 4
                    bD, bW, bR, bU = PB[1 + h % 2], PB[3 + h % 2], PB[5 + h % 2], PB[7]
                    kTh = qkT[:, 8 + h, :]
                    qTh = qkT[:, h, :]
                    P.ts(gh[p][:], K.m_ge[:], g[:, h:h + 1], ALU.mult)
                    P.mm(bD[:, 0:128], K.ones_f[:], gh[p][:])
                    P.mm(bD[:, 128:256], K.m_lt[:], gh[p][:])
                    P.mm(bD[:, 256:384], kTh, kTh)
                    P.mm(bD[:, 384:512], kTh, qTh)
                    P.act(E2[p][:], bD[:, 0:256], AF.Exp)
                    P.tt(EMi[p][:], E2[p][:, 128:256], K.m_ge[:], ALU.mult, eng=POOL)
                    P.tt(EMs[p][:], E2[p][:, 128:256], K.m_gt[:], ALU.mult, eng=POOL)
                    W_ = WW[p]
                    R_ = Rm[p]
                    P.stt(W_[0][:, 0:128], bD[:, 256:384], nbeta[:, h:h + 1], EMs[p][:], ALU.mult, ALU.mult)
                    P.tt(qkd[h][:], bD[:, 384:512], EMi[p][:], ALU.mult)
                    P.tt(qdT[h][:], qTh, E2[p][:, 0:128], ALU.mult, eng=POOL)
                    yield
                    P.transpose(bW[:, 128:256], W_[0][:, 0:128], K.ident_f[:])
                    P.copy(W_[0][:, 128:256], bW[:, 128:256], eng=ACT)
                    P.tt(R_[:], W_[0][:, 0:128], K.ident_f[:], ALU.add)
                    yield
                    for m in range(1, 7):
                        a, b = (m - 1) % 2, m % 2
                        Wa, WTa = W_[a][:, 0:128], W_[a][:, 128:256]
                        if m < 6:
                            P.mm(bW[:, 0:128], WTa, Wa)
                        P.mm(bW[:, 128:256], Wa, WTa)
                        if m < 6:
                            P.copy(W_[b][:], bW[:, 0:256], eng=(ACT if m % 2 else DVE))
                        else:
                            P.copy(W_[b][:, 128:256], bW[:, 128:256], eng=ACT)
                        yield
                        P.mm(bR[:, 0:128], W_[b][:, 128:256], R_[:])
                        P.tt(R_[:], bR[:, 0:128], R_[:], ALU.add)
                        yield
                    P.copy(Rb[p][:], R_[:], eng=ACT)
                    P.transpose(B0[:, 0:128], kTh, K.ident_b[:])
                    P.transpose(B0[:, 128:256], vT[:, h, :], K.ident_b[:])
                    P.ts(Xk[h][:], B0[:, 0:128], sm["egc"][:, h:h + 1], ALU.mult)
                    P.ts(kdec[h][:], B0[:, 0:128], sm["kdsc"][:, h:h + 1], ALU.mult)
                    P.copy(vtm[p][:], B0[:, 128:256], eng=ACT)
                    yield
                    P.mm(bU[:, 0:128], Rb[p][:], vtm[p][:])
                    P.mm(bU[:, 128:256], Xk[h][:], Rb[p][:])
                    P.ts(uinb[h][:], bU[:, 0:128], beta[:, h:h + 1], ALU.mult)
                    P.copy(wT[h][:], bU[:, 128:256], eng=ACT)
                    yield

                for h0 in range(0, 8, 4):
                    run_interleaved([head_prep(h0 + q) for q in range(4)])

                def recur(hg):
                    hs = [hg * 4 + q for q in range(4)]
                    bT, bO = PB[1 + 2 * hg], PB[2 + 2 * hg]
                    for q, h in enumerate(hs):
                        P.mm(bT[:, cs(q)], wT[h][:], S_b.k(h, (ALL, h, ALL)))
                    yield
                    for q, h in enumerate(hs):
                        P.stt(uu[h][:], bT[:, cs(q)], nbeta[:, h:h + 1], uinb[h][:], ALU.mult, ALU.add)
                    yield
                    for q, h in enumerate(hs):
                        P.mm(bO[:, cs(q)], qdT[h][:], S_b.k(h, (ALL, h, ALL)), start=True, stop=False)
                        P.mm(bO[:, cs(q)], qkd[h][:], uu[h][:], start=False, stop=True)
                    for q, h in enumerate(hs):
                        P.mm(bT[:, cs(q)], kdec[h][:], uu[h][:])
                    yield
                    for q, h in enumerate(hs):
                        Sfh = S_f.k(h, (ALL, h, ALL))
                        P.stt(Sfh, Sfh, sm["gtot"][:, h:h + 1], bT[:, cs(q)], ALU.mult, ALU.add)
                        P.copy(S_b.k(h, (ALL, h, ALL)), Sfh, eng=POOL)
                    yield
                    for q, h in enumerate(hs):
                        P.act(junk2[hg][:], bO[:, cs(q)], AF.Square, accum_out=ssq[h][:])
                    yield
                    for q, h in enumerate(hs):
                        P.rsqrt(rsq[h][:], ssq[h][:], 1.0 / 128, EPS)
                    yield
                    for q, h in enumerate(hs):
                        P.stt(og.k(h, (ALL, cs(h))), bO[:, cs(q)], rsq[h][:], gz[:, cs(h)], ALU.mult, ALU.mult)
                    yield

                run_interleaved([recur(0), recur(1)])
                for hc in range(8):
                    P.transpose(B0[:, cs(hc)], og.k(hc, (ALL, cs(hc))), K.ident_b[:])
                P.copy(ogT[:, :, :], B0.v(B0.base()[:, :].rearrange("p (c t) -> p c t", t=128)), eng=ACT)
                for half in range(2):
                    bo = PB[5 + half]
                    for hc in range(8):
                        P.mm(bo[:], ogT[:, hc, :], wout[:, hc, half * 512:(half + 1) * 512], start=(hc == 0), stop=(hc == 7))
                    P.tt(xs[:, half * 512:(half + 1) * 512], bo[:], xs[:, half * 512:(half + 1) * 512], ALU.add)
                P.dma(xr.k(("r", ti), rows), xs[:], q=POOL)


_uid = [0]


def dv(t, ap):
    _uid[0] += 1
    return V(ap, (t.id, ("u", _uid[0]), False))


def outproj_phase(P, K, C, xr, w_out, os_):
    with P.phase():
        wout = P.sbuf("wout", [128, 8, 1024], BF16)
        P.dma(wout[:], V(w_out.rearrange("(kc p) d -> p kc d", p=128), ("w", "wout")), q=POOL)
        PB = [P.psum(f"PO{i}", [128, 512], F32) for i in range(4)]
        oT = [P.sbuf(f"oT{i}", [128, 8, 512], BF16) for i in range(2)]
        xt = [P.sbuf(f"xt{i}", [128, D_MODEL], F32) for i in range(4)]
        nb = 0
        for ti in range(C.NTOK // 512):
            r0 = ti * 512
            o_ = oT[ti % 2]
            P.dma(o_[:], dv(os_, os_.base()[:, :, r0:r0 + 512].rearrange("h p t -> p h t")))
            for s_ in range(4):
                x = xt[s_]
                rows = (sl(r0 + s_ * 128, r0 + (s_ + 1) * 128), ALL)
                P.dma(x[:], xr.k(("r", ti * 4 + s_), rows))
                for half in range(2):
                    pb = PB[nb % 4]
                    nb += 1
                    for h in range(8):
                        P.mm(pb[:], o_[:, h, s_ * 128:(s_ + 1) * 128], wout[:, h, half * 512:(half + 1) * 512],
                             start=(h == 0), stop=(h == 7))
                    P.tt(x[:, half * 512:(half + 1) * 512], pb[:], x[:, half * 512:(half + 1) * 512], ALU.add)
                P.dma(xr.k(("r", ti * 4 + s_), rows), x[:], q=POOL)


def sb_phase(P, K, C, xr, W, li, j, SC):
    S = C.S
    NT = S // 128
    NG = S // 512
    qs, ks, vs, os_ = SC["qs"], SC["ks"], SC["vs"], SC["os"]
    scale = 128.0 ** -0.5

    def cs(i, n=1):
        return sl(i * 128, (i + n) * 128)

    with P.phase():
        win = P.sbuf("win", [128, 8, 3072], BF16)
        wsrc = W['sb_w_in'][j].rearrange("(kc p) f -> p kc f", p=128)
        for pi in range(6):
            P.dma(win.k(pi, (ALL, ALL, sl(pi * 512, (pi + 1) * 512))), V(wsrc[:, :, pi * 512:(pi + 1) * 512], ("w", "sb_in")), q=POOL)
        gb = P.sbuf("gb", [128, D_MODEL], F32)
        P.dma(gb[:], V(W['mix_norm'][li].partition_broadcast(128), ("w", "mixg")))
        B0 = P.psum("B0", [128, 1024], BF16)
        PB = [None] + [P.psum(f"PB{i}", [128, 512], F32) for i in range(1, 8)]
        xt = [P.sbuf(f"xt{i}", [128, D_MODEL], F32) for i in range(4)]
        hT = [P.sbuf(f"hT{i}", [128, 8, 512], BF16) for i in range(2)]
        scr = (P.sbuf("junk", [128, D_MODEL], BF16), P.sbuf("ss", [128, 1], F32),
               P.sbuf("rstd", [128, 1], F32), P.sbuf("xn", [128, D_MODEL], BF16))
        qst = [P.sbuf(f"qst{i}", [128, 512], BF16) for i in range(4)]
        vst = [P.sbuf(f"vst{i}", [128, 1024], BF16) for i in range(2)]
        for ti in range(C.NTOK // 512):
            r0 = ti * 512
            h_ = hT[ti % 2]
            for s_ in range(4):
                rows = (sl(r0 + s_ * 128, r0 + (s_ + 1) * 128), ALL)
                P.dma(xt[s_][:], xr.k(("r", ti * 4 + s_), rows))
                norm_T(P, K, xt[s_][:], gb[:], h_[:, :, s_ * 128:(s_ + 1) * 128], scr, B0)
            for c in range(16):
                pb = PB[1 + c % 4]
                for kc in range(8):
                    P.mm(pb[:], win.k(c // 4, (ALL, kc, cs(c))), h_[:, kc, :], start=(kc == 0), stop=(kc == 7))
                st = qst[c % 4]
                if c < 8:
                    P.act(st[:], pb[:], AF.Copy, scale=scale)
                    P.dma(qs.k(("q", c, ti), (c, ALL, sl(r0, r0 + 512))), st[:], q=SP)
                else:
                    P.copy(st[:], pb[:], eng=DVE)
                    P.dma(ks.k(("k", c - 8, ti), (c - 8, ALL, sl(r0, r0 + 512))), st[:], q=SP)
            for s_ in range(4):
                vv = vst[s_ % 2]
                for half in range(2):
                    pb = PB[5 + half]
                    for kc in range(8):
                        P.mm(pb[:], h_[:, kc, s_ * 128:(s_ + 1) * 128],
                             win.k(4 + half, (ALL, kc, sl(2048 + half * 512, 2048 + (half + 1) * 512))),
                             start=(kc == 0), stop=(kc == 7))
                    P.copy(vv[:, half * 512:(half + 1) * 512], pb[:], eng=(ACT if half else DVE))
                P.dma(vs.k(("v", ti * 4 + s_), (sl(r0 + s_ * 128, r0 + (s_ + 1) * 128), ALL)), vv[:], q=SP)

    with P.phase():
        PA = [P.psum(f"PA{i}", [128, 512], F32) for i in range(6)]
        negtri_f = P.sbuf("negtri_f", [128, 128], F32)
        negtri = P.sbuf("negtri", [128, 128], BF16)
        negones = P.sbuf("negones", [128, 128], BF16)
        P.tt(negtri_f[:], K.m_lt[:], K.ident_f[:], ALU.add)
        P.ts(negtri[:], negtri_f[:], -1.0, ALU.mult)
        P.memset(negones[:], -1.0)
        maskf = [P.sbuf(f"maskf{r}", [128, 512], F32) for r in range(4)]
        maskb = [P.sbuf(f"maskb{r}", [128, 512], BF16) for r in range(4)]
        maskf_ = maskf
        maskf = maskb
        for r in range(4):
            P.memset(maskf_[r][:], 1.0)
            P.op(POOL, (lambda t, r: lambda e: e.affine_select(t[:].ap, t[:].ap, [[1, 512]], ALU.is_gt, 0.0,
                                                               base=-r * 128, channel_multiplier=-1))(maskf_[r], r),
                 [maskf_[r][:]], [maskf_[r][:]])
            P.copy(maskb[r][:], maskf_[r][:])
        hd = [[{"k": P.sbuf(f"kT{a}{b}", [128, S], BF16), "q": P.sbuf(f"qT{a}{b}", [128, S], BF16),
                "v": P.sbuf(f"v{a}{b}", [128, NT, 128], BF16)} for b in range(2)] for a in range(2)]
        wk = [{"e": [P.sbuf(f"e{b}{i}", [128, 512], F32) for i in range(2)],
               "sp": [P.sbuf(f"sp{b}{i}", [128, 512], BF16) for i in range(2)],
               "accb": P.sbuf(f"accb{b}", [128, 512], BF16),
               "att": [P.sbuf(f"att{b}{i}", [128, 512], BF16) for i in range(2)],
               "acc": P.sbuf(f"acc{b}", [128, 512], F32),
               "ost": P.sbuf(f"ost{b}", [128, 512], BF16),
               "banks": (PA[3 * b], PA[3 * b + 1], PA[3 * b + 2])} for b in range(2)]

        def load_pair(sq_i, hp, a):
            for b in range(2):
                h = 2 * hp + b
                d = hd[a][b]
                cols = sl(sq_i * S, (sq_i + 1) * S)
                P.dma(d["k"][:], ks.k(("kall",), (h, ALL, cols)))
                P.dma(d["q"][:], qs.k(("qall",), (h, ALL, cols)))
                P.dma(d["v"][:], vs.v(vs.base()[sq_i * S:(sq_i + 1) * S, h * 128:(h + 1) * 128].rearrange("(t p) d -> p t d", p=128), ("vall",)))

        def stream(sq_i, h, g, d, w):
            bA, bB, bO = w["banks"]
            kT, qT, v = d["k"], d["q"], d["v"]
            qcols = sl(g * 512, (g + 1) * 512)
            nkb = 4 * g + 4
            kbs = list(range(nkb - 1, -1, -1))
            P.mm(bA[:], kT[:, cs(kbs[0])], qT[:, qcols])
            yield
            for idx, kb in enumerate(kbs):
                first = idx == 0
                last = kb == 0
                r = kb - 4 * g
                e, sp, att = w["e"][idx % 2], w["sp"][idx % 2], w["att"][idx % 2]
                P.act(e[:], bA[:], AF.Exp)
                P.act(sp[:], e[:], AF.Ln, bias=1.0)
                if r >= 0:
                    P.tt(sp[:], sp[:], maskf[r][:], ALU.mult, eng=POOL)
                yield
                P.mm(bB[:], kT[:, cs(kb)], qT[:, qcols], start=True, stop=False)
                P.mm(bB[:], negtri[:], sp[:], start=False, stop=first)
                if not first:
                    P.mm(bB[:], negones[:], w["accb"][:], start=False, stop=True)
                if not last:
                    P.mm(bA[:], kT[:, cs(kbs[idx + 1])], qT[:, qcols])
                yield
                P.act(att[:], bB[:], AF.Exp)
                if r >= 0:
                    P.tt(att[:], att[:], maskb[r][:], ALU.mult)
                if not last:
                    if first:
                        P.copy(w["acc"][:], sp[:])
                    else:
                        P.tt(w["acc"][:], w["acc"][:], sp[:], ALU.add)
                    P.copy(w["accb"][:], w["acc"][:])
                yield
                P.mm(bO[:], v[:, kb, :], att[:], start=first, stop=last)
                yield
            P.copy(w["ost"][:], bO[:])
            P.dma(os_.k(("o", h, sq_i, g), (h, ALL, sl(sq_i * S + g * 512, sq_i * S + (g + 1) * 512))), w["ost"][:], q=POOL)

        pairs = [(sq_i, hp) for sq_i in range(C.NSEQ) for hp in range(4)]
        load_pair(pairs[0][0], pairs[0][1], 0)
        for pi, (sq_i, hp) in enumerate(pairs):
            a = pi % 2
            if pi + 1 < len(pairs):
                load_pair(pairs[pi + 1][0], pairs[pi + 1][1], 1 - a)
            for g in range(NG):
                run_interleaved([stream(sq_i, 2 * hp + b, g, hd[a][b], wk[b]) for b in range(2)])

    outproj_phase(P, K, C, xr, W['sb_w_out'][j], os_)


def rope_tm(P, x1, x2, cos, sin, o1, o2, t1, t2):
    P.tt(t1, x1, cos, ALU.mult)
    P.tt(t2, x2, sin, ALU.mult)
    P.tt(o1, t1, t2, ALU.subtract)
    P.tt(t1, x2, cos, ALU.mult)
    P.tt(t2, x1, sin, ALU.mult)
    P.tt(o2, t1, t2, ALU.add)


def dsa_phase(P, K, C, xr, W, li, j, SC):
    S = C.S
    NT = S // 128
    NG = S // 512
    topk = C.topk
    NIT = 16
    att_scale = 128.0 ** -0.5
    widx_scale = (8.0 ** -0.5) * (64.0 ** -0.5)
    NEG = -30000.0
    kl, kvtm, kiT, wiS = SC["kl"], SC["kvtm"], SC["kiT"], SC["wi"]
    qaT, qrT, qiT, negT, os_ = SC["qaT"], SC["qrT"], SC["qiT"], SC["negT"], SC["os"]
    ropeA, ropeB = SC["ropeA"], SC["ropeB"]

    def cs(i, n=1):
        return sl(i * 128, (i + n) * 128)

    with P.phase():
        B0 = P.psum("B0", [128, 1024], BF16)
        PB = [None] + [P.psum(f"PB{i}", [128, 512], F32) for i in range(1, 8)]
        win = P.sbuf("win", [128, 8, 744], BF16)
        P.dma(win[:], V(W['dsa_w_in'][j].rearrange("(kc p) f -> p kc f", p=128), ("w", "dsa_in")), q=POOL)
        wuq4 = W['dsa_w_uq'][j].rearrange("(kc p) (h d) -> p kc h d", p=128, d=128)
        wuq_r = P.sbuf("wuq_r", [128, 3, 8, 32], BF16)
        for kc in range(3):
            P.dma(wuq_r[:, kc, :, :], V(wuq4[:, kc, :, 0:32], ("w", "uq_r")), q=POOL)
        wuq_n = P.sbuf("wuq_n", [128, 3, 8, 96], BF16)
        for kc in range(3):
            P.dma(wuq_n[:, kc, :, :], V(wuq4[:, kc, :, 32:128], ("w", "uq_n")), q=POOL)
        wuk = P.sbuf("wuk", [128, 2, 768], BF16)
        P.dma(wuk[:], V(W['dsa_w_uk'][j].rearrange("(cc p) f -> p cc f", p=128), ("w", "uk")), q=POOL)
        wqidx = P.sbuf("wqidx", [128, 3, 512], BF16)
        P.dma(wqidx[:], V(W['dsa_w_qidx'][j].rearrange("(kc p) f -> p kc f", p=128), ("w", "qidx")), q=POOL)
        gb = P.sbuf("gb", [128, D_MODEL], F32)
        P.dma(gb[:], V(W['mix_norm'][li].partition_broadcast(128), ("w", "mixg")))
        cqg = P.sbuf("cqg", [128, 384], F32)
        ckvg = P.sbuf("ckvg", [128, 256], F32)
        kidxg = P.sbuf("kidxg", [128, 64], F32)
        P.dma(cqg[:], V(W['dsa_cq_norm'][j].partition_broadcast(128), ("w", "cqg")))
        P.dma(ckvg[:], V(W['dsa_ckv_norm'][j].partition_broadcast(128), ("w", "ckvg")))
        P.dma(kidxg[:], V(W['dsa_kidx_norm'][j].partition_broadcast(128), ("w", "kidxg")))
        AT = P.sbuf("AT", [96, 8, 384], BF16)
        BT = P.sbuf("BT", [96, 8, 256], BF16)
        Wabs = P.sbuf("Wabs", [128, 3, 8, 256], BF16)
        for h in range(8):
            for kc in range(3):
                P.transpose(B0[0:96, cs(kc)], wuq_n[:, kc, h, :], K.ident_b[:])
            for cc in range(2):
                P.transpose(B0[0:96, cs(3 + cc)], wuk[:, cc, h * 96:(h + 1) * 96], K.ident_b[:])
            P.copy(AT[:, h, :], B0[0:96, 0:384], eng=ACT)
            P.copy(BT[:, h, :], B0[0:96, 384:640])
        for h in range(8):
            for kc in range(3):
                n = h * 3 + kc
                pb = PB[1 + n % 4]
                P.mm(pb[:, 0:256], AT[:, h, cs(kc)], BT[:, h, :])
                P.copy(Wabs[:, kc, h, :], pb[:, 0:256], eng=(ACT if n % 2 else DVE))
        xt = [P.sbuf(f"xt{i}", [128, D_MODEL], F32) for i in range(2)]
        hT = [P.sbuf(f"hT{i}", [128, 8, 128], BF16) for i in range(2)]
        scr = (P.sbuf("junk", [128, D_MODEL], BF16), P.sbuf("ss", [128, 1], F32),
               P.sbuf("rstd", [128, 1], F32), P.sbuf("xn", [128, D_MODEL], BF16))
        junk = scr[0]
        ra = [P.sbuf(f"ra{i}", [128, 256], F32) for i in range(2)]
        rb = [P.sbuf(f"rb{i}", [128, 128], F32) for i in range(2)]
        pj = P.sbuf("pj", [128, 744], F32)
        sA = {n: P.sbuf(n, [128, 1], F32) for n in ("ssA", "rsA", "ssB", "rsB", "ssC", "rsC", "kn2a", "kn2b", "kn2", "rm", "nkmax")}
        km1 = P.sbuf("km1", [1, 1], F32)
        cqn = P.sbuf("cqn", [128, 384], BF16)
        cqT = P.sbuf("cqT", [128, 3, 128], BF16)
        ckv = P.sbuf("ckv", [128, 256], BF16)
        ckT = P.sbuf("ckT", [128, 2, 128], BF16)
        kra = P.sbuf("kra", [128, 33], F32)
        krb = P.sbuf("krb", [128, 33], BF16)
        krT = P.sbuf("krT", [33, 128], BF16)
        t1 = P.sbuf("t1", [128, 128], F32)
        t2 = P.sbuf("t2", [128, 128], F32)
        kin = P.sbuf("kin", [128, 64], F32)
        kib = P.sbuf("kib", [128, 64], BF16)
        kiTt = P.sbuf("kiTt", [64, 128], BF16)
        wit = P.sbuf("wit", [128, 8], F32)
        qab = P.sbuf("qab", [128, 16, 128], BF16)
        sqa = P.sbuf("sqa", [128, 16, 128], BF16)
        qr = P.sbuf("qr", [128, 8, 32], F32)
        qra = P.sbuf("qra", [128, 8, 33], F32)
        qrb = P.sbuf("qrb", [128, 8, 33], BF16)
        qrTt = P.sbuf("qrTt", [33, 8, 128], BF16)
        t3 = P.sbuf("t3", [128, 8, 32], F32)
        qr2 = P.sbuf("qr2", [128, 8], F32)
        qn = P.sbuf("qn", [128, 8], F32)
        qi = P.sbuf("qi", [128, 8, 64], F32)
        qib = P.sbuf("qib", [128, 8, 64], BF16)
        qiTt = P.sbuf("qiTt", [64, 8, 128], BF16)
        P.memset(kra[:, 32:33], 1.0)

        def v3(t, a, b, d):
            return t.v(t.base()[:, a:b].rearrange("p (h d) -> p h d", d=d))

        for sq_i in range(C.NSEQ):
            P.memset(sA["rm"][:], 0.0)
            for t in range(NT):
                r0 = sq_i * S + t * 128
                ti = r0 // 128
                toks = sl(r0, r0 + 128)
                xs, h_ = xt[t % 2], hT[t % 2]
                ra_, rb_ = ra[t % 2], rb[t % 2]
                P.dma(xs[:], xr.k(("r", ti), (toks, ALL)))
                P.dma(ra_[:], V(ropeA[t * 128:(t + 1) * 128, :], ("w", "ropeA")))
                P.dma(rb_[:], V(ropeB[t * 128:(t + 1) * 128, :], ("w", "ropeB")))
                norm_T(P, K, xs[:], gb[:], h_[:, :, :], scr, B0)
                for kc in range(8):
                    P.mm(PB[1][:], h_[:, kc, :], win[:, kc, 0:512], start=(kc == 0), stop=(kc == 7))
                for kc in range(8):
                    P.mm(PB[2][:, 0:232], h_[:, kc, :], win[:, kc, 512:744], start=(kc == 0), stop=(kc == 7))
                P.copy(pj[:, 0:512], PB[1][:], eng=ACT)
                P.copy(pj[:, 512:744], PB[2][:, 0:232])
                P.act(junk[:, 0:384], pj[:, 0:384], AF.Square, accum_out=sA["ssA"][:])
                P.rsqrt(sA["rsA"][:], sA["ssA"][:], 1.0 / 384, EPS)
                P.stt(cqn[:], pj[:, 0:384], sA["rsA"][:], cqg[:], ALU.mult, ALU.mult)
                for cc in range(3):
                    P.transpose(B0[:, cs(cc)], cqn[:, cs(cc)], K.ident_b[:])
                P.copy(cqT[:, :, :], B0.v(B0.base()[:, 0:384].rearrange("p (c t) -> p c t", t=128)), eng=ACT)
                P.act(junk[:, 0:256], pj[:, 384:640], AF.Square, accum_out=sA["ssB"][:])
                P.rsqrt(sA["rsB"][:], sA["ssB"][:], 1.0 / 256, EPS)
                P.stt(ckv[:], pj[:, 384:640], sA["rsB"][:], ckvg[:], ALU.mult, ALU.mult)
                P.act(junk[:, 0:256], ckv[:], AF.Square, accum_out=sA["kn2a"][:])
                P.dma(dv(kvtm, kvtm.base()[toks, :]), ckv[:])
                for cc in range(2):
                    P.transpose(B0[:, cs(cc)], ckv[:, cs(cc)], K.ident_b[:])
                P.copy(ckT[:, :, :], B0.v(B0.base()[:, 0:256].rearrange("p (c t) -> p c t", t=128)))
                P.dma(dv(kl, kl.base()[0:2, :, toks].rearrange("c p t -> p c t")), ckT[:])
                rope_tm(P, pj[:, 640:656], pj[:, 656:672], ra_[:, 0:16], ra_[:, 128:144],
                        kra[:, 0:16], kra[:, 16:32], t1[:, 0:16], t2[:, 0:16])
                P.act(junk[:, 0:32], kra[:, 0:32], AF.Square, accum_out=sA["kn2b"][:])
                P.tt(sA["kn2"][:], sA["kn2a"][:], sA["kn2b"][:], ALU.add)
                P.tt(sA["rm"][:], sA["rm"][:], sA["kn2"][:], ALU.max)
                P.copy(krb[:], kra[:], eng=POOL)
                P.transpose(B0[0:33, 0:128], krb[:], K.ident_b[:])
                P.copy(krT[:], B0[0:33, 0:128])
                P.dma(dv(kl, kl.base()[2, 0:33, toks]), krT[:])
                P.act(junk[:, 0:64], pj[:, 672:736], AF.Square, accum_out=sA["ssC"][:])
                P.rsqrt(sA["rsC"][:], sA["ssC"][:], 1.0 / 64, EPS)
                P.stt(kin[:], pj[:, 672:736], sA["rsC"][:], kidxg[:], ALU.mult, ALU.mult)
                P.copy(kib[:], kin[:], eng=POOL)
                rope_tm(P, kin[:, 0:8], kin[:, 8:16], rb_[:, 0:8], rb_[:, 64:72],
                        kib[:, 0:8], kib[:, 8:16], t1[:, 0:8], t2[:, 0:8])
                P.transpose(B0[0:64, 0:128], kib[:], K.ident_b[:])
                P.copy(kiTt[:], B0[0:64, 0:128], eng=ACT)
                P.dma(dv(kiT, kiT.base()[:, toks]), kiTt[:])
                P.ts(wit[:], pj[:, 736:744], widx_scale, ALU.mult)
                P.dma(dv(wiS, wiS.base()[toks, :]), wit[:])
                P.transpose(PB[3][0:1, 0:128], sA["rm"][:], K.ident_f[:])
                P.reduce(km1[:], PB[3][0:1, 0:128], ALU.max)
                P.act(km1[:], km1[:], AF.Ln, bias=1e-30)
                P.act(km1[:], km1[:], AF.Exp, scale=0.5)
                P.ts(km1[:], km1[:], -1.0, ALU.mult)
                P.mm(PB[3][:, 128:129], K.ones_f[0:1, 0:128], km1[0:1, 0:1])
                P.copy(sA["nkmax"][:], PB[3][:, 128:129])
                for b4 in range(4):
                    pb = PB[4 + b4 % 2]
                    for i in range(4):
                        idx = b4 * 4 + i
                        h, cc = idx // 2, idx % 2
                        for kc in range(3):
                            P.mm(pb[:, cs(i)], Wabs[:, kc, h, cs(cc)], cqT[:, kc, :], start=(kc == 0), stop=(kc == 2))
                    P.copy(qab[:, b4 * 4:(b4 + 1) * 4, :], pb.v(pb.base()[:, :].rearrange("p (c t) -> p c t", t=128)),
                           eng=(ACT if b4 % 2 else DVE))
                P.dma(dv(qaT, qaT.base()[:, :, :, toks].rearrange("h c p t -> p (h c) t")), qab[:])
                P.tt(sqa[:], qab[:], qab[:], ALU.mult, eng=POOL)
                for idx in range(16):
                    h, cc = idx // 2, idx % 2
                    P.mm(PB[6][:, h:h + 1], sqa[:, idx, :], K.ones_b[:, 0:1], start=(cc == 0), stop=(cc == 1))
                for kc in range(3):
                    P.mm(PB[7][:, 0:256], cqT[:, kc, :], wuq_r.v(wuq_r.base()[:, kc, :, :].rearrange("p h d -> p (h d)")),
                         start=(kc == 0), stop=(kc == 2))
                P.copy(qr[:, :, :], PB[7].v(PB[7].base()[:, 0:256].rearrange("p (h d) -> p h d", d=32)), eng=ACT)
                rope_tm(P, qr[:, :, 0:16], qr[:, :, 16:32], v3(ra_, 0, 128, 16), v3(ra_, 128, 256, 16),
                        qra[:, :, 0:16], qra[:, :, 16:32], v3(t1, 0, 128, 16), v3(t2, 0, 128, 16))
                P.tt(t3[:, :, :], qra[:, :, 0:32], qra[:, :, 0:32], ALU.mult)
                P.reduce(qr2[:], t3[:, :, :], ALU.add)
                P.tt(qn[:], qr2[:], PB[6][:, 0:8], ALU.add)
                P.act(qn[:], qn[:], AF.Ln, bias=1e-30)
                P.act(qn[:], qn[:], AF.Exp, scale=0.5)
                P.ts(qra[:, :, 32:33], qn.v(qn.base()[:, :].rearrange("p (h o) -> p h o", o=1)), sA["nkmax"][:], ALU.mult)
                P.copy(qrb[:, :, :], qra[:, :, :], eng=POOL)
                for h in range(8):
                    P.transpose(B0[0:33, cs(h)], qrb[:, h, :], K.ident_b[:])
                P.copy(qrTt[:, :, :], B0.v(B0.base()[0:33, :].rearrange("p (h t) -> p h t", t=128)), eng=ACT)
                P.dma(dv(qrT, qrT.base()[:, :, toks].rearrange("h p t -> p h t")), qrTt[:])
                for kc in range(3):
                    P.mm(PB[1][:], cqT[:, kc, :], wqidx[:, kc, :], start=(kc == 0), stop=(kc == 2))
                P.copy(qi[:, :, :], PB[1].v(PB[1].base()[:, :].rearrange("p (h d) -> p h d", d=64)))
                P.copy(qib[:, :, :], qi[:, :, :], eng=POOL)
                rope_tm(P, qi[:, :, 0:8], qi[:, :, 8:16], v3(rb_, 0, 64, 8), v3(rb_, 64, 128, 8),
                        qib[:, :, 0:8], qib[:, :, 8:16], v3(t1, 0, 64, 8), v3(t2, 0, 64, 8))
                for h in range(8):
                    P.transpose(B0[0:64, cs(h)], qib[:, h, :], K.ident_b[:])
                P.copy(qiTt[:, :, :], B0.v(B0.base()[0:64, :].rearrange("p (h t) -> p h t", t=128)))
                P.dma(dv(qiT, qiT.base()[:, :, toks].rearrange("h p t -> p h t")), qiTt[:])

    with P.phase():
        B0 = P.psum("B0", [128, 1024], BF16)
        PI = [P.psum(f"PI{i}", [128, 512], F32) for i in range(2)]
        PS = [P.psum(f"PS{i}", [128, 512], F32) for i in range(2)]
        kis = P.sbuf("kis", [64, S], BF16)
        sc = [P.sbuf(f"sc{i}", [128, S], F32) for i in range(2)]
        jk = P.sbuf("jk", [128, S], BF16)
        neg = [P.sbuf(f"neg{i}", [128, S], BF16) for i in range(2)]
        ngt = [P.sbuf(f"ngt{i}", [128, NT, 128], BF16) for i in range(2)]
        rr = [[P.sbuf(f"rr{i}{k}", [128, 512], F32) for k in range(2)] for i in range(2)]
        dg = [P.sbuf(f"dg{i}", [128, 8, 128], F32) for i in range(2)]
        qiq = [P.sbuf(f"qiq{i}", [64, 8, 128], BF16) for i in range(2)]
        wiq = [P.sbuf(f"wiq{i}", [128, 8], F32) for i in range(2)]
        m_le = P.sbuf("m_le", [128, 128], F32)
        nbig = P.sbuf("nbig", [128, 128], F32)
        P.ts(m_le[:], K.m_gt[:], -1.0, ALU.mult, 1.0, ALU.add)
        P.ts(nbig[:], K.m_gt[:], -1e30, ALU.mult)
        pw2 = P.sbuf("pw2", [128, NIT], F32)
        for it in range(NIT):
            P.memset(pw2[:, it:it + 1], 2.0 ** -(it + 1))
        sB = [{n: P.sbuf(f"{n}{i}", [128, 1], F32) for n in ("lo", "hi", "mid", "cnt", "t")} for i in range(2)]
        stp = [P.sbuf(f"stp{i}", [128, NIT], F32) for i in range(2)]
        jks = [jk, P.sbuf("jk2", [128, S], BF16)]

        def idx_gen(sq_i, qb):
            par = qb % 2
            L = (qb + 1) * 128
            r0 = sq_i * S + qb * 128
            toks = sl(r0, r0 + 128)
            s_, q_, w_, d_ = sc[par], qiq[par], wiq[par], dg[par]
            P.dma(q_[:], dv(qiT, qiT.base()[:, :, toks].rearrange("h p t -> p h t")))
            P.dma(w_[:], dv(wiS, wiS.base()[toks, :]))
            for h in range(8):
                P.ts(d_[:, h, :], K.ident_f[:], w_[:, h:h + 1], ALU.mult)
            yield
            n = 0
            for kg in range((L + 511) // 512):
                w = min(512, L - kg * 512)
                cols = sl(kg * 512, kg * 512 + w)
                sb_ = PS[kg % 2]
                pend = None
                for h in range(8):
                    pb, r_ = PI[n % 2], rr[n % 2][0]
                    n += 1
                    P.mm(pb[:, 0:w], q_[:, h, :], kis[:, cols])
                    if pend is not None:
                        P.mm(sb_[:, 0:w], d_[:, pend[0], :], pend[1][:, 0:w], start=(pend[0] == 0), stop=False)
                    P.act(r_[:, 0:w], pb[:, 0:w], AF.Relu)
                    pend = (h, r_)
                    yield
                P.mm(sb_[:, 0:w], d_[:, 7, :], pend[1][:, 0:w], start=False, stop=True)
                P.copy(s_[:, cols], sb_[:, 0:w], eng=ACT)
                yield

        def bis_gen(sq_i, qb):
            par = qb % 2
            L = (qb + 1) * 128
            Lg = (4 * (qb // 4) + 4) * 128
            s_, n_, g_ = sc[par], neg[par], ngt[par]
            lo, hi, mid, cnt, tt_ = (sB[par][n] for n in ("lo", "hi", "mid", "cnt", "t"))
            st_, jk_ = stp[par], jks[par]
            P.reduce(hi[:], s_[:, 0:L], ALU.max)
            P.reduce(lo[:], s_[:, 0:L], ALU.min)
            yield
            P.tt(hi[:], hi[:], lo[:], ALU.subtract)
            P.ts(lo[:], lo[:], -1.0, ALU.add)
            P.ts(hi[:], hi[:], 2.0, ALU.add)
            P.ts(st_[:], pw2[:], hi[:], ALU.mult)
            blk = s_[:, qb * 128:L]
            P.tt(blk, blk, m_le[:], ALU.mult)
            P.tt(blk, blk, nbig[:], ALU.add)
            yield
            for it in range(NIT):
                P.tt(mid[:], lo[:], st_[:, it:it + 1], ALU.add)
                P.ts(jk_[:, 0:L], s_[:, 0:L], mid[:], ALU.is_gt, 0.0, ALU.add, accum_out=cnt[:])
                yield
                P.ts(tt_[:], cnt[:], float(topk) - 0.5, ALU.is_ge, st_[:, it:it + 1], ALU.mult)
                P.tt(lo[:], lo[:], tt_[:], ALU.add)
                yield
            P.ts(n_[:, 0:L], s_[:, 0:L], lo[:], ALU.is_gt)
            if Lg > L:
                P.memset(n_[:, L:Lg], 0.0, eng=POOL)
            yield
            nkb = Lg // 128
            for kb0 in range(0, nkb, 8):
                n8 = min(8, nkb - kb0)
                for i in range(n8):
                    P.transpose(B0[:, cs(i)], n_[:, cs(kb0 + i)], K.ident_b[:])
                P.copy(g_[:, kb0:kb0 + n8, :], B0.v(B0.base()[:, 0:n8 * 128].rearrange("p (c t) -> p c t", t=128)),
                       eng=ACT)
                yield
            P.dma(dv(negT, negT.base()[sq_i, 0:nkb, :, qb * 128:(qb + 1) * 128].rearrange("kb k q -> k kb q")),
                  g_[:, 0:nkb, :], q=POOL)

        prev = None
        for sq_i in range(C.NSEQ):
            P.dma(kis[:], dv(kiT, kiT.base()[:, sq_i * S:(sq_i + 1) * S]))
            for qb in range(NT):
                gens = [idx_gen(sq_i, qb)]
                if prev is not None:
                    gens.append(bis_gen(*prev))
                run_interleaved(gens)
                prev = (sq_i, qb)
        run_interleaved([bis_gen(*prev)])

    with P.phase():
        PA = [P.psum(f"PA{i}", [128, 512], F32) for i in range(2)]
        O0 = P.psum("O0", [128, 512], F32)
        O1 = P.psum("O1", [128, 512], F32)
        Dn = P.psum("Dn", [128, 512], F32)
        Ov = P.psum("Ov", [128, 512], F32)
        wuv = P.sbuf("wuv", [128, 2, 1024], BF16)
        P.dma(wuv[:], V(W['dsa_w_uv'][j].rearrange("(cc p) f -> p cc f", p=128), ("w", "uv")), q=POOL)
        kl01 = P.sbuf("kl01", [128, 2, S], BF16)
        kl2 = P.sbuf("kl2", [33, S], BF16)
        ckv = P.sbuf("ckvs", [128, NT, 256], BF16)
        ngc = [P.sbuf(f"ngc{i}", [128, NT, 512], BF16) for i in range(2)]
        qa = [P.sbuf(f"qa{i}", [128, 2, 512], BF16) for i in range(2)]
        qrr = [P.sbuf(f"qrr{i}", [33, 512], BF16) for i in range(2)]
        pT = [P.sbuf(f"pT{i}", [128, 512], BF16) for i in range(2)]
        ol = P.sbuf("ol", [128, 2, 512], BF16)
        rden = P.sbuf("rden", [128, 512], F32)
        ost = [P.sbuf(f"ost{i}", [128, 512], BF16) for i in range(2)]
        cnt_ = 0
        for sq_i in range(C.NSEQ):
            seq = sl(sq_i * S, (sq_i + 1) * S)
            P.dma(kl01[:], dv(kl, kl.base()[0:2, :, seq].rearrange("c p t -> p c t")))
            P.dma(kl2[:], dv(kl, kl.base()[2, 0:33, seq]))
            P.dma(ckv[:], dv(kvtm, kvtm.base()[seq, :].rearrange("(t p) c -> p t c", p=128)))
            for g in range(NG):
                nkb = 4 * g + 4
                ng_ = ngc[g % 2]
                gt = sl(sq_i * S + g * 512, sq_i * S + (g + 1) * 512)
                P.dma(ng_[:, 0:nkb, :], dv(negT, negT.base()[sq_i, 0:nkb, :, g * 512:(g + 1) * 512].rearrange("kb k q -> k kb q")))
                for h in range(8):
                    qa_, qr_ = qa[cnt_ % 2], qrr[cnt_ % 2]
                    o_ = ost[cnt_ % 2]
                    cnt_ += 1
                    P.dma(qa_[:], dv(qaT, qaT.base()[h, :, :, gt].rearrange("c p t -> p c t")))
                    P.dma(qr_[:], dv(qrT, qrT.base()[h, 0:33, gt]))

                    def pv(i):
                        p_ = pT[i % 2]
                        P.mm(O0[:], ckv[:, i, 0:128], p_[:], start=(i == 0), stop=(i == nkb - 1))
                        P.mm(O1[:], ckv[:, i, 128:256], p_[:], start=(i == 0), stop=(i == nkb - 1))
                        P.mm(Dn[:], K.ones_b[:], p_[:], start=(i == 0), stop=(i == nkb - 1))

                    for kb in range(nkb):
                        A = PA[kb % 2]
                        P.mm(A[:], kl01[:, 0, cs(kb)], qa_[:, 0, :], start=True, stop=False)
                        P.mm(A[:], kl01[:, 1, cs(kb)], qa_[:, 1, :], start=False, stop=False)
                        P.mm(A[:], kl2[0:33, cs(kb)], qr_[0:33, :], start=False, stop=True)
                        if kb > 0:
                            pv(kb - 1)
                        P.act(pT[kb % 2][:], A[:], AF.Exp, scale=att_scale)
                        P.tt(pT[kb % 2][:], pT[kb % 2][:], ng_[:, kb, :], ALU.mult)
                    pv(nkb - 1)
                    P.copy(ol[:, 0, :], O0[:], eng=ACT)
                    P.copy(ol[:, 1, :], O1[:])
                    P.recip(rden[:], Dn[:])
                    P.mm(Ov[:], wuv[:, 0, cs(h)], ol[:, 0, :], start=True, stop=False)
                    P.mm(Ov[:], wuv[:, 1, cs(h)], ol[:, 1, :], start=False, stop=True)
                    P.tt(o_[:], Ov[:], rden[:], ALU.mult)
                    P.dma(dv(os_, os_.base()[h, :, gt]), o_[:], q=POOL)

    outproj_phase(P, K, C, xr, W['dsa_w_out'][j], os_)


def rope_tables(S):
    pos = np.arange(S, dtype=np.float32)[:, None]

    def tab(r):
        half = r // 2
        inv = (np.float32(500000.0) ** (-np.arange(half, dtype=np.float32) * np.float32(2.0 / r))).astype(np.float32)
        ang = pos * inv[None, :]
        c = np.tile(np.cos(ang).astype(np.float32), (1, 8))
        s_ = np.tile(np.sin(ang).astype(np.float32), (1, 8))
        return np.ascontiguousarray(np.concatenate([c, s_], axis=1), dtype=np.float32)

    return tab(32), tab(16)


def run(C, inputs, n_cores):
    nc = build(C)
    x = np.ascontiguousarray(inputs['x'], dtype=np.float32).reshape(n_cores, C.NTOK, D_MODEL)
    in_maps = []
    for c in range(n_cores):
        m = {"x": x[c]}
        m["ropeA"], m["ropeB"] = rope_tables(C.S)
        for name in INPUT_SHAPES:
            m[name] = np.ascontiguousarray(inputs[name], dtype=np.float32)
        in_maps.append(m)
    res = run_bass_kernel_spmd(nc, in_maps, core_ids=list(range(n_cores)))
    return np.stack([np.asarray(r["y"]) for r in res.results], axis=0)


def kernel(**inputs):
    C = Cfg()
    x = np.asarray(inputs['x'])
    B, S, D = x.shape
    y = run(C, inputs, N_CORES)
    return y.reshape(B, S, D).astype(np.float32)
```

```python
import contextlib
from contextlib import ExitStack

import numpy as np
import concourse.bass as bass
import concourse.mybir as mybir
from concourse.bass_utils import run_bass_kernel_spmd

F32 = mybir.dt.float32
BF16 = mybir.dt.bfloat16
AF = mybir.ActivationFunctionType
ALU = mybir.AluOpType
AX = mybir.AxisListType

D_MODEL = 1024
D_FF = 2816
EPS = 1e-6
N_CORES = 8

PE, ACT, DVE, POOL, SP = "pe", "act", "dve", "pool", "sp"
SEM_EPOCH = 20000
NDMA_SLOTS = 24


class V:
    __slots__ = ("ap", "key")

    def __init__(self, ap, key):
        self.ap = ap
        self.key = key


class T:
    _n = 0

    def __init__(self, handle, name, ap=None, is_psum=False):
        self.h = handle
        self.name = name
        T._n += 1
        self.id = T._n
        self._ap = ap
        self.is_psum = is_psum

    def base(self):
        return self._ap if self._ap is not None else self.h

    def __getitem__(self, idx):
        return V(self.base()[idx], (self.id, None, self.is_psum))

    def k(self, sub, idx=slice(None)):
        return V(self.base()[idx], (self.id, None if self.is_psum else sub, self.is_psum))

    def v(self, ap, sub=None):
        return V(ap, (self.id, None if self.is_psum else sub, self.is_psum))


class Op:
    __slots__ = ("eng", "fn", "reads", "writes", "is_dma", "idx", "deps", "signal", "sig_no", "slot", "slot_val")

    def __init__(self, eng, fn, reads, writes, is_dma=False):
        self.eng = eng
        self.fn = fn
        self.reads = reads
        self.writes = writes
        self.is_dma = is_dma
        self.deps = []
        self.signal = False
        self.sig_no = None


class Prog:
    def __init__(self, nc, es):
        self.nc = nc
        self.es_outer = es
        self.es = es
        self.ops = []
        self.engs = {PE: nc.tensor, ACT: nc.scalar, DVE: nc.vector, POOL: nc.gpsimd, SP: nc.sync}
        self.sig_count = {e: 0 for e in self.engs}
        self.dma_count = {e: 0 for e in self.engs}
        self.sems = {e: [] for e in self.engs}
        self.dsems = {e: [] for e in self.engs}
        self.n_emitted = 0

    def sbuf(self, name, shape, dt):
        T._n += 1
        name = f"{name}_{T._n}"
        h = self.es.enter_context(self.nc.sbuf_tensor(name, list(shape), dt))
        return T(h, name)

    def psum(self, name, shape, dt=F32):
        T._n += 1
        name = f"{name}_{T._n}"
        h = self.es.enter_context(self.nc.psum_tensor(name, list(shape), dt))
        return T(h, name, is_psum=True)

    def dram(self, name, shape, dt, kind="Internal"):
        h = self.nc.dram_tensor(name, list(shape), dt, kind=kind)
        return T(h, name, ap=h.ap())

    @contextlib.contextmanager
    def phase(self):
        old = self.es
        with ExitStack() as es:
            self.es = es
            yield
            self.flush()
        self.es = old

    def op(self, eng, fn, reads, writes, is_dma=False):
        rk = [v.key for v in reads]
        wk = [v.key for v in writes]
        wk += [k for k in rk if len(k) == 3 and k[2] is True]
        o = Op(eng, fn, rk, wk, is_dma)
        o.idx = len(self.ops)
        self.ops.append(o)
        return o

    def dma(self, out, in_, q=SP, **kw):
        return self.op(q, lambda e: e.dma_start(out=out.ap, in_=in_.ap, **kw), [in_], [out], is_dma=True)

    def mm(self, out, lhsT, rhs, start=True, stop=True):
        return self.op(PE, lambda e: e.matmul(out.ap, lhsT=lhsT.ap, rhs=rhs.ap, start=start, stop=stop),
                       [lhsT, rhs] + ([] if start else [out]), [out])

    def transpose(self, out, in_, ident):
        return self.op(PE, lambda e: e.transpose(out.ap, in_.ap, ident.ap), [in_, ident], [out])

    def act(self, out, in_, func, bias=None, scale=None, accum_out=None):
        kw = {}
        reads = [in_]
        writes = [out]
        if bias is not None:
            if isinstance(bias, V):
                kw["bias"] = bias.ap
                reads.append(bias)
            else:
                kw["bias"] = bias
        if scale is not None:
            if isinstance(scale, V):
                kw["scale"] = scale.ap
                reads.append(scale)
            else:
                kw["scale"] = scale
        if accum_out is not None:
            kw["accum_out"] = accum_out.ap
            writes.append(accum_out)
        return self.op(ACT, lambda e: e.activation(out.ap, in_.ap, func, **kw), reads, writes)

    def copy(self, out, in_, eng=DVE):
        if eng == ACT:
            return self.op(ACT, lambda e: e.copy(out.ap, in_.ap), [in_], [out])
        return self.op(eng, lambda e: e.tensor_copy(out=out.ap, in_=in_.ap), [in_], [out])

    def tt(self, out, in0, in1, op, eng=DVE):
        return self.op(eng, lambda e: e.tensor_tensor(out.ap, in0.ap, in1.ap, op), [in0, in1], [out])

    def ts(self, out, in0, s1, op0, s2=None, op1=None, accum_out=None, eng=DVE):
        reads = [in0]
        writes = [out]
        a1 = s1.ap if isinstance(s1, V) else s1
        a2 = s2.ap if isinstance(s2, V) else s2
        if isinstance(s1, V):
            reads.append(s1)
        if isinstance(s2, V):
            reads.append(s2)
        kw = {}
        if op1 is not None:
            kw["op1"] = op1
        if accum_out is not None:
            kw["accum_out"] = accum_out.ap
            writes.append(accum_out)
        return self.op(eng, lambda e: e.tensor_scalar(out.ap, in0.ap, a1, a2, op0, **kw), reads, writes)

    def stt(self, out, in0, s, in1, op0, op1, eng=DVE):
        reads = [in0, in1]
        a = s.ap if isinstance(s, V) else s
        if isinstance(s, V):
            reads.append(s)
        return self.op(eng, lambda e: e.scalar_tensor_tensor(out.ap, in0.ap, a, in1.ap, op0, op1), reads, [out])

    def reduce(self, out, in_, op, axis=AX.X, eng=DVE):
        return self.op(eng, lambda e: e.tensor_reduce(out.ap, in_.ap, axis, op), [in_], [out])

    def recip(self, out, in_):
        return self.op(DVE, lambda e: e.reciprocal(out.ap, in_.ap), [in_], [out])

    def rsqrt(self, out, in_, mult, add):
        self.act(out, in_, AF.Ln, bias=add, scale=mult)
        return self.act(out, out, AF.Exp, scale=-0.5)

    def memset(self, out, val, eng=DVE):
        return self.op(eng, lambda e: e.memset(out.ap, val), [], [out])

    def _sem(self, eng, n):
        i = n // SEM_EPOCH
        while len(self.sems[eng]) <= i:
            self.sems[eng].append(self.es_outer.enter_context(self.nc.semaphore(f"s_{eng}_{len(self.sems[eng])}")))
        return self.sems[eng][i], n % SEM_EPOCH + 1

    def _dsem(self, eng, slot):
        while len(self.dsems[eng]) <= slot:
            self.dsems[eng].append(self.es_outer.enter_context(self.nc.semaphore(f"d_{eng}_{len(self.dsems[eng])}")))
        return self.dsems[eng][slot]

    def flush(self):
        ops = self.ops
        if not ops:
            return
        last_w = {}
        readers = {}
        for o in ops:
            deps = set()
            for k in o.reads:
                w = last_w.get(k)
                if w is not None:
                    deps.add(w)
            for k in o.writes:
                w = last_w.get(k)
                if w is not None:
                    deps.add(w)
                for r in readers.get(k, ()):
                    deps.add(r)
            deps.discard(o.idx)
            o.deps = deps
            for k in o.reads:
                readers.setdefault(k, []).append(o.idx)
            for k in o.writes:
                last_w[k] = o.idx
                readers[k] = []
        waited = {e: {} for e in self.engs}
        waited_dma = {e: set() for e in self.engs}
        for o in ops:
            best = {}
            keep = []
            for d in o.deps:
                p = ops[d]
                if p.is_dma:
                    if d not in waited_dma[o.eng]:
                        waited_dma[o.eng].add(d)
                        keep.append(d)
                    continue
                if p.eng == PE and o.eng == PE and not o.is_dma:
                    continue
                if waited[o.eng].get(p.eng, -1) >= d:
                    continue
                if best.get(p.eng, -1) < d:
                    best[p.eng] = d
            for e, d in best.items():
                keep.append(d)
                waited[o.eng][e] = d
            o.deps = sorted(keep)
            for d in o.deps:
                ops[d].signal = True
        last_op = {}
        for o in ops:
            if not o.is_dma:
                last_op[o.eng] = o
        for o in last_op.values():
            o.signal = True
        last_dma = {}
        for o in ops:
            if o.is_dma:
                o.slot = self.dma_count[o.eng] % NDMA_SLOTS
                o.slot_val = 16 * (self.dma_count[o.eng] // NDMA_SLOTS + 1)
                self.dma_count[o.eng] += 1
                last_dma[(o.eng, o.slot)] = o
            elif o.signal:
                o.sig_no = self.sig_count[o.eng]
                self.sig_count[o.eng] += 1

        def sem_of(p):
            if p.is_dma:
                return self._dsem(p.eng, p.slot), p.slot_val
            return self._sem(p.eng, p.sig_no)

        for o in ops:
            e = self.engs[o.eng]
            for d in o.deps:
                s, v = sem_of(ops[d])
                e.wait_ge(s, v)
            if o.is_dma and o.slot_val > 16:
                e.wait_ge(self._dsem(o.eng, o.slot), o.slot_val - 16)
            ins = o.fn(e)
            if o.is_dma:
                ins.then_inc(self._dsem(o.eng, o.slot), 16)
            elif o.signal:
                s, _ = self._sem(o.eng, o.sig_no)
                ins.then_inc(s, 1)
        for en, e in self.engs.items():
            for pe_, o in last_op.items():
                if pe_ == en:
                    continue
                s, v = sem_of(o)
                e.wait_ge(s, v)
            for o in last_dma.values():
                s, v = sem_of(o)
                e.wait_ge(s, v)
        self.n_emitted += len(ops)
        self.ops = []


class Cfg:
    def __init__(self, S=4096, NSEQ=2, layers=(0, 1, 2, 3), ffn=True, mixers=True, final=True):
        self.S = S
        self.NSEQ = NSEQ
        self.NTOK = S * NSEQ
        self.layers = tuple(layers)
        self.ffn = ffn
        self.mixers = mixers
        self.final = final
        self.topk = min(256, S // 4)


INPUT_SHAPES = {
    'ffn1_norm': (4, 1024), 'ffn1_w_gu': (4, 1024, 5632), 'ffn1_w_down': (4, 2816, 1024),
    'mix_norm': (4, 1024), 'ffn2_norm': (4, 1024), 'ffn2_w_gu': (4, 1024, 5632), 'ffn2_w_down': (4, 2816, 1024),
    'gdn_w_in': (2, 1024, 4112), 'gdn_conv': (2, 4, 3072), 'gdn_a_log': (2, 8), 'gdn_dt_bias': (2, 8),
    'gdn_norm': (2, 128), 'gdn_w_out': (2, 1024, 1024),
    'sb_w_in': (1, 1024, 3072), 'sb_w_out': (1, 1024, 1024),
    'dsa_w_in': (1, 1024, 744), 'dsa_cq_norm': (1, 384), 'dsa_ckv_norm': (1, 256), 'dsa_kidx_norm': (1, 64),
    'dsa_w_uq': (1, 384, 1024), 'dsa_w_qidx': (1, 384, 512), 'dsa_w_uk': (1, 256, 768), 'dsa_w_uv': (1, 256, 1024),
    'dsa_w_out': (1, 1024, 1024), 'final_norm': (1024,),
}


class Ctx:
    pass


def make_consts(P, K):
    K.ident_f = P.sbuf("ident_f", [128, 128], F32)
    K.ident_b = P.sbuf("ident_b", [128, 128], BF16)
    K.ones_f = P.sbuf("ones_f", [128, 128], F32)
    K.ones_b = P.sbuf("ones_b", [128, 128], BF16)
    P.memset(K.ones_f[:], 1.0)
    P.memset(K.ones_b[:], 1.0)
    P.memset(K.ident_f[:], 1.0)
    P.op(POOL, lambda e: e.affine_select(K.ident_f[:].ap, K.ident_f[:].ap, [[-1, 128]], ALU.is_equal, 0.0,
                                         base=0, channel_multiplier=1), [K.ident_f[:]], [K.ident_f[:]])
    P.copy(K.ident_b[:], K.ident_f[:])
    K.m_ge = P.sbuf("m_ge", [128, 128], F32)
    K.m_gt = P.sbuf("m_gt", [128, 128], F32)
    K.m_lt = P.sbuf("m_lt", [128, 128], F32)
    for t, pat, cm, cmp_ in ((K.m_ge, 1, -1, ALU.is_ge), (K.m_gt, 1, -1, ALU.is_gt), (K.m_lt, -1, 1, ALU.is_gt)):
        P.memset(t[:], 1.0)
        P.op(POOL, (lambda t, pat, cm, cmp_: lambda e: e.affine_select(t[:].ap, t[:].ap, [[pat, 128]], cmp_, 0.0,
                                                                       base=0, channel_multiplier=cm))(t, pat, cm, cmp_),
             [t[:]], [t[:]])


def norm_T(P, K, xs, gb, hT_view, scr, psT, n_feat=1024, eps=EPS):
    junk, ss, rstd, xn = scr
    P.act(junk[:, 0:n_feat], xs, AF.Square, accum_out=ss[:])
    P.rsqrt(rstd[:], ss[:], 1.0 / n_feat, eps)
    P.stt(xn[:, 0:n_feat], xs, rstd[:], gb, ALU.mult, ALU.mult)
    nch = n_feat // 128
    for kc in range(nch):
        P.transpose(psT[:, kc * 128:(kc + 1) * 128], xn[:, kc * 128:(kc + 1) * 128], K.ident_b[:])
    P.copy(hT_view, psT.v(psT.base()[:, 0:n_feat].rearrange("p (c t) -> p c t", t=128)), eng=ACT)


def ffn_phase(P, K, C, src, dst, g_row, w_gu, w_down, tag):
    TT = 256
    NS = TT // 128
    NFC = D_FF // 128
    with P.phase():
        wgu = P.sbuf("wgu", [128, 8, 2 * D_FF], BF16)
        wd = P.sbuf("wd", [128, NFC, D_MODEL], BF16)
        gb = P.sbuf("gb", [128, D_MODEL], F32)
        P.dma(gb[:], V(g_row.partition_broadcast(128), ("w", tag, "g")))
        wgu_src = w_gu.rearrange("(kc p) f -> p kc f", p=128)
        for fi in range(11):
            P.dma(wgu.k(fi, (slice(None), slice(None), slice(fi * 512, (fi + 1) * 512))),
                  V(wgu_src[:, :, fi * 512:(fi + 1) * 512], ("w", tag, "gu")), q=POOL)
        wd_src = w_down.rearrange("(fc p) d -> p fc d", p=128)
        for pi in range(2):
            P.dma(wd.k(pi, (slice(None), slice(pi * 11, (pi + 1) * 11), slice(None))),
                  V(wd_src[:, pi * 11:(pi + 1) * 11, :], ("w", tag, "d")), q=POOL)
        xt = [P.sbuf(f"xt{i}", [128, D_MODEL], F32) for i in range(2 * NS)]
        hT = [P.sbuf(f"hT{i}", [128, 8, TT], BF16) for i in range(2)]
        aT = P.sbuf("aT", [128, NFC, TT], BF16)
        sg = [P.sbuf(f"sg{i}", [128, TT], F32) for i in range(2)]
        scr = (P.sbuf("junk", [128, D_MODEL], BF16), P.sbuf("ss", [128, 1], F32),
               P.sbuf("rstd", [128, 1], F32), P.sbuf("xn", [128, D_MODEL], BF16))
        psT = [P.psum(f"psT{i}", [128, D_MODEL], BF16) for i in range(2)]
        psg = [P.psum(f"psg{i}", [128, TT], F32) for i in range(2)]
        psu = [P.psum(f"psu{i}", [128, TT], F32) for i in range(2)]
        psd = [P.psum(f"psd{i}", [128, 512], F32) for i in range(2)]
        ntile = C.NTOK // TT
        nd = 0
        for ti in range(ntile):
            r0 = ti * TT
            xs = [xt[(ti % 2) * NS + s] for s in range(NS)]
            h = hT[ti % 2]
            for s in range(NS):
                P.dma(xs[s][:], src.k(("r", r0 // 128 + s), (slice(r0 + s * 128, r0 + (s + 1) * 128), slice(None))))
                norm_T(P, K, xs[s][:], gb[:], h[:, :, s * 128:(s + 1) * 128], scr, psT[s % 2])
            for fc in range(NFC):
                pg = psg[fc % 2]
                pu = psu[fc % 2]
                for kc in range(8):
                    P.mm(pg[:], wgu.k(fc // 4, (slice(None), kc, slice(fc * 128, (fc + 1) * 128))), h[:, kc, :],
                         start=(kc == 0), stop=(kc == 7))
                cu = NFC + fc
                for kc in range(8):
                    P.mm(pu[:], wgu.k(cu // 4, (slice(None), kc, slice(cu * 128, (cu + 1) * 128))), h[:, kc, :],
                         start=(kc == 0), stop=(kc == 7))
                P.act(sg[fc % 2][:], pg[:], AF.Silu)
                P.tt(aT[:, fc, :], sg[fc % 2][:], pu[:], ALU.mult)
            for s in range(NS):
                for half in range(2):
                    pd = psd[nd % 2]
                    nd += 1
                    for fc in range(NFC):
                        P.mm(pd[:], aT[:, fc, s * 128:(s + 1) * 128],
                             wd.k(fc // 11, (slice(None), fc, slice(half * 512, (half + 1) * 512))),
                             start=(fc == 0), stop=(fc == NFC - 1))
                    P.stt(xs[s][:, half * 512:(half + 1) * 512], pd[:], 0.5, xs[s][:, half * 512:(half + 1) * 512],
                          ALU.mult, ALU.add)
                P.dma(dst.k(("r", r0 // 128 + s), (slice(r0 + s * 128, r0 + (s + 1) * 128), slice(None))), xs[s][:],
                      q=POOL)


def final_phase(P, K, C, src, dst, g_row):
    with P.phase():
        gb = P.sbuf("gb", [128, D_MODEL], F32)
        P.dma(gb[:], V(g_row.partition_broadcast(128), ("w", "fin", "g")))
        xt = [P.sbuf(f"xt{i}", [128, D_MODEL], F32) for i in range(4)]
        junk = P.sbuf("junk", [128, D_MODEL], BF16)
        ss = [P.sbuf(f"ss{i}", [128, 1], F32) for i in range(4)]
        for ti in range(C.NTOK // 128):
            x = xt[ti % 4]
            s = ss[ti % 4]
            rows = (slice(ti * 128, (ti + 1) * 128), slice(None))
            P.dma(x[:], src.k(("r", ti), rows))
            P.act(junk[:], x[:], AF.Square, accum_out=s[:])
            P.rsqrt(s[:], s[:], 1.0 / D_MODEL, EPS)
            P.stt(x[:], x[:], s[:], gb[:], ALU.mult, ALU.mult)
            P.dma(dst.k(("r", ti), rows), x[:], q=POOL)


def copy_phase(P, C, src, dst):
    with P.phase():
        xt = [P.sbuf(f"xt{i}", [128, D_MODEL], F32) for i in range(4)]
        for ti in range(C.NTOK // 128):
            rows = (slice(ti * 128, (ti + 1) * 128), slice(None))
            P.dma(xt[ti % 4][:], src.k(("r", ti), rows))
            P.dma(dst.k(("r", ti), rows), xt[ti % 4][:], q=POOL)


def build(C):
    nc = bass.Bass("TRN2", target_bir_lowering=False)
    with ExitStack() as es:
        P = Prog(nc, es)
        K = Ctx()
        x_in = P.dram("x", [C.NTOK, D_MODEL], F32, kind="ExternalInput")
        y_out = P.dram("y", [C.NTOK, D_MODEL], F32, kind="ExternalOutput")
        xr = P.dram("xr", [C.NTOK, D_MODEL], F32)
        W = {}
        for name, shp in INPUT_SHAPES.items():
            W[name] = nc.dram_tensor(name, list(shp), F32, kind="ExternalInput").ap()
        SC = {}
        if C.mixers and any(l % 3 in (1, 2) for l in C.layers):
            SC["qs"] = P.dram("sc_qs", [8, 128, C.NTOK], BF16)
            SC["ks"] = P.dram("sc_ks", [8, 128, C.NTOK], BF16)
            SC["vs"] = P.dram("sc_vs", [C.NTOK, 1024], BF16)
            SC["os"] = P.dram("sc_os", [8, 128, C.NTOK], BF16)
        if C.mixers and any(l % 3 == 2 for l in C.layers):
            NT_ = C.S // 128
            SC["kl"] = P.dram("sc_kl", [3, 128, C.NTOK], BF16)
            SC["kvtm"] = P.dram("sc_kvtm", [C.NTOK, 256], BF16)
            SC["kiT"] = P.dram("sc_kiT", [64, C.NTOK], BF16)
            SC["wi"] = P.dram("sc_wi", [C.NTOK, 8], F32)
            SC["qaT"] = P.dram("sc_qaT", [8, 2, 128, C.NTOK], BF16)
            SC["qrT"] = P.dram("sc_qrT", [8, 33, C.NTOK], BF16)
            SC["qiT"] = P.dram("sc_qiT", [8, 64, C.NTOK], BF16)
            SC["negT"] = P.dram("sc_negT", [C.NSEQ, NT_, 128, C.S], BF16)
        SC["ropeA"] = nc.dram_tensor("ropeA", [C.S, 256], F32, kind="ExternalInput").ap()
        SC["ropeB"] = nc.dram_tensor("ropeB", [C.S, 128], F32, kind="ExternalInput").ap()
        make_consts(P, K)
        P.flush()
        cur = x_in
        for li in C.layers:
            kind, j = li % 3, li // 3
            if C.ffn:
                ffn_phase(P, K, C, cur, xr, W['ffn1_norm'][li], W['ffn1_w_gu'][li], W['ffn1_w_down'][li], f"f1_{li}")
                cur = xr
            if C.mixers:
                if cur is x_in:
                    copy_phase(P, C, x_in, xr)
                    cur = xr
                if kind == 0:
                    gdn_phase(P, K, C, xr, W, li, j)
                elif kind == 1:
                    sb_phase(P, K, C, xr, W, li, j, SC)
                else:
                    dsa_phase(P, K, C, xr, W, li, j, SC)
            if C.ffn:
                ffn_phase(P, K, C, cur, xr, W['ffn2_norm'][li], W['ffn2_w_gu'][li], W['ffn2_w_down'][li], f"f2_{li}")
                cur = xr
        if C.final:
            final_phase(P, K, C, cur, y_out, W['final_norm'])
        else:
            copy_phase(P, C, cur, y_out)
        P.flush()
        C.n_ops = P.n_emitted
    return nc


def sl(a, b):
    return slice(a, b)


ALL = slice(None)


def run_interleaved(gens):
    alive = list(gens)
    while alive:
        for g in list(alive):
            try:
                next(g)
            except StopIteration:
                alive.remove(g)


def gdn_phase(P, K, C, xr, W, li, j):
    S = C.S
    NT = S // 128
    with P.phase():
        win = P.sbuf("win", [128, 8, 4112], BF16)
        wsrc = W['gdn_w_in'][j].rearrange("(kc p) f -> p kc f", p=128)
        pieces = [(i * 512, (i + 1) * 512) for i in range(8)] + [(4096, 4112)]
        for pi, (a, b) in enumerate(pieces):
            P.dma(win.k(pi, (ALL, ALL, sl(a, b))), V(wsrc[:, :, a:b], ("w", "gdn_in")), q=POOL)
        wout = P.sbuf("wout", [128, 8, 1024], BF16)
        P.dma(wout[:], V(W['gdn_w_out'][j].rearrange("(kc p) d -> p kc d", p=128), ("w", "gdn_out")), q=POOL)
        gb = P.sbuf("gb", [128, D_MODEL], F32)
        P.dma(gb[:], V(W['mix_norm'][li].partition_broadcast(128), ("w", "mixg")))
        gnb8 = P.sbuf("gnb8", [128, 1024], F32)
        for h in range(8):
            P.dma(gnb8[:, h * 128:(h + 1) * 128], V(W['gdn_norm'][j].partition_broadcast(128), ("w", "gn")))
        alb = P.sbuf("alb", [128, 8], F32)
        dtb = P.sbuf("dtb", [128, 8], F32)
        nea = P.sbuf("nea", [128, 8], F32)
        P.dma(alb[:], V(W['gdn_a_log'][j].partition_broadcast(128), ("w", "alog")))
        P.dma(dtb[:], V(W['gdn_dt_bias'][j].partition_broadcast(128), ("w", "dtb")))
        P.act(nea[:], alb[:], AF.Exp)
        P.ts(nea[:], nea[:], -1.0, ALU.mult)
        cw4 = P.sbuf("cw4", [96, 128], F32)
        P.dma(cw4[:], V(W['gdn_conv'][j].rearrange("i (c p) -> (i c) p", p=128), ("w", "conv")))
        cw = P.sbuf("cw", [128, 96], F32)
        diagw = P.sbuf("diagw", [128, 96, 128], BF16)
        B0 = P.psum("B0", [128, 1024], BF16)
        PB = [None] + [P.psum(f"PB{i}", [128, 512], F32) for i in range(1, 8)]
        P.transpose(PB[1][:, 0:96], cw4[:], K.ident_f[0:96, 0:96])
        P.copy(cw[:], PB[1][:, 0:96])
        for ci in range(96):
            P.ts(diagw.k(ci, (ALL, ci, ALL)), K.ident_f[:], cw[:, ci:ci + 1], ALU.mult)
        xt = [P.sbuf(f"xt{i}", [128, D_MODEL], F32) for i in range(2)]
        hT = [P.sbuf(f"hT{i}", [128, 8, 128], BF16) for i in range(2)]
        scr = (P.sbuf("junk", [128, D_MODEL], BF16), P.sbuf("ss", [128, 1], F32),
               P.sbuf("rstd", [128, 1], F32), P.sbuf("xn", [128, D_MODEL], BF16))
        cb = P.sbuf("cb", [128, 24, 131], BF16)
        sil = [P.sbuf(f"sil{i}", [128, 512], F32) for i in range(2)]
        sqb = [P.sbuf(f"sqb{i}", [128, 512], BF16) for i in range(2)]
        rs = [P.sbuf(f"rs{i}", [128, 512], F32) for i in range(2)]
        qkT = P.sbuf("qkT", [128, 16, 128], BF16)
        vT = P.sbuf("vT", [128, 8, 128], BF16)
        gz = P.sbuf("gz", [128, 1024], F32)
        sm = {n: P.sbuf(n, [128, 8], F32) for n in ("eb", "beta", "nbeta", "ta", "g", "egc", "gtot", "dk", "kdsc")}
        gc = P.sbuf("gc", [128, 16], F32)
        S_f = P.sbuf("S_f", [128, 8, 128], F32)
        S_b = P.sbuf("S_b", [128, 8, 128], BF16)
        gh = [P.sbuf(f"gh{i}", [128, 128], F32) for i in range(4)]
        E2 = [P.sbuf(f"E2{i}", [128, 256], F32) for i in range(4)]
        EMi = [P.sbuf(f"EMi{i}", [128, 128], F32) for i in range(4)]
        EMs = [P.sbuf(f"EMs{i}", [128, 128], F32) for i in range(4)]
        WW = [[P.sbuf(f"WW{p}{i}", [128, 256], F32) for i in range(2)] for p in range(4)]
        Rm = [P.sbuf(f"R{p}", [128, 128], F32) for p in range(4)]
        Rb = [P.sbuf(f"Rb{p}", [128, 128], BF16) for p in range(4)]
        vtm = [P.sbuf(f"vtm{p}", [128, 128], BF16) for p in range(4)]
        Xk = [P.sbuf(f"Xk{h}", [128, 128], BF16) for h in range(8)]
        kdec = [P.sbuf(f"kdec{h}", [128, 128], BF16) for h in range(8)]
        qkd = [P.sbuf(f"qkd{h}", [128, 128], BF16) for h in range(8)]
        qdT = [P.sbuf(f"qdT{h}", [128, 128], BF16) for h in range(8)]
        uinb = [P.sbuf(f"uinb{h}", [128, 128], F32) for h in range(8)]
        wT = [P.sbuf(f"wT{h}", [128, 128], BF16) for h in range(8)]
        uu = [P.sbuf(f"u{h}", [128, 128], BF16) for h in range(8)]
        ssq = [P.sbuf(f"ssq{h}", [128, 1], F32) for h in range(8)]
        rsq = [P.sbuf(f"rsq{h}", [128, 1], F32) for h in range(8)]
        junk2 = [P.sbuf(f"junk2{i}", [128, 128], BF16) for i in range(2)]
        og = P.sbuf("og", [128, 1024], BF16)
        ogT = P.sbuf("ogT", [128, 8, 128], BF16)

        def cs(i, n=1):
            return sl(i * 128, (i + n) * 128)

        for sq_i in range(C.NSEQ):
            for h in range(8):
                P.memset(S_f.k(h, (ALL, h, ALL)), 0.0)
                P.memset(S_b.k(h, (ALL, h, ALL)), 0.0)
            for gq in range(6):
                P.memset(cb.k(gq, (ALL, sl(gq * 4, gq * 4 + 4), sl(0, 3))), 0.0)
            for t in range(NT):
                r0 = sq_i * S + t * 128
                ti = r0 // 128
                xs = xt[t % 2]
                h_ = hT[t % 2]
                rows = (sl(r0, r0 + 128), ALL)
                P.dma(xs[:], xr.k(("r", ti), rows))
                norm_T(P, K, xs[:], gb[:], h_[:, :, :], scr, B0)

                def stage_a(gq):
                    pp = PB[1 + gq % 2]
                    pc = PB[3 + gq % 2]
                    for cc in range(4):
                        c = gq * 4 + cc
                        for kc in range(8):
                            P.mm(pp[:, cs(cc)], win.k(c // 4, (ALL, kc, cs(c))), h_[:, kc, :],
                                 start=(kc == 0), stop=(kc == 7))
                    cbg = cb.k(gq, (ALL, sl(gq * 4, gq * 4 + 4), sl(3, 131)))
                    P.copy(cbg, pp.v(pp.base()[:, :].rearrange("p (c t) -> p c t", t=128)), eng=ACT)
                    yield
                    for cc in range(4):
                        c = gq * 4 + cc
                        for i in range(4):
                            P.mm(pc[:, cs(cc)], diagw.k(i * 24 + c, (ALL, i * 24 + c, ALL)),
                                 cb.k(gq, (ALL, c, sl(i, i + 128))), start=(i == 0), stop=(i == 3))
                    if gq < 4:
                        s_ = sil[gq % 2]
                        P.act(s_[:], pc[:], AF.Silu)
                        P.tt(sqb[gq % 2][:], s_[:], s_[:], ALU.mult)
                    else:
                        P.act(vT.v(vT.base()[:, (gq - 4) * 4:(gq - 4) * 4 + 4, :]),
                              pc.v(pc.base()[:, :].rearrange("p (c t) -> p c t", t=128)), AF.Silu)
                    P.copy(cb.k(gq, (ALL, sl(gq * 4, gq * 4 + 4), sl(0, 3))),
                           cb.k(gq, (ALL, sl(gq * 4, gq * 4 + 4), sl(128, 131))), eng=POOL)
                    yield
                    if gq < 4:
                        for cc in range(4):
                            P.mm(PB[5][:, cs(cc)], K.ones_b[:], sqb[gq % 2][:, cs(cc)])
                        r_ = rs[gq % 2]
                        if gq < 2:
                            P.rsqrt(r_[:], PB[5][:], 128.0, 128.0 * EPS)
                        else:
                            P.rsqrt(r_[:], PB[5][:], 1.0, EPS)
                        P.tt(qkT.v(qkT.base()[:, gq * 4:gq * 4 + 4, :]),
                             s_.v(s_.base()[:, :].rearrange("p (c t) -> p c t", t=128)),
                             r_.v(r_.base()[:, :].rearrange("p (c t) -> p c t", t=128)), ALU.mult)
                    yield

                gens = [stage_a(gq) for gq in range(6)]
                for step in range(6 + 2):
                    for gq in range(6):
                        if 0 <= step - gq < 3:
                            next(gens[gq])
                for hf in range(2):
                    for kc in range(8):
                        P.mm(PB[1 + hf][:], h_[:, kc, :], win.k(6 + hf, (ALL, kc, sl(3072 + hf * 512, 3072 + (hf + 1) * 512))),
                             start=(kc == 0), stop=(kc == 7))
                    P.act(gz[:, hf * 512:(hf + 1) * 512], PB[1 + hf][:], AF.Silu)
                P.tt(gz[:], gz[:], gnb8[:], ALU.mult)
                for kc in range(8):
                    P.mm(PB[6][:, 0:16], h_[:, kc, :], win.k(8, (ALL, kc, sl(4096, 4112))), start=(kc == 0), stop=(kc == 7))
                P.act(sm["eb"][:], PB[6][:, 0:8], AF.Exp, scale=-1.0)
                P.tt(sm["ta"][:], PB[6][:, 8:16], dtb[:], ALU.add)
                P.ts(sm["eb"][:], sm["eb"][:], 1.0, ALU.add)
                P.recip(sm["beta"][:], sm["eb"][:])
                P.ts(sm["nbeta"][:], sm["beta"][:], -1.0, ALU.mult)
                P.act(sm["ta"][:], sm["ta"][:], AF.Exp)
                P.act(sm["ta"][:], sm["ta"][:], AF.Ln, bias=1.0)
                P.tt(sm["g"][:], sm["ta"][:], nea[:], ALU.mult)
                P.mm(PB[7][:, 0:8], K.m_ge[:], sm["g"][:])
                P.mm(PB[7][:, 8:16], K.ones_f[:], sm["g"][:])
                P.copy(gc[:], PB[7][:, 0:16])
                P.act(sm["egc"][:], gc[:, 0:8], AF.Exp)
                P.act(sm["gtot"][:], gc[:, 8:16], AF.Exp)
                P.tt(sm["dk"][:], gc[:, 8:16], gc[:, 0:8], ALU.subtract)
                P.act(sm["kdsc"][:], sm["dk"][:], AF.Exp)
                beta, nbeta, g = sm["beta"], sm["nbeta"], sm["g"]

                def head_prep(h):
                    p = h % 4
                    bD, bW, bR, bU = PB[1 + h % 2], PB[3 + h % 2], PB[5 + h % 2], PB[7]
                    kTh = qkT[:, 8 + h, :]
                    qTh = qkT[:, h, :]
                    P.ts(gh[p][:], K.m_ge[:], g[:, h:h + 1], ALU.mult)
                    P.mm(bD[:, 0:128], K.ones_f[:], gh[p][:])
                    P.mm(bD[:, 128:256], K.m_lt[:], gh[p][:])
                    P.mm(bD[:, 256:384], kTh, kTh)
                    P.mm(bD[:, 384:512], kTh, qTh)
                    P.act(E2[p][:], bD[:, 0:256], AF.Exp)
                    P.tt(EMi[p][:], E2[p][:, 128:256], K.m_ge[:], ALU.mult)
                    P.tt(EMs[p][:], E2[p][:, 128:256], K.m_gt[:], ALU.mult)
                    W_ = WW[p]
                    R_ = Rm[p]
                    P.stt(W_[0][:, 0:128], bD[:, 256:384], nbeta[:, h:h + 1], EMs[p][:], ALU.mult, ALU.mult)
                    P.tt(qkd[h][:], bD[:, 384:512], EMi[p][:], ALU.mult)
                    P.tt(qdT[h][:], qTh, E2[p][:, 0:128], ALU.mult)
                    yield
                    P.transpose(bW[:, 128:256], W_[0][:, 0:128], K.ident_f[:])
                    P.copy(W_[0][:, 128:256], bW[:, 128:256], eng=ACT)
                    P.tt(R_[:], W_[0][:, 0:128], K.ident_f[:], ALU.add)
                    yield
                    for m in range(1, 7):
                        a, b = (m - 1) % 2, m % 2
                        Wa, WTa = W_[a][:, 0:128], W_[a][:, 128:256]
                        if m < 6:
                            P.mm(bW[:, 0:128], WTa, Wa)
                        P.mm(bW[:, 128:256], Wa, WTa)
                        if m < 6:
                            P.copy(W_[b][:], bW[:, 0:256], eng=(ACT if m % 2 else DVE))
                        else:
                            P.copy(W_[b][:, 128:256], bW[:, 128:256], eng=ACT)
                        yield
                        P.mm(bR[:, 0:128], W_[b][:, 128:256], R_[:])
                        P.tt(R_[:], bR[:, 0:128], R_[:], ALU.add)
                        yield
                    P.copy(Rb[p][:], R_[:], eng=ACT)
                    P.transpose(B0[:, 0:128], kTh, K.ident_b[:])
                    P.transpose(B0[:, 128:256], vT[:, h, :], K.ident_b[:])
                    P.ts(Xk[h][:], B0[:, 0:128], sm["egc"][:, h:h + 1], ALU.mult)
                    P.ts(kdec[h][:], B0[:, 0:128], sm["kdsc"][:, h:h + 1], ALU.mult)
                    P.copy(vtm[p][:], B0[:, 128:256], eng=ACT)
                    yield
                    P.mm(bU[:, 0:128], Rb[p][:], vtm[p][:])
                    P.mm(bU[:, 128:256], Xk[h][:], Rb[p][:])
                    P.ts(uinb[h][:], bU[:, 0:128], beta[:, h:h + 1], ALU.mult)
                    P.copy(wT[h][:], bU[:, 128:256], eng=ACT)
                    yield

                for h0 in range(0, 8, 4):
                    run_interleaved([head_prep(h0 + q) for q in range(4)])

                def recur(hg):
                    hs = [hg * 4 + q for q in range(4)]
                    bT, bO = PB[1 + 2 * hg], PB[2 + 2 * hg]
                    for q, h in enumerate(hs):
                        P.mm(bT[:, cs(q)], wT[h][:], S_b.k(h, (ALL, h, ALL)))
                    yield
                    for q, h in enumerate(hs):
                        P.stt(uu[h][:], bT[:, cs(q)], nbeta[:, h:h + 1], uinb[h][:], ALU.mult, ALU.add)
                    yield
                    for q, h in enumerate(hs):
                        P.mm(bO[:, cs(q)], qdT[h][:], S_b.k(h, (ALL, h, ALL)), start=True, stop=False)
                        P.mm(bO[:, cs(q)], qkd[h][:], uu[h][:], start=False, stop=True)
                    for q, h in enumerate(hs):
                        P.mm(bT[:, cs(q)], kdec[h][:], uu[h][:])
                    yield
                    for q, h in enumerate(hs):
                        Sfh = S_f.k(h, (ALL, h, ALL))
                        P.stt(Sfh, Sfh, sm["gtot"][:, h:h + 1], bT[:, cs(q)], ALU.mult, ALU.add)
                        P.copy(S_b.k(h, (ALL, h, ALL)), Sfh, eng=POOL)
                    yield
                    for q, h in enumerate(hs):
                        P.act(junk2[hg][:], bO[:, cs(q)], AF.Square, accum_out=ssq[h][:])
                    yield
                    for q, h in enumerate(hs):
                        P.rsqrt(rsq[h][:], ssq[h][:], 1.0 / 128, EPS)
                    yield
                    for q, h in enumerate(hs):
                        P.stt(og.k(h, (ALL, cs(h))), bO[:, cs(q)], rsq[h][:], gz[:, cs(h)], ALU.mult, ALU.mult)
                    yield

                run_interleaved([recur(0), recur(1)])
                for hc in range(8):
                    P.transpose(B0[:, cs(hc)], og.k(hc, (ALL, cs(hc))), K.ident_b[:])
                P.copy(ogT[:, :, :], B0.v(B0.base()[:, :].rearrange("p (c t) -> p c t", t=128)), eng=ACT)
                for half in range(2):
                    bo = PB[5 + half]
                    for hc in range(8):
                        P.mm(bo[:], ogT[:, hc, :], wout[:, hc, half * 512:(half + 1) * 512], start=(hc == 0), stop=(hc == 7))
                    P.tt(xs[:, half * 512:(half + 1) * 512], bo[:], xs[:, half * 512:(half + 1) * 512], ALU.add)
                P.dma(xr.k(("r", ti), rows), xs[:], q=POOL)


_uid = [0]


def dv(t, ap):
    _uid[0] += 1
    return V(ap, (t.id, ("u", _uid[0]), False))


def outproj_phase(P, K, C, xr, w_out, os_):
    with P.phase():
        wout = P.sbuf("wout", [128, 8, 1024], BF16)
        P.dma(wout[:], V(w_out.rearrange("(kc p) d -> p kc d", p=128), ("w", "wout")), q=POOL)
        PB = [P.psum(f"PO{i}", [128, 512], F32) for i in range(4)]
        oT = [P.sbuf(f"oT{i}", [128, 8, 512], BF16) for i in range(2)]
        xt = [P.sbuf(f"xt{i}", [128, D_MODEL], F32) for i in range(4)]
        nb = 0
        for ti in range(C.NTOK // 512):
            r0 = ti * 512
            o_ = oT[ti % 2]
            P.dma(o_[:], dv(os_, os_.base()[:, :, r0:r0 + 512].rearrange("h p t -> p h t")))
            for s_ in range(4):
                x = xt[s_]
                rows = (sl(r0 + s_ * 128, r0 + (s_ + 1) * 128), ALL)
                P.dma(x[:], xr.k(("r", ti * 4 + s_), rows))
                for half in range(2):
                    pb = PB[nb % 4]
                    nb += 1
                    for h in range(8):
                        P.mm(pb[:], o_[:, h, s_ * 128:(s_ + 1) * 128], wout[:, h, half * 512:(half + 1) * 512],
                             start=(h == 0), stop=(h == 7))
                    P.tt(x[:, half * 512:(half + 1) * 512], pb[:], x[:, half * 512:(half + 1) * 512], ALU.add)
                P.dma(xr.k(("r", ti * 4 + s_), rows), x[:], q=POOL)


def sb_phase(P, K, C, xr, W, li, j, SC):
    S = C.S
    NT = S // 128
    NG = S // 512
    qs, ks, vs, os_ = SC["qs"], SC["ks"], SC["vs"], SC["os"]
    scale = 128.0 ** -0.5

    def cs(i, n=1):
        return sl(i * 128, (i + n) * 128)

    with P.phase():
        win = P.sbuf("win", [128, 8, 3072], BF16)
        wsrc = W['sb_w_in'][j].rearrange("(kc p) f -> p kc f", p=128)
        for pi in range(6):
            P.dma(win.k(pi, (ALL, ALL, sl(pi * 512, (pi + 1) * 512))), V(wsrc[:, :, pi * 512:(pi + 1) * 512], ("w", "sb_in")), q=POOL)
        gb = P.sbuf("gb", [128, D_MODEL], F32)
        P.dma(gb[:], V(W['mix_norm'][li].partition_broadcast(128), ("w", "mixg")))
        B0 = P.psum("B0", [128, 1024], BF16)
        PB = [None] + [P.psum(f"PB{i}", [128, 512], F32) for i in range(1, 8)]
        xt = [P.sbuf(f"xt{i}", [128, D_MODEL], F32) for i in range(4)]
        hT = [P.sbuf(f"hT{i}", [128, 8, 512], BF16) for i in range(2)]
        scr = (P.sbuf("junk", [128, D_MODEL], BF16), P.sbuf("ss", [128, 1], F32),
               P.sbuf("rstd", [128, 1], F32), P.sbuf("xn", [128, D_MODEL], BF16))
        qst = [P.sbuf(f"qst{i}", [128, 512], BF16) for i in range(4)]
        vst = [P.sbuf(f"vst{i}", [128, 1024], BF16) for i in range(2)]
        for ti in range(C.NTOK // 512):
            r0 = ti * 512
            h_ = hT[ti % 2]
            for s_ in range(4):
                rows = (sl(r0 + s_ * 128, r0 + (s_ + 1) * 128), ALL)
                P.dma(xt[s_][:], xr.k(("r", ti * 4 + s_), rows))
                norm_T(P, K, xt[s_][:], gb[:], h_[:, :, s_ * 128:(s_ + 1) * 128], scr, B0)
            for c in range(16):
                pb = PB[1 + c % 4]
                for kc in range(8):
                    P.mm(pb[:], win.k(c // 4, (ALL, kc, cs(c))), h_[:, kc, :], start=(kc == 0), stop=(kc == 7))
                st = qst[c % 4]
                if c < 8:
                    P.act(st[:], pb[:], AF.Copy, scale=scale)
                    P.dma(qs.k(("q", c, ti), (c, ALL, sl(r0, r0 + 512))), st[:], q=SP)
                else:
                    P.copy(st[:], pb[:], eng=DVE)
                    P.dma(ks.k(("k", c - 8, ti), (c - 8, ALL, sl(r0, r0 + 512))), st[:], q=SP)
            for s_ in range(4):
                vv = vst[s_ % 2]
                for half in range(2):
                    pb = PB[5 + half]
                    for kc in range(8):
                        P.mm(pb[:], h_[:, kc, s_ * 128:(s_ + 1) * 128],
                             win.k(4 + half, (ALL, kc, sl(2048 + half * 512, 2048 + (half + 1) * 512))),
                             start=(kc == 0), stop=(kc == 7))
                    P.copy(vv[:, half * 512:(half + 1) * 512], pb[:], eng=(ACT if half else DVE))
                P.dma(vs.k(("v", ti * 4 + s_), (sl(r0 + s_ * 128, r0 + (s_ + 1) * 128), ALL)), vv[:], q=SP)

    with P.phase():
        PA = [P.psum(f"PA{i}", [128, 512], F32) for i in range(6)]
        negtri_f = P.sbuf("negtri_f", [128, 128], F32)
        negtri = P.sbuf("negtri", [128, 128], BF16)
        negones = P.sbuf("negones", [128, 128], BF16)
        P.tt(negtri_f[:], K.m_lt[:], K.ident_f[:], ALU.add)
        P.ts(negtri[:], negtri_f[:], -1.0, ALU.mult)
        P.memset(negones[:], -1.0)
        maskf = [P.sbuf(f"maskf{r}", [128, 512], F32) for r in range(4)]
        maskb = [P.sbuf(f"maskb{r}", [128, 512], BF16) for r in range(4)]
        maskf_ = maskf
        maskf = maskb
        for r in range(4):
            P.memset(maskf_[r][:], 1.0)
            P.op(POOL, (lambda t, r: lambda e: e.affine_select(t[:].ap, t[:].ap, [[1, 512]], ALU.is_gt, 0.0,
                                                               base=-r * 128, channel_multiplier=-1))(maskf_[r], r),
                 [maskf_[r][:]], [maskf_[r][:]])
            P.copy(maskb[r][:], maskf_[r][:])
        hd = [[{"k": P.sbuf(f"kT{a}{b}", [128, S], BF16), "q": P.sbuf(f"qT{a}{b}", [128, S], BF16),
                "v": P.sbuf(f"v{a}{b}", [128, NT, 128], BF16)} for b in range(2)] for a in range(2)]
        wk = [{"e": [P.sbuf(f"e{b}{i}", [128, 512], F32) for i in range(2)],
               "sp": [P.sbuf(f"sp{b}{i}", [128, 512], BF16) for i in range(2)],
               "accb": P.sbuf(f"accb{b}", [128, 512], BF16),
               "att": [P.sbuf(f"att{b}{i}", [128, 512], BF16) for i in range(2)],
               "acc": P.sbuf(f"acc{b}", [128, 512], F32),
               "ost": P.sbuf(f"ost{b}", [128, 512], BF16),
               "banks": (PA[3 * b], PA[3 * b + 1], PA[3 * b + 2])} for b in range(2)]

        def load_pair(sq_i, hp, a):
            for b in range(2):
                h = 2 * hp + b
                d = hd[a][b]
                cols = sl(sq_i * S, (sq_i + 1) * S)
                P.dma(d["k"][:], ks.k(("kall",), (h, ALL, cols)))
                P.dma(d["q"][:], qs.k(("qall",), (h, ALL, cols)))
                P.dma(d["v"][:], vs.v(vs.base()[sq_i * S:(sq_i + 1) * S, h * 128:(h + 1) * 128].rearrange("(t p) d -> p t d", p=128), ("vall",)))

        def stream(sq_i, h, g, d, w):
            bA, bB, bO = w["banks"]
            kT, qT, v = d["k"], d["q"], d["v"]
            qcols = sl(g * 512, (g + 1) * 512)
            nkb = 4 * g + 4
            kbs = list(range(nkb - 1, -1, -1))
            P.mm(bA[:], kT[:, cs(kbs[0])], qT[:, qcols])
            yield
            for idx, kb in enumerate(kbs):
                first = idx == 0
                last = kb == 0
                r = kb - 4 * g
                e, sp, att = w["e"][idx % 2], w["sp"][idx % 2], w["att"][idx % 2]
                P.act(e[:], bA[:], AF.Exp)
                P.act(sp[:], e[:], AF.Ln, bias=1.0)
                if r >= 0:
                    P.tt(sp[:], sp[:], maskf[r][:], ALU.mult, eng=POOL)
                yield
                P.mm(bB[:], kT[:, cs(kb)], qT[:, qcols], start=True, stop=False)
                P.mm(bB[:], negtri[:], sp[:], start=False, stop=first)
                if not first:
                    P.mm(bB[:], negones[:], w["accb"][:], start=False, stop=True)
                if not last:
                    P.mm(bA[:], kT[:, cs(kbs[idx + 1])], qT[:, qcols])
                yield
                P.act(att[:], bB[:], AF.Exp)
                if r >= 0:
                    P.tt(att[:], att[:], maskb[r][:], ALU.mult)
                if not last:
                    if first:
                        P.copy(w["acc"][:], sp[:])
                    else:
                        P.tt(w["acc"][:], w["acc"][:], sp[:], ALU.add)
                    P.copy(w["accb"][:], w["acc"][:])
                yield
                P.mm(bO[:], v[:, kb, :], att[:], start=first, stop=last)
                yield
            P.copy(w["ost"][:], bO[:])
            P.dma(os_.k(("o", h, sq_i, g), (h, ALL, sl(sq_i * S + g * 512, sq_i * S + (g + 1) * 512))), w["ost"][:], q=POOL)

        pairs = [(sq_i, hp) for sq_i in range(C.NSEQ) for hp in range(4)]
        load_pair(pairs[0][0], pairs[0][1], 0)
        for pi, (sq_i, hp) in enumerate(pairs):
            a = pi % 2
            if pi + 1 < len(pairs):
                load_pair(pairs[pi + 1][0], pairs[pi + 1][1], 1 - a)
            for g in range(NG):
                run_interleaved([stream(sq_i, 2 * hp + b, g, hd[a][b], wk[b]) for b in range(2)])

    outproj_phase(P, K, C, xr, W['sb_w_out'][j], os_)


def rope_tm(P, x1, x2, cos, sin, o1, o2, t1, t2):
    P.tt(t1, x1, cos, ALU.mult)
    P.tt(t2, x2, sin, ALU.mult)
    P.tt(o1, t1, t2, ALU.subtract)
    P.tt(t1, x2, cos, ALU.mult)
    P.tt(t2, x1, sin, ALU.mult)
    P.tt(o2, t1, t2, ALU.add)


def dsa_phase(P, K, C, xr, W, li, j, SC):
    S = C.S
    NT = S // 128
    NG = S // 512
    topk = C.topk
    NIT = 16
    att_scale = 128.0 ** -0.5
    widx_scale = (8.0 ** -0.5) * (64.0 ** -0.5)
    NEG = -30000.0
    kl, kvtm, kiT, wiS = SC["kl"], SC["kvtm"], SC["kiT"], SC["wi"]
    qaT, qrT, qiT, negT, os_ = SC["qaT"], SC["qrT"], SC["qiT"], SC["negT"], SC["os"]
    ropeA, ropeB = SC["ropeA"], SC["ropeB"]

    def cs(i, n=1):
        return sl(i * 128, (i + n) * 128)

    with P.phase():
        B0 = P.psum("B0", [128, 1024], BF16)
        PB = [None] + [P.psum(f"PB{i}", [128, 512], F32) for i in range(1, 8)]
        win = P.sbuf("win", [128, 8, 744], BF16)
        P.dma(win[:], V(W['dsa_w_in'][j].rearrange("(kc p) f -> p kc f", p=128), ("w", "dsa_in")), q=POOL)
        wuq4 = W['dsa_w_uq'][j].rearrange("(kc p) (h d) -> p kc h d", p=128, d=128)
        wuq_r = P.sbuf("wuq_r", [128, 3, 8, 32], BF16)
        for kc in range(3):
            P.dma(wuq_r[:, kc, :, :], V(wuq4[:, kc, :, 0:32], ("w", "uq_r")), q=POOL)
        wuq_n = P.sbuf("wuq_n", [128, 3, 8, 96], BF16)
        for kc in range(3):
            P.dma(wuq_n[:, kc, :, :], V(wuq4[:, kc, :, 32:128], ("w", "uq_n")), q=POOL)
        wuk = P.sbuf("wuk", [128, 2, 768], BF16)
        P.dma(wuk[:], V(W['dsa_w_uk'][j].rearrange("(cc p) f -> p cc f", p=128), ("w", "uk")), q=POOL)
        wqidx = P.sbuf("wqidx", [128, 3, 512], BF16)
        P.dma(wqidx[:], V(W['dsa_w_qidx'][j].rearrange("(kc p) f -> p kc f", p=128), ("w", "qidx")), q=POOL)
        gb = P.sbuf("gb", [128, D_MODEL], F32)
        P.dma(gb[:], V(W['mix_norm'][li].partition_broadcast(128), ("w", "mixg")))
        cqg = P.sbuf("cqg", [128, 384], F32)
        ckvg = P.sbuf("ckvg", [128, 256], F32)
        kidxg = P.sbuf("kidxg", [128, 64], F32)
        P.dma(cqg[:], V(W['dsa_cq_norm'][j].partition_broadcast(128), ("w", "cqg")))
        P.dma(ckvg[:], V(W['dsa_ckv_norm'][j].partition_broadcast(128), ("w", "ckvg")))
        P.dma(kidxg[:], V(W['dsa_kidx_norm'][j].partition_broadcast(128), ("w", "kidxg")))
        AT = P.sbuf("AT", [96, 8, 384], BF16)
        BT = P.sbuf("BT", [96, 8, 256], BF16)
        Wabs = P.sbuf("Wabs", [128, 3, 8, 256], BF16)
        for h in range(8):
            for kc in range(3):
                P.transpose(B0[0:96, cs(kc)], wuq_n[:, kc, h, :], K.ident_b[:])
            for cc in range(2):
                P.transpose(B0[0:96, cs(3 + cc)], wuk[:, cc, h * 96:(h + 1) * 96], K.ident_b[:])
            P.copy(AT[:, h, :], B0[0:96, 0:384], eng=ACT)
            P.copy(BT[:, h, :], B0[0:96, 384:640])
        for h in range(8):
            for kc in range(3):
                n = h * 3 + kc
                pb = PB[1 + n % 4]
                P.mm(pb[:, 0:256], AT[:, h, cs(kc)], BT[:, h, :])
                P.copy(Wabs[:, kc, h, :], pb[:, 0:256], eng=(ACT if n % 2 else DVE))
        xt = [P.sbuf(f"xt{i}", [128, D_MODEL], F32) for i in range(2)]
        hT = [P.sbuf(f"hT{i}", [128, 8, 128], BF16) for i in range(2)]
        scr = (P.sbuf("junk", [128, D_MODEL], BF16), P.sbuf("ss", [128, 1], F32),
               P.sbuf("rstd", [128, 1], F32), P.sbuf("xn", [128, D_MODEL], BF16))
        junk = scr[0]
        ra = [P.sbuf(f"ra{i}", [128, 256], F32) for i in range(2)]
        rb = [P.sbuf(f"rb{i}", [128, 128], F32) for i in range(2)]
        pj = P.sbuf("pj", [128, 744], F32)
        sA = {n: P.sbuf(n, [128, 1], F32) for n in ("ssA", "rsA", "ssB", "rsB", "ssC", "rsC", "kn2a", "kn2b", "kn2", "rm", "nkmax")}
        km1 = P.sbuf("km1", [1, 1], F32)
        cqn = P.sbuf("cqn", [128, 384], BF16)
        cqT = P.sbuf("cqT", [128, 3, 128], BF16)
        ckv = P.sbuf("ckv", [128, 256], BF16)
        ckT = P.sbuf("ckT", [128, 2, 128], BF16)
        kra = P.sbuf("kra", [128, 33], F32)
        krb = P.sbuf("krb", [128, 33], BF16)
        krT = P.sbuf("krT", [33, 128], BF16)
        t1 = P.sbuf("t1", [128, 128], F32)
        t2 = P.sbuf("t2", [128, 128], F32)
        kin = P.sbuf("kin", [128, 64], F32)
        kib = P.sbuf("kib", [128, 64], BF16)
        kiTt = P.sbuf("kiTt", [64, 128], BF16)
        wit = P.sbuf("wit", [128, 8], F32)
        qab = P.sbuf("qab", [128, 16, 128], BF16)
        sqa = P.sbuf("sqa", [128, 16, 128], BF16)
        qr = P.sbuf("qr", [128, 8, 32], F32)
        qra = P.sbuf("qra", [128, 8, 33], F32)
        qrb = P.sbuf("qrb", [128, 8, 33], BF16)
        qrTt = P.sbuf("qrTt", [33, 8, 128], BF16)
        t3 = P.sbuf("t3", [128, 8, 32], F32)
        qr2 = P.sbuf("qr2", [128, 8], F32)
        qn = P.sbuf("qn", [128, 8], F32)
        qi = P.sbuf("qi", [128, 8, 64], F32)
        qib = P.sbuf("qib", [128, 8, 64], BF16)
        qiTt = P.sbuf("qiTt", [64, 8, 128], BF16)
        P.memset(kra[:, 32:33], 1.0)

        def v3(t, a, b, d):
            return t.v(t.base()[:, a:b].rearrange("p (h d) -> p h d", d=d))

        for sq_i in range(C.NSEQ):
            P.memset(sA["rm"][:], 0.0)
            for t in range(NT):
                r0 = sq_i * S + t * 128
                ti = r0 // 128
                toks = sl(r0, r0 + 128)
                xs, h_ = xt[t % 2], hT[t % 2]
                ra_, rb_ = ra[t % 2], rb[t % 2]
                P.dma(xs[:], xr.k(("r", ti), (toks, ALL)))
                P.dma(ra_[:], V(ropeA[t * 128:(t + 1) * 128, :], ("w", "ropeA")))
                P.dma(rb_[:], V(ropeB[t * 128:(t + 1) * 128, :], ("w", "ropeB")))
                norm_T(P, K, xs[:], gb[:], h_[:, :, :], scr, B0)
                for kc in range(8):
                    P.mm(PB[1][:], h_[:, kc, :], win[:, kc, 0:512], start=(kc == 0), stop=(kc == 7))
                for kc in range(8):
                    P.mm(PB[2][:, 0:232], h_[:, kc, :], win[:, kc, 512:744], start=(kc == 0), stop=(kc == 7))
                P.copy(pj[:, 0:512], PB[1][:], eng=ACT)
                P.copy(pj[:, 512:744], PB[2][:, 0:232])
                P.act(junk[:, 0:384], pj[:, 0:384], AF.Square, accum_out=sA["ssA"][:])
                P.rsqrt(sA["rsA"][:], sA["ssA"][:], 1.0 / 384, EPS)
                P.stt(cqn[:], pj[:, 0:384], sA["rsA"][:], cqg[:], ALU.mult, ALU.mult)
                for cc in range(3):
                    P.transpose(B0[:, cs(cc)], cqn[:, cs(cc)], K.ident_b[:])
                P.copy(cqT[:, :, :], B0.v(B0.base()[:, 0:384].rearrange("p (c t) -> p c t", t=128)), eng=ACT)
                P.act(junk[:, 0:256], pj[:, 384:640], AF.Square, accum_out=sA["ssB"][:])
                P.rsqrt(sA["rsB"][:], sA["ssB"][:], 1.0 / 256, EPS)
                P.stt(ckv[:], pj[:, 384:640], sA["rsB"][:], ckvg[:], ALU.mult, ALU.mult)
                P.act(junk[:, 0:256], ckv[:], AF.Square, accum_out=sA["kn2a"][:])
                P.dma(dv(kvtm, kvtm.base()[toks, :]), ckv[:])
                for cc in range(2):
                    P.transpose(B0[:, cs(cc)], ckv[:, cs(cc)], K.ident_b[:])
                P.copy(ckT[:, :, :], B0.v(B0.base()[:, 0:256].rearrange("p (c t) -> p c t", t=128)))
                P.dma(dv(kl, kl.base()[0:2, :, toks].rearrange("c p t -> p c t")), ckT[:])
                rope_tm(P, pj[:, 640:656], pj[:, 656:672], ra_[:, 0:16], ra_[:, 128:144],
                        kra[:, 0:16], kra[:, 16:32], t1[:, 0:16], t2[:, 0:16])
                P.act(junk[:, 0:32], kra[:, 0:32], AF.Square, accum_out=sA["kn2b"][:])
                P.tt(sA["kn2"][:], sA["kn2a"][:], sA["kn2b"][:], ALU.add)
                P.tt(sA["rm"][:], sA["rm"][:], sA["kn2"][:], ALU.max)
                P.copy(krb[:], kra[:], eng=POOL)
                P.transpose(B0[0:33, 0:128], krb[:], K.ident_b[:])
                P.copy(krT[:], B0[0:33, 0:128])
                P.dma(dv(kl, kl.base()[2, 0:33, toks]), krT[:])
                P.act(junk[:, 0:64], pj[:, 672:736], AF.Square, accum_out=sA["ssC"][:])
                P.rsqrt(sA["rsC"][:], sA["ssC"][:], 1.0 / 64, EPS)
                P.stt(kin[:], pj[:, 672:736], sA["rsC"][:], kidxg[:], ALU.mult, ALU.mult)
                P.copy(kib[:], kin[:], eng=POOL)
                rope_tm(P, kin[:, 0:8], kin[:, 8:16], rb_[:, 0:8], rb_[:, 64:72],
                        kib[:, 0:8], kib[:, 8:16], t1[:, 0:8], t2[:, 0:8])
                P.transpose(B0[0:64, 0:128], kib[:], K.ident_b[:])
                P.copy(kiTt[:], B0[0:64, 0:128], eng=ACT)
                P.dma(dv(kiT, kiT.base()[:, toks]), kiTt[:])
                P.ts(wit[:], pj[:, 736:744], widx_scale, ALU.mult)
                P.dma(dv(wiS, wiS.base()[toks, :]), wit[:])
                P.transpose(PB[3][0:1, 0:128], sA["rm"][:], K.ident_f[:])
                P.reduce(km1[:], PB[3][0:1, 0:128], ALU.max)
                P.act(km1[:], km1[:], AF.Ln, bias=1e-30)
                P.act(km1[:], km1[:], AF.Exp, scale=0.5)
                P.ts(km1[:], km1[:], -1.0, ALU.mult)
                P.mm(PB[3][:, 128:129], K.ones_f[0:1, 0:128], km1[0:1, 0:1])
                P.copy(sA["nkmax"][:], PB[3][:, 128:129])
                for b4 in range(4):
                    pb = PB[4 + b4 % 2]
                    for i in range(4):
                        idx = b4 * 4 + i
                        h, cc = idx // 2, idx % 2
                        for kc in range(3):
                            P.mm(pb[:, cs(i)], Wabs[:, kc, h, cs(cc)], cqT[:, kc, :], start=(kc == 0), stop=(kc == 2))
                    P.copy(qab[:, b4 * 4:(b4 + 1) * 4, :], pb.v(pb.base()[:, :].rearrange("p (c t) -> p c t", t=128)),
                           eng=(ACT if b4 % 2 else DVE))
                P.dma(dv(qaT, qaT.base()[:, :, :, toks].rearrange("h c p t -> p (h c) t")), qab[:])
                P.tt(sqa[:], qab[:], qab[:], ALU.mult, eng=POOL)
                for idx in range(16):
                    h, cc = idx // 2, idx % 2
                    P.mm(PB[6][:, h:h + 1], sqa[:, idx, :], K.ones_b[:, 0:1], start=(cc == 0), stop=(cc == 1))
                for kc in range(3):
                    P.mm(PB[7][:, 0:256], cqT[:, kc, :], wuq_r.v(wuq_r.base()[:, kc, :, :].rearrange("p h d -> p (h d)")),
                         start=(kc == 0), stop=(kc == 2))
                P.copy(qr[:, :, :], PB[7].v(PB[7].base()[:, 0:256].rearrange("p (h d) -> p h d", d=32)), eng=ACT)
                rope_tm(P, qr[:, :, 0:16], qr[:, :, 16:32], v3(ra_, 0, 128, 16), v3(ra_, 128, 256, 16),
                        qra[:, :, 0:16], qra[:, :, 16:32], v3(t1, 0, 128, 16), v3(t2, 0, 128, 16))
                P.tt(t3[:, :, :], qra[:, :, 0:32], qra[:, :, 0:32], ALU.mult)
                P.reduce(qr2[:], t3[:, :, :], ALU.add)
                P.tt(qn[:], qr2[:], PB[6][:, 0:8], ALU.add)
                P.act(qn[:], qn[:], AF.Ln, bias=1e-30)
                P.act(qn[:], qn[:], AF.Exp, scale=0.5)
                P.ts(qra[:, :, 32:33], qn.v(qn.base()[:, :].rearrange("p (h o) -> p h o", o=1)), sA["nkmax"][:], ALU.mult)
                P.copy(qrb[:, :, :], qra[:, :, :], eng=POOL)
                for h in range(8):
                    P.transpose(B0[0:33, cs(h)], qrb[:, h, :], K.ident_b[:])
                P.copy(qrTt[:, :, :], B0.v(B0.base()[0:33, :].rearrange("p (h t) -> p h t", t=128)), eng=ACT)
                P.dma(dv(qrT, qrT.base()[:, :, toks].rearrange("h p t -> p h t")), qrTt[:])
                for kc in range(3):
                    P.mm(PB[1][:], cqT[:, kc, :], wqidx[:, kc, :], start=(kc == 0), stop=(kc == 2))
                P.copy(qi[:, :, :], PB[1].v(PB[1].base()[:, :].rearrange("p (h d) -> p h d", d=64)))
                P.copy(qib[:, :, :], qi[:, :, :], eng=POOL)
                rope_tm(P, qi[:, :, 0:8], qi[:, :, 8:16], v3(rb_, 0, 64, 8), v3(rb_, 64, 128, 8),
                        qib[:, :, 0:8], qib[:, :, 8:16], v3(t1, 0, 64, 8), v3(t2, 0, 64, 8))
                for h in range(8):
                    P.transpose(B0[0:64, cs(h)], qib[:, h, :], K.ident_b[:])
                P.copy(qiTt[:, :, :], B0.v(B0.base()[0:64, :].rearrange("p (h t) -> p h t", t=128)))
                P.dma(dv(qiT, qiT.base()[:, :, toks].rearrange("h p t -> p h t")), qiTt[:])

    with P.phase():
        B0 = P.psum("B0", [128, 1024], BF16)
        PI = [P.psum(f"PI{i}", [128, 512], F32) for i in range(2)]
        PS = [P.psum(f"PS{i}", [128, 512], F32) for i in range(2)]
        kis = P.sbuf("kis", [64, S], BF16)
        sc = [P.sbuf(f"sc{i}", [128, S], F32) for i in range(2)]
        jk = P.sbuf("jk", [128, S], BF16)
        neg = [P.sbuf(f"neg{i}", [128, S], BF16) for i in range(2)]
        ngt = [P.sbuf(f"ngt{i}", [128, NT, 128], BF16) for i in range(2)]
        rr = [[P.sbuf(f"rr{i}{k}", [128, 512], F32) for k in range(2)] for i in range(2)]
        dg = [P.sbuf(f"dg{i}", [128, 8, 128], F32) for i in range(2)]
        qiq = [P.sbuf(f"qiq{i}", [64, 8, 128], BF16) for i in range(2)]
        wiq = [P.sbuf(f"wiq{i}", [128, 8], F32) for i in range(2)]
        m_le = P.sbuf("m_le", [128, 128], F32)
        nbig = P.sbuf("nbig", [128, 128], F32)
        P.ts(m_le[:], K.m_gt[:], -1.0, ALU.mult, 1.0, ALU.add)
        P.ts(nbig[:], K.m_gt[:], -1e30, ALU.mult)
        pw2 = P.sbuf("pw2", [128, NIT], F32)
        for it in range(NIT):
            P.memset(pw2[:, it:it + 1], 2.0 ** -(it + 1))
        sB = [{n: P.sbuf(f"{n}{i}", [128, 1], F32) for n in ("lo", "hi", "mid", "cnt", "t")} for i in range(2)]
        stp = [P.sbuf(f"stp{i}", [128, NIT], F32) for i in range(2)]
        jks = [jk, P.sbuf("jk2", [128, S], BF16)]

        def idx_gen(sq_i, qb):
            par = qb % 2
            L = (qb + 1) * 128
            r0 = sq_i * S + qb * 128
            toks = sl(r0, r0 + 128)
            s_, q_, w_, d_ = sc[par], qiq[par], wiq[par], dg[par]
            P.dma(q_[:], dv(qiT, qiT.base()[:, :, toks].rearrange("h p t -> p h t")))
            P.dma(w_[:], dv(wiS, wiS.base()[toks, :]))
            for h in range(8):
                P.ts(d_[:, h, :], K.ident_f[:], w_[:, h:h + 1], ALU.mult)
            yield
            n = 0
            for kg in range((L + 511) // 512):
                w = min(512, L - kg * 512)
                cols = sl(kg * 512, kg * 512 + w)
                sb_ = PS[kg % 2]
                pend = None
                for h in range(8):
                    pb, r_ = PI[n % 2], rr[n % 2][0]
                    n += 1
                    P.mm(pb[:, 0:w], q_[:, h, :], kis[:, cols])
                    if pend is not None:
                        P.mm(sb_[:, 0:w], d_[:, pend[0], :], pend[1][:, 0:w], start=(pend[0] == 0), stop=False)
                    P.act(r_[:, 0:w], pb[:, 0:w], AF.Relu)
                    pend = (h, r_)
                    yield
                P.mm(sb_[:, 0:w], d_[:, 7, :], pend[1][:, 0:w], start=False, stop=True)
                P.copy(s_[:, cols], sb_[:, 0:w], eng=ACT)
                yield

        def bis_gen(sq_i, qb):
            par = qb % 2
            L = (qb + 1) * 128
            Lg = (4 * (qb // 4) + 4) * 128
            s_, n_, g_ = sc[par], neg[par], ngt[par]
            lo, hi, mid, cnt, tt_ = (sB[par][n] for n in ("lo", "hi", "mid", "cnt", "t"))
            st_, jk_ = stp[par], jks[par]
            P.reduce(hi[:], s_[:, 0:L], ALU.max)
            P.reduce(lo[:], s_[:, 0:L], ALU.min)
            yield
            P.tt(hi[:], hi[:], lo[:], ALU.subtract)
            P.ts(lo[:], lo[:], -1.0, ALU.add)
            P.ts(hi[:], hi[:], 2.0, ALU.add)
            P.ts(st_[:], pw2[:], hi[:], ALU.mult)
            blk = s_[:, qb * 128:L]
            P.tt(blk, blk, m_le[:], ALU.mult)
            P.tt(blk, blk, nbig[:], ALU.add)
            yield
            for it in range(NIT):
                P.tt(mid[:], lo[:], st_[:, it:it + 1], ALU.add)
                P.ts(jk_[:, 0:L], s_[:, 0:L], mid[:], ALU.is_gt, 0.0, ALU.add, accum_out=cnt[:])
                yield
                P.ts(tt_[:], cnt[:], float(topk) - 0.5, ALU.is_ge, st_[:, it:it + 1], ALU.mult)
                P.tt(lo[:], lo[:], tt_[:], ALU.add)
                yield
            P.ts(n_[:, 0:L], s_[:, 0:L], lo[:], ALU.is_gt)
            if Lg > L:
                P.memset(n_[:, L:Lg], 0.0, eng=POOL)
            yield
            nkb = Lg // 128
            for kb0 in range(0, nkb, 8):
                n8 = min(8, nkb - kb0)
                for i in range(n8):
                    P.transpose(B0[:, cs(i)], n_[:, cs(kb0 + i)], K.ident_b[:])
                P.copy(g_[:, kb0:kb0 + n8, :], B0.v(B0.base()[:, 0:n8 * 128].rearrange("p (c t) -> p c t", t=128)),
                       eng=ACT)
                yield
            P.dma(dv(negT, negT.base()[sq_i, 0:nkb, :, qb * 128:(qb + 1) * 128].rearrange("kb k q -> k kb q")),
                  g_[:, 0:nkb, :], q=POOL)

        prev = None
        for sq_i in range(C.NSEQ):
            P.dma(kis[:], dv(kiT, kiT.base()[:, sq_i * S:(sq_i + 1) * S]))
            for qb in range(NT):
                gens = [idx_gen(sq_i, qb)]
                if prev is not None:
                    gens.append(bis_gen(*prev))
                run_interleaved(gens)
                prev = (sq_i, qb)
        run_interleaved([bis_gen(*prev)])

    with P.phase():
        PA = [P.psum(f"PA{i}", [128, 512], F32) for i in range(2)]
        O0 = P.psum("O0", [128, 512], F32)
        O1 = P.psum("O1", [128, 512], F32)
        Dn = P.psum("Dn", [128, 512], F32)
        Ov = P.psum("Ov", [128, 512], F32)
        wuv = P.sbuf("wuv", [128, 2, 1024], BF16)
        P.dma(wuv[:], V(W['dsa_w_uv'][j].rearrange("(cc p) f -> p cc f", p=128), ("w", "uv")), q=POOL)
        kl01 = P.sbuf("kl01", [128, 2, S], BF16)
        kl2 = P.sbuf("kl2", [33, S], BF16)
        ckv = P.sbuf("ckvs", [128, NT, 256], BF16)
        ngc = [P.sbuf(f"ngc{i}", [128, NT, 512], BF16) for i in range(2)]
        qa = [P.sbuf(f"qa{i}", [128, 2, 512], BF16) for i in range(2)]
        qrr = [P.sbuf(f"qrr{i}", [33, 512], BF16) for i in range(2)]
        pT = [P.sbuf(f"pT{i}", [128, 512], BF16) for i in range(2)]
        ol = P.sbuf("ol", [128, 2, 512], BF16)
        rden = P.sbuf("rden", [128, 512], F32)
        ost = [P.sbuf(f"ost{i}", [128, 512], BF16) for i in range(2)]
        cnt_ = 0
        for sq_i in range(C.NSEQ):
            seq = sl(sq_i * S, (sq_i + 1) * S)
            P.dma(kl01[:], dv(kl, kl.base()[0:2, :, seq].rearrange("c p t -> p c t")))
            P.dma(kl2[:], dv(kl, kl.base()[2, 0:33, seq]))
            P.dma(ckv[:], dv(kvtm, kvtm.base()[seq, :].rearrange("(t p) c -> p t c", p=128)))
            for g in range(NG):
                nkb = 4 * g + 4
                ng_ = ngc[g % 2]
                gt = sl(sq_i * S + g * 512, sq_i * S + (g + 1) * 512)
                P.dma(ng_[:, 0:nkb, :], dv(negT, negT.base()[sq_i, 0:nkb, :, g * 512:(g + 1) * 512].rearrange("kb k q -> k kb q")))
                for h in range(8):
                    qa_, qr_ = qa[cnt_ % 2], qrr[cnt_ % 2]
                    o_ = ost[cnt_ % 2]
                    cnt_ += 1
                    P.dma(qa_[:], dv(qaT, qaT.base()[h, :, :, gt].rearrange("c p t -> p c t")))
                    P.dma(qr_[:], dv(qrT, qrT.base()[h, 0:33, gt]))

                    def pv(i):
                        p_ = pT[i % 2]
                        P.mm(O0[:], ckv[:, i, 0:128], p_[:], start=(i == 0), stop=(i == nkb - 1))
                        P.mm(O1[:], ckv[:, i, 128:256], p_[:], start=(i == 0), stop=(i == nkb - 1))
                        P.mm(Dn[:], K.ones_b[:], p_[:], start=(i == 0), stop=(i == nkb - 1))

                    for kb in range(nkb):
                        A = PA[kb % 2]
                        P.mm(A[:], kl01[:, 0, cs(kb)], qa_[:, 0, :], start=True, stop=False)
                        P.mm(A[:], kl01[:, 1, cs(kb)], qa_[:, 1, :], start=False, stop=False)
                        P.mm(A[:], kl2[0:33, cs(kb)], qr_[0:33, :], start=False, stop=True)
                        if kb > 0:
                            pv(kb - 1)
                        P.act(pT[kb % 2][:], A[:], AF.Exp, scale=att_scale)
                        P.tt(pT[kb % 2][:], pT[kb % 2][:], ng_[:, kb, :], ALU.mult)
                    pv(nkb - 1)
                    P.copy(ol[:, 0, :], O0[:], eng=ACT)
                    P.copy(ol[:, 1, :], O1[:])
                    P.recip(rden[:], Dn[:])
                    P.mm(Ov[:], wuv[:, 0, cs(h)], ol[:, 0, :], start=True, stop=False)
                    P.mm(Ov[:], wuv[:, 1, cs(h)], ol[:, 1, :], start=False, stop=True)
                    P.tt(o_[:], Ov[:], rden[:], ALU.mult)
                    P.dma(dv(os_, os_.base()[h, :, gt]), o_[:], q=POOL)

    outproj_phase(P, K, C, xr, W['dsa_w_out'][j], os_)


def rope_tables(S):
    pos = np.arange(S, dtype=np.float32)[:, None]

    def tab(r):
        half = r // 2
        inv = (np.float32(500000.0) ** (-np.arange(half, dtype=np.float32) * np.float32(2.0 / r))).astype(np.float32)
        ang = pos * inv[None, :]
        c = np.tile(np.cos(ang).astype(np.float32), (1, 8))
        s_ = np.tile(np.sin(ang).astype(np.float32), (1, 8))
        return np.ascontiguousarray(np.concatenate([c, s_], axis=1), dtype=np.float32)

    return tab(32), tab(16)


def run(C, inputs, n_cores):
    nc = build(C)
    x = np.ascontiguousarray(inputs['x'], dtype=np.float32).reshape(n_cores, C.NTOK, D_MODEL)
    in_maps = []
    for c in range(n_cores):
        m = {"x": x[c]}
        m["ropeA"], m["ropeB"] = rope_tables(C.S)
        for name in INPUT_SHAPES:
            m[name] = np.ascontiguousarray(inputs[name], dtype=np.float32)
        in_maps.append(m)
    res = run_bass_kernel_spmd(nc, in_maps, core_ids=list(range(n_cores)))
    return np.stack([np.asarray(r["y"]) for r in res.results], axis=0)


def kernel(**inputs):
    C = Cfg()
    x = np.asarray(inputs['x'])
    B, S, D = x.shape
    y = run(C, inputs, N_CORES)
    return y.reshape(B, S, D).astype(np.float32)
```
